# Optimizing a Trainium2 kernel written in Bass

```python
import math
import jax, jax.numpy as jnp
from jax import lax
import numpy as np


D_MODEL = 1024
BATCH = 2
SEQ = 16384
DEPTH = 2

BLOCK = 128
EPS = 1e-6
SB_HEADS = 4
SB_DIM = 64
SB_SUB = 32
MLA_HEADS = 4
MLA_Q_RANK = 256
MLA_KV_RANK = 256
MLA_NOPE = 64
MLA_ROPE = 32
MLA_V = 64
ROPE_THETA = 10000.0
SW_Q_HEADS = 8
SW_KV_HEADS = 2
SW_DIM = 32
SW_WINDOW = 128
DIL_PATTERNS = ((128, 1), (512, 4), (2048, 16))
DIL_HEADS = 4
DIL_DIM = 32
REL_BUCKETS = 32
REL_MAX_DIST = 2048
GATE_RANK = 128
N_BRANCH = 4
D_FF_DENSE = 2048
N_EXPERTS = 8
TOP_K = 2
D_FF_EXPERT = 768
MOE_BLOCK = 512

SB_W = SB_HEADS * SB_DIM
MLA_QK = MLA_NOPE + MLA_ROPE
SW_W = SW_Q_HEADS * SW_DIM
SW_KV_W = SW_KV_HEADS * SW_DIM
N_DIL = len(DIL_PATTERNS)
DIL_W = N_DIL * DIL_HEADS * DIL_DIM
REL_HEADS = SW_Q_HEADS + N_DIL * DIL_HEADS
IN_SIZES = (SB_W, SB_W, SB_W, MLA_Q_RANK, MLA_KV_RANK, MLA_ROPE, SW_W, SW_KV_W, SW_KV_W, DIL_W, DIL_W, DIL_W)
IN_COLS = sum(IN_SIZES)
BR_SIZES = (SB_W, MLA_HEADS * MLA_V, SW_W, DIL_HEADS * DIL_DIM)
BR_ROWS = sum(BR_SIZES)

kernel_name = 'hybrid_parallel_gated_mixers_moe'


def rmsnorm(x, g):
    xf = x.astype(jnp.float32)
    y = xf * lax.rsqrt(jnp.mean(xf * xf, axis=-1, keepdims=True) + EPS)
    return (y * g.astype(jnp.float32)).astype(x.dtype)


def rope(x, pos):
    half = x.shape[-1] // 2
    inv = ROPE_THETA ** (-jnp.arange(half, dtype=jnp.float32) / half)
    ang = pos[:, None] * inv[None, :]
    cos = jnp.cos(ang)[:, None, :]
    sin = jnp.sin(ang)[:, None, :]
    xf = x.astype(jnp.float32)
    x1, x2 = xf[..., :half], xf[..., half:]
    return jnp.concatenate([x1 * cos - x2 * sin, x2 * cos + x1 * sin], axis=-1).astype(x.dtype)


def t5_bucket(dist):
    max_exact = REL_BUCKETS // 2
    d = jnp.maximum(dist, 1).astype(jnp.float32)
    large = max_exact + (jnp.log(d / max_exact) / math.log(REL_MAX_DIST / max_exact) * (REL_BUCKETS - max_exact)).astype(jnp.int32)
    large = jnp.minimum(large, REL_BUCKETS - 1)
    return jnp.where(dist < max_exact, dist, large)


def stick_breaking_attention(q, k, v):
    B_, S, H, Dh = q.shape
    scale = Dh ** -0.5
    qf = q.astype(jnp.float32)
    kf = k.astype(jnp.float32)
    incl = (jnp.arange(SB_SUB)[:, None] >= jnp.arange(SB_SUB)[None, :]).astype(jnp.float32)
    outs = []
    for i in range(S // BLOCK):
        L = (i + 1) * BLOCK
        n_sub = L // SB_SUB
        z = jnp.einsum('bqhd,bkhd->bhqk', qf[:, i * BLOCK:L], kf[:, :L]) * scale
        mask = jnp.arange(L)[None, :] < (i * BLOCK + jnp.arange(BLOCK))[:, None]
        lk = jnp.where(mask, jax.nn.log_sigmoid(-z), 0.0)
        within = jnp.einsum('bhqnj,js->bhqns', lk.reshape(B_, H, BLOCK, n_sub, SB_SUB), incl)
        tot = within[..., 0]
        after = lax.cumsum(tot, axis=3, reverse=True) - tot
        later = (within + after[..., None]).reshape(B_, H, BLOCK, L) - lk
        a = jnp.where(mask, jnp.exp(jax.nn.log_sigmoid(z) + later), 0.0)
        outs.append(jnp.einsum('bhqk,bkhd->bqhd', a.astype(v.dtype), v[:, :L]))
    return jnp.concatenate(outs, axis=1).reshape(B_, S, H * Dh)


def causal_softmax_attention(q, k, v):
    B_, S, H, Dqk = q.shape
    scale = Dqk ** -0.5
    qf = q.astype(jnp.float32)
    kf = k.astype(jnp.float32)
    outs = []
    for i in range(S // BLOCK):
        L = (i + 1) * BLOCK
        s = jnp.einsum('bqhd,bkhd->bhqk', qf[:, i * BLOCK:L], kf[:, :L]) * scale
        mask = jnp.arange(L)[None, :] <= (i * BLOCK + jnp.arange(BLOCK))[:, None]
        p = jax.nn.softmax(jnp.where(mask, s, -jnp.inf), axis=-1)
        outs.append(jnp.einsum('bhqk,bkhd->bqhd', p.astype(v.dtype), v[:, :L]))
    out = jnp.concatenate(outs, axis=1)
    return out.reshape(B_, S, H * v.shape[-1])


def sliding_window_sink_attention(q, k, v, sinks, bias):
    B_, S, Hq, Dh = q.shape
    Hkv = k.shape[2]
    G = Hq // Hkv
    nb = S // BLOCK
    qb = q.reshape(B_, nb, BLOCK, Hkv, G, Dh).astype(jnp.float32)

    def band(t):
        tb = t.reshape(B_, nb, BLOCK, Hkv, Dh)
        prev = jnp.concatenate([jnp.zeros_like(tb[:, :1]), tb[:, :-1]], axis=1)
        return jnp.concatenate([prev, tb], axis=2)

    kb, vb = band(k), band(v)
    s = jnp.einsum('bnqkgd,bnskd->bnkgqs', qb, kb.astype(jnp.float32)) * (Dh ** -0.5)
    s = s + bias.reshape(Hkv, G, BLOCK, 2 * BLOCK).astype(jnp.float32)
    dist = (BLOCK + jnp.arange(BLOCK))[:, None] - jnp.arange(2 * BLOCK)[None, :]
    in_window = (dist >= 0) & (dist < SW_WINDOW)
    kvalid = (jnp.arange(nb)[:, None] * BLOCK + jnp.arange(2 * BLOCK)[None, :] - BLOCK) >= 0
    mask = in_window[None] & kvalid[:, None, :]
    s = jnp.where(mask[None, :, None, None], s, -jnp.inf)
    sink = sinks.astype(jnp.float32).reshape(Hkv, G)[None, None, :, :, None, None]
    m = jnp.maximum(jnp.max(s, axis=-1, keepdims=True), sink)
    p = jnp.exp(s - m)
    denom = jnp.sum(p, axis=-1, keepdims=True) + jnp.exp(sink - m)
    o = jnp.einsum('bnkgqs,bnskd->bnqkgd', (p / denom).astype(v.dtype), vb)
    return o.reshape(B_, S, Hq * Dh)


def dilated_group_attention(q, k, v, bias, window, dil):
    B_, S, H, Dh = q.shape
    M = S // dil
    nb = -(-M // BLOCK)
    Mp = nb * BLOCK
    jmax = window // dil

    def streams(t):
        t = t.astype(jnp.float32).reshape(B_, M, dil, H, Dh).transpose(0, 2, 1, 3, 4)
        t = jnp.pad(t, ((0, 0), (0, 0), (0, Mp - M), (0, 0), (0, 0)))
        return t.reshape(B_, dil, nb, BLOCK, H, Dh)

    def band(t):
        prev = jnp.concatenate([jnp.zeros_like(t[:, :, :1]), t[:, :, :-1]], axis=2)
        return jnp.concatenate([prev, t], axis=3)

    qs = streams(q)
    ks = band(streams(k))
    vs = band(streams(v))
    s = jnp.einsum('brnqhd,brnkhd->brnhqk', qs, ks) * (Dh ** -0.5) + bias.astype(jnp.float32)
    dist = (BLOCK + jnp.arange(BLOCK))[:, None] - jnp.arange(2 * BLOCK)[None, :]
    in_window = (dist >= 0) & (dist <= jmax)
    kvalid = (jnp.arange(nb)[:, None] * BLOCK + jnp.arange(2 * BLOCK)[None, :] - BLOCK) >= 0
    mask = in_window[None] & kvalid[:, None, :]
    s = jnp.where(mask[None, None, :, None], s, -jnp.inf)
    m = jnp.max(s, axis=-1, keepdims=True)
    p = jnp.exp(s - m)
    l = jnp.sum(p, axis=-1, keepdims=True)
    o = jnp.einsum('brnhqk,brnkhd->brnqhd', p / l, vs)
    lse = (m + jnp.log(l))[..., 0].transpose(0, 1, 2, 4, 3)
    o = o.reshape(B_, dil, Mp, H, Dh)[:, :, :M].transpose(0, 2, 1, 3, 4).reshape(B_, S, H, Dh)
    lse = lse.reshape(B_, dil, Mp, H)[:, :, :M].transpose(0, 2, 1, 3).reshape(B_, S, H)
    return o, lse


def dilated_mixture_attention(q, k, v, biases):
    B_, S, _, H, Dh = q.shape
    os_, lses = [], []
    for g, (w, dil) in enumerate(DIL_PATTERNS):
        o, lse = dilated_group_attention(q[:, :, g], k[:, :, g], v[:, :, g], biases[g], w, dil)
        os_.append(o)
        lses.append(lse)
    wgt = jax.nn.softmax(jnp.stack(lses), axis=0)
    out = jnp.sum(wgt[..., None] * jnp.stack(os_), axis=0)
    return out.reshape(B_, S, H * Dh).astype(v.dtype)


def mixer_sublayer(x, norm_g, w_in, g_qa, w_qb, g_kva, w_kvb, qk_g_mla, qk_g_sw, qk_g_dil, sinks, sw_bias, dil_biases, w_gate_a, w_gate_b, b_gate, w_branch, w_out):
    B_, S, D = x.shape
    h = rmsnorm(x, norm_g)
    proj = h @ w_in
    offs = np.cumsum(IN_SIZES)[:-1].tolist()
    (a_q, a_k, a_v, b_cq, b_ckv, b_kpe, c_q, c_k, c_v, d_q, d_k, d_v) = jnp.split(proj, offs, axis=-1)
    pos = jnp.arange(S, dtype=jnp.float32)

    sh = (B_, S, SB_HEADS, SB_DIM)
    out_a = stick_breaking_attention(a_q.reshape(sh), a_k.reshape(sh), a_v.reshape(sh))

    qm = (rmsnorm(b_cq, g_qa) @ w_qb).reshape(B_, S, MLA_HEADS, MLA_QK)
    q_mla = jnp.concatenate([qm[..., :MLA_NOPE], rope(qm[..., MLA_NOPE:], pos)], axis=-1)
    kv = (rmsnorm(b_ckv, g_kva) @ w_kvb).reshape(B_, S, MLA_HEADS, MLA_NOPE + MLA_V)
    k_pe = jnp.broadcast_to(rope(b_kpe[:, :, None, :], pos), (B_, S, MLA_HEADS, MLA_ROPE))
    k_mla = jnp.concatenate([kv[..., :MLA_NOPE], k_pe], axis=-1)
    q_mla = rmsnorm(q_mla, qk_g_mla[0])
    k_mla = rmsnorm(k_mla, qk_g_mla[1])
    out_b = causal_softmax_attention(q_mla, k_mla, kv[..., MLA_NOPE:])

    q_sw = rmsnorm(c_q.reshape(B_, S, SW_Q_HEADS, SW_DIM), qk_g_sw[0])
    k_sw = rmsnorm(c_k.reshape(B_, S, SW_KV_HEADS, SW_DIM), qk_g_sw[1])
    out_c = sliding_window_sink_attention(q_sw, k_sw, c_v.reshape(B_, S, SW_KV_HEADS, SW_DIM), sinks, sw_bias)

    dsh = (B_, S, N_DIL, DIL_HEADS, DIL_DIM)
    q_d = rmsnorm(d_q.reshape(dsh), qk_g_dil[0])
    k_d = rmsnorm(d_k.reshape(dsh), qk_g_dil[1])
    out_d = dilated_mixture_attention(q_d, k_d, d_v.reshape(dsh), dil_biases)

    g_low = h @ w_gate_a
    row_offs = np.concatenate([[0], np.cumsum(BR_SIZES)]).tolist()
    y = None
    for i, o in enumerate((out_a, out_b, out_c, out_d)):
        pre = g_low @ w_gate_b[:, i * D:(i + 1) * D] + b_gate[i * D:(i + 1) * D]
        gate = jax.nn.sigmoid(pre.astype(jnp.float32)).astype(x.dtype)
        term = gate * (o @ w_branch[row_offs[i]:row_offs[i + 1]])
        y = term if y is None else y + term
    return x + y @ w_out


def swiglu(h, w_gu, w_down):
    gu = h @ w_gu
    g, u = jnp.split(gu, 2, axis=-1)
    return (jax.nn.silu(g) * u) @ w_down


def moe_swiglu(h, w_router, b_router, w_gu, w_down):
    D = h.shape[-1]
    tok = h.reshape(-1, D)
    N = tok.shape[0]
    logits = (tok @ w_router).astype(jnp.float32) + b_router.astype(jnp.float32)
    top_val, top_idx = lax.top_k(logits, TOP_K)
    top_w = jax.nn.softmax(top_val, axis=-1)
    NK = N * TOP_K
    slot_e = top_idx.reshape(NK)
    slot_tok = jnp.arange(NK, dtype=jnp.int32) // TOP_K
    slot_w = top_w.reshape(NK)
    order = jnp.argsort(slot_e)
    sorted_e = slot_e[order]
    counts = jnp.zeros((N_EXPERTS,), jnp.int32).at[slot_e].add(1)
    padded = (counts + MOE_BLOCK - 1) // MOE_BLOCK * MOE_BLOCK
    pad_end = jnp.cumsum(padded)
    pad_start = pad_end - padded
    start = jnp.cumsum(counts) - counts
    dest = pad_start[sorted_e] + jnp.arange(NK, dtype=jnp.int32) - start[sorted_e]
    n_blk = -(-NK // MOE_BLOCK) + N_EXPERTS
    P = n_blk * MOE_BLOCK
    row_tok = jnp.zeros((P,), jnp.int32).at[dest].set(slot_tok[order])
    row_w = jnp.zeros((P,), jnp.float32).at[dest].set(slot_w[order])
    blk_start = jnp.arange(n_blk, dtype=jnp.int32) * MOE_BLOCK
    blk_e = jnp.minimum(jnp.sum((pad_end[None, :] <= blk_start[:, None]).astype(jnp.int32), axis=1), N_EXPERTS - 1)
    xs = tok[row_tok].reshape(n_blk, MOE_BLOCK, D)

    def expert_block(args):
        xb, e = args
        return swiglu(xb, w_gu[e], w_down[e])

    yb = lax.map(expert_block, (xs, blk_e)).reshape(P, D)
    y = jax.ops.segment_sum(yb * row_w[:, None].astype(yb.dtype), row_tok, num_segments=N)
    return y.reshape(h.shape)


def setup_inputs(seed: int = 0) -> dict:
    key = jax.random.key(seed)
    ks = jax.random.split(key, 26)
    f32 = jnp.float32

    def nrm(k, shape, scale):
        return jax.random.normal(k, shape, f32) * scale

    def gain(k, shape):
        return 1.0 + 0.05 * jax.random.normal(k, shape, f32)

    n_dense = (DEPTH + 1) // 2
    n_moe = DEPTH // 2
    row_scale = jnp.asarray(np.concatenate([np.full((s,), s ** -0.5, np.float32) for s in BR_SIZES]))
    return {
        'x': nrm(ks[0], (BATCH, SEQ, D_MODEL), 1.0),
        'norm1_g': gain(ks[1], (DEPTH, D_MODEL)),
        'w_in': nrm(ks[2], (DEPTH, D_MODEL, IN_COLS), D_MODEL ** -0.5),
        'g_qa': gain(ks[3], (DEPTH, MLA_Q_RANK)),
        'w_qb': nrm(ks[4], (DEPTH, MLA_Q_RANK, MLA_HEADS * MLA_QK), MLA_Q_RANK ** -0.5),
        'g_kva': gain(ks[5], (DEPTH, MLA_KV_RANK)),
        'w_kvb': nrm(ks[6], (DEPTH, MLA_KV_RANK, MLA_HEADS * (MLA_NOPE + MLA_V)), MLA_KV_RANK ** -0.5),
        'qk_g_mla': gain(ks[7], (DEPTH, 2, MLA_QK)),
        'qk_g_sw': gain(ks[8], (DEPTH, 2, SW_DIM)),
        'qk_g_dil': gain(ks[9], (DEPTH, 2, DIL_DIM)),
        'sinks': nrm(ks[10], (DEPTH, SW_Q_HEADS), 0.5),
        'rel_bias': nrm(ks[11], (REL_BUCKETS, REL_HEADS), 0.5),
        'w_gate_a': nrm(ks[12], (DEPTH, D_MODEL, GATE_RANK), D_MODEL ** -0.5),
        'w_gate_b': nrm(ks[13], (DEPTH, GATE_RANK, N_BRANCH * D_MODEL), GATE_RANK ** -0.5),
        'b_gate': nrm(ks[14], (DEPTH, N_BRANCH * D_MODEL), 0.02),
        'w_branch': nrm(ks[15], (DEPTH, BR_ROWS, D_MODEL), 1.0) * row_scale[None, :, None],
        'w_out': nrm(ks[16], (DEPTH, D_MODEL, D_MODEL), D_MODEL ** -0.5),
        'norm2_g': gain(ks[17], (DEPTH, D_MODEL)),
        'w_gu_dense': nrm(ks[18], (n_dense, D_MODEL, 2 * D_FF_DENSE), D_MODEL ** -0.5),
        'w_down_dense': nrm(ks[19], (n_dense, D_FF_DENSE, D_MODEL), D_FF_DENSE ** -0.5),
        'w_router': nrm(ks[20], (n_moe, D_MODEL, N_EXPERTS), D_MODEL ** -0.5),
        'b_router': nrm(ks[21], (n_moe, N_EXPERTS), 0.01),
        'w_gu_exp': nrm(ks[22], (n_moe, N_EXPERTS, D_MODEL, 2 * D_FF_EXPERT), D_MODEL ** -0.5),
        'w_down_exp': nrm(ks[23], (n_moe, N_EXPERTS, D_FF_EXPERT, D_MODEL), D_FF_EXPERT ** -0.5),
    }


def reference(x, norm1_g, w_in, g_qa, w_qb, g_kva, w_kvb, qk_g_mla, qk_g_sw, qk_g_dil, sinks, rel_bias, w_gate_a, w_gate_b, b_gate, w_branch, w_out, norm2_g, w_gu_dense, w_down_dense, w_router, b_router, w_gu_exp, w_down_exp):
    dist_band = (BLOCK + jnp.arange(BLOCK))[:, None] - jnp.arange(2 * BLOCK)[None, :]
    dist_pos = jnp.maximum(dist_band, 0)
    sw_bias = rel_bias[t5_bucket(dist_pos)][..., :SW_Q_HEADS].transpose(2, 0, 1)
    dil_biases = []
    for g, (w, dil) in enumerate(DIL_PATTERNS):
        lo = SW_Q_HEADS + g * DIL_HEADS
        b = rel_bias[t5_bucket(dist_pos * dil)][..., lo:lo + DIL_HEADS]
        dil_biases.append(b.transpose(2, 0, 1))

    for i in range(DEPTH):
        x = mixer_sublayer(x, norm1_g[i], w_in[i], g_qa[i], w_qb[i], g_kva[i], w_kvb[i], qk_g_mla[i], qk_g_sw[i], qk_g_dil[i], sinks[i], sw_bias, dil_biases, w_gate_a[i], w_gate_b[i], b_gate[i], w_branch[i], w_out[i])
        h = rmsnorm(x, norm2_g[i])
        if i % 2 == 0:
            x = x + swiglu(h, w_gu_dense[i // 2], w_down_dense[i // 2])
        else:
            x = x + moe_swiglu(h, w_router[i // 2], b_router[i // 2], w_gu_exp[i // 2], w_down_exp[i // 2])
    return x
```

```python
import math
from contextlib import ExitStack

import numpy as np
import ml_dtypes

import concourse.bass as bass
import concourse.mybir as mybir
from concourse.bass_utils import run_bass_kernel_spmd

F32 = mybir.dt.float32
BF16 = mybir.dt.bfloat16
U8 = mybir.dt.uint8
AF = mybir.ActivationFunctionType
ALU = mybir.AluOpType
AX = mybir.AxisListType
ENGS = ['sync', 'scalar', 'vector', 'gpsimd', 'tensor']

D = 1024
SEQ = 16384
BATCH = 2
EPS = 1e-6
NEG = -30000.0
O_AQ, O_AK, O_AV, O_CQ, O_CKV, O_KPE, O_SWQ, O_SWK, O_SWV, O_DQ, O_DK, O_DV = (
    0, 256, 512, 768, 1024, 1280, 1312, 1568, 1632, 1696, 2080, 2464)
DILS = (1, 4, 16)
STQ = 'sync'
LIMIT = 10 ** 12
SKIP = ()


class Prog:
    def __init__(self, nc, es):
        self.nc, self.es = nc, es
        self.ops = {e: [] for e in ENGS}
        self.sems, self.cnt = {}, {}
        self.known = {e: {} for e in ENGS}
        self.lastw, self.readers = {}, {}
        self.n_instr = 0
        for e in ENGS:
            self._mksem('E_' + e)

    def _mksem(self, name):
        if name not in self.sems:
            self.sems[name] = self.es.enter_context(self.nc.semaphore(name))
            self.cnt[name] = 0
        return self.sems[name]

    def _deps(self, reads, writes):
        deps = {}

        def add(d):
            if d is not None and deps.get(d[0], 0) < d[1]:
                deps[d[0]] = d[1]
        for k in reads:
            add(self.lastw.get(k))
        for k in writes:
            add(self.lastw.get(k))
            for s, v in self.readers.get(k, {}).items():
                add((s, v))
        return deps

    def _waits(self, eng, deps):
        for s, v in deps.items():
            if eng == 'tensor' and s == 'E_tensor':
                continue
            if self.known[eng].get(s, 0) >= v:
                continue
            self.known[eng][s] = v
            h = self.sems[s]
            self.ops[eng].append(lambda e, h=h, v=v: e.wait_ge(h, v))
            self.n_instr += 1

    def _record(self, s, v, reads, writes):
        for k in writes:
            self.lastw[k] = (s, v)
            self.readers[k] = {}
        for k in reads:
            self.readers.setdefault(k, {})[s] = v

    def op(self, eng, fn, reads=(), writes=()):
        self.n_ops = getattr(self, 'n_ops', 0) + 1
        if self.n_ops > LIMIT or self.n_ops in SKIP:
            return
        self._waits(eng, self._deps(reads, writes))
        s = 'E_' + eng
        self.cnt[s] += 1
        h = self.sems[s]
        self.ops[eng].append(lambda e, fn=fn, h=h: fn(e).then_inc(h, 1))
        self._record(s, self.cnt[s], reads, writes)
        self.n_instr += 1

    def dma(self, q, out, in_, reads, writes, slot):
        self.n_ops = getattr(self, 'n_ops', 0) + 1
        if self.n_ops > LIMIT:
            return
        self._waits(q, self._deps(reads, writes))
        s = 'D_' + slot
        h = self._mksem(s)
        self.cnt[s] += 16
        self.ops[q].append(lambda e, h=h, out=out, in_=in_: e.dma_start(out=out, in_=in_).then_inc(h, 16))
        self._record(s, self.cnt[s], reads, writes)
        self.n_instr += 1

    def cc(self, kind, op, groups, in_, out, reads, writes):
        self._waits('gpsimd', self._deps(reads, writes))
        s = 'C_all'
        h = self._mksem(s)
        self.cnt[s] += 1
        self.ops['gpsimd'].append(lambda e, h=h: e.collective_compute(kind, op, replica_groups=groups, ins=[in_], outs=[out]).then_inc(h, 1))
        self._record(s, self.cnt[s], reads, writes)
        self.n_instr += 1

    def barrier(self, exclude_cc=False, keep=()):
        allv = {s: v for s, v in self.cnt.items() if v > 0 and not (exclude_cc and s == 'C_all')}
        for e in ENGS:
            self._waits(e, allv)
        kept = {k: v for k, v in self.lastw.items() if isinstance(k, tuple) and k[0] in keep}
        self.lastw, self.readers = kept, {}

    def finish(self, block):
        for name in ENGS:
            def body(e, name=name):
                for f in self.ops[name]:
                    f(e)
            getattr(block, name)(body)

    def mm(self, out, lhsT, rhs, start, stop, reads, writes, skip=False):
        self.op('tensor', lambda e: e.matmul(out, lhsT, rhs, start=start, stop=stop, skip_group_check=skip), reads, writes)

    def tr(self, out, in_, ident, reads, writes):
        self.op('tensor', lambda e: e.transpose(out, in_, ident), reads, writes)

    def act(self, out, in_, func, reads, writes, bias=None, scale=None, accum_out=None):
        kw = {}
        if bias is not None:
            kw['bias'] = bias
        if scale is not None:
            kw['scale'] = scale
        if accum_out is not None:
            kw['accum_out'] = accum_out
        self.op('scalar', lambda e: e.activation(out=out, in_=in_, func=func, **kw), reads, writes)

    def tt(self, eng, out, in0, in1, op, reads, writes):
        self.op(eng, lambda e: e.tensor_tensor(out=out, in0=in0, in1=in1, op=op), reads, writes)

    def ts(self, eng, out, in0, s1, op0, reads, writes, s2=None, op1=None):
        if op1 is None:
            self.op(eng, lambda e: e.tensor_scalar(out=out, in0=in0, scalar1=s1, scalar2=None, op0=op0), reads, writes)
        else:
            self.op(eng, lambda e: e.tensor_scalar(out=out, in0=in0, scalar1=s1, scalar2=s2, op0=op0, op1=op1), reads, writes)

    def stt(self, out, in0, scalar, in1, op0, op1, reads, writes):
        self.op('vector', lambda e: e.scalar_tensor_tensor(out=out, in0=in0, scalar=scalar, in1=in1, op0=op0, op1=op1), reads, writes)

    def copy(self, eng, out, in_, reads, writes):
        if eng == 'scalar':
            self.op(eng, lambda e: e.copy(out=out, in_=in_), reads, writes)
        else:
            self.op(eng, lambda e: e.tensor_copy(out=out, in_=in_), reads, writes)

    def recip(self, out, in_, reads, writes):
        self.op('vector', lambda e: e.reciprocal(out=out, in_=in_), reads, writes)

    def memset(self, eng, ap, val, writes):
        self.op(eng, lambda e: e.memset(ap, val), (), writes)


class Arena:
    def __init__(self, nc, es, nbytes):
        self.big = es.enter_context(nc.sbuf_tensor("arena", [128, nbytes], U8))
        self.nbytes = nbytes
        self.off = 0

    def reset(self):
        self.off = 0

    def __call__(self, name, shape, dt):
        esz = 2 if dt == BF16 else 4
        n = int(np.prod(shape[1:]))
        nb = (n * esz + 63) // 64 * 64
        assert self.off + nb <= self.nbytes, (name, self.off, nb)
        ap = self.big[:, self.off:self.off + n * esz].bitcast(dt)
        self.off += nb
        if len(shape) > 2:
            names = " ".join("d%d" % i for i in range(len(shape) - 1))
            ap = ap.rearrange("p (%s) -> p %s" % (names, names), **{"d%d" % i: shape[i + 1] for i in range(len(shape) - 2)})
        if shape[0] < 128:
            ap = ap[0:shape[0]]
        return ap


def PK(i):
    return ('ps', i)


def core_cols(h):
    r = lambda o, n: list(range(o, o + n))
    swq0 = r(O_SWQ + (2 * h) * 32, 32)
    swq1 = r(O_SWQ + (2 * h + 1) * 32, 32)
    swk = r(O_SWK + (h // 2) * 32, 32)
    swv = r(O_SWV + (h // 2) * 32, 32)
    dq = [r(O_DQ + (g * 4 + h) * 32, 32) for g in range(3)]
    dk = [r(O_DK + (g * 4 + h) * 32, 32) for g in range(3)]
    dv = [r(O_DV + (g * 4 + h) * 32, 32) for g in range(3)]
    g1 = r(O_CQ, 256) + r(O_CKV, 256)
    n32 = swq0 + dq[0] + dq[1] + swk + dk[0] + dk[1] + swq1 + dq[2] + dk[2]
    g2 = n32 + r(O_AQ + h * 64, 64) + r(O_AK + h * 64, 64) + r(O_KPE, 32) + r(O_AV + h * 64, 64)
    g3 = swv + dv[0] + dv[1] + dv[2]
    return np.array(g1 + g2 + g3)


def emit_inproj(p, sb, PS, d, S):
    NT = S // 128
    identb = sb("identb", [128, 128], BF16)
    p.dma('sync', identb, d['ident'][:, :], [], ['identb'], 'identb')
    g1t = sb("g1t", [128, 8], F32)
    p.dma('sync', g1t, d['g1'][:, :], [], ['g1t'], 'g1t')
    gqat = sb("gqat", [128, 2], F32)
    p.dma('sync', gqat, d['gqa'][:, :], [], ['gqat'], 'gqat')
    gkvat = sb("gkvat", [128, 2], F32)
    p.dma('sync', gkvat, d['gkva'][:, :], [], ['gkvat'], 'gkvat')
    G32 = sb("G32", [128, 288], F32)
    p.dma('sync', G32, d['g32'].partition_broadcast(128), [], ['G32'], 'G32')
    G96 = sb("G96", [128, 2, 96], F32)
    p.dma('sync', G96.rearrange("p a d -> p (a d)"), d['g96'].partition_broadcast(128), [], ['G96'], 'G96')
    COS = sb("COS", [128, NT, 16], F32)
    SIN = sb("SIN", [128, NT, 16], F32)
    p.dma('sync', COS, d['cos'][:, 0:NT, :], [], ['COS'], 'COS')
    p.dma('sync', SIN, d['sin'][:, 0:NT, :], [], ['SIN'], 'SIN')
    Wb = sb("Wb", [128, 8, 1280], BF16)
    wst = [sb("wst%d" % i, [128, 1280], F32) for i in range(2)]
    for kc in range(8):
        b = kc % 2
        p.dma('sync', wst[b], d['w_in'][kc * 128:(kc + 1) * 128, :], [], [('wst', b)], 'wst%d' % b)
        p.act(Wb[:, kc, :], wst[b], AF.Copy, [('wst', b), 'g1t'], ['Wb'], scale=g1t[:, kc:kc + 1])
    Wqb = sb("Wqb", [128, 2, 96], BF16)
    Wkvb = sb("Wkvb", [128, 2, 128], BF16)
    for i in range(2):
        p.dma('sync', wst[0][:, 0:96], d['wqb'][i * 128:(i + 1) * 128, :], [], [('wst', 0)], 'wst0')
        p.act(Wqb[:, i, :], wst[0][:, 0:96], AF.Copy, [('wst', 0), 'gqat'], ['Wqb'], scale=gqat[:, i:i + 1])
        p.dma('sync', wst[1][:, 0:128], d['wkvb'][i * 128:(i + 1) * 128, :], [], [('wst', 1)], 'wst1')
        p.act(Wkvb[:, i, :], wst[1][:, 0:128], AF.Copy, [('wst', 1), 'gkvat'], ['Wkvb'], scale=gkvat[:, i:i + 1])

    X = [sb("x%d" % i, [128, 1024], F32) for i in range(2)]
    junk = sb("junk", [128, 1024], BF16)
    hb = [sb("hb%d" % i, [128, 1024], BF16) for i in range(2)]
    hT = [sb("hT%d" % i, [128, 8, 128], BF16) for i in range(2)]
    st = sb("stats", [128, 32], F32)
    cb = sb("cb", [128, 512], BF16)
    cT = sb("cT", [128, 4, 128], BF16)
    qk96 = sb("qk96", [128, 2, 96], F32)
    tmp96 = sb("tmp96", [128, 2, 96], F32)
    qkb = sb("qkb", [128, 2, 96], BF16)
    rA = sb("ropeA", [128, 2, 2, 16], F32)
    rB = sb("ropeB", [128, 2, 2, 16], F32)
    sq32 = sb("sq32", [128, 288], F32)
    tmp32 = sb("tmp32", [128, 288], F32)
    n32b = sb("n32b", [128, 288], BF16)
    sbqk = sb("sbqk", [128, 128], BF16)
    stageT = [sb("stageT%d" % i, [128, 8, 512], BF16) for i in range(2)]
    stVS = [sb("stVS%d" % i, [128, 4, 64], BF16) for i in range(2)]
    stVM = [sb("stVM%d" % i, [128, 4, 65], BF16) for i in range(2)]
    stVG = [sb("stVG%d" % i, [128, 4, 4, 33], BF16) for i in range(2)]
    stGL = [sb("stGL%d" % i, [128, 512], BF16) for i in range(2)]
    glb = sb("glb", [128, 128], BF16)
    for i in range(2):
        p.memset('gpsimd', stageT[i], 0.0, [('stageT', i)])
        p.memset('gpsimd', stVM[i], 1.0, [('stVM', i)])
        p.memset('gpsimd', stVG[i], 1.0, [('stVG', i)])
    psTb = PS[0].bitcast(BF16)
    ps1, ps2, ps3 = PS[1], PS[2], PS[3]
    ps4b = PS[4].bitcast(BF16)
    ps5 = PS[5]
    ps6b = PS[6].bitcast(BF16)
    xrows = d['xrows']

    xkeys = d.get('xkeys', lambda t: [])
    p.dma('sync', X[0], xrows(0), xkeys(0), [('x', 0)], 'x0')
    for t in range(NT):
        b = t % 2
        tt = t % 4
        sg = (t // 4) % 2
        xk, hbk, hTk = ('x', b), ('hb', b), ('hT', b)
        if t + 1 < NT:
            p.dma('sync', X[1 - b], xrows(t + 1), xkeys(t + 1), [('x', 1 - b)], 'x%d' % (1 - b))
        p.act(junk, X[b], AF.Square, [xk], ['junk', 'ssq'], accum_out=st[:, 0:1])
        p.act(st[:, 1:2], st[:, 0:1], AF.Sqrt, ['ssq'], ['rs'], scale=1.0 / D, bias=EPS)
        p.recip(st[:, 2:3], st[:, 1:2], ['rs'], ['rstd'])
        p.act(hb[b], X[b], AF.Copy, [xk, 'rstd'], [hbk], scale=st[:, 2:3])
        for kc in range(8):
            p.tr(psTb[:, kc * 128:(kc + 1) * 128], hb[b][:, kc * 128:(kc + 1) * 128], identb, [hbk, 'identb'], [PK(0)])
        p.copy('vector', hT[b].rearrange("p a d -> p (a d)"), psTb, [PK(0)], [hTk])
        for (ps, key, c0, c1) in ((ps1, PK(1), 0, 512), (ps2, PK(2), 512, 1024), (ps3, PK(3), 1024, 1280)):
            for kc in range(8):
                p.mm(ps[:, 0:c1 - c0], hT[b][:, kc, :], Wb[:, kc, c0:c1], kc == 0, kc == 7, [hTk, 'Wb'], [key])
        p.act(junk[:, 0:256], ps1[:, 0:256], AF.Square, [PK(1)], ['junk', 'ssq2a'], accum_out=st[:, 3:4])
        p.act(junk[:, 0:256], ps1[:, 256:512], AF.Square, [PK(1)], ['junk', 'ssq2b'], accum_out=st[:, 4:5])
        p.act(st[:, 5:7], st[:, 3:5], AF.Sqrt, ['ssq2a', 'ssq2b'], ['rs2'], scale=1.0 / 256, bias=EPS)
        p.recip(st[:, 7:9], st[:, 5:7], ['rs2'], ['rstd2'])
        p.copy('vector', cb, ps1, [PK(1)], ['cb'])
        for i in range(4):
            p.tr(ps4b[:, i * 128:(i + 1) * 128], cb[:, i * 128:(i + 1) * 128], identb, ['cb', 'identb'], [PK(4)])
        p.copy('vector', cT.rearrange("p a d -> p (a d)"), ps4b[:, 0:512], [PK(4)], ['cT'])
        for i in range(2):
            p.mm(ps5[:, 0:96], cT[:, i, :], Wqb[:, i, :], i == 0, i == 1, ['cT', 'Wqb'], [PK(5)])
        for i in range(2):
            p.mm(ps5[:, 128:256], cT[:, 2 + i, :], Wkvb[:, i, :], i == 0, i == 1, ['cT', 'Wkvb'], [PK(5)])
        p.act(qk96[:, 0, :], ps5[:, 0:96], AF.Copy, [PK(5), 'rstd2'], ['qk96'], scale=st[:, 7:8])
        p.act(qk96[:, 1, 0:64], ps5[:, 128:192], AF.Copy, [PK(5), 'rstd2'], ['qk96'], scale=st[:, 8:9])
        p.act(stVM[sg][:, tt, 0:64], ps5[:, 192:256], AF.Copy, [PK(5), 'rstd2'], [('stVM', sg)], scale=st[:, 8:9])
        p.copy('vector', qk96[:, 1, 64:96], ps2[:, 416:448], [PK(2)], ['qk96'])
        R = qk96[:, :, 64:96].rearrange("p a (h d) -> p a h d", h=2)
        cosb = COS[:, t, :].unsqueeze(1).unsqueeze(1).broadcast_to([128, 2, 2, 16])
        sinb = SIN[:, t, :].unsqueeze(1).broadcast_to([128, 2, 16])
        p.tt('vector', rA, R, cosb, ALU.mult, ['qk96', 'COS'], ['rA'])
        p.tt('vector', rB[:, :, 0, :], R[:, :, 1, :], sinb, ALU.mult, ['qk96', 'SIN'], ['rB'])
        p.tt('vector', rB[:, :, 1, :], R[:, :, 0, :], sinb, ALU.mult, ['qk96', 'SIN'], ['rB'])
        p.tt('vector', R[:, :, 0, :], rA[:, :, 0, :], rB[:, :, 0, :], ALU.subtract, ['rA', 'rB'], ['qk96'])
        p.tt('vector', R[:, :, 1, :], rA[:, :, 1, :], rB[:, :, 1, :], ALU.add, ['rA', 'rB'], ['qk96'])
        p.tt('vector', tmp96, qk96, qk96, ALU.mult, ['qk96'], ['tmp96'])
        p.op('vector', lambda e: e.tensor_reduce(out=st[:, 9:11], in_=tmp96, axis=AX.X, op=ALU.add), ['tmp96'], ['ssq96'])
        p.act(st[:, 11:13], st[:, 9:11], AF.Sqrt, ['ssq96'], ['rs96'], scale=1.0 / 96, bias=EPS)
        p.recip(st[:, 13:15], st[:, 11:13], ['rs96'], ['rstd96'])
        p.tt('vector', tmp96, qk96, G96, ALU.mult, ['qk96', 'G96'], ['tmp96'])
        p.tt('vector', qkb, tmp96, st[:, 13:15].unsqueeze(2).broadcast_to([128, 2, 96]), ALU.mult, ['tmp96', 'rstd96'], ['qkb'])
        p.act(sq32, ps2[:, 0:288], AF.Square, [PK(2)], ['sq32'])
        p.op('vector', lambda e: e.tensor_reduce(out=st[:, 15:24], in_=sq32.rearrange("p (a d) -> p a d", d=32), axis=AX.X, op=ALU.add), ['sq32'], ['ssq32'])
        p.act(st[:, 15:24], st[:, 15:24], AF.Sqrt, ['ssq32'], ['ssq32'], scale=1.0 / 32, bias=EPS)
        p.recip(st[:, 15:24], st[:, 15:24], ['ssq32'], ['ssq32'])
        p.tt('vector', tmp32, ps2[:, 0:288], G32, ALU.mult, [PK(2), 'G32'], ['tmp32'])
        p.tt('vector', n32b.rearrange("p (a d) -> p a d", d=32), tmp32.rearrange("p (a d) -> p a d", d=32),
             st[:, 15:24].unsqueeze(2).broadcast_to([128, 9, 32]), ALU.mult, ['tmp32', 'ssq32'], ['n32b'])
        p.ts('vector', sbqk[:, 0:64], ps2[:, 288:352], 0.125, ALU.mult, [PK(2)], ['sbqk'])
        p.copy('vector', sbqk[:, 64:128], ps2[:, 352:416], [PK(2)], ['sbqk'])
        p.copy('vector', stVS[sg][:, tt, :], ps2[:, 448:512], [PK(2)], [('stVS', sg)])
        p.copy('vector', stVG[sg][:, tt, :, 0:32], ps3[:, 0:128].rearrange("p (a d) -> p a d", d=32), [PK(3)], [('stVG', sg)])
        p.copy('vector', glb, ps3[:, 128:256], [PK(3)], ['glb'])
        p.tr(ps4b[:, 512:640], glb, identb, ['glb', 'identb'], [PK(4)])
        p.copy('vector', stGL[sg][:, tt * 128:(tt + 1) * 128], ps4b[:, 512:640], [PK(4)], [('stGL', sg)])
        p.tr(ps6b[0:64, 0:128], sbqk[:, 0:64], identb, ['sbqk', 'identb'], [PK(6)])
        p.tr(ps6b[0:64, 128:256], sbqk[:, 64:128], identb, ['sbqk', 'identb'], [PK(6)])
        p.tr(ps6b[0:96, 256:384], qkb[:, 0, :], identb, ['qkb', 'identb'], [PK(6)])
        p.tr(ps6b[0:96, 384:512], qkb[:, 1, :], identb, ['qkb', 'identb'], [PK(6)])
        p.tr(ps6b[0:96, 512:640], n32b[:, 0:96], identb, ['n32b', 'identb'], [PK(6)])
        p.tr(ps6b[0:96, 640:768], n32b[:, 96:192], identb, ['n32b', 'identb'], [PK(6)])
        p.tr(ps6b[0:64, 768:896], n32b[:, 192:256], identb, ['n32b', 'identb'], [PK(6)])
        p.tr(ps6b[0:64, 896:1024], n32b[:, 224:288], identb, ['n32b', 'identb'], [PK(6)])
        p.copy('vector', stageT[sg][0:96, 2:6, tt * 128:(tt + 1) * 128], ps6b[0:96, 256:768].rearrange("p (a d) -> p a d", d=128), [PK(6)], [('stageT', sg)])
        p.copy('vector', stageT[sg][0:64, 0:2, tt * 128:(tt + 1) * 128], ps6b[0:64, 0:256].rearrange("p (a d) -> p a d", d=128), [PK(6)], [('stageT', sg)])
        p.copy('vector', stageT[sg][0:64, 6:8, tt * 128:(tt + 1) * 128], ps6b[0:64, 768:1024].rearrange("p (a d) -> p a d", d=128), [PK(6)], [('stageT', sg)])
        if tt == 3:
            T0 = (t - 3) * 128
            j0 = t - 3
            p.dma(STQ, d['QKT'][0:96, :, T0:T0 + 512], stageT[sg][0:96], [('stageT', sg)], [('QKT', sg)], 'stageT%d' % sg)
            p.dma(STQ, d['GL'][:, T0:T0 + 512], stGL[sg], [('stGL', sg)], [('GL', sg)], 'stGL%d' % sg)
            p.dma(STQ, d['VSB'][:, j0:j0 + 4, :], stVS[sg], [('stVS', sg)], [('VSB', sg)], 'stVS%d' % sg)
            p.dma(STQ, d['VML'][:, j0:j0 + 4, :], stVM[sg], [('stVM', sg)], [('VML', sg)], 'stVM%d' % sg)
            p.dma(STQ, d['VSW'][:, j0:j0 + 4, :], stVG[sg][:, :, 0, :], [('stVG', sg)], [('VG', sg)], 'stVG%d' % sg)
            for g in range(3):
                p.dma(STQ, d['VD%d' % g][T0:T0 + 512, :].rearrange("(a p) d -> p a d", p=128), stVG[sg][:, :, 1 + g, :],
                      [('stVG', sg)], [('VG', sg)], 'stVG%d' % sg)


def load_cols(p, dst, dst_key, src, src_keys, slot, nsplit=4):
    n = src.shape[-1]
    w = n // nsplit
    for i in range(nsplit):
        p.dma('sync', dst[:, i * w:(i + 1) * w], src[:, i * w:(i + 1) * w], src_keys, [dst_key], slot)


def emit_sb(p, sb, PS, d, S, side=None):
    NT = S // 128
    QT = sb("sbQT", [64, S], BF16)
    KT = sb("sbKT", [64, S], BF16)
    V = sb("sbV", [128, NT, 64], BF16)
    load_cols(p, QT, 'sbQT', d['QKT'][0:64, 0, :], [], 'sbQT')
    load_cols(p, KT, 'sbKT', d['QKT'][0:64, 1, :], [], 'sbKT')
    p.dma('sync', V, d['VSB'][:, :, :], [], ['sbV'], 'sbV')
    triN = sb("triN", [128, 128], BF16)
    onesb = sb("onesb", [128, 128], BF16)
    maskS = sb("maskS", [128, 4, 512], BF16)
    p.dma('sync', triN, d['triN'][:, :], [], ['triN'], 'triN')
    p.dma('sync', onesb, d['onesb'][:, :], [], ['onesb'], 'onesb')
    p.dma('sync', maskS, d['maskS'][:, :, :], [], ['maskS'], 'maskS')
    e_t = [sb("sb_e%d" % i, [128, 512], F32) for i in range(2)]
    sp_t = [sb("sb_sp%d" % i, [128, 512], BF16) for i in range(2)]
    ex_t = [sb("sb_ex%d" % i, [128, 512], F32) for i in range(2)]
    A_t = [sb("sb_A%d" % i, [128, 512], BF16) for i in range(2)]
    Cb = sb("sb_Cb", [128, 512], F32)
    ost = [sb("sb_ost%d" % i, [64, 512], BF16) for i in range(2)]
    steps = []
    for c in range(S // 512):
        for idx, j in enumerate(range(4 * c + 3, -1, -1)):
            steps.append((c, idx, j))
    NS = len(steps)

    def stage1(s):
        c, idx, j = steps[s]
        b = s % 2
        r = j - 4 * c
        zk, ek, spk = PK(b), ('e', b), ('sp', b)
        p.mm(PS[b], KT[:, j * 128:(j + 1) * 128], QT[:, c * 512:(c + 1) * 512], True, True, ['sbKT', 'sbQT'], [zk])
        p.act(e_t[b], PS[b], AF.Exp, [zk], [ek])
        p.act(sp_t[b], e_t[b], AF.Ln, [ek], [spk], bias=1.0)
        if r >= 0:
            p.tt('gpsimd', sp_t[b], sp_t[b], maskS[:, r, :], ALU.mult, [spk, 'maskS'], [spk])

    def stage2(s):
        c, idx, j = steps[s]
        b = s % 2
        r = j - 4 * c
        zk, ck, spk, exk, Ak = PK(b), PK(2 + b), ('sp', b), ('ex', b), ('A', b)
        p.mm(PS[b], triN, sp_t[b], False, True, [spk, 'triN'], [zk], skip=True)
        p.mm(PS[2 + b], onesb, sp_t[b], True, True, [spk, 'onesb'], [ck])
        if idx == 0:
            p.copy('vector', ex_t[b], PS[b], [zk], [exk])
            p.copy('vector', Cb, PS[2 + b], [ck], ['Cb'])
        else:
            p.tt('vector', ex_t[b], PS[b], Cb, ALU.subtract, [zk, 'Cb'], [exk])
            if j > 0:
                p.tt('vector', Cb, PS[2 + b], Cb, ALU.add, [ck, 'Cb'], ['Cb'])
        p.act(A_t[b], ex_t[b], AF.Exp, [exk], [Ak])
        if r >= 0:
            p.tt('gpsimd', A_t[b], A_t[b], maskS[:, r, :], ALU.mult, [Ak, 'maskS'], [Ak])

    def stage3(s):
        c, idx, j = steps[s]
        b = s % 2
        ob = c % 2
        p.mm(PS[4 + ob][0:64, :], V[:, j, :], A_t[b], idx == 0, j == 0, [('A', b), 'sbV'], [PK(4 + ob)])
        if j == 0:
            p.copy('vector', ost[ob], PS[4 + ob][0:64, :], [PK(4 + ob)], [('ost', ob)])
            p.dma(STQ, d['OUT'][0:64, c * 512:(c + 1) * 512], ost[ob], [('ost', ob)], [('OUT', ob)], 'sb_ost%d' % ob)

    side_it = side(sb) if side is not None else None
    n_side = d.get('n_side', 0)
    every = max(1, NS // max(n_side, 1))
    for i in range(NS + 2):
        if i < NS:
            stage1(i)
        if 0 <= i - 1 < NS:
            stage2(i - 1)
        if 0 <= i - 2 < NS:
            stage3(i - 2)
        if side_it is not None and i % every == 0:
            next(side_it, None)
    if side_it is not None:
        for _ in side_it:
            pass


def emit_mla(p, sb, PS, d, S):
    NT = S // 128
    QT = sb("mlQT", [96, S], BF16)
    KT = sb("mlKT", [96, S], BF16)
    V = sb("mlV", [128, NT, 65], BF16)
    load_cols(p, QT, 'mlQT', d['QKT'][0:96, 2, :], [], 'mlQT')
    load_cols(p, KT, 'mlKT', d['QKT'][0:96, 3, :], [], 'mlKT')
    p.dma('sync', V, d['VML'][:, :, :], [], ['mlV'], 'mlV')
    maskI = sb("maskI", [128, 4, 512], BF16)
    p.dma('sync', maskI, d['maskI'][:, :, :], [], ['maskI'], 'maskI')
    onesf = sb("onesf", [128, 64], F32)
    p.dma('sync', onesf, d['onesf'][:, :], [], ['onesf'], 'onesf')
    P_t = [sb("ml_P%d" % i, [128, 512], BF16) for i in range(3)]
    osb = sb("ml_osb", [128, 512], F32)
    rrow = sb("ml_rrow", [128, 512], F32)
    ost = [sb("ml_ost%d" % i, [64, 512], BF16) for i in range(2)]
    scale = 96 ** -0.5
    steps = []
    for c in range(S // 512):
        for idx, j in enumerate(range(4 * c + 3, -1, -1)):
            steps.append((c, idx, j))
    NS = len(steps)

    def stage1(s):
        c, idx, j = steps[s]
        b = s % 3
        r = j - 4 * c
        sk, Pk = PK(b), ('P', b)
        p.mm(PS[b], KT[:, j * 128:(j + 1) * 128], QT[:, c * 512:(c + 1) * 512], True, True, ['mlKT', 'mlQT'], [sk])
        p.act(P_t[b], PS[b], AF.Exp, [sk], [Pk], scale=scale)
        if r >= 0:
            p.tt('gpsimd', P_t[b], P_t[b], maskI[:, r, :], ALU.mult, [Pk, 'maskI'], [Pk])

    def stage2(s):
        c, idx, j = steps[s]
        b = s % 3
        ob = c % 2
        oD, ok = PS[4 + ob], PK(4 + ob)
        p.mm(oD[0:65, :], V[:, j, :], P_t[b], idx == 0, j == 0, [('P', b), 'mlV'], [ok])
        if j == 0:
            p.copy('vector', osb[0:65, :], oD[0:65, :], [ok], ['ml_osb'])
            p.act(rrow[64:65, :], osb[64:65, :], AF.Ln, ['ml_osb'], ['ml_rrow'])
            p.act(rrow[64:65, :], rrow[64:65, :], AF.Exp, ['ml_rrow'], ['ml_rrow'], scale=-1.0)
            p.mm(PS[6][0:64, :], onesf[64:65, :], rrow[64:65, :], True, True, ['ml_rrow', 'onesf'], [PK(6)])
            p.tt('vector', ost[ob], osb[0:64, :], PS[6][0:64, :], ALU.mult, ['ml_osb', PK(6)], [('ml_ost', ob)])
            p.dma(STQ, d['OUT'][64:128, c * 512:(c + 1) * 512], ost[ob], [('ml_ost', ob)], [('OUT', 2 + ob)], 'ml_ost%d' % ob)

    for i in range(NS + 2):
        if i < NS:
            stage1(i)
        if 0 <= i - 2 < NS:
            stage2(i - 2)


def toeplitz(bmat, idx, rev=False):
    return bmat[idx, :, :]


def run_band_units(p, PS, units, t_sb, P_sb, scale):
    def front(ui):
        slots, Btile, Bkey, _ = units[ui]
        b = ui % 2
        s_ps, sk = PS[b], PK(b)
        for u, sl in enumerate(slots):
            p.mm(s_ps[:, u * 256:u * 256 + 128], sl['kc'], sl['q'], True, True, sl['rk'], [sk])
            p.mm(s_ps[:, u * 256 + 128:(u + 1) * 256], sl['kp'], sl['q'], True, True, sl['rk'], [sk])
        p.stt(t_sb[b], s_ps, scale, Btile, ALU.mult, ALU.add, [sk, Bkey], [('bt', b)])
        p.act(P_sb[b], t_sb[b], AF.Exp, [('bt', b)], [('bP', b)])

    def back(ui):
        slots, _, _, epi = units[ui]
        b = ui % 2
        o_ps, ok = PS[2 + b], PK(2 + b)
        for u, sl in enumerate(slots):
            p.mm(o_ps[0:33, u * 128:(u + 1) * 128], sl['vc'], P_sb[b][:, u * 256:u * 256 + 128], True, False, [('bP', b)] + sl['vk'], [ok])
            p.mm(o_ps[0:33, u * 128:(u + 1) * 128], sl['vp'], P_sb[b][:, u * 256 + 128:(u + 1) * 256], False, True, [('bP', b)] + sl['vk'], [ok])
        epi(o_ps, ok)

    n = len(units)
    for i in range(n + 1):
        if i < n:
            front(i)
        if i >= 1:
            back(i - 1)


def emit_sw(p, sb, PS, d, S):
    NT = S // 128
    Q = [sb("swQ%d" % i, [32, S], BF16) for i in range(2)]
    K = sb("swK", [32, S], BF16)
    V = sb("swV", [128, NT, 33], BF16)
    load_cols(p, Q[0], 'swQ0', d['QKT'][0:32, 4, :], [], 'swQ0')
    load_cols(p, Q[1], 'swQ1', d['QKT'][0:32, 6, :], [], 'swQ1')
    load_cols(p, K, 'swK', d['QKT'][0:32, 5, :], [], 'swK')
    p.dma('sync', V, d['VSW'][:, :, :], [], ['swV'], 'swV')
    B = sb("swB", [128, 2, 2, 128], F32)
    B0 = sb("swB0", [128, 2, 2, 128], F32)
    for h in range(2):
        for sel in range(2):
            p.dma('sync', B[:, h, sel, :], toeplitz(d['bvec'], h * 2 + sel), [], ['swB'], 'swB')
        p.dma('sync', B0[:, h, 0, :], toeplitz(d['bvec'], h * 2), [], ['swB0'], 'swB0')
        p.memset('gpsimd', B0[:, h, 1, :], NEG, ['swB0'])
    onesf = sb("onesf", [128, 64], F32)
    p.dma('sync', onesf, d['onesf'][:, :], [], ['onesf'], 'onesf')
    es = sb("sw_es", [128, 2], F32)
    p.dma('sync', es[32:33, :], d['sinks'][:, :], [], ['sw_es'], 'sw_es')
    p.act(es[32:33, :], es[32:33, :], AF.Exp, ['sw_es'], ['sw_es'])
    t_sb = [sb("bt%d" % i, [128, 512], F32) for i in range(2)]
    P_sb = [sb("bP%d" % i, [128, 512], BF16) for i in range(2)]
    osb = sb("sw_osb", [128, 2, 512], F32)
    rrow = sb("sw_rrow", [128, 2, 512], F32)
    ost = [sb("sw_ost%d" % i, [32, 2, 512], BF16) for i in range(2)]
    scale = 32 ** -0.5
    units = []
    for n in range(NT):
        cs = slice(n * 128, (n + 1) * 128)
        ps_ = slice(max(n - 1, 0) * 128, (max(n - 1, 0) + 1) * 128)
        slots = [dict(kc=K[:, cs], kp=K[:, ps_], q=Q[h][:, cs], vc=V[:, n, :], vp=V[:, max(n - 1, 0), :],
                      rk=['swK', 'swQ%d' % h], vk=['swV']) for h in range(2)]

        def epi(o_ps, ok, n=n):
            tt = n % 4
            p.copy('scalar', osb[0:33, :, tt * 128:(tt + 1) * 128], o_ps[0:33, 0:256].rearrange("p (a d) -> p a d", d=128), [ok], ['sw_osb'])
            if tt == 3:
                gi = (n // 4) % 2
                T0 = (n - 3) * 128
                for h in range(2):
                    p.act(rrow[32:33, h, :], osb[32:33, h, :], AF.Ln, ['sw_osb', 'sw_es'], ['sw_rrow'], bias=es[32:33, h:h + 1])
                    p.act(rrow[32:33, h, :], rrow[32:33, h, :], AF.Exp, ['sw_rrow'], ['sw_rrow'], scale=-1.0)
                    p.mm(PS[4 + h][0:32, :], onesf[32:33, 0:32], rrow[32:33, h, :], True, True, ['sw_rrow', 'onesf'], [PK(4 + h)])
                    p.tt('vector', ost[gi][:, h, :], osb[0:32, h, :], PS[4 + h][0:32, :], ALU.mult, ['sw_osb', PK(4 + h)], [('sw_ost', gi)])
                p.dma(STQ, d['OUT'][128:192, T0:T0 + 512].rearrange("(h p) t -> p h t", p=32), ost[gi], [('sw_ost', gi)], [('OUT', 4 + gi)], 'sw_ost%d' % gi)
        units.append((slots, (B0 if n == 0 else B).rearrange("p a b c -> p (a b c)"), 'swB0' if n == 0 else 'swB', epi))
    run_band_units(p, PS, units, t_sb, P_sb, scale)


def emit_dil(p, sb, PS, d, S):
    Qd = sb("dlQ", [96, S], BF16)
    Kd = sb("dlK", [96, S], BF16)
    Oacc = sb("dlO", [128, S], F32)
    onesf = sb("onesf", [128, 64], F32)
    p.dma('sync', onesf, d['onesf'][:, :], [], ['onesf'], 'onesf')
    Vg = sb("dlV", [128, S // 128, 33], BF16)
    Bt = [sb("dlB%d" % i, [128, 2, 2, 128], F32) for i in range(2)]
    t_sb = [sb("bt%d" % i, [128, 512], F32) for i in range(2)]
    P_sb = [sb("bP%d" % i, [128, 512], BF16) for i in range(2)]
    rrow = sb("dl_rrow", [128, 512], F32)
    ost = [sb("dl_ost%d" % i, [32, 512], BF16) for i in range(2)]
    scale = 32 ** -0.5
    src = [(4, 5, 32), (4, 5, 64), (6, 7, 32)]
    for g, dil in enumerate(DILS):
        qs_, ks_, pb = src[g]
        rows = slice(pb, pb + 32)
        M = S // dil
        nb = M // 128
        load_cols(p, Qd[rows], 'dlQ', d['QKT'][rows, qs_, :], [], 'dlQ')
        load_cols(p, Kd[rows], 'dlK', d['QKT'][rows, ks_, :], [], 'dlK')
        Vv = Vg.rearrange("p (r n) d -> p r n d", r=dil)
        vd = d['VD%d' % g]
        for r0 in range(dil):
            for n0 in range(0, nb, 16):
                nn = min(16, nb - n0)
                srcap = bass.AP(vd.tensor, r0 * 33 + n0 * 128 * dil * 33, [[dil * 33, 128], [128 * dil * 33, nn], [1, 33]])
                p.dma('sync', Vv[:, r0, n0:n0 + nn, :], srcap, [], ['dlV'], 'dlV')
        for v in range(2):
            for u in range(2):
                for sel in range(2):
                    if v == 1 and u == 0 and sel == 1:
                        p.memset('gpsimd', Bt[v][:, u, sel, :], NEG, [('dlB', v)])
                    else:
                        p.dma('sync', Bt[v][:, u, sel, :], toeplitz(d['bvec'], (2 + g) * 2 + sel), [], [('dlB', v)], 'dlB%d' % v)
        units = []
        for r in range(dil):
            for n in range(0, nb, 2):
                slots = []
                for u in range(2):
                    n1 = n + u
                    np_ = max(n1 - 1, 0)
                    qc = slice(r + dil * 128 * n1, r + dil * 128 * n1 + dil * 127 + 1, dil)
                    kc = slice(r + dil * 128 * np_, r + dil * 128 * np_ + dil * 127 + 1, dil)
                    slots.append(dict(kc=Kd[rows, qc], kp=Kd[rows, kc], q=Qd[rows, qc], vc=Vv[:, r, n1, :], vp=Vv[:, r, np_, :],
                                      rk=['dlK', 'dlQ'], vk=['dlV']))
                v = 1 if n == 0 else 0
                oc = slice(r + dil * 128 * n, r + dil * 128 * n + dil * 255 + 1, dil)

                def epi(o_ps, ok, oc=oc, g=g):
                    if g == 0:
                        p.copy('vector', Oacc[0:33, oc], o_ps[0:33, 0:256], [ok], ['dlO'])
                    else:
                        p.tt('vector', Oacc[0:33, oc], o_ps[0:33, 0:256], Oacc[0:33, oc], ALU.add, [ok, 'dlO'], ['dlO'])
                units.append((slots, Bt[v].rearrange("p a b c -> p (a b c)"), ('dlB', v), epi))
        run_band_units(p, PS, units, t_sb, P_sb, scale)
    for c in range(S // 512):
        gi = c % 2
        cs = slice(c * 512, (c + 1) * 512)
        p.act(rrow[32:33, :], Oacc[32:33, cs], AF.Ln, ['dlO'], ['dl_rrow'])
        p.act(rrow[32:33, :], rrow[32:33, :], AF.Exp, ['dl_rrow'], ['dl_rrow'], scale=-1.0)
        p.mm(PS[4 + gi][0:32, :], onesf[32:33, 0:32], rrow[32:33, :], True, True, ['dl_rrow', 'onesf'], [PK(4 + gi)])
        p.tt('vector', ost[gi], Oacc[0:32, cs], PS[4 + gi][0:32, :], ALU.mult, ['dlO', PK(4 + gi)], [('dl_ost', gi)])
        p.dma(STQ, d['OUT'][192:224, cs], ost[gi], [('dl_ost', gi)], [('OUT', 6 + gi)], 'dl_ost%d' % gi)


def build_A(S, phases=('inproj', 'sb', 'mla', 'sw', 'dil'), dbg=False):
    nc = bass.Bass("TRN2", target_bir_lowering=False)
    NT = S // 128
    d = {}

    def din(name, shape, dt=F32):
        d[name] = nc.dram_tensor(name, shape, dt, kind="ExternalInput").ap()

    din('x', [S, D])
    din('w_in', [D, 1280])
    din('g1', [128, 8])
    din('wqb', [256, 96])
    din('gqa', [128, 2])
    din('wkvb', [256, 128])
    din('gkva', [128, 2])
    din('g32', [288])
    din('g96', [192])
    din('cos', [128, SEQ // 128, 16])
    din('sin', [128, SEQ // 128, 16])
    din('ident', [128, 128], BF16)
    din('triN', [128, 128], BF16)
    din('onesb', [128, 128], BF16)
    din('maskS', [128, 4, 512], BF16)
    din('maskI', [128, 4, 512], BF16)
    din('onesf', [128, 64])
    din('bvec', [10, 128, 128])
    din('sinks', [1, 2])
    kind = "ExternalOutput" if dbg else "Internal"
    d['QKT'] = nc.dram_tensor('QKT', [128, 8, S], BF16, kind=kind).ap()
    d['VSB'] = nc.dram_tensor('VSB', [128, NT, 64], BF16, kind=kind).ap()
    d['VML'] = nc.dram_tensor('VML', [128, NT, 65], BF16, kind=kind).ap()
    d['VSW'] = nc.dram_tensor('VSW', [128, NT, 33], BF16, kind=kind).ap()
    for g in range(3):
        d['VD%d' % g] = nc.dram_tensor('VD%d' % g, [S, 33], BF16, kind=kind).ap()
    d['OUT'] = nc.dram_tensor('OUT', [224, S], BF16, kind="ExternalOutput").ap()
    with ExitStack() as es:
        sb = Arena(nc, es, 196 * 1024)
        PS = [es.enter_context(nc.psum_tensor("ps%d" % i, [128, 512], F32))[:, :] for i in range(8)]
        p = Prog(nc, es)
        block = es.enter_context(nc.Block())
        d['xrows'] = lambda t: d['x'][t * 128:(t + 1) * 128, :]
        d['GL'] = nc.dram_tensor('GL', [128, S], BF16, kind="Internal").ap()
        emitters = dict(inproj=emit_inproj, sb=emit_sb, mla=emit_mla, sw=emit_sw, dil=emit_dil)
        for ph in phases:
            sb.reset()
            emitters[ph](p, sb, PS, d, S)
            p.barrier()
        p.finish(block)
    return nc, p


def bf(a):
    return np.ascontiguousarray(a).astype(ml_dtypes.bfloat16)


def t5_bucket_np(dist):
    dist = np.asarray(dist, np.int64)
    d_ = np.maximum(dist, 1).astype(np.float32)
    large = 16 + (np.log(d_ / np.float32(16)) / np.float32(math.log(2048 / 16)) * np.float32(16)).astype(np.int32)
    large = np.minimum(large, 31)
    return np.where(dist < 16, dist, large)


def consts_A():
    k = np.arange(128)
    c = {}
    c['ident'] = bf(np.eye(128, dtype=np.float32))
    c['triN'] = bf(-(k[:, None] >= k[None, :]).astype(np.float32))
    c['onesb'] = bf(np.ones((128, 128), np.float32))
    qi = np.arange(512)
    mS = np.zeros((128, 4, 512), np.float32)
    mI = np.zeros((128, 4, 512), np.float32)
    for r in range(4):
        mS[:, r, :] = (128 * r + k[:, None]) < qi[None, :]
        mI[:, r, :] = (128 * r + k[:, None]) <= qi[None, :]
    c['maskS'] = bf(mS)
    c['maskI'] = bf(mI)
    c['onesf'] = np.ones((128, 64), np.float32)
    half = 16
    inv = (10000.0 ** (-np.arange(half, dtype=np.float32) / half)).astype(np.float32)
    ang = np.arange(SEQ, dtype=np.float32)[:, None] * inv[None, :]
    c['cos'] = np.ascontiguousarray(np.cos(ang).astype(np.float32).reshape(SEQ // 128, 128, 16).transpose(1, 0, 2))
    c['sin'] = np.ascontiguousarray(np.sin(ang).astype(np.float32).reshape(SEQ // 128, 128, 16).transpose(1, 0, 2))
    return c


def band_bias_vecs(rel_bias, h):
    dd = np.arange(-127, 128)
    out = np.full((10, 255), NEG, np.float32)
    for i, hq in enumerate((2 * h, 2 * h + 1)):
        cur = dd >= 0
        out[2 * i, cur] = rel_bias[t5_bucket_np(dd[cur]), hq]
        prv = dd < 0
        out[2 * i + 1, prv] = rel_bias[t5_bucket_np(128 + dd[prv]), hq]
    for g, dil in enumerate(DILS):
        col = 8 + g * 4 + h
        cur = dd >= 0
        out[4 + 2 * g, cur] = rel_bias[t5_bucket_np(dd[cur] * dil), col]
        prv = dd <= 0
        out[5 + 2 * g, prv] = rel_bias[t5_bucket_np((128 + dd[prv]) * dil), col]
    return out


def inputs_A(c, l, b, h, inp, x_b, S):
    cols = core_cols(h)
    m = dict(c)
    m['x'] = np.ascontiguousarray(x_b[:S])
    m['w_in'] = np.ascontiguousarray(np.concatenate([inp['w_in'][l][:, cols], inp['w_gate_a'][l]], axis=1))
    m['g1'] = np.ascontiguousarray(inp['norm1_g'][l].reshape(8, 128).T)
    m['wqb'] = np.ascontiguousarray(inp['w_qb'][l][:, h * 96:(h + 1) * 96])
    m['gqa'] = np.ascontiguousarray(inp['g_qa'][l].reshape(2, 128).T)
    m['wkvb'] = np.ascontiguousarray(inp['w_kvb'][l][:, h * 128:(h + 1) * 128])
    m['gkva'] = np.ascontiguousarray(inp['g_kva'][l].reshape(2, 128).T)
    gs, gd = inp['qk_g_sw'][l], inp['qk_g_dil'][l]
    m['g32'] = np.concatenate([gs[0], gd[0], gd[0], gs[1], gd[1], gd[1], gs[0], gd[0], gd[1]]).astype(np.float32)
    m['g96'] = np.concatenate([inp['qk_g_mla'][l][0], inp['qk_g_mla'][l][1]]).astype(np.float32)
    bv = band_bias_vecs(inp['rel_bias'], h)
    kk = np.arange(128)
    m['bvec'] = np.ascontiguousarray(bv[:, kk[None, :] - kk[:, None] + 127])
    m['sinks'] = np.ascontiguousarray(inp['sinks'][l][2 * h:2 * h + 2].reshape(1, 2))
    return m


BR_CHUNKS = ((0, 1), (2, 3), (4, 5), (6,))


def castw_iter(p, sb, d, nblk, F, engs=('vector', 'gpsimd', 'scalar')):
    stg = [sb("cw_stg%d" % i, [128, 2 * F], F32) for i in range(2)]
    stb = [sb("cw_stb%d" % i, [128, 2 * F], BF16) for i in range(2)]
    it = 0
    for blk in range(nblk):
        for kc in range(8):
            b = it % 2
            rows = slice(kc * 128, (kc + 1) * 128)
            p.dma('sync', stg[b][:, 0:F], d['wg'](blk)[rows, :], [], [('cw_stg', b)], 'cw_stg%d' % b)
            p.dma('sync', stg[b][:, F:2 * F], d['wu'](blk)[rows, :], [], [('cw_stg', b)], 'cw_stg%d' % b)
            p.copy(engs[it % len(engs)], stb[b], stg[b], [('cw_stg', b)], [('cw_stb', b)])
            p.dma(STQ, d['WGU'][blk, :, kc, :], stb[b], [('cw_stb', b)], [('WGU', b)], 'cw_stb%d' % b)
            it += 1
            yield
        for fc in range(F // 128):
            b = it % 2
            p.dma('sync', stg[b][:, 0:1024], d['wd'](blk)[fc * 128:(fc + 1) * 128, :], [], [('cw_stg', b)], 'cw_stg%d' % b)
            p.copy(engs[it % len(engs)], stb[b][:, 0:1024], stg[b][:, 0:1024], [('cw_stg', b)], [('cw_stb', b)])
            p.dma(STQ, d['WD'][blk, :, fc, :], stb[b][:, 0:1024], [('cw_stb', b)], [('WD', b)], 'cw_stb%d' % b)
            it += 1
            yield


def emit_castw(p, sb, PS, d, nblk, F):
    for _ in castw_iter(p, sb, d, nblk, F):
        pass


def emit_B(p, sb, PS, d, NTOK, nblk, F, moe):
    NCH = NTOK // 512
    FC = F // 128
    identb = sb("identb", [128, 128], BF16)
    p.dma('sync', identb, d['ident'][:, :], [], ['identb'], 'identb')
    G1 = sb("G1", [128, 1024], F32)
    G2 = sb("G2", [128, 1024], F32)
    p.dma('sync', G1, d['g1'].partition_broadcast(128), [], ['G1'], 'G1')
    p.dma('sync', G2, d['g2'].partition_broadcast(128), [], ['G2'], 'G2')
    bgt = sb("bgt", [128, 32], F32)
    p.dma('sync', bgt, d['bgate'][:, :], [], ['bgt'], 'bgt')
    Wga = sb("Wga", [128, 8, 128], BF16)
    Wgb = sb("Wgb", [128, 4096], BF16)
    Wbr = sb("Wbr", [128, 7, 1024], BF16)
    Wout = sb("Wout", [128, 8, 1024], BF16)
    stg = [sb("w_stg%d" % i, [128, 1024], F32) for i in range(2)]
    engs = ['vector', 'gpsimd', 'scalar']
    it = 0

    def cast_in(dst, src, n):
        nonlocal it
        b = it % 2
        p.dma('sync', stg[b][:, 0:n], src, [], [('w_stg', b)], 'w_stg%d' % b)
        p.copy(engs[it % 3], dst, stg[b][:, 0:n], [('w_stg', b)], ['Wres'])
        it += 1
    for kc in range(8):
        cast_in(Wga[:, kc, :], d['wga'][kc * 128:(kc + 1) * 128, :], 128)
        cast_in(Wout[:, kc, :], d['wout'][kc * 128:(kc + 1) * 128, :], 1024)
    for i in range(4):
        cast_in(Wgb[:, i * 1024:(i + 1) * 1024], d['wgb'][:, i * 1024:(i + 1) * 1024], 1024)
    for rc in range(7):
        cast_in(Wbr[:, rc, :], d['wbr'][rc * 128:(rc + 1) * 128, :], 1024)
    if moe:
        identf = sb("identf", [128, 128], F32)
        p.dma('sync', identf, d['identf'][:, :], [], ['identf'], 'identf')
        Wr = sb("Wr", [128, 8, 8], F32)
        p.dma('sync', Wr, d['wr'].rearrange("(kc p) e -> p kc e", p=128), [], ['Wr'], 'Wr')
        brt = sb("brt", [128, 8], F32)
        p.dma('sync', brt, d['br'].partition_broadcast(128), [], ['brt'], 'brt')
        h2f = sb("h2f", [128, 1024], F32)
        h2fT = sb("h2fT", [128, 8, 128], F32)
        rt = sb("rt", [128, 64], F32)
        GW = sb("GW", [128, 4, 8], F32)
    X4 = sb("X4", [128, 4, 1024], F32)
    oTc = sb("oTc", [128, 7, 512], BF16)
    hT = sb("hT", [128, 8, 512], BF16)
    hb = sb("hb", [128, 1024], BF16)
    junk = sb("junk", [128, 1024], BF16)
    st = sb("st", [128, 8], F32)
    glT = sb("glT", [128, 512], BF16)
    gate = [sb("gate%d" % i, [128, 512], F32) for i in range(2)]
    tmpy = sb("tmpy", [128, 512], F32)
    yacc = sb("yacc", [128, 512], F32)
    yT = sb("yT", [128, 8, 512], BF16)
    sg = [sb("sg%d" % i, [128, 512], F32) for i in range(2)]
    WGU_t = [sb("WGU_t%d" % i, [128, 8, 2 * F], BF16) for i in range(2)]
    WD_t = [sb("WD_t%d" % i, [128, FC, 1024], BF16) for i in range(2)]
    psTb = PS[0].bitcast(BF16)
    x, out = d['x'], d['out']
    wit = 0

    def norm_T(tt, G, Gk):
        xt = X4[:, tt, :]
        p.act(junk, xt, AF.Square, ['X4'], ['junk', 'ssq'], accum_out=st[:, 0:1])
        p.act(st[:, 1:2], st[:, 0:1], AF.Sqrt, ['ssq'], ['rs'], scale=1.0 / D, bias=EPS)
        p.recip(st[:, 2:3], st[:, 1:2], ['rs'], ['rstd'])
        p.stt(hb, xt, st[:, 2:3], G, ALU.mult, ALU.mult, ['X4', 'rstd', Gk], ['hb'])
        for kc in range(8):
            p.tr(psTb[:, kc * 128:(kc + 1) * 128], hb[:, kc * 128:(kc + 1) * 128], identb, ['hb', 'identb'], [PK(0)])
        p.copy('vector', hT[:, :, tt * 128:(tt + 1) * 128], psTb.rearrange("p (a d) -> p a d", d=128), [PK(0)], ['hT'])

    for c in range(NCH):
        T0 = c * 512
        p.dma('sync', X4, x[T0:T0 + 512, :].rearrange("(t p) d -> p t d", p=128), [], ['X4'], 'X4')
        p.dma('sync', oTc, d['oT'][:, T0:T0 + 512].rearrange("(r p) t -> p r t", p=128), [], ['oTc'], 'oTc')
        for tt in range(4):
            norm_T(tt, G1, 'G1')
        for kc in range(8):
            p.mm(PS[1], Wga[:, kc, :], hT[:, kc, :], kc == 0, kc == 7, ['hT', 'Wres'], [PK(1)])
        p.copy('vector', glT, PS[1], [PK(1)], ['glT'])
        k = 0
        for oc in range(8):
            for i in range(4):
                b = k % 2
                k += 1
                gp, gk = PS[2 + b], PK(2 + b)
                bp, bk = PS[4 + b], PK(4 + b)
                col = i * 1024 + oc * 128
                p.mm(gp, Wgb[:, col:col + 128], glT, True, True, ['glT', 'Wres'], [gk])
                p.act(gate[b], gp, AF.Sigmoid, [gk, 'bgt'], [('gate', b)], bias=bgt[:, i * 8 + oc:i * 8 + oc + 1])
                rcs = BR_CHUNKS[i]
                for n_, rc in enumerate(rcs):
                    p.mm(bp, Wbr[:, rc, oc * 128:(oc + 1) * 128], oTc[:, rc, :], n_ == 0, n_ == len(rcs) - 1, ['oTc', 'Wres'], [bk])
                if i == 0:
                    p.tt('vector', yacc, gate[b], bp, ALU.mult, [('gate', b), bk], ['yacc'])
                else:
                    p.tt('vector', tmpy, gate[b], bp, ALU.mult, [('gate', b), bk], ['tmpy'])
                    if i < 3:
                        p.tt('gpsimd', yacc, yacc, tmpy, ALU.add, ['yacc', 'tmpy'], ['yacc'])
                    else:
                        p.tt('gpsimd', yT[:, oc, :], yacc, tmpy, ALU.add, ['yacc', 'tmpy'], ['yT'])
        k = 0
        for tt in range(4):
            for half in range(2):
                b = k % 2
                k += 1
                ps, pk = PS[6 + b], PK(6 + b)
                for oc in range(8):
                    p.mm(ps, yT[:, oc, tt * 128:(tt + 1) * 128], Wout[:, oc, half * 512:(half + 1) * 512], oc == 0, oc == 7, ['yT', 'Wres'], [pk])
                xs = X4[:, tt, half * 512:(half + 1) * 512]
                p.tt('vector', xs, ps, xs, ALU.add, [pk, 'X4'], ['X4'])
        for tt in range(4):
            norm_T(tt, G2, 'G2')
            if moe:
                p.stt(h2f, X4[:, tt, :], st[:, 2:3], G2, ALU.mult, ALU.mult, ['X4', 'rstd', 'G2'], ['h2f'])
                for kc in range(8):
                    bnk = 2 + kc // 4
                    p.tr(PS[bnk][:, (kc % 4) * 128:(kc % 4 + 1) * 128], h2f[:, kc * 128:(kc + 1) * 128], identf, ['h2f', 'identf'], [PK(bnk)])
                p.copy('vector', h2fT[:, 0:4, :].rearrange("p a d -> p (a d)"), PS[2], [PK(2)], ['h2fT'])
                p.copy('vector', h2fT[:, 4:8, :].rearrange("p a d -> p (a d)"), PS[3], [PK(3)], ['h2fT'])
                for kc in range(8):
                    p.mm(PS[1][:, 0:8], h2fT[:, kc, :], Wr[:, kc, :], kc == 0, kc == 7, ['h2fT', 'Wr'], [PK(1)])
                lg, m8, ee, mk = rt[:, 0:8], rt[:, 8:16], rt[:, 16:24], rt[:, 24:32]
                p.tt('vector', lg, PS[1][:, 0:8], brt, ALU.add, [PK(1), 'brt'], ['lg'])
                p.op('vector', lambda e, lg=lg, m8=m8: e.max(out=m8, in_=lg), ['lg'], ['m8'])
                p.ts('vector', rt[:, 32:33], m8[:, 0:1], -1.0, ALU.mult, ['m8'], ['negm'])
                p.act(ee, lg, AF.Exp, ['lg', 'negm'], ['ee'], bias=rt[:, 32:33])
                p.ts('vector', mk, lg, m8[:, 1:2], ALU.is_ge, ['lg', 'm8'], ['mk'])
                p.tt('vector', ee, ee, mk, ALU.mult, ['ee', 'mk'], ['ee'])
                p.op('vector', lambda e, ee=ee: e.tensor_reduce(out=rt[:, 33:34], in_=ee, axis=AX.X, op=ALU.add), ['ee'], ['den'])
                p.recip(rt[:, 34:35], rt[:, 33:34], ['den'], ['rden'])
                p.ts('vector', GW[:, tt, :], ee, rt[:, 34:35], ALU.mult, ['ee', 'rden'], ['GW'])
        aT = yT
        for blk in range(nblk):
            wb = wit % 2
            wit += 1
            p.dma('sync', WGU_t[wb], d['WGU'][blk], [], [('WGU_t', wb)], 'WGU_t%d' % wb)
            p.dma('sync', WD_t[wb], d['WD'][blk], [], [('WD_t', wb)], 'WD_t%d' % wb)
            for fc in range(FC):
                b = fc % 2
                gps, gk = PS[2 + b], PK(2 + b)
                ups, uk = PS[4 + b], PK(4 + b)
                for kc in range(8):
                    p.mm(gps, WGU_t[wb][:, kc, fc * 128:(fc + 1) * 128], hT[:, kc, :], kc == 0, kc == 7, ['hT', ('WGU_t', wb)], [gk])
                for kc in range(8):
                    p.mm(ups, WGU_t[wb][:, kc, F + fc * 128:F + (fc + 1) * 128], hT[:, kc, :], kc == 0, kc == 7, ['hT', ('WGU_t', wb)], [uk])
                p.act(sg[b], gps, AF.Silu, [gk], [('sg', b)])
                p.tt('vector', aT[:, fc, :], sg[b], ups, ALU.mult, [('sg', b), uk], ['yT'])
            k = 0
            for tt in range(4):
                for half in range(2):
                    b = k % 2
                    k += 1
                    ps, pk = PS[6 + b], PK(6 + b)
                    for fc in range(FC):
                        p.mm(ps, aT[:, fc, tt * 128:(tt + 1) * 128], WD_t[wb][:, fc, half * 512:(half + 1) * 512], fc == 0, fc == FC - 1, ['yT', ('WD_t', wb)], [pk])
                    xs = X4[:, tt, half * 512:(half + 1) * 512]
                    if moe:
                        p.stt(xs, ps, GW[:, tt, blk:blk + 1], xs, ALU.mult, ALU.add, [pk, 'GW', 'X4'], ['X4'])
                    else:
                        p.tt('vector', xs, ps, xs, ALU.add, [pk, 'X4'], ['X4'])
        p.dma(STQ, out[T0:T0 + 512, :].rearrange("(t p) d -> p t d", p=128), X4, ['X4'], ['out'], 'X4o')


def build_B(NTOK, moe):
    nc = bass.Bass("TRN2", target_bir_lowering=False)
    d = {}

    def din(name, shape, dt=F32):
        d[name] = nc.dram_tensor(name, shape, dt, kind="ExternalInput").ap()

    din('x', [NTOK, D])
    din('oT', [896, NTOK], BF16)
    din('ident', [128, 128], BF16)
    din('g1', [1024])
    din('g2', [1024])
    din('bgate', [128, 32])
    din('wga', [1024, 128])
    din('wgb', [128, 4096])
    din('wbr', [896, 1024])
    din('wout', [1024, 1024])
    if moe:
        nblk, F = 8, 768
        din('identf', [128, 128])
        din('wr', [1024, 8])
        din('br', [8])
        din('wgu', [8, 1024, 1536])
        din('wdn', [8, 768, 1024])
        d['wg'] = lambda blk: d['wgu'][blk, :, 0:768]
        d['wu'] = lambda blk: d['wgu'][blk, :, 768:1536]
        d['wd'] = lambda blk: d['wdn'][blk, :, :]
    else:
        nblk, F = 4, 512
        din('wgu', [1024, 4096])
        din('wdn', [2048, 1024])
        d['wg'] = lambda blk: d['wgu'][:, blk * 512:(blk + 1) * 512]
        d['wu'] = lambda blk: d['wgu'][:, 2048 + blk * 512:2048 + (blk + 1) * 512]
        d['wd'] = lambda blk: d['wdn'][blk * 512:(blk + 1) * 512, :]
    d['WGU'] = nc.dram_tensor('WGU', [nblk, 128, 8, 2 * F], BF16, kind="Internal").ap()
    d['WD'] = nc.dram_tensor('WD', [nblk, 128, F // 128, 1024], BF16, kind="Internal").ap()
    d['out'] = nc.dram_tensor('out', [NTOK, D], F32, kind="ExternalOutput").ap()
    with ExitStack() as es:
        sb = Arena(nc, es, 200 * 1024)
        PS = [es.enter_context(nc.psum_tensor("ps%d" % i, [128, 512], F32))[:, :] for i in range(8)]
        p = Prog(nc, es)
        block = es.enter_context(nc.Block())
        emit_castw(p, sb, PS, d, nblk, F)
        p.barrier()
        sb.reset()
        emit_B(p, sb, PS, d, NTOK, nblk, F, moe)
        p.barrier()
        p.finish(block)
    return nc, p


def inputs_B(l, inp, x_tok, oT_tok, moe):
    m = {}
    m['x'] = np.ascontiguousarray(x_tok)
    m['oT'] = np.ascontiguousarray(oT_tok)
    m['ident'] = bf(np.eye(128, dtype=np.float32))
    m['g1'] = np.ascontiguousarray(inp['norm1_g'][l])
    m['g2'] = np.ascontiguousarray(inp['norm2_g'][l])
    m['bgate'] = np.ascontiguousarray(inp['b_gate'][l].reshape(32, 128).T)
    m['wga'] = np.ascontiguousarray(inp['w_gate_a'][l])
    m['wgb'] = np.ascontiguousarray(inp['w_gate_b'][l])
    m['wbr'] = np.ascontiguousarray(inp['w_branch'][l])
    m['wout'] = np.ascontiguousarray(inp['w_out'][l])
    if moe:
        m['identf'] = np.eye(128, dtype=np.float32)
        m['wr'] = np.ascontiguousarray(inp['w_router'][l // 2])
        m['br'] = np.ascontiguousarray(inp['b_router'][l // 2])
        m['wgu'] = np.ascontiguousarray(inp['w_gu_exp'][l // 2])
        m['wdn'] = np.ascontiguousarray(inp['w_down_exp'][l // 2])
    else:
        m['wgu'] = np.ascontiguousarray(inp['w_gu_dense'][l // 2])
        m['wdn'] = np.ascontiguousarray(inp['w_down_dense'][l // 2])
    return m


_CACHE = {}


def _prog(key, fn):
    if key not in _CACHE:
        _CACHE[key] = fn()
    return _CACHE[key]


def assemble_oT(outs):
    res = []
    for b in range(BATCH):
        rows = [None] * 4
        o = [np.asarray(outs[b * 4 + h]) for h in range(4)]
        sbp = np.concatenate([o[h][0:64] for h in range(4)], axis=0)
        mlp = np.concatenate([o[h][64:128] for h in range(4)], axis=0)
        swp = np.concatenate([o[h][128:192] for h in range(4)], axis=0)
        dlp = np.concatenate([o[h][192:224] for h in range(4)], axis=0)
        res.append(np.concatenate([sbp, mlp, swp, dlp], axis=0))
    return res


def kernel(**inp):
    inp = {k: np.asarray(v) for k, v in inp.items()}
    return kernel_fused(inp)


GROUPS = [[0, 1, 2, 3], [4, 5, 6, 7]]


def emit_merge(p, sb, PS, d, S):
    SL = S // 4
    bgt = sb("bgt", [128, 32], F32)
    p.dma('sync', bgt, d['bgate'][:, :], [], ['bgt'], 'bgt')
    Wgb = sb("Wgb", [128, 4096], BF16)
    Wbr = sb("Wbrc", [64, 4, 1024], BF16)
    stg = [sb("w_stg%d" % i, [128, 1024], F32) for i in range(2)]
    engs = ['vector', 'gpsimd', 'scalar']
    it = 0
    for i in range(4):
        b = it % 2
        p.dma('sync', stg[b], d['wgb'][:, i * 1024:(i + 1) * 1024], [], [('w_stg', b)], 'w_stg%d' % b)
        p.copy(engs[it % 3], Wgb[:, i * 1024:(i + 1) * 1024], stg[b], [('w_stg', b)], ['Wres'])
        it += 1
    rows = ((0, 64), (64, 64), (128, 64), (192, 32))
    for i, (r0, nr) in enumerate(rows):
        b = it % 2
        p.dma('sync', stg[b][0:nr, :], d['wbrc'][r0:r0 + nr, :], [], [('w_stg', b)], 'w_stg%d' % b)
        p.copy(engs[it % 3], Wbr[0:nr, i, :], stg[b][0:nr, :], [('w_stg', b)], ['Wres'])
        it += 1
    glT = [sb("glT%d" % i, [128, 512], BF16) for i in range(2)]
    oc_t = [sb("oc_t%d" % i, [64, 4, 512], BF16) for i in range(2)]
    gate = [sb("gate%d" % i, [128, 512], F32) for i in range(2)]
    tmpy = sb("tmpy", [128, 512], F32)
    yacc = sb("yacc", [128, 512], F32)
    yst = [sb("yst%d" % i, [128, 8, 512], F32) for i in range(2)]
    k = 0
    SLp = min(SL, 1024)
    NPc = SL // SLp
    order = [(sl * SL + q * SLp) // 512 + cc for q in range(NPc) for sl in range(4) for cc in range(SLp // 512)]
    for ci, c in enumerate(order):
        cb_ = ci % 2
        cs = slice(c * 512, (c + 1) * 512)
        p.dma('sync', glT[cb_], d['GL'][:, cs], [], [('glT', cb_)], 'glT%d' % cb_)
        for i, (r0, nr) in enumerate(rows):
            p.dma('sync', oc_t[cb_][0:nr, i, :], d['OUT'][r0:r0 + nr, cs], [], [('oc_t', cb_)], 'oc_t%d' % cb_)
        for oc in range(8):
            for i, (r0, nr) in enumerate(rows):
                b = k % 2
                k += 1
                gp, gk = PS[2 + b], PK(2 + b)
                bp, bk = PS[4 + b], PK(4 + b)
                col = i * 1024 + oc * 128
                p.mm(gp, Wgb[:, col:col + 128], glT[cb_], True, True, [('glT', cb_), 'Wres'], [gk])
                p.act(gate[b], gp, AF.Sigmoid, [gk, 'bgt'], [('gate', b)], bias=bgt[:, i * 8 + oc:i * 8 + oc + 1])
                p.mm(bp, Wbr[0:nr, i, oc * 128:(oc + 1) * 128], oc_t[cb_][0:nr, i, :], True, True, [('oc_t', cb_), 'Wres'], [bk])
                if i == 0:
                    p.tt('vector', yacc, gate[b], bp, ALU.mult, [('gate', b), bk], ['yacc'])
                else:
                    p.tt('vector', tmpy, gate[b], bp, ALU.mult, [('gate', b), bk], ['tmpy'])
                    if i < 3:
                        p.tt('gpsimd', yacc, yacc, tmpy, ALU.add, ['yacc', 'tmpy'], ['yacc'])
                    else:
                        p.tt('gpsimd', yst[cb_][:, oc, :], yacc, tmpy, ALU.add, ['yacc', 'tmpy'], [('yst', cb_)])
        sl, w = (c * 512) // SL, (c * 512) % SL
        q, t0 = w // SLp, w % SLp
        p.dma('sync', d['YP'][q, sl, :, t0:t0 + 512].rearrange("(oc p) t -> p oc t", p=128), yst[cb_], [('yst', cb_)], [('YP', cb_)], 'yst%d' % cb_)
        if (ci + 1) % (4 * (SLp // 512)) == 0:
            p.cc("ReduceScatter", ALU.add, GROUPS, d['YP'][q].rearrange("a c t -> (a c) t"), d['YS'][q], [('YP', 0), ('YP', 1)], [('YS', q)])


def emit_B2(p, sb, PS, d, NTOK, nblk, F, moe):
    NCH = NTOK // 512
    FC = F // 128
    identb = sb("identb", [128, 128], BF16)
    p.dma('sync', identb, d['ident'][:, :], [], ['identb'], 'identb')
    G2 = sb("G2", [128, 1024], F32)
    p.dma('sync', G2, d['g2'].partition_broadcast(128), [], ['G2'], 'G2')
    Wout = sb("Wout", [128, 8, 1024], BF16)
    stg = [sb("w_stg%d" % i, [128, 1024], F32) for i in range(2)]
    engs = ['vector', 'gpsimd', 'scalar']
    for kc in range(8):
        b = kc % 2
        p.dma('sync', stg[b], d['wout'][kc * 128:(kc + 1) * 128, :], [], [('w_stg', b)], 'w_stg%d' % b)
        p.copy(engs[kc % 3], Wout[:, kc, :], stg[b], [('w_stg', b)], ['Wres'])
    if moe:
        identf = sb("identf", [128, 128], F32)
        p.dma('sync', identf, d['identf'][:, :], [], ['identf'], 'identf')
        Wr = sb("Wr", [128, 8, 8], F32)
        p.dma('sync', Wr, d['wr'].rearrange("(kc p) e -> p kc e", p=128), [], ['Wr'], 'Wr')
        brt = sb("brt", [128, 8], F32)
        p.dma('sync', brt, d['br'].partition_broadcast(128), [], ['brt'], 'brt')
        h2f = sb("h2f", [128, 1024], F32)
        h2fT = sb("h2fT", [128, 8, 128], F32)
        rt = sb("rt", [128, 64], F32)
        GW = sb("GW", [128, 4, 8], F32)
    X4 = sb("X4", [128, 4, 1024], F32)
    yf = sb("yf", [128, 8, 512], F32)
    hT = sb("hT", [128, 8, 512], BF16)
    hb = sb("hb", [128, 1024], BF16)
    junk = sb("junk", [128, 1024], BF16)
    st = sb("st", [128, 8], F32)
    yT = sb("yT", [128, 8, 512], BF16)
    sg = [sb("sg%d" % i, [128, 512], F32) for i in range(2)]
    WGU_t = [sb("WGU_t%d" % i, [128, 8, 2 * F], BF16) for i in range(2)]
    WD_t = [sb("WD_t%d" % i, [128, FC, 1024], BF16) for i in range(2)]
    psTb = PS[0].bitcast(BF16)
    wit = 0

    def norm_T(tt, G, Gk):
        xt = X4[:, tt, :]
        p.act(junk, xt, AF.Square, ['X4'], ['junk', 'ssq'], accum_out=st[:, 0:1])
        p.act(st[:, 1:2], st[:, 0:1], AF.Sqrt, ['ssq'], ['rs'], scale=1.0 / D, bias=EPS)
        p.recip(st[:, 2:3], st[:, 1:2], ['rs'], ['rstd'])
        p.stt(hb, xt, st[:, 2:3], G, ALU.mult, ALU.mult, ['X4', 'rstd', Gk], ['hb'])
        for kc in range(8):
            p.tr(psTb[:, kc * 128:(kc + 1) * 128], hb[:, kc * 128:(kc + 1) * 128], identb, ['hb', 'identb'], [PK(0)])
        p.copy('vector', hT[:, :, tt * 128:(tt + 1) * 128], psTb.rearrange("p (a d) -> p a d", d=128), [PK(0)], ['hT'])

    for c in range(NCH):
        T0 = c * 512
        p.dma('sync', X4, d['xtok'][T0:T0 + 512, :].rearrange("(t p) d -> p t d", p=128), [], ['X4'], 'X4')
        SLp = min(NTOK, 1024)
        p.dma('sync', yf, d['YS'][T0 // SLp, :, T0 % SLp:T0 % SLp + 512].rearrange("(oc p) t -> p oc t", p=128), [('YS', T0 // SLp)], ['yf'], 'yf')
        p.copy('gpsimd', yT[:, 0:4, :], yf[:, 0:4, :], ['yf'], ['yT'])
        p.copy('vector', yT[:, 4:8, :], yf[:, 4:8, :], ['yf'], ['yT'])
        k = 0
        for tt in range(4):
            for half in range(2):
                b = k % 2
                k += 1
                ps, pk = PS[6 + b], PK(6 + b)
                for oc in range(8):
                    p.mm(ps, yT[:, oc, tt * 128:(tt + 1) * 128], Wout[:, oc, half * 512:(half + 1) * 512], oc == 0, oc == 7, ['yT', 'Wres'], [pk])
                xs = X4[:, tt, half * 512:(half + 1) * 512]
                p.tt('vector', xs, ps, xs, ALU.add, [pk, 'X4'], ['X4'])
        for tt in range(4):
            norm_T(tt, G2, 'G2')
            if moe:
                p.stt(h2f, X4[:, tt, :], st[:, 2:3], G2, ALU.mult, ALU.mult, ['X4', 'rstd', 'G2'], ['h2f'])
                for kc in range(8):
                    bnk = 2 + kc // 4
                    p.tr(PS[bnk][:, (kc % 4) * 128:(kc % 4 + 1) * 128], h2f[:, kc * 128:(kc + 1) * 128], identf, ['h2f', 'identf'], [PK(bnk)])
                p.copy('vector', h2fT[:, 0:4, :].rearrange("p a d -> p (a d)"), PS[2], [PK(2)], ['h2fT'])
                p.copy('vector', h2fT[:, 4:8, :].rearrange("p a d -> p (a d)"), PS[3], [PK(3)], ['h2fT'])
                for kc in range(8):
                    p.mm(PS[1][:, 0:8], h2fT[:, kc, :], Wr[:, kc, :], kc == 0, kc == 7, ['h2fT', 'Wr'], [PK(1)])
                lg, m8, ee, mk = rt[:, 0:8], rt[:, 8:16], rt[:, 16:24], rt[:, 24:32]
                p.tt('vector', lg, PS[1][:, 0:8], brt, ALU.add, [PK(1), 'brt'], ['lg'])
                p.op('vector', lambda e, lg=lg, m8=m8: e.max(out=m8, in_=lg), ['lg'], ['m8'])
                p.ts('vector', rt[:, 32:33], m8[:, 0:1], -1.0, ALU.mult, ['m8'], ['negm'])
                p.act(ee, lg, AF.Exp, ['lg', 'negm'], ['ee'], bias=rt[:, 32:33])
                p.ts('vector', mk, lg, m8[:, 1:2], ALU.is_ge, ['lg', 'm8'], ['mk'])
                p.tt('vector', ee, ee, mk, ALU.mult, ['ee', 'mk'], ['ee'])
                p.op('vector', lambda e, ee=ee: e.tensor_reduce(out=rt[:, 33:34], in_=ee, axis=AX.X, op=ALU.add), ['ee'], ['den'])
                p.recip(rt[:, 34:35], rt[:, 33:34], ['den'], ['rden'])
                p.ts('vector', GW[:, tt, :], ee, rt[:, 34:35], ALU.mult, ['ee', 'rden'], ['GW'])
        aT = yT
        for blk in range(nblk):
            wb = wit % 2
            wit += 1
            p.dma('sync', WGU_t[wb], d['WGU'][blk], [], [('WGU_t', wb)], 'WGU_t%d' % wb)
            p.dma('sync', WD_t[wb], d['WD'][blk], [], [('WD_t', wb)], 'WD_t%d' % wb)
            for fc in range(FC):
                b = fc % 2
                gps, gk = PS[2 + b], PK(2 + b)
                ups, uk = PS[4 + b], PK(4 + b)
                for kc in range(8):
                    p.mm(gps, WGU_t[wb][:, kc, fc * 128:(fc + 1) * 128], hT[:, kc, :], kc == 0, kc == 7, ['hT', ('WGU_t', wb)], [gk])
                for kc in range(8):
                    p.mm(ups, WGU_t[wb][:, kc, F + fc * 128:F + (fc + 1) * 128], hT[:, kc, :], kc == 0, kc == 7, ['hT', ('WGU_t', wb)], [uk])
                p.act(sg[b], gps, AF.Silu, [gk], [('sg', b)])
                p.tt('vector', aT[:, fc, :], sg[b], ups, ALU.mult, [('sg', b), uk], ['yT'])
            k = 0
            for tt in range(4):
                for half in range(2):
                    b = k % 2
                    k += 1
                    ps, pk = PS[6 + b], PK(6 + b)
                    for fc in range(FC):
                        p.mm(ps, aT[:, fc, tt * 128:(tt + 1) * 128], WD_t[wb][:, fc, half * 512:(half + 1) * 512], fc == 0, fc == FC - 1, ['yT', ('WD_t', wb)], [pk])
                    xs = X4[:, tt, half * 512:(half + 1) * 512]
                    if moe:
                        p.stt(xs, ps, GW[:, tt, blk:blk + 1], xs, ALU.mult, ALU.add, [pk, 'GW', 'X4'], ['X4'])
                    else:
                        p.tt('vector', xs, ps, xs, ALU.add, [pk, 'X4'], ['X4'])
        p.dma('sync', d['xout'][T0:T0 + 512, :].rearrange("(t p) d -> p t d", p=128), X4, ['X4'], ['xout'], 'X4o')
        if d.get('XG') is not None:
            for kk in (2 * c, 2 * c + 1):
                p.cc("AllGather", ALU.bypass, GROUPS, d['xout'][kk * 256:(kk + 1) * 256, :], d['XG'][kk], ['xout'], [('XG', kk)])


def build_fused(S, n_layers=2):
    nc = bass.Bass("TRN2", target_bir_lowering=False)
    NT, NTOK, SL = S // 128, S // 4, S // 4
    g = {}

    def din(name, shape, dt=F32):
        g[name] = nc.dram_tensor(name, shape, dt, kind="ExternalInput").ap()

    def dint(name, shape, dt):
        g[name] = nc.dram_tensor(name, shape, dt, kind="Internal").ap()

    din('x', [S, D])
    din('xtok', [NTOK, D])
    for nm, shp, dt in (('cos', [128, SEQ // 128, 16], F32), ('sin', [128, SEQ // 128, 16], F32), ('ident', [128, 128], BF16),
                        ('triN', [128, 128], BF16), ('onesb', [128, 128], BF16), ('maskS', [128, 4, 512], BF16),
                        ('maskI', [128, 4, 512], BF16), ('onesf', [128, 64], F32), ('identf', [128, 128], F32), ('bvec', [10, 128, 128], F32)):
        din(nm, shp, dt)
    for L in range(n_layers):
        sfx = str(L)
        for nm, shp in (('w_in', [D, 1280]), ('g1', [128, 8]), ('wqb', [256, 96]), ('gqa', [128, 2]), ('wkvb', [256, 128]),
                        ('gkva', [128, 2]), ('g32', [288]), ('g96', [192]), ('sinks', [1, 2]), ('wgb', [128, 4096]),
                        ('bgate', [128, 32]), ('wbrc', [224, 1024]), ('g2', [1024]), ('wout', [1024, 1024])):
            din(nm + sfx, shp)
    din('wgu0', [1024, 4096])
    din('wdn0', [2048, 1024])
    if n_layers > 1:
        din('wr1', [1024, 8])
        din('br1', [8])
        din('wgu1', [8, 1024, 1536])
        din('wdn1', [8, 768, 1024])
    dint('QKT', [128, 8, S], BF16)
    dint('VSB', [128, NT, 64], BF16)
    dint('VML', [128, NT, 65], BF16)
    dint('VSW', [128, NT, 33], BF16)
    for i in range(3):
        dint('VD%d' % i, [S, 33], BF16)
    dint('GL', [128, S], BF16)
    dint('OUT', [224, S], BF16)
    SLp = min(SL, 1024)
    NP = SL // SLp
    NAG = NTOK // 256
    dint('YP', [NP, 4, 1024, SLp], F32)
    dint('YS', [NP, 1024, SLp], F32)
    dint('XS', [NTOK, D], F32)
    dint('XG', [NAG, 4 * 256, D], F32)
    dint('WGU0', [4, 128, 8, 1024], BF16)
    dint('WD0', [4, 128, 4, 1024], BF16)
    if n_layers > 1:
        dint('WGU1', [8, 128, 8, 1536], BF16)
        dint('WD1', [8, 128, 6, 1024], BF16)
    g['final'] = nc.dram_tensor('final', [NTOK, D], F32, kind="ExternalOutput").ap()
    shared = ('cos', 'sin', 'ident', 'triN', 'onesb', 'maskS', 'maskI', 'onesf', 'identf', 'bvec',
              'QKT', 'VSB', 'VML', 'VSW', 'VD0', 'VD1', 'VD2', 'GL', 'OUT', 'YP', 'YS')
    with ExitStack() as es:
        sb = Arena(nc, es, 200 * 1024)
        PS = [es.enter_context(nc.psum_tensor("ps%d" % i, [128, 512], F32))[:, :] for i in range(8)]
        p = Prog(nc, es)
        block = es.enter_context(nc.Block())
        for L in range(n_layers):
            sfx = str(L)
            moe = (L % 2 == 1)
            d = {k: g[k] for k in shared}
            for nm in ('w_in', 'g1', 'wqb', 'gqa', 'wkvb', 'gkva', 'g32', 'g96', 'sinks', 'wgb', 'bgate', 'wbrc', 'g2', 'wout'):
                d[nm] = g[nm + sfx]
            if L == 0:
                d['xrows'] = lambda t: g['x'][t * 128:(t + 1) * 128, :]
            else:
                def xrows(t):
                    T = t * 128
                    r, w = T // NTOK, T % NTOK
                    return g['XG'][w // 256, r * 256 + (w % 256):r * 256 + (w % 256) + 128, :]
                d['xrows'] = xrows
                d['xkeys'] = lambda t: [('XG', ((t * 128) % NTOK) // 256)]
            d['xtok'] = g['xtok'] if L == 0 else g['XS']
            d['xout'] = g['final'] if L == n_layers - 1 else g['XS']
            d['WGU'], d['WD'] = g['WGU' + sfx], g['WD' + sfx]
            if moe:
                nblk, F = 8, 768
                d['wr'], d['br'] = g['wr1'], g['br1']
                d['wg'] = lambda blk: g['wgu1'][blk, :, 0:768]
                d['wu'] = lambda blk: g['wgu1'][blk, :, 768:1536]
                d['wd'] = lambda blk: g['wdn1'][blk, :, :]
            else:
                nblk, F = 4, 512
                d['wg'] = lambda blk: g['wgu0'][:, blk * 512:(blk + 1) * 512]
                d['wu'] = lambda blk: g['wgu0'][:, 2048 + blk * 512:2048 + (blk + 1) * 512]
                d['wd'] = lambda blk: g['wdn0'][blk * 512:(blk + 1) * 512, :]
            d['XG'] = g['XG'] if L < n_layers - 1 else None
            d['n_side'] = nblk * (8 + F // 128)
            for ph in (emit_inproj, emit_sb, emit_mla, emit_sw, emit_dil):
                sb.reset()
                if ph is emit_sb:
                    ph(p, sb, PS, d, S, side=lambda sb_, d=d, nblk=nblk, F=F: castw_iter(p, sb_, d, nblk, F, engs=('gpsimd',)))
                else:
                    ph(p, sb, PS, d, S)
                p.barrier(exclude_cc=True, keep=('XG',))
            sb.reset()
            emit_merge(p, sb, PS, d, S)
            p.barrier(exclude_cc=True, keep=('YS',))
            sb.reset()
            emit_B2(p, sb, PS, d, NTOK, nblk, F, moe)
            p.barrier(exclude_cc=True, keep=('XG',))
        p.barrier()
        p.finish(block)
    return nc, p


def inputs_fused(cA, inp, core, S, n_layers=2):
    b, h = core // 4, core % 4
    NTOK = S // 4
    m = {k: cA[k] for k in ('cos', 'sin', 'ident', 'triN', 'onesb', 'maskS', 'maskI', 'onesf')}
    m['identf'] = np.eye(128, dtype=np.float32)
    x = np.asarray(inp['x'], np.float32)
    m['x'] = np.ascontiguousarray(x[b, :S])
    m['xtok'] = np.ascontiguousarray(x[b, h * NTOK:(h + 1) * NTOK])
    bv = band_bias_vecs(inp['rel_bias'], h)
    kk = np.arange(128)
    m['bvec'] = np.ascontiguousarray(bv[:, kk[None, :] - kk[:, None] + 127])
    cols = core_cols(h)
    for l in range(n_layers):
        s = str(l)
        m['w_in' + s] = np.ascontiguousarray(np.concatenate([inp['w_in'][l][:, cols], inp['w_gate_a'][l]], axis=1))
        m['g1' + s] = np.ascontiguousarray(inp['norm1_g'][l].reshape(8, 128).T)
        m['wqb' + s] = np.ascontiguousarray(inp['w_qb'][l][:, h * 96:(h + 1) * 96])
        m['gqa' + s] = np.ascontiguousarray(inp['g_qa'][l].reshape(2, 128).T)
        m['wkvb' + s] = np.ascontiguousarray(inp['w_kvb'][l][:, h * 128:(h + 1) * 128])
        m['gkva' + s] = np.ascontiguousarray(inp['g_kva'][l].reshape(2, 128).T)
        gs, gd = inp['qk_g_sw'][l], inp['qk_g_dil'][l]
        m['g32' + s] = np.concatenate([gs[0], gd[0], gd[0], gs[1], gd[1], gd[1], gs[0], gd[0], gd[1]]).astype(np.float32)
        m['g96' + s] = np.concatenate([inp['qk_g_mla'][l][0], inp['qk_g_mla'][l][1]]).astype(np.float32)
        m['sinks' + s] = np.ascontiguousarray(inp['sinks'][l][2 * h:2 * h + 2].reshape(1, 2))
        m['wgb' + s] = np.ascontiguousarray(inp['w_gate_b'][l])
        m['bgate' + s] = np.ascontiguousarray(inp['b_gate'][l].reshape(32, 128).T)
        wb = inp['w_branch'][l]
        m['wbrc' + s] = np.ascontiguousarray(np.concatenate([wb[h * 64:(h + 1) * 64], wb[256 + h * 64:256 + (h + 1) * 64],
                                                             wb[512 + h * 64:512 + (h + 1) * 64], wb[768 + h * 32:768 + (h + 1) * 32]], axis=0))
        m['g2' + s] = np.ascontiguousarray(inp['norm2_g'][l])
        m['wout' + s] = np.ascontiguousarray(inp['w_out'][l])
    m['wgu0'] = np.ascontiguousarray(inp['w_gu_dense'][0])
    m['wdn0'] = np.ascontiguousarray(inp['w_down_dense'][0])
    if n_layers > 1:
        m['wr1'] = np.ascontiguousarray(inp['w_router'][0])
        m['br1'] = np.ascontiguousarray(inp['b_router'][0])
        m['wgu1'] = np.ascontiguousarray(inp['w_gu_exp'][0])
        m['wdn1'] = np.ascontiguousarray(inp['w_down_exp'][0])
    return m


def kernel_fused(inp, S=SEQ, n_layers=2):
    cA = consts_A()
    ncF, _ = _prog(('F', S, n_layers), lambda: build_fused(S, n_layers))
    in_maps = [inputs_fused(cA, inp, c, S, n_layers) for c in range(8)]
    res = run_bass_kernel_spmd(ncF, in_maps, core_ids=list(range(8)))
    NTOK = S // 4
    out = np.empty((BATCH, S, D), np.float32)
    for c in range(8):
        out[c // 4, (c % 4) * NTOK:(c % 4 + 1) * NTOK] = np.asarray(res.results[c]['final'])
    return out
```

```python
import math
from contextlib import ExitStack

import numpy as np
import ml_dtypes

import concourse.bass as bass
import concourse.mybir as mybir
from concourse.bass_utils import run_bass_kernel_spmd

F32 = mybir.dt.float32
BF16 = mybir.dt.bfloat16
U8 = mybir.dt.uint8
AF = mybir.ActivationFunctionType
ALU = mybir.AluOpType
AX = mybir.AxisListType
ENGS = ['sync', 'scalar', 'vector', 'gpsimd', 'tensor']

D = 1024
SEQ = 16384
BATCH = 2
EPS = 1e-6
NEG = -30000.0
O_AQ, O_AK, O_AV, O_CQ, O_CKV, O_KPE, O_SWQ, O_SWK, O_SWV, O_DQ, O_DK, O_DV = (
    0, 256, 512, 768, 1024, 1280, 1312, 1568, 1632, 1696, 2080, 2464)
DILS = (1, 4, 16)
STQ = 'sync'
LIMIT = 10 ** 12
SKIP = ()


class Prog:
    def __init__(self, nc, es):
        self.nc, self.es = nc, es
        self.ops = {e: [] for e in ENGS}
        self.sems, self.cnt = {}, {}
        self.known = {e: {} for e in ENGS}
        self.lastw, self.readers = {}, {}
        self.n_instr = 0
        for e in ENGS:
            self._mksem('E_' + e)

    def _mksem(self, name):
        if name not in self.sems:
            self.sems[name] = self.es.enter_context(self.nc.semaphore(name))
            self.cnt[name] = 0
        return self.sems[name]

    def _deps(self, reads, writes):
        deps = {}

        def add(d):
            if d is not None and deps.get(d[0], 0) < d[1]:
                deps[d[0]] = d[1]
        for k in reads:
            add(self.lastw.get(k))
        for k in writes:
            add(self.lastw.get(k))
            for s, v in self.readers.get(k, {}).items():
                add((s, v))
        return deps

    def _waits(self, eng, deps):
        for s, v in deps.items():
            if eng == 'tensor' and s == 'E_tensor':
                continue
            if self.known[eng].get(s, 0) >= v:
                continue
            self.known[eng][s] = v
            h = self.sems[s]
            self.ops[eng].append(lambda e, h=h, v=v: e.wait_ge(h, v))
            self.n_instr += 1

    def _record(self, s, v, reads, writes):
        for k in writes:
            self.lastw[k] = (s, v)
            self.readers[k] = {}
        for k in reads:
            self.readers.setdefault(k, {})[s] = v

    def op(self, eng, fn, reads=(), writes=()):
        self.n_ops = getattr(self, 'n_ops', 0) + 1
        if self.n_ops > LIMIT or self.n_ops in SKIP:
            return
        self._waits(eng, self._deps(reads, writes))
        s = 'E_' + eng
        self.cnt[s] += 1
        h = self.sems[s]
        self.ops[eng].append(lambda e, fn=fn, h=h: fn(e).then_inc(h, 1))
        self._record(s, self.cnt[s], reads, writes)
        self.n_instr += 1

    def dma(self, q, out, in_, reads, writes, slot):
        self.n_ops = getattr(self, 'n_ops', 0) + 1
        if self.n_ops > LIMIT:
            return
        self._waits(q, self._deps(reads, writes))
        s = 'D_' + slot
        h = self._mksem(s)
        self.cnt[s] += 16
        self.ops[q].append(lambda e, h=h, out=out, in_=in_: e.dma_start(out=out, in_=in_).then_inc(h, 16))
        self._record(s, self.cnt[s], reads, writes)
        self.n_instr += 1

    def cc(self, kind, op, groups, in_, out, reads, writes):
        self._waits('gpsimd', self._deps(reads, writes))
        s = 'C_all'
        h = self._mksem(s)
        self.cnt[s] += 1
        self.ops['gpsimd'].append(lambda e, h=h: e.collective_compute(kind, op, replica_groups=groups, ins=[in_], outs=[out]).then_inc(h, 1))
        self._record(s, self.cnt[s], reads, writes)
        self.n_instr += 1

    def barrier(self, exclude_cc=False, keep=()):
        allv = {s: v for s, v in self.cnt.items() if v > 0 and not (exclude_cc and s == 'C_all')}
        for e in ENGS:
            self._waits(e, allv)
        kept = {k: v for k, v in self.lastw.items() if isinstance(k, tuple) and k[0] in keep}
        self.lastw, self.readers = kept, {}

    def finish(self, block):
        for name in ENGS:
            def body(e, name=name):
                for f in self.ops[name]:
                    f(e)
            getattr(block, name)(body)

    def mm(self, out, lhsT, rhs, start, stop, reads, writes, skip=False):
        self.op('tensor', lambda e: e.matmul(out, lhsT, rhs, start=start, stop=stop, skip_group_check=skip), reads, writes)

    def tr(self, out, in_, ident, reads, writes):
        self.op('tensor', lambda e: e.transpose(out, in_, ident), reads, writes)

    def act(self, out, in_, func, reads, writes, bias=None, scale=None, accum_out=None):
        kw = {}
        if bias is not None:
            kw['bias'] = bias
        if scale is not None:
            kw['scale'] = scale
        if accum_out is not None:
            kw['accum_out'] = accum_out
        self.op('scalar', lambda e: e.activation(out=out, in_=in_, func=func, **kw), reads, writes)

    def tt(self, eng, out, in0, in1, op, reads, writes):
        self.op(eng, lambda e: e.tensor_tensor(out=out, in0=in0, in1=in1, op=op), reads, writes)

    def ts(self, eng, out, in0, s1, op0, reads, writes, s2=None, op1=None):
        if op1 is None:
            self.op(eng, lambda e: e.tensor_scalar(out=out, in0=in0, scalar1=s1, scalar2=None, op0=op0), reads, writes)
        else:
            self.op(eng, lambda e: e.tensor_scalar(out=out, in0=in0, scalar1=s1, scalar2=s2, op0=op0, op1=op1), reads, writes)

    def stt(self, out, in0, scalar, in1, op0, op1, reads, writes):
        self.op('vector', lambda e: e.scalar_tensor_tensor(out=out, in0=in0, scalar=scalar, in1=in1, op0=op0, op1=op1), reads, writes)

    def copy(self, eng, out, in_, reads, writes):
        if eng == 'scalar':
            self.op(eng, lambda e: e.copy(out=out, in_=in_), reads, writes)
        else:
            self.op(eng, lambda e: e.tensor_copy(out=out, in_=in_), reads, writes)

    def recip(self, out, in_, reads, writes):
        self.op('vector', lambda e: e.reciprocal(out=out, in_=in_), reads, writes)

    def memset(self, eng, ap, val, writes):
        self.op(eng, lambda e: e.memset(ap, val), (), writes)


class Arena:
    def __init__(self, nc, es, nbytes):
        self.big = es.enter_context(nc.sbuf_tensor("arena", [128, nbytes], U8))
        self.nbytes = nbytes
        self.off = 0

    def reset(self):
        self.off = 0

    def __call__(self, name, shape, dt):
        esz = 2 if dt == BF16 else 4
        n = int(np.prod(shape[1:]))
        nb = (n * esz + 63) // 64 * 64
        assert self.off + nb <= self.nbytes, (name, self.off, nb)
        ap = self.big[:, self.off:self.off + n * esz].bitcast(dt)
        self.off += nb
        if len(shape) > 2:
            names = " ".join("d%d" % i for i in range(len(shape) - 1))
            ap = ap.rearrange("p (%s) -> p %s" % (names, names), **{"d%d" % i: shape[i + 1] for i in range(len(shape) - 2)})
        if shape[0] < 128:
            ap = ap[0:shape[0]]
        return ap


def PK(i):
    return ('ps', i)


def core_cols(h):
    r = lambda o, n: list(range(o, o + n))
    swq0 = r(O_SWQ + (2 * h) * 32, 32)
    swq1 = r(O_SWQ + (2 * h + 1) * 32, 32)
    swk = r(O_SWK + (h // 2) * 32, 32)
    swv = r(O_SWV + (h // 2) * 32, 32)
    dq = [r(O_DQ + (g * 4 + h) * 32, 32) for g in range(3)]
    dk = [r(O_DK + (g * 4 + h) * 32, 32) for g in range(3)]
    dv = [r(O_DV + (g * 4 + h) * 32, 32) for g in range(3)]
    g1 = r(O_CQ, 256) + r(O_CKV, 256)
    n32 = swq0 + dq[0] + dq[1] + swk + dk[0] + dk[1] + swq1 + dq[2] + dk[2]
    g2 = n32 + r(O_AQ + h * 64, 64) + r(O_AK + h * 64, 64) + r(O_KPE, 32) + r(O_AV + h * 64, 64)
    g3 = swv + dv[0] + dv[1] + dv[2]
    return np.array(g1 + g2 + g3)


def emit_inproj(p, sb, PS, d, S):
    NT = S // 128
    identb = sb("identb", [128, 128], BF16)
    p.dma('sync', identb, d['ident'][:, :], [], ['identb'], 'identb')
    g1t = sb("g1t", [128, 8], F32)
    p.dma('sync', g1t, d['g1'][:, :], [], ['g1t'], 'g1t')
    gqat = sb("gqat", [128, 2], F32)
    p.dma('sync', gqat, d['gqa'][:, :], [], ['gqat'], 'gqat')
    gkvat = sb("gkvat", [128, 2], F32)
    p.dma('sync', gkvat, d['gkva'][:, :], [], ['gkvat'], 'gkvat')
    G32 = sb("G32", [128, 288], F32)
    p.dma('sync', G32, d['g32'].partition_broadcast(128), [], ['G32'], 'G32')
    G96 = sb("G96", [128, 2, 96], F32)
    p.dma('sync', G96.rearrange("p a d -> p (a d)"), d['g96'].partition_broadcast(128), [], ['G96'], 'G96')
    COS = sb("COS", [128, NT, 16], F32)
    SIN = sb("SIN", [128, NT, 16], F32)
    p.dma('sync', COS, d['cos'][:, 0:NT, :], [], ['COS'], 'COS')
    p.dma('sync', SIN, d['sin'][:, 0:NT, :], [], ['SIN'], 'SIN')
    Wb = sb("Wb", [128, 8, 1280], BF16)
    wst = [sb("wst%d" % i, [128, 1280], F32) for i in range(2)]
    for kc in range(8):
        b = kc % 2
        p.dma('sync', wst[b], d['w_in'][kc * 128:(kc + 1) * 128, :], [], [('wst', b)], 'wst%d' % b)
        p.act(Wb[:, kc, :], wst[b], AF.Copy, [('wst', b), 'g1t'], ['Wb'], scale=g1t[:, kc:kc + 1])
    Wqb = sb("Wqb", [128, 2, 96], BF16)
    Wkvb = sb("Wkvb", [128, 2, 128], BF16)
    for i in range(2):
        p.dma('sync', wst[0][:, 0:96], d['wqb'][i * 128:(i + 1) * 128, :], [], [('wst', 0)], 'wst0')
        p.act(Wqb[:, i, :], wst[0][:, 0:96], AF.Copy, [('wst', 0), 'gqat'], ['Wqb'], scale=gqat[:, i:i + 1])
        p.dma('sync', wst[1][:, 0:128], d['wkvb'][i * 128:(i + 1) * 128, :], [], [('wst', 1)], 'wst1')
        p.act(Wkvb[:, i, :], wst[1][:, 0:128], AF.Copy, [('wst', 1), 'gkvat'], ['Wkvb'], scale=gkvat[:, i:i + 1])

    X = [sb("x%d" % i, [128, 1024], F32) for i in range(2)]
    junk = sb("junk", [128, 1024], BF16)
    hb = [sb("hb%d" % i, [128, 1024], BF16) for i in range(2)]
    hT = [sb("hT%d" % i, [128, 8, 128], BF16) for i in range(2)]
    st = sb("stats", [128, 32], F32)
    cb = sb("cb", [128, 512], BF16)
    cT = sb("cT", [128, 4, 128], BF16)
    qk96 = sb("qk96", [128, 2, 96], F32)
    tmp96 = sb("tmp96", [128, 2, 96], F32)
    qkb = sb("qkb", [128, 2, 96], BF16)
    rA = sb("ropeA", [128, 2, 2, 16], F32)
    rB = sb("ropeB", [128, 2, 2, 16], F32)
    sq32 = sb("sq32", [128, 288], F32)
    tmp32 = sb("tmp32", [128, 288], F32)
    n32b = sb("n32b", [128, 288], BF16)
    sbqk = sb("sbqk", [128, 128], BF16)
    stageT = [sb("stageT%d" % i, [128, 8, 512], BF16) for i in range(2)]
    stVS = [sb("stVS%d" % i, [128, 4, 64], BF16) for i in range(2)]
    stVM = [sb("stVM%d" % i, [128, 4, 65], BF16) for i in range(2)]
    stVG = [sb("stVG%d" % i, [128, 4, 4, 33], BF16) for i in range(2)]
    stGL = [sb("stGL%d" % i, [128, 512], BF16) for i in range(2)]
    glb = sb("glb", [128, 128], BF16)
    for i in range(2):
        p.memset('gpsimd', stageT[i], 0.0, [('stageT', i)])
        p.memset('gpsimd', stVM[i], 1.0, [('stVM', i)])
        p.memset('gpsimd', stVG[i], 1.0, [('stVG', i)])
    psTb = PS[0].bitcast(BF16)
    ps1, ps2, ps3 = PS[1], PS[2], PS[3]
    ps4b = PS[4].bitcast(BF16)
    ps5 = PS[5]
    ps6b = PS[6].bitcast(BF16)
    xrows = d['xrows']

    xkeys = d.get('xkeys', lambda t: [])
    p.dma('sync', X[0], xrows(0), xkeys(0), [('x', 0)], 'x0')
    for t in range(NT):
        b = t % 2
        tt = t % 4
        sg = (t // 4) % 2
        xk, hbk, hTk = ('x', b), ('hb', b), ('hT', b)
        if t + 1 < NT:
            p.dma('sync', X[1 - b], xrows(t + 1), xkeys(t + 1), [('x', 1 - b)], 'x%d' % (1 - b))
        p.act(junk, X[b], AF.Square, [xk], ['junk', 'ssq'], accum_out=st[:, 0:1])
        p.act(st[:, 1:2], st[:, 0:1], AF.Sqrt, ['ssq'], ['rs'], scale=1.0 / D, bias=EPS)
        p.recip(st[:, 2:3], st[:, 1:2], ['rs'], ['rstd'])
        p.act(hb[b], X[b], AF.Copy, [xk, 'rstd'], [hbk], scale=st[:, 2:3])
        for kc in range(8):
            p.tr(psTb[:, kc * 128:(kc + 1) * 128], hb[b][:, kc * 128:(kc + 1) * 128], identb, [hbk, 'identb'], [PK(0)])
        p.copy('vector', hT[b].rearrange("p a d -> p (a d)"), psTb, [PK(0)], [hTk])
        for (ps, key, c0, c1) in ((ps1, PK(1), 0, 512), (ps2, PK(2), 512, 1024), (ps3, PK(3), 1024, 1280)):
            for kc in range(8):
                p.mm(ps[:, 0:c1 - c0], hT[b][:, kc, :], Wb[:, kc, c0:c1], kc == 0, kc == 7, [hTk, 'Wb'], [key])
        p.act(junk[:, 0:256], ps1[:, 0:256], AF.Square, [PK(1)], ['junk', 'ssq2a'], accum_out=st[:, 3:4])
        p.act(junk[:, 0:256], ps1[:, 256:512], AF.Square, [PK(1)], ['junk', 'ssq2b'], accum_out=st[:, 4:5])
        p.act(st[:, 5:7], st[:, 3:5], AF.Sqrt, ['ssq2a', 'ssq2b'], ['rs2'], scale=1.0 / 256, bias=EPS)
        p.recip(st[:, 7:9], st[:, 5:7], ['rs2'], ['rstd2'])
        p.copy('vector', cb, ps1, [PK(1)], ['cb'])
        for i in range(4):
            p.tr(ps4b[:, i * 128:(i + 1) * 128], cb[:, i * 128:(i + 1) * 128], identb, ['cb', 'identb'], [PK(4)])
        p.copy('vector', cT.rearrange("p a d -> p (a d)"), ps4b[:, 0:512], [PK(4)], ['cT'])
        for i in range(2):
            p.mm(ps5[:, 0:96], cT[:, i, :], Wqb[:, i, :], i == 0, i == 1, ['cT', 'Wqb'], [PK(5)])
        for i in range(2):
            p.mm(ps5[:, 128:256], cT[:, 2 + i, :], Wkvb[:, i, :], i == 0, i == 1, ['cT', 'Wkvb'], [PK(5)])
        p.act(qk96[:, 0, :], ps5[:, 0:96], AF.Copy, [PK(5), 'rstd2'], ['qk96'], scale=st[:, 7:8])
        p.act(qk96[:, 1, 0:64], ps5[:, 128:192], AF.Copy, [PK(5), 'rstd2'], ['qk96'], scale=st[:, 8:9])
        p.act(stVM[sg][:, tt, 0:64], ps5[:, 192:256], AF.Copy, [PK(5), 'rstd2'], [('stVM', sg)], scale=st[:, 8:9])
        p.copy('vector', qk96[:, 1, 64:96], ps2[:, 416:448], [PK(2)], ['qk96'])
        R = qk96[:, :, 64:96].rearrange("p a (h d) -> p a h d", h=2)
        cosb = COS[:, t, :].unsqueeze(1).unsqueeze(1).broadcast_to([128, 2, 2, 16])
        sinb = SIN[:, t, :].unsqueeze(1).broadcast_to([128, 2, 16])
        p.tt('vector', rA, R, cosb, ALU.mult, ['qk96', 'COS'], ['rA'])
        p.tt('vector', rB[:, :, 0, :], R[:, :, 1, :], sinb, ALU.mult, ['qk96', 'SIN'], ['rB'])
        p.tt('vector', rB[:, :, 1, :], R[:, :, 0, :], sinb, ALU.mult, ['qk96', 'SIN'], ['rB'])
        p.tt('vector', R[:, :, 0, :], rA[:, :, 0, :], rB[:, :, 0, :], ALU.subtract, ['rA', 'rB'], ['qk96'])
        p.tt('vector', R[:, :, 1, :], rA[:, :, 1, :], rB[:, :, 1, :], ALU.add, ['rA', 'rB'], ['qk96'])
        p.tt('vector', tmp96, qk96, qk96, ALU.mult, ['qk96'], ['tmp96'])
        p.op('vector', lambda e: e.tensor_reduce(out=st[:, 9:11], in_=tmp96, axis=AX.X, op=ALU.add), ['tmp96'], ['ssq96'])
        p.act(st[:, 11:13], st[:, 9:11], AF.Sqrt, ['ssq96'], ['rs96'], scale=1.0 / 96, bias=EPS)
        p.recip(st[:, 13:15], st[:, 11:13], ['rs96'], ['rstd96'])
        p.tt('vector', tmp96, qk96, G96, ALU.mult, ['qk96', 'G96'], ['tmp96'])
        p.tt('vector', qkb, tmp96, st[:, 13:15].unsqueeze(2).broadcast_to([128, 2, 96]), ALU.mult, ['tmp96', 'rstd96'], ['qkb'])
        p.act(sq32, ps2[:, 0:288], AF.Square, [PK(2)], ['sq32'])
        p.op('vector', lambda e: e.tensor_reduce(out=st[:, 15:24], in_=sq32.rearrange("p (a d) -> p a d", d=32), axis=AX.X, op=ALU.add), ['sq32'], ['ssq32'])
        p.act(st[:, 15:24], st[:, 15:24], AF.Sqrt, ['ssq32'], ['ssq32'], scale=1.0 / 32, bias=EPS)
        p.recip(st[:, 15:24], st[:, 15:24], ['ssq32'], ['ssq32'])
        p.tt('vector', tmp32, ps2[:, 0:288], G32, ALU.mult, [PK(2), 'G32'], ['tmp32'])
        p.tt('vector', n32b.rearrange("p (a d) -> p a d", d=32), tmp32.rearrange("p (a d) -> p a d", d=32),
             st[:, 15:24].unsqueeze(2).broadcast_to([128, 9, 32]), ALU.mult, ['tmp32', 'ssq32'], ['n32b'])
        p.ts('vector', sbqk[:, 0:64], ps2[:, 288:352], 0.125, ALU.mult, [PK(2)], ['sbqk'])
        p.copy('vector', sbqk[:, 64:128], ps2[:, 352:416], [PK(2)], ['sbqk'])
        p.copy('vector', stVS[sg][:, tt, :], ps2[:, 448:512], [PK(2)], [('stVS', sg)])
        p.copy('vector', stVG[sg][:, tt, :, 0:32], ps3[:, 0:128].rearrange("p (a d) -> p a d", d=32), [PK(3)], [('stVG', sg)])
        p.copy('vector', glb, ps3[:, 128:256], [PK(3)], ['glb'])
        p.tr(ps4b[:, 512:640], glb, identb, ['glb', 'identb'], [PK(4)])
        p.copy('vector', stGL[sg][:, tt * 128:(tt + 1) * 128], ps4b[:, 512:640], [PK(4)], [('stGL', sg)])
        p.tr(ps6b[0:64, 0:128], sbqk[:, 0:64], identb, ['sbqk', 'identb'], [PK(6)])
        p.tr(ps6b[0:64, 128:256], sbqk[:, 64:128], identb, ['sbqk', 'identb'], [PK(6)])
        p.tr(ps6b[0:96, 256:384], qkb[:, 0, :], identb, ['qkb', 'identb'], [PK(6)])
        p.tr(ps6b[0:96, 384:512], qkb[:, 1, :], identb, ['qkb', 'identb'], [PK(6)])
        p.tr(ps6b[0:96, 512:640], n32b[:, 0:96], identb, ['n32b', 'identb'], [PK(6)])
        p.tr(ps6b[0:96, 640:768], n32b[:, 96:192], identb, ['n32b', 'identb'], [PK(6)])
        p.tr(ps6b[0:64, 768:896], n32b[:, 192:256], identb, ['n32b', 'identb'], [PK(6)])
        p.tr(ps6b[0:64, 896:1024], n32b[:, 224:288], identb, ['n32b', 'identb'], [PK(6)])
        p.copy('vector', stageT[sg][0:96, 2:6, tt * 128:(tt + 1) * 128], ps6b[0:96, 256:768].rearrange("p (a d) -> p a d", d=128), [PK(6)], [('stageT', sg)])
        p.copy('vector', stageT[sg][0:64, 0:2, tt * 128:(tt + 1) * 128], ps6b[0:64, 0:256].rearrange("p (a d) -> p a d", d=128), [PK(6)], [('stageT', sg)])
        p.copy('vector', stageT[sg][0:64, 6:8, tt * 128:(tt + 1) * 128], ps6b[0:64, 768:1024].rearrange("p (a d) -> p a d", d=128), [PK(6)], [('stageT', sg)])
        if tt == 3:
            T0 = (t - 3) * 128
            j0 = t - 3
            p.dma(STQ, d['QKT'][0:96, :, T0:T0 + 512], stageT[sg][0:96], [('stageT', sg)], [('QKT', sg)], 'stageT%d' % sg)
            p.dma(STQ, d['GL'][:, T0:T0 + 512], stGL[sg], [('stGL', sg)], [('GL', sg)], 'stGL%d' % sg)
            p.dma(STQ, d['VSB'][:, j0:j0 + 4, :], stVS[sg], [('stVS', sg)], [('VSB', sg)], 'stVS%d' % sg)
            p.dma(STQ, d['VML'][:, j0:j0 + 4, :], stVM[sg], [('stVM', sg)], [('VML', sg)], 'stVM%d' % sg)
            p.dma(STQ, d['VSW'][:, j0:j0 + 4, :], stVG[sg][:, :, 0, :], [('stVG', sg)], [('VG', sg)], 'stVG%d' % sg)
            for g in range(3):
                p.dma(STQ, d['VD%d' % g][T0:T0 + 512, :].rearrange("(a p) d -> p a d", p=128), stVG[sg][:, :, 1 + g, :],
                      [('stVG', sg)], [('VG', sg)], 'stVG%d' % sg)


def load_cols(p, dst, dst_key, src, src_keys, slot, nsplit=4):
    n = src.shape[-1]
    w = n // nsplit
    for i in range(nsplit):
        p.dma('sync', dst[:, i * w:(i + 1) * w], src[:, i * w:(i + 1) * w], src_keys, [dst_key], slot)


def emit_sb(p, sb, PS, d, S, side=None):
    NT = S // 128
    QT = sb("sbQT", [64, S], BF16)
    KT = sb("sbKT", [64, S], BF16)
    V = sb("sbV", [128, NT, 64], BF16)
    load_cols(p, QT, 'sbQT', d['QKT'][0:64, 0, :], [], 'sbQT')
    load_cols(p, KT, 'sbKT', d['QKT'][0:64, 1, :], [], 'sbKT')
    p.dma('sync', V, d['VSB'][:, :, :], [], ['sbV'], 'sbV')
    triN = sb("triN", [128, 128], BF16)
    onesb = sb("onesb", [128, 128], BF16)
    maskS = sb("maskS", [128, 4, 512], BF16)
    p.dma('sync', triN, d['triN'][:, :], [], ['triN'], 'triN')
    p.dma('sync', onesb, d['onesb'][:, :], [], ['onesb'], 'onesb')
    p.dma('sync', maskS, d['maskS'][:, :, :], [], ['maskS'], 'maskS')
    e_t = [sb("sb_e%d" % i, [128, 1024], F32) for i in range(2)]
    sp_t = [sb("sb_sp%d" % i, [128, 1024], BF16) for i in range(2)]
    ex_t = [sb("sb_ex%d" % i, [128, 1024], F32) for i in range(2)]
    A_t = [sb("sb_A%d" % i, [128, 1024], BF16) for i in range(2)]
    Cb = sb("sb_Cb", [128, 512], F32)
    ost = [sb("sb_ost%d" % i, [64, 512], BF16) for i in range(2)]
    PSB = d['PSB']
    pairs = []
    for c in range(S // 512):
        js = list(range(4 * c + 3, -1, -1))
        for m in range(0, len(js), 2):
            pairs.append((c, m, js[m], js[m + 1]))
    NS = len(pairs)

    def stage1(pi):
        c, m, j0, j1 = pairs[pi]
        pb = pi % 2
        ek, spk = ('e', pb), ('sp', pb)
        for h, j in enumerate((j0, j1)):
            p.mm(PS[2 * pb + h], KT[:, j * 128:(j + 1) * 128], QT[:, c * 512:(c + 1) * 512], True, True, ['sbKT', 'sbQT'], [PK(2 * pb + h)])
        p.act(e_t[pb], PSB[pb], AF.Exp, [PK(2 * pb), PK(2 * pb + 1)], [ek])
        p.act(sp_t[pb], e_t[pb], AF.Ln, [ek], [spk], bias=1.0)
        for h, j in enumerate((j0, j1)):
            r = j - 4 * c
            if r >= 0:
                hs = slice(h * 512, (h + 1) * 512)
                p.tt('gpsimd', sp_t[pb][:, hs], sp_t[pb][:, hs], maskS[:, r, :], ALU.mult, [spk, 'maskS'], [spk])

    def stage2(pi):
        c, m, j0, j1 = pairs[pi]
        pb = pi % 2
        spk, exk, Ak = ('sp', pb), ('ex', pb), ('A', pb)
        for h, j in enumerate((j0, j1)):
            hs = slice(h * 512, (h + 1) * 512)
            zk, ck = PK(2 * pb + h), PK(4 + h)
            p.mm(PS[2 * pb + h], triN, sp_t[pb][:, hs], False, True, [spk, 'triN'], [zk], skip=True)
            p.mm(PS[4 + h], onesb, sp_t[pb][:, hs], True, True, [spk, 'onesb'], [ck])
            if m == 0 and h == 0:
                p.copy('vector', ex_t[pb][:, hs], PS[2 * pb + h], [zk], [exk])
                p.copy('vector', Cb, PS[4 + h], [ck], ['Cb'])
            else:
                p.tt('vector', ex_t[pb][:, hs], PS[2 * pb + h], Cb, ALU.subtract, [zk, 'Cb'], [exk])
                if j > 0:
                    p.tt('vector', Cb, PS[4 + h], Cb, ALU.add, [ck, 'Cb'], ['Cb'])
        p.act(A_t[pb], ex_t[pb], AF.Exp, [exk], [Ak])
        for h, j in enumerate((j0, j1)):
            r = j - 4 * c
            if r >= 0:
                hs = slice(h * 512, (h + 1) * 512)
                p.tt('gpsimd', A_t[pb][:, hs], A_t[pb][:, hs], maskS[:, r, :], ALU.mult, [Ak, 'maskS'], [Ak])

    def stage3(pi):
        c, m, j0, j1 = pairs[pi]
        pb = pi % 2
        ob = c % 2
        for h, j in enumerate((j0, j1)):
            hs = slice(h * 512, (h + 1) * 512)
            p.mm(PS[6 + ob][0:64, :], V[:, j, :], A_t[pb][:, hs], m == 0 and h == 0, j == 0, [('A', pb), 'sbV'], [PK(6 + ob)])
        if j1 == 0:
            p.copy('vector', ost[ob], PS[6 + ob][0:64, :], [PK(6 + ob)], [('ost', ob)])
            p.dma(STQ, d['OUT'][0:64, c * 512:(c + 1) * 512], ost[ob], [('ost', ob)], [('OUT', ob)], 'sb_ost%d' % ob)

    side_it = side(sb) if side is not None else None
    n_side = d.get('n_side', 0)
    every = max(1, NS // max(n_side, 1))
    for i in range(NS + 2):
        if i < NS:
            stage1(i)
        if 0 <= i - 1 < NS:
            stage2(i - 1)
        if 0 <= i - 2 < NS:
            stage3(i - 2)
        if side_it is not None and i % every == 0:
            next(side_it, None)
    if side_it is not None:
        for _ in side_it:
            pass


def emit_mla(p, sb, PS, d, S):
    NT = S // 128
    QT = sb("mlQT", [96, S], BF16)
    KT = sb("mlKT", [96, S], BF16)
    V = sb("mlV", [128, NT, 65], BF16)
    load_cols(p, QT, 'mlQT', d['QKT'][0:96, 2, :], [], 'mlQT')
    load_cols(p, KT, 'mlKT', d['QKT'][0:96, 3, :], [], 'mlKT')
    p.dma('sync', V, d['VML'][:, :, :], [], ['mlV'], 'mlV')
    maskI = sb("maskI", [128, 4, 512], BF16)
    p.dma('sync', maskI, d['maskI'][:, :, :], [], ['maskI'], 'maskI')
    onesf = sb("onesf", [128, 64], F32)
    p.dma('sync', onesf, d['onesf'][:, :], [], ['onesf'], 'onesf')
    P_t = [sb("ml_P%d" % i, [128, 512], BF16) for i in range(3)]
    osb = sb("ml_osb", [128, 512], F32)
    rrow = sb("ml_rrow", [128, 512], F32)
    ost = [sb("ml_ost%d" % i, [64, 512], BF16) for i in range(2)]
    scale = 96 ** -0.5
    steps = []
    for c in range(S // 512):
        for idx, j in enumerate(range(4 * c + 3, -1, -1)):
            steps.append((c, idx, j))
    NS = len(steps)

    def stage1(s):
        c, idx, j = steps[s]
        b = s % 3
        r = j - 4 * c
        sk, Pk = PK(b), ('P', b)
        p.mm(PS[b], KT[:, j * 128:(j + 1) * 128], QT[:, c * 512:(c + 1) * 512], True, True, ['mlKT', 'mlQT'], [sk])
        p.act(P_t[b], PS[b], AF.Exp, [sk], [Pk], scale=scale)
        if r >= 0:
            p.tt('gpsimd', P_t[b], P_t[b], maskI[:, r, :], ALU.mult, [Pk, 'maskI'], [Pk])

    def stage2(s):
        c, idx, j = steps[s]
        b = s % 3
        ob = c % 2
        oD, ok = PS[4 + ob], PK(4 + ob)
        p.mm(oD[0:65, :], V[:, j, :], P_t[b], idx == 0, j == 0, [('P', b), 'mlV'], [ok])
        if j == 0:
            p.copy('vector', osb[0:65, :], oD[0:65, :], [ok], ['ml_osb'])
            p.act(rrow[64:65, :], osb[64:65, :], AF.Ln, ['ml_osb'], ['ml_rrow'])
            p.act(rrow[64:65, :], rrow[64:65, :], AF.Exp, ['ml_rrow'], ['ml_rrow'], scale=-1.0)
            p.mm(PS[6][0:64, :], onesf[64:65, :], rrow[64:65, :], True, True, ['ml_rrow', 'onesf'], [PK(6)])
            p.tt('vector', ost[ob], osb[0:64, :], PS[6][0:64, :], ALU.mult, ['ml_osb', PK(6)], [('ml_ost', ob)])
            p.dma(STQ, d['OUT'][64:128, c * 512:(c + 1) * 512], ost[ob], [('ml_ost', ob)], [('OUT', 2 + ob)], 'ml_ost%d' % ob)

    for i in range(NS + 2):
        if i < NS:
            stage1(i)
        if 0 <= i - 2 < NS:
            stage2(i - 2)


def toeplitz(bmat, idx, rev=False):
    return bmat[idx, :, :]


def run_band_units(p, PS, units, t_sb, P_sb, scale):
    def front(ui):
        slots, Btile, Bkey, _ = units[ui]
        b = ui % 2
        s_ps, sk = PS[b], PK(b)
        for u, sl in enumerate(slots):
            p.mm(s_ps[:, u * 256:u * 256 + 128], sl['kc'], sl['q'], True, True, sl['rk'], [sk])
            p.mm(s_ps[:, u * 256 + 128:(u + 1) * 256], sl['kp'], sl['q'], True, True, sl['rk'], [sk])
        p.stt(t_sb[b], s_ps, scale, Btile, ALU.mult, ALU.add, [sk, Bkey], [('bt', b)])
        p.act(P_sb[b], t_sb[b], AF.Exp, [('bt', b)], [('bP', b)])

    def back(ui):
        slots, _, _, epi = units[ui]
        b = ui % 2
        o_ps, ok = PS[2 + b], PK(2 + b)
        for u, sl in enumerate(slots):
            p.mm(o_ps[0:33, u * 128:(u + 1) * 128], sl['vc'], P_sb[b][:, u * 256:u * 256 + 128], True, False, [('bP', b)] + sl['vk'], [ok])
            p.mm(o_ps[0:33, u * 128:(u + 1) * 128], sl['vp'], P_sb[b][:, u * 256 + 128:(u + 1) * 256], False, True, [('bP', b)] + sl['vk'], [ok])
        epi(o_ps, ok)

    n = len(units)
    for i in range(n + 1):
        if i < n:
            front(i)
        if i >= 1:
            back(i - 1)


def emit_sw(p, sb, PS, d, S):
    NT = S // 128
    Q = [sb("swQ%d" % i, [32, S], BF16) for i in range(2)]
    K = sb("swK", [32, S], BF16)
    V = sb("swV", [128, NT, 33], BF16)
    load_cols(p, Q[0], 'swQ0', d['QKT'][0:32, 4, :], [], 'swQ0')
    load_cols(p, Q[1], 'swQ1', d['QKT'][0:32, 6, :], [], 'swQ1')
    load_cols(p, K, 'swK', d['QKT'][0:32, 5, :], [], 'swK')
    p.dma('sync', V, d['VSW'][:, :, :], [], ['swV'], 'swV')
    B = sb("swB", [128, 2, 2, 128], F32)
    B0 = sb("swB0", [128, 2, 2, 128], F32)
    for h in range(2):
        for sel in range(2):
            p.dma('sync', B[:, h, sel, :], toeplitz(d['bvec'], h * 2 + sel), [], ['swB'], 'swB')
        p.dma('sync', B0[:, h, 0, :], toeplitz(d['bvec'], h * 2), [], ['swB0'], 'swB0')
        p.memset('gpsimd', B0[:, h, 1, :], NEG, ['swB0'])
    onesf = sb("onesf", [128, 64], F32)
    p.dma('sync', onesf, d['onesf'][:, :], [], ['onesf'], 'onesf')
    es = sb("sw_es", [128, 2], F32)
    p.dma('sync', es[32:33, :], d['sinks'][:, :], [], ['sw_es'], 'sw_es')
    p.act(es[32:33, :], es[32:33, :], AF.Exp, ['sw_es'], ['sw_es'])
    t_sb = [sb("bt%d" % i, [128, 512], F32) for i in range(2)]
    P_sb = [sb("bP%d" % i, [128, 512], BF16) for i in range(2)]
    osb = sb("sw_osb", [128, 2, 512], F32)
    rrow = sb("sw_rrow", [128, 2, 512], F32)
    ost = [sb("sw_ost%d" % i, [32, 2, 512], BF16) for i in range(2)]
    scale = 32 ** -0.5
    units = []
    for n in range(NT):
        cs = slice(n * 128, (n + 1) * 128)
        ps_ = slice(max(n - 1, 0) * 128, (max(n - 1, 0) + 1) * 128)
        slots = [dict(kc=K[:, cs], kp=K[:, ps_], q=Q[h][:, cs], vc=V[:, n, :], vp=V[:, max(n - 1, 0), :],
                      rk=['swK', 'swQ%d' % h], vk=['swV']) for h in range(2)]

        def epi(o_ps, ok, n=n):
            tt = n % 4
            p.copy('scalar', osb[0:33, :, tt * 128:(tt + 1) * 128], o_ps[0:33, 0:256].rearrange("p (a d) -> p a d", d=128), [ok], ['sw_osb'])
            if tt == 3:
                gi = (n // 4) % 2
                T0 = (n - 3) * 128
                for h in range(2):
                    p.act(rrow[32:33, h, :], osb[32:33, h, :], AF.Ln, ['sw_osb', 'sw_es'], ['sw_rrow'], bias=es[32:33, h:h + 1])
                    p.act(rrow[32:33, h, :], rrow[32:33, h, :], AF.Exp, ['sw_rrow'], ['sw_rrow'], scale=-1.0)
                    p.mm(PS[4 + h][0:32, :], onesf[32:33, 0:32], rrow[32:33, h, :], True, True, ['sw_rrow', 'onesf'], [PK(4 + h)])
                    p.tt('vector', ost[gi][:, h, :], osb[0:32, h, :], PS[4 + h][0:32, :], ALU.mult, ['sw_osb', PK(4 + h)], [('sw_ost', gi)])
                p.dma(STQ, d['OUT'][128:192, T0:T0 + 512].rearrange("(h p) t -> p h t", p=32), ost[gi], [('sw_ost', gi)], [('OUT', 4 + gi)], 'sw_ost%d' % gi)
        units.append((slots, (B0 if n == 0 else B).rearrange("p a b c -> p (a b c)"), 'swB0' if n == 0 else 'swB', epi))
    run_band_units(p, PS, units, t_sb, P_sb, scale)


def emit_dil(p, sb, PS, d, S):
    Qd = sb("dlQ", [96, S], BF16)
    Kd = sb("dlK", [96, S], BF16)
    Oacc = sb("dlO", [128, S], F32)
    onesf = sb("onesf", [128, 64], F32)
    p.dma('sync', onesf, d['onesf'][:, :], [], ['onesf'], 'onesf')
    Vg = sb("dlV", [128, S // 128, 33], BF16)
    Bt = [sb("dlB%d" % i, [128, 2, 2, 128], F32) for i in range(2)]
    t_sb = [sb("bt%d" % i, [128, 512], F32) for i in range(2)]
    P_sb = [sb("bP%d" % i, [128, 512], BF16) for i in range(2)]
    rrow = sb("dl_rrow", [128, 512], F32)
    ost = [sb("dl_ost%d" % i, [32, 512], BF16) for i in range(2)]
    scale = 32 ** -0.5
    src = [(4, 5, 32), (4, 5, 64), (6, 7, 32)]
    for g, dil in enumerate(DILS):
        qs_, ks_, pb = src[g]
        rows = slice(pb, pb + 32)
        M = S // dil
        nb = M // 128
        load_cols(p, Qd[rows], 'dlQ', d['QKT'][rows, qs_, :], [], 'dlQ')
        load_cols(p, Kd[rows], 'dlK', d['QKT'][rows, ks_, :], [], 'dlK')
        Vv = Vg.rearrange("p (r n) d -> p r n d", r=dil)
        vd = d['VD%d' % g]
        for r0 in range(dil):
            for n0 in range(0, nb, 16):
                nn = min(16, nb - n0)
                srcap = bass.AP(vd.tensor, r0 * 33 + n0 * 128 * dil * 33, [[dil * 33, 128], [128 * dil * 33, nn], [1, 33]])
                p.dma('sync', Vv[:, r0, n0:n0 + nn, :], srcap, [], ['dlV'], 'dlV')
        for v in range(2):
            for u in range(2):
                for sel in range(2):
                    if v == 1 and u == 0 and sel == 1:
                        p.memset('gpsimd', Bt[v][:, u, sel, :], NEG, [('dlB', v)])
                    else:
                        p.dma('sync', Bt[v][:, u, sel, :], toeplitz(d['bvec'], (2 + g) * 2 + sel), [], [('dlB', v)], 'dlB%d' % v)
        units = []
        for r in range(dil):
            for n in range(0, nb, 2):
                slots = []
                for u in range(2):
                    n1 = n + u
                    np_ = max(n1 - 1, 0)
                    qc = slice(r + dil * 128 * n1, r + dil * 128 * n1 + dil * 127 + 1, dil)
                    kc = slice(r + dil * 128 * np_, r + dil * 128 * np_ + dil * 127 + 1, dil)
                    slots.append(dict(kc=Kd[rows, qc], kp=Kd[rows, kc], q=Qd[rows, qc], vc=Vv[:, r, n1, :], vp=Vv[:, r, np_, :],
                                      rk=['dlK', 'dlQ'], vk=['dlV']))
                v = 1 if n == 0 else 0
                oc = slice(r + dil * 128 * n, r + dil * 128 * n + dil * 255 + 1, dil)

                def epi(o_ps, ok, oc=oc, g=g):
                    if g == 0:
                        p.copy('vector', Oacc[0:33, oc], o_ps[0:33, 0:256], [ok], ['dlO'])
                    else:
                        p.tt('vector', Oacc[0:33, oc], o_ps[0:33, 0:256], Oacc[0:33, oc], ALU.add, [ok, 'dlO'], ['dlO'])
                units.append((slots, Bt[v].rearrange("p a b c -> p (a b c)"), ('dlB', v), epi))
        run_band_units(p, PS, units, t_sb, P_sb, scale)
    for c in range(S // 512):
        gi = c % 2
        cs = slice(c * 512, (c + 1) * 512)
        p.act(rrow[32:33, :], Oacc[32:33, cs], AF.Ln, ['dlO'], ['dl_rrow'])
        p.act(rrow[32:33, :], rrow[32:33, :], AF.Exp, ['dl_rrow'], ['dl_rrow'], scale=-1.0)
        p.mm(PS[4 + gi][0:32, :], onesf[32:33, 0:32], rrow[32:33, :], True, True, ['dl_rrow', 'onesf'], [PK(4 + gi)])
        p.tt('vector', ost[gi], Oacc[0:32, cs], PS[4 + gi][0:32, :], ALU.mult, ['dlO', PK(4 + gi)], [('dl_ost', gi)])
        p.dma(STQ, d['OUT'][192:224, cs], ost[gi], [('dl_ost', gi)], [('OUT', 6 + gi)], 'dl_ost%d' % gi)


def build_A(S, phases=('inproj', 'sb', 'mla', 'sw', 'dil'), dbg=False):
    nc = bass.Bass("TRN2", target_bir_lowering=False)
    NT = S // 128
    d = {}

    def din(name, shape, dt=F32):
        d[name] = nc.dram_tensor(name, shape, dt, kind="ExternalInput").ap()

    din('x', [S, D])
    din('w_in', [D, 1280])
    din('g1', [128, 8])
    din('wqb', [256, 96])
    din('gqa', [128, 2])
    din('wkvb', [256, 128])
    din('gkva', [128, 2])
    din('g32', [288])
    din('g96', [192])
    din('cos', [128, SEQ // 128, 16])
    din('sin', [128, SEQ // 128, 16])
    din('ident', [128, 128], BF16)
    din('triN', [128, 128], BF16)
    din('onesb', [128, 128], BF16)
    din('maskS', [128, 4, 512], BF16)
    din('maskI', [128, 4, 512], BF16)
    din('onesf', [128, 64])
    din('bvec', [10, 128, 128])
    din('sinks', [1, 2])
    kind = "ExternalOutput" if dbg else "Internal"
    d['QKT'] = nc.dram_tensor('QKT', [128, 8, S], BF16, kind=kind).ap()
    d['VSB'] = nc.dram_tensor('VSB', [128, NT, 64], BF16, kind=kind).ap()
    d['VML'] = nc.dram_tensor('VML', [128, NT, 65], BF16, kind=kind).ap()
    d['VSW'] = nc.dram_tensor('VSW', [128, NT, 33], BF16, kind=kind).ap()
    for g in range(3):
        d['VD%d' % g] = nc.dram_tensor('VD%d' % g, [S, 33], BF16, kind=kind).ap()
    d['OUT'] = nc.dram_tensor('OUT', [224, S], BF16, kind="ExternalOutput").ap()
    with ExitStack() as es:
        sb = Arena(nc, es, 196 * 1024)
        PSB = [es.enter_context(nc.psum_tensor("psb%d" % i, [128, 1024], F32))[:, :] for i in range(4)]
        PS = [PSB[i // 2][:, (i % 2) * 512:(i % 2 + 1) * 512] for i in range(8)]
        d_psb = PSB
        p = Prog(nc, es)
        block = es.enter_context(nc.Block())
        d['xrows'] = lambda t: d['x'][t * 128:(t + 1) * 128, :]
        d['GL'] = nc.dram_tensor('GL', [128, S], BF16, kind="Internal").ap()
        d['PSB'] = d_psb
        emitters = dict(inproj=emit_inproj, sb=emit_sb, mla=emit_mla, sw=emit_sw, dil=emit_dil)
        for ph in phases:
            sb.reset()
            emitters[ph](p, sb, PS, d, S)
            p.barrier()
        p.finish(block)
    return nc, p


def bf(a):
    return np.ascontiguousarray(a).astype(ml_dtypes.bfloat16)


def t5_bucket_np(dist):
    dist = np.asarray(dist, np.int64)
    d_ = np.maximum(dist, 1).astype(np.float32)
    large = 16 + (np.log(d_ / np.float32(16)) / np.float32(math.log(2048 / 16)) * np.float32(16)).astype(np.int32)
    large = np.minimum(large, 31)
    return np.where(dist < 16, dist, large)


def consts_A():
    k = np.arange(128)
    c = {}
    c['ident'] = bf(np.eye(128, dtype=np.float32))
    c['triN'] = bf(-(k[:, None] >= k[None, :]).astype(np.float32))
    c['onesb'] = bf(np.ones((128, 128), np.float32))
    qi = np.arange(512)
    mS = np.zeros((128, 4, 512), np.float32)
    mI = np.zeros((128, 4, 512), np.float32)
    for r in range(4):
        mS[:, r, :] = (128 * r + k[:, None]) < qi[None, :]
        mI[:, r, :] = (128 * r + k[:, None]) <= qi[None, :]
    c['maskS'] = bf(mS)
    c['maskI'] = bf(mI)
    c['onesf'] = np.ones((128, 64), np.float32)
    half = 16
    inv = (10000.0 ** (-np.arange(half, dtype=np.float32) / half)).astype(np.float32)
    ang = np.arange(SEQ, dtype=np.float32)[:, None] * inv[None, :]
    c['cos'] = np.ascontiguousarray(np.cos(ang).astype(np.float32).reshape(SEQ // 128, 128, 16).transpose(1, 0, 2))
    c['sin'] = np.ascontiguousarray(np.sin(ang).astype(np.float32).reshape(SEQ // 128, 128, 16).transpose(1, 0, 2))
    return c


def band_bias_vecs(rel_bias, h):
    dd = np.arange(-127, 128)
    out = np.full((10, 255), NEG, np.float32)
    for i, hq in enumerate((2 * h, 2 * h + 1)):
        cur = dd >= 0
        out[2 * i, cur] = rel_bias[t5_bucket_np(dd[cur]), hq]
        prv = dd < 0
        out[2 * i + 1, prv] = rel_bias[t5_bucket_np(128 + dd[prv]), hq]
    for g, dil in enumerate(DILS):
        col = 8 + g * 4 + h
        cur = dd >= 0
        out[4 + 2 * g, cur] = rel_bias[t5_bucket_np(dd[cur] * dil), col]
        prv = dd <= 0
        out[5 + 2 * g, prv] = rel_bias[t5_bucket_np((128 + dd[prv]) * dil), col]
    return out


def inputs_A(c, l, b, h, inp, x_b, S):
    cols = core_cols(h)
    m = dict(c)
    m['x'] = np.ascontiguousarray(x_b[:S])
    m['w_in'] = np.ascontiguousarray(np.concatenate([inp['w_in'][l][:, cols], inp['w_gate_a'][l]], axis=1))
    m['g1'] = np.ascontiguousarray(inp['norm1_g'][l].reshape(8, 128).T)
    m['wqb'] = np.ascontiguousarray(inp['w_qb'][l][:, h * 96:(h + 1) * 96])
    m['gqa'] = np.ascontiguousarray(inp['g_qa'][l].reshape(2, 128).T)
    m['wkvb'] = np.ascontiguousarray(inp['w_kvb'][l][:, h * 128:(h + 1) * 128])
    m['gkva'] = np.ascontiguousarray(inp['g_kva'][l].reshape(2, 128).T)
    gs, gd = inp['qk_g_sw'][l], inp['qk_g_dil'][l]
    m['g32'] = np.concatenate([gs[0], gd[0], gd[0], gs[1], gd[1], gd[1], gs[0], gd[0], gd[1]]).astype(np.float32)
    m['g96'] = np.concatenate([inp['qk_g_mla'][l][0], inp['qk_g_mla'][l][1]]).astype(np.float32)
    bv = band_bias_vecs(inp['rel_bias'], h)
    kk = np.arange(128)
    m['bvec'] = np.ascontiguousarray(bv[:, kk[None, :] - kk[:, None] + 127])
    m['sinks'] = np.ascontiguousarray(inp['sinks'][l][2 * h:2 * h + 2].reshape(1, 2))
    return m


BR_CHUNKS = ((0, 1), (2, 3), (4, 5), (6,))


def castw_iter(p, sb, d, nblk, F, engs=('vector', 'gpsimd', 'scalar')):
    stg = [sb("cw_stg%d" % i, [128, 2 * F], F32) for i in range(2)]
    stb = [sb("cw_stb%d" % i, [128, 2 * F], BF16) for i in range(2)]
    it = 0
    for blk in range(nblk):
        for kc in range(8):
            b = it % 2
            rows = slice(kc * 128, (kc + 1) * 128)
            p.dma('sync', stg[b][:, 0:F], d['wg'](blk)[rows, :], [], [('cw_stg', b)], 'cw_stg%d' % b)
            p.dma('sync', stg[b][:, F:2 * F], d['wu'](blk)[rows, :], [], [('cw_stg', b)], 'cw_stg%d' % b)
            p.copy(engs[it % len(engs)], stb[b], stg[b], [('cw_stg', b)], [('cw_stb', b)])
            p.dma(STQ, d['WGU'][blk, :, kc, :], stb[b], [('cw_stb', b)], [('WGU', b)], 'cw_stb%d' % b)
            it += 1
            yield
        for fc in range(F // 128):
            b = it % 2
            p.dma('sync', stg[b][:, 0:1024], d['wd'](blk)[fc * 128:(fc + 1) * 128, :], [], [('cw_stg', b)], 'cw_stg%d' % b)
            p.copy(engs[it % len(engs)], stb[b][:, 0:1024], stg[b][:, 0:1024], [('cw_stg', b)], [('cw_stb', b)])
            p.dma(STQ, d['WD'][blk, :, fc, :], stb[b][:, 0:1024], [('cw_stb', b)], [('WD', b)], 'cw_stb%d' % b)
            it += 1
            yield


def emit_castw(p, sb, PS, d, nblk, F):
    for _ in castw_iter(p, sb, d, nblk, F):
        pass


def emit_B(p, sb, PS, d, NTOK, nblk, F, moe):
    NCH = NTOK // 512
    FC = F // 128
    identb = sb("identb", [128, 128], BF16)
    p.dma('sync', identb, d['ident'][:, :], [], ['identb'], 'identb')
    G1 = sb("G1", [128, 1024], F32)
    G2 = sb("G2", [128, 1024], F32)
    p.dma('sync', G1, d['g1'].partition_broadcast(128), [], ['G1'], 'G1')
    p.dma('sync', G2, d['g2'].partition_broadcast(128), [], ['G2'], 'G2')
    bgt = sb("bgt", [128, 32], F32)
    p.dma('sync', bgt, d['bgate'][:, :], [], ['bgt'], 'bgt')
    Wga = sb("Wga", [128, 8, 128], BF16)
    Wgb = sb("Wgb", [128, 4096], BF16)
    Wbr = sb("Wbr", [128, 7, 1024], BF16)
    Wout = sb("Wout", [128, 8, 1024], BF16)
    stg = [sb("w_stg%d" % i, [128, 1024], F32) for i in range(2)]
    engs = ['vector', 'gpsimd', 'scalar']
    it = 0

    def cast_in(dst, src, n):
        nonlocal it
        b = it % 2
        p.dma('sync', stg[b][:, 0:n], src, [], [('w_stg', b)], 'w_stg%d' % b)
        p.copy(engs[it % 3], dst, stg[b][:, 0:n], [('w_stg', b)], ['Wres'])
        it += 1
    for kc in range(8):
        cast_in(Wga[:, kc, :], d['wga'][kc * 128:(kc + 1) * 128, :], 128)
        cast_in(Wout[:, kc, :], d['wout'][kc * 128:(kc + 1) * 128, :], 1024)
    for i in range(4):
        cast_in(Wgb[:, i * 1024:(i + 1) * 1024], d['wgb'][:, i * 1024:(i + 1) * 1024], 1024)
    for rc in range(7):
        cast_in(Wbr[:, rc, :], d['wbr'][rc * 128:(rc + 1) * 128, :], 1024)
    if moe:
        identf = sb("identf", [128, 128], F32)
        p.dma('sync', identf, d['identf'][:, :], [], ['identf'], 'identf')
        Wr = sb("Wr", [128, 8, 8], F32)
        p.dma('sync', Wr, d['wr'].rearrange("(kc p) e -> p kc e", p=128), [], ['Wr'], 'Wr')
        brt = sb("brt", [128, 8], F32)
        p.dma('sync', brt, d['br'].partition_broadcast(128), [], ['brt'], 'brt')
        h2f = sb("h2f", [128, 1024], F32)
        h2fT = sb("h2fT", [128, 8, 128], F32)
        rt = sb("rt", [128, 64], F32)
        GW = sb("GW", [128, 4, 8], F32)
    X4 = sb("X4", [128, 4, 1024], F32)
    oTc = sb("oTc", [128, 7, 512], BF16)
    hT = sb("hT", [128, 8, 512], BF16)
    hb = sb("hb", [128, 1024], BF16)
    junk = sb("junk", [128, 1024], BF16)
    st = sb("st", [128, 8], F32)
    glT = sb("glT", [128, 512], BF16)
    gate = [sb("gate%d" % i, [128, 512], F32) for i in range(2)]
    tmpy = sb("tmpy", [128, 512], F32)
    yacc = sb("yacc", [128, 512], F32)
    yT = sb("yT", [128, 8, 512], BF16)
    sg = [sb("sg%d" % i, [128, 512], F32) for i in range(2)]
    WGU_t = [sb("WGU_t%d" % i, [128, 8, 2 * F], BF16) for i in range(2)]
    WD_t = [sb("WD_t%d" % i, [128, FC, 1024], BF16) for i in range(2)]
    psTb = PS[0].bitcast(BF16)
    x, out = d['x'], d['out']
    wit = 0

    def norm_T(tt, G, Gk):
        xt = X4[:, tt, :]
        p.act(junk, xt, AF.Square, ['X4'], ['junk', 'ssq'], accum_out=st[:, 0:1])
        p.act(st[:, 1:2], st[:, 0:1], AF.Sqrt, ['ssq'], ['rs'], scale=1.0 / D, bias=EPS)
        p.recip(st[:, 2:3], st[:, 1:2], ['rs'], ['rstd'])
        p.stt(hb, xt, st[:, 2:3], G, ALU.mult, ALU.mult, ['X4', 'rstd', Gk], ['hb'])
        for kc in range(8):
            p.tr(psTb[:, kc * 128:(kc + 1) * 128], hb[:, kc * 128:(kc + 1) * 128], identb, ['hb', 'identb'], [PK(0)])
        p.copy('vector', hT[:, :, tt * 128:(tt + 1) * 128], psTb.rearrange("p (a d) -> p a d", d=128), [PK(0)], ['hT'])

    for c in range(NCH):
        T0 = c * 512
        p.dma('sync', X4, x[T0:T0 + 512, :].rearrange("(t p) d -> p t d", p=128), [], ['X4'], 'X4')
        p.dma('sync', oTc, d['oT'][:, T0:T0 + 512].rearrange("(r p) t -> p r t", p=128), [], ['oTc'], 'oTc')
        for tt in range(4):
            norm_T(tt, G1, 'G1')
        for kc in range(8):
            p.mm(PS[1], Wga[:, kc, :], hT[:, kc, :], kc == 0, kc == 7, ['hT', 'Wres'], [PK(1)])
        p.copy('vector', glT, PS[1], [PK(1)], ['glT'])
        k = 0
        for oc in range(8):
            for i in range(4):
                b = k % 2
                k += 1
                gp, gk = PS[2 + b], PK(2 + b)
                bp, bk = PS[4 + b], PK(4 + b)
                col = i * 1024 + oc * 128
                p.mm(gp, Wgb[:, col:col + 128], glT, True, True, ['glT', 'Wres'], [gk])
                p.act(gate[b], gp, AF.Sigmoid, [gk, 'bgt'], [('gate', b)], bias=bgt[:, i * 8 + oc:i * 8 + oc + 1])
                rcs = BR_CHUNKS[i]
                for n_, rc in enumerate(rcs):
                    p.mm(bp, Wbr[:, rc, oc * 128:(oc + 1) * 128], oTc[:, rc, :], n_ == 0, n_ == len(rcs) - 1, ['oTc', 'Wres'], [bk])
                if i == 0:
                    p.tt('vector', yacc, gate[b], bp, ALU.mult, [('gate', b), bk], ['yacc'])
                else:
                    p.tt('vector', tmpy, gate[b], bp, ALU.mult, [('gate', b), bk], ['tmpy'])
                    if i < 3:
                        p.tt('gpsimd', yacc, yacc, tmpy, ALU.add, ['yacc', 'tmpy'], ['yacc'])
                    else:
                        p.tt('gpsimd', yT[:, oc, :], yacc, tmpy, ALU.add, ['yacc', 'tmpy'], ['yT'])
        k = 0
        for tt in range(4):
            for half in range(2):
                b = k % 2
                k += 1
                ps, pk = PS[6 + b], PK(6 + b)
                for oc in range(8):
                    p.mm(ps, yT[:, oc, tt * 128:(tt + 1) * 128], Wout[:, oc, half * 512:(half + 1) * 512], oc == 0, oc == 7, ['yT', 'Wres'], [pk])
                xs = X4[:, tt, half * 512:(half + 1) * 512]
                p.tt('vector', xs, ps, xs, ALU.add, [pk, 'X4'], ['X4'])
        for tt in range(4):
            norm_T(tt, G2, 'G2')
            if moe:
                p.stt(h2f, X4[:, tt, :], st[:, 2:3], G2, ALU.mult, ALU.mult, ['X4', 'rstd', 'G2'], ['h2f'])
                for kc in range(8):
                    bnk = 2 + kc // 4
                    p.tr(PS[bnk][:, (kc % 4) * 128:(kc % 4 + 1) * 128], h2f[:, kc * 128:(kc + 1) * 128], identf, ['h2f', 'identf'], [PK(bnk)])
                p.copy('vector', h2fT[:, 0:4, :].rearrange("p a d -> p (a d)"), PS[2], [PK(2)], ['h2fT'])
                p.copy('vector', h2fT[:, 4:8, :].rearrange("p a d -> p (a d)"), PS[3], [PK(3)], ['h2fT'])
                for kc in range(8):
                    p.mm(PS[1][:, 0:8], h2fT[:, kc, :], Wr[:, kc, :], kc == 0, kc == 7, ['h2fT', 'Wr'], [PK(1)])
                lg, m8, ee, mk = rt[:, 0:8], rt[:, 8:16], rt[:, 16:24], rt[:, 24:32]
                p.tt('vector', lg, PS[1][:, 0:8], brt, ALU.add, [PK(1), 'brt'], ['lg'])
                p.op('vector', lambda e, lg=lg, m8=m8: e.max(out=m8, in_=lg), ['lg'], ['m8'])
                p.ts('vector', rt[:, 32:33], m8[:, 0:1], -1.0, ALU.mult, ['m8'], ['negm'])
                p.act(ee, lg, AF.Exp, ['lg', 'negm'], ['ee'], bias=rt[:, 32:33])
                p.ts('vector', mk, lg, m8[:, 1:2], ALU.is_ge, ['lg', 'm8'], ['mk'])
                p.tt('vector', ee, ee, mk, ALU.mult, ['ee', 'mk'], ['ee'])
                p.op('vector', lambda e, ee=ee: e.tensor_reduce(out=rt[:, 33:34], in_=ee, axis=AX.X, op=ALU.add), ['ee'], ['den'])
                p.recip(rt[:, 34:35], rt[:, 33:34], ['den'], ['rden'])
                p.ts('vector', GW[:, tt, :], ee, rt[:, 34:35], ALU.mult, ['ee', 'rden'], ['GW'])
        aT = yT
        for blk in range(nblk):
            wb = wit % 2
            wit += 1
            p.dma('sync', WGU_t[wb], d['WGU'][blk], [], [('WGU_t', wb)], 'WGU_t%d' % wb)
            p.dma('sync', WD_t[wb], d['WD'][blk], [], [('WD_t', wb)], 'WD_t%d' % wb)
            for fc in range(FC):
                b = fc % 2
                gps, gk = PS[2 + b], PK(2 + b)
                ups, uk = PS[4 + b], PK(4 + b)
                for kc in range(8):
                    p.mm(gps, WGU_t[wb][:, kc, fc * 128:(fc + 1) * 128], hT[:, kc, :], kc == 0, kc == 7, ['hT', ('WGU_t', wb)], [gk])
                for kc in range(8):
                    p.mm(ups, WGU_t[wb][:, kc, F + fc * 128:F + (fc + 1) * 128], hT[:, kc, :], kc == 0, kc == 7, ['hT', ('WGU_t', wb)], [uk])
                p.act(sg[b], gps, AF.Silu, [gk], [('sg', b)])
                p.tt('vector', aT[:, fc, :], sg[b], ups, ALU.mult, [('sg', b), uk], ['yT'])
            k = 0
            for tt in range(4):
                for half in range(2):
                    b = k % 2
                    k += 1
                    ps, pk = PS[6 + b], PK(6 + b)
                    for fc in range(FC):
                        p.mm(ps, aT[:, fc, tt * 128:(tt + 1) * 128], WD_t[wb][:, fc, half * 512:(half + 1) * 512], fc == 0, fc == FC - 1, ['yT', ('WD_t', wb)], [pk])
                    xs = X4[:, tt, half * 512:(half + 1) * 512]
                    if moe:
                        p.stt(xs, ps, GW[:, tt, blk:blk + 1], xs, ALU.mult, ALU.add, [pk, 'GW', 'X4'], ['X4'])
                    else:
                        p.tt('vector', xs, ps, xs, ALU.add, [pk, 'X4'], ['X4'])
        p.dma(STQ, out[T0:T0 + 512, :].rearrange("(t p) d -> p t d", p=128), X4, ['X4'], ['out'], 'X4o')


def build_B(NTOK, moe):
    nc = bass.Bass("TRN2", target_bir_lowering=False)
    d = {}

    def din(name, shape, dt=F32):
        d[name] = nc.dram_tensor(name, shape, dt, kind="ExternalInput").ap()

    din('x', [NTOK, D])
    din('oT', [896, NTOK], BF16)
    din('ident', [128, 128], BF16)
    din('g1', [1024])
    din('g2', [1024])
    din('bgate', [128, 32])
    din('wga', [1024, 128])
    din('wgb', [128, 4096])
    din('wbr', [896, 1024])
    din('wout', [1024, 1024])
    if moe:
        nblk, F = 8, 768
        din('identf', [128, 128])
        din('wr', [1024, 8])
        din('br', [8])
        din('wgu', [8, 1024, 1536])
        din('wdn', [8, 768, 1024])
        d['wg'] = lambda blk: d['wgu'][blk, :, 0:768]
        d['wu'] = lambda blk: d['wgu'][blk, :, 768:1536]
        d['wd'] = lambda blk: d['wdn'][blk, :, :]
    else:
        nblk, F = 4, 512
        din('wgu', [1024, 4096])
        din('wdn', [2048, 1024])
        d['wg'] = lambda blk: d['wgu'][:, blk * 512:(blk + 1) * 512]
        d['wu'] = lambda blk: d['wgu'][:, 2048 + blk * 512:2048 + (blk + 1) * 512]
        d['wd'] = lambda blk: d['wdn'][blk * 512:(blk + 1) * 512, :]
    d['WGU'] = nc.dram_tensor('WGU', [nblk, 128, 8, 2 * F], BF16, kind="Internal").ap()
    d['WD'] = nc.dram_tensor('WD', [nblk, 128, F // 128, 1024], BF16, kind="Internal").ap()
    d['out'] = nc.dram_tensor('out', [NTOK, D], F32, kind="ExternalOutput").ap()
    with ExitStack() as es:
        sb = Arena(nc, es, 200 * 1024)
        PSB = [es.enter_context(nc.psum_tensor("psb%d" % i, [128, 1024], F32))[:, :] for i in range(4)]
        PS = [PSB[i // 2][:, (i % 2) * 512:(i % 2 + 1) * 512] for i in range(8)]
        d_psb = PSB
        p = Prog(nc, es)
        block = es.enter_context(nc.Block())
        emit_castw(p, sb, PS, d, nblk, F)
        p.barrier()
        sb.reset()
        emit_B(p, sb, PS, d, NTOK, nblk, F, moe)
        p.barrier()
        p.finish(block)
    return nc, p


def inputs_B(l, inp, x_tok, oT_tok, moe):
    m = {}
    m['x'] = np.ascontiguousarray(x_tok)
    m['oT'] = np.ascontiguousarray(oT_tok)
    m['ident'] = bf(np.eye(128, dtype=np.float32))
    m['g1'] = np.ascontiguousarray(inp['norm1_g'][l])
    m['g2'] = np.ascontiguousarray(inp['norm2_g'][l])
    m['bgate'] = np.ascontiguousarray(inp['b_gate'][l].reshape(32, 128).T)
    m['wga'] = np.ascontiguousarray(inp['w_gate_a'][l])
    m['wgb'] = np.ascontiguousarray(inp['w_gate_b'][l])
    m['wbr'] = np.ascontiguousarray(inp['w_branch'][l])
    m['wout'] = np.ascontiguousarray(inp['w_out'][l])
    if moe:
        m['identf'] = np.eye(128, dtype=np.float32)
        m['wr'] = np.ascontiguousarray(inp['w_router'][l // 2])
        m['br'] = np.ascontiguousarray(inp['b_router'][l // 2])
        m['wgu'] = np.ascontiguousarray(inp['w_gu_exp'][l // 2])
        m['wdn'] = np.ascontiguousarray(inp['w_down_exp'][l // 2])
    else:
        m['wgu'] = np.ascontiguousarray(inp['w_gu_dense'][l // 2])
        m['wdn'] = np.ascontiguousarray(inp['w_down_dense'][l // 2])
    return m


_CACHE = {}


def _prog(key, fn):
    if key not in _CACHE:
        _CACHE[key] = fn()
    return _CACHE[key]


def assemble_oT(outs):
    res = []
    for b in range(BATCH):
        rows = [None] * 4
        o = [np.asarray(outs[b * 4 + h]) for h in range(4)]
        sbp = np.concatenate([o[h][0:64] for h in range(4)], axis=0)
        mlp = np.concatenate([o[h][64:128] for h in range(4)], axis=0)
        swp = np.concatenate([o[h][128:192] for h in range(4)], axis=0)
        dlp = np.concatenate([o[h][192:224] for h in range(4)], axis=0)
        res.append(np.concatenate([sbp, mlp, swp, dlp], axis=0))
    return res


def kernel(**inp):
    inp = {k: np.asarray(v) for k, v in inp.items()}
    return kernel_fused(inp)


GROUPS = [[0, 1, 2, 3], [4, 5, 6, 7]]


def emit_merge(p, sb, PS, d, S):
    SL = S // 4
    bgt = sb("bgt", [128, 32], F32)
    p.dma('sync', bgt, d['bgate'][:, :], [], ['bgt'], 'bgt')
    Wgb = sb("Wgb", [128, 4096], BF16)
    Wbr = sb("Wbrc", [64, 4, 1024], BF16)
    stg = [sb("w_stg%d" % i, [128, 1024], F32) for i in range(2)]
    engs = ['vector', 'gpsimd', 'scalar']
    it = 0
    for i in range(4):
        b = it % 2
        p.dma('sync', stg[b], d['wgb'][:, i * 1024:(i + 1) * 1024], [], [('w_stg', b)], 'w_stg%d' % b)
        p.copy(engs[it % 3], Wgb[:, i * 1024:(i + 1) * 1024], stg[b], [('w_stg', b)], ['Wres'])
        it += 1
    rows = ((0, 64), (64, 64), (128, 64), (192, 32))
    for i, (r0, nr) in enumerate(rows):
        b = it % 2
        p.dma('sync', stg[b][0:nr, :], d['wbrc'][r0:r0 + nr, :], [], [('w_stg', b)], 'w_stg%d' % b)
        p.copy(engs[it % 3], Wbr[0:nr, i, :], stg[b][0:nr, :], [('w_stg', b)], ['Wres'])
        it += 1
    glT = [sb("glT%d" % i, [128, 512], BF16) for i in range(2)]
    oc_t = [sb("oc_t%d" % i, [64, 4, 512], BF16) for i in range(2)]
    gate = [sb("gate%d" % i, [128, 512], F32) for i in range(2)]
    tmpy = sb("tmpy", [128, 512], F32)
    yacc = sb("yacc", [128, 512], F32)
    yst = [sb("yst%d" % i, [128, 8, 512], F32) for i in range(2)]
    k = 0
    SLp = min(SL, 1024)
    NPc = SL // SLp
    order = [(sl * SL + q * SLp) // 512 + cc for q in range(NPc) for sl in range(4) for cc in range(SLp // 512)]
    for ci, c in enumerate(order):
        cb_ = ci % 2
        cs = slice(c * 512, (c + 1) * 512)
        p.dma('sync', glT[cb_], d['GL'][:, cs], [], [('glT', cb_)], 'glT%d' % cb_)
        for i, (r0, nr) in enumerate(rows):
            p.dma('sync', oc_t[cb_][0:nr, i, :], d['OUT'][r0:r0 + nr, cs], [], [('oc_t', cb_)], 'oc_t%d' % cb_)
        for oc in range(8):
            for i, (r0, nr) in enumerate(rows):
                b = k % 2
                k += 1
                gp, gk = PS[2 + b], PK(2 + b)
                bp, bk = PS[4 + b], PK(4 + b)
                col = i * 1024 + oc * 128
                p.mm(gp, Wgb[:, col:col + 128], glT[cb_], True, True, [('glT', cb_), 'Wres'], [gk])
                p.act(gate[b], gp, AF.Sigmoid, [gk, 'bgt'], [('gate', b)], bias=bgt[:, i * 8 + oc:i * 8 + oc + 1])
                p.mm(bp, Wbr[0:nr, i, oc * 128:(oc + 1) * 128], oc_t[cb_][0:nr, i, :], True, True, [('oc_t', cb_), 'Wres'], [bk])
                if i == 0:
                    p.tt('vector', yacc, gate[b], bp, ALU.mult, [('gate', b), bk], ['yacc'])
                else:
                    p.tt('vector', tmpy, gate[b], bp, ALU.mult, [('gate', b), bk], ['tmpy'])
                    if i < 3:
                        p.tt('gpsimd', yacc, yacc, tmpy, ALU.add, ['yacc', 'tmpy'], ['yacc'])
                    else:
                        p.tt('gpsimd', yst[cb_][:, oc, :], yacc, tmpy, ALU.add, ['yacc', 'tmpy'], [('yst', cb_)])
        sl, w = (c * 512) // SL, (c * 512) % SL
        q, t0 = w // SLp, w % SLp
        p.dma('sync', d['YP'][q, sl, :, t0:t0 + 512].rearrange("(oc p) t -> p oc t", p=128), yst[cb_], [('yst', cb_)], [('YP', cb_)], 'yst%d' % cb_)
        if (ci + 1) % (4 * (SLp // 512)) == 0:
            p.cc("ReduceScatter", ALU.add, GROUPS, d['YP'][q].rearrange("a c t -> (a c) t"), d['YS'][q], [('YP', 0), ('YP', 1)], [('YS', q)])


def emit_B2(p, sb, PS, d, NTOK, nblk, F, moe):
    NCH = NTOK // 512
    FC = F // 128
    identb = sb("identb", [128, 128], BF16)
    p.dma('sync', identb, d['ident'][:, :], [], ['identb'], 'identb')
    G2 = sb("G2", [128, 1024], F32)
    p.dma('sync', G2, d['g2'].partition_broadcast(128), [], ['G2'], 'G2')
    Wout = sb("Wout", [128, 8, 1024], BF16)
    stg = [sb("w_stg%d" % i, [128, 1024], F32) for i in range(2)]
    engs = ['vector', 'gpsimd', 'scalar']
    for kc in range(8):
        b = kc % 2
        p.dma('sync', stg[b], d['wout'][kc * 128:(kc + 1) * 128, :], [], [('w_stg', b)], 'w_stg%d' % b)
        p.copy(engs[kc % 3], Wout[:, kc, :], stg[b], [('w_stg', b)], ['Wres'])
    if moe:
        identf = sb("identf", [128, 128], F32)
        p.dma('sync', identf, d['identf'][:, :], [], ['identf'], 'identf')
        Wr = sb("Wr", [128, 8, 8], F32)
        p.dma('sync', Wr, d['wr'].rearrange("(kc p) e -> p kc e", p=128), [], ['Wr'], 'Wr')
        brt = sb("brt", [128, 8], F32)
        p.dma('sync', brt, d['br'].partition_broadcast(128), [], ['brt'], 'brt')
        h2f = sb("h2f", [128, 1024], F32)
        h2fT = sb("h2fT", [128, 8, 128], F32)
        rt = sb("rt", [128, 64], F32)
        GW = sb("GW", [128, 4, 8], F32)
    X4 = sb("X4", [128, 4, 1024], F32)
    yf = sb("yf", [128, 8, 512], F32)
    hT = sb("hT", [128, 8, 512], BF16)
    hb = sb("hb", [128, 1024], BF16)
    junk = sb("junk", [128, 1024], BF16)
    st = sb("st", [128, 8], F32)
    yT = sb("yT", [128, 8, 512], BF16)
    sg = [sb("sg%d" % i, [128, 512], F32) for i in range(2)]
    WGU_t = [sb("WGU_t%d" % i, [128, 8, 2 * F], BF16) for i in range(2)]
    WD_t = [sb("WD_t%d" % i, [128, FC, 1024], BF16) for i in range(2)]
    psTb = PS[0].bitcast(BF16)
    wit = 0

    def norm_T(tt, G, Gk):
        xt = X4[:, tt, :]
        p.act(junk, xt, AF.Square, ['X4'], ['junk', 'ssq'], accum_out=st[:, 0:1])
        p.act(st[:, 1:2], st[:, 0:1], AF.Sqrt, ['ssq'], ['rs'], scale=1.0 / D, bias=EPS)
        p.recip(st[:, 2:3], st[:, 1:2], ['rs'], ['rstd'])
        p.stt(hb, xt, st[:, 2:3], G, ALU.mult, ALU.mult, ['X4', 'rstd', Gk], ['hb'])
        for kc in range(8):
            p.tr(psTb[:, kc * 128:(kc + 1) * 128], hb[:, kc * 128:(kc + 1) * 128], identb, ['hb', 'identb'], [PK(0)])
        p.copy('vector', hT[:, :, tt * 128:(tt + 1) * 128], psTb.rearrange("p (a d) -> p a d", d=128), [PK(0)], ['hT'])

    for c in range(NCH):
        T0 = c * 512
        p.dma('sync', X4, d['xtok'][T0:T0 + 512, :].rearrange("(t p) d -> p t d", p=128), [], ['X4'], 'X4')
        SLp = min(NTOK, 1024)
        p.dma('sync', yf, d['YS'][T0 // SLp, :, T0 % SLp:T0 % SLp + 512].rearrange("(oc p) t -> p oc t", p=128), [('YS', T0 // SLp)], ['yf'], 'yf')
        p.copy('gpsimd', yT[:, 0:4, :], yf[:, 0:4, :], ['yf'], ['yT'])
        p.copy('vector', yT[:, 4:8, :], yf[:, 4:8, :], ['yf'], ['yT'])
        k = 0
        for tt in range(4):
            for half in range(2):
                b = k % 2
                k += 1
                ps, pk = PS[6 + b], PK(6 + b)
                for oc in range(8):
                    p.mm(ps, yT[:, oc, tt * 128:(tt + 1) * 128], Wout[:, oc, half * 512:(half + 1) * 512], oc == 0, oc == 7, ['yT', 'Wres'], [pk])
                xs = X4[:, tt, half * 512:(half + 1) * 512]
                p.tt('vector', xs, ps, xs, ALU.add, [pk, 'X4'], ['X4'])
        for tt in range(4):
            norm_T(tt, G2, 'G2')
            if moe:
                p.stt(h2f, X4[:, tt, :], st[:, 2:3], G2, ALU.mult, ALU.mult, ['X4', 'rstd', 'G2'], ['h2f'])
                for kc in range(8):
                    bnk = 2 + kc // 4
                    p.tr(PS[bnk][:, (kc % 4) * 128:(kc % 4 + 1) * 128], h2f[:, kc * 128:(kc + 1) * 128], identf, ['h2f', 'identf'], [PK(bnk)])
                p.copy('vector', h2fT[:, 0:4, :].rearrange("p a d -> p (a d)"), PS[2], [PK(2)], ['h2fT'])
                p.copy('vector', h2fT[:, 4:8, :].rearrange("p a d -> p (a d)"), PS[3], [PK(3)], ['h2fT'])
                for kc in range(8):
                    p.mm(PS[1][:, 0:8], h2fT[:, kc, :], Wr[:, kc, :], kc == 0, kc == 7, ['h2fT', 'Wr'], [PK(1)])
                lg, m8, ee, mk = rt[:, 0:8], rt[:, 8:16], rt[:, 16:24], rt[:, 24:32]
                p.tt('vector', lg, PS[1][:, 0:8], brt, ALU.add, [PK(1), 'brt'], ['lg'])
                p.op('vector', lambda e, lg=lg, m8=m8: e.max(out=m8, in_=lg), ['lg'], ['m8'])
                p.ts('vector', rt[:, 32:33], m8[:, 0:1], -1.0, ALU.mult, ['m8'], ['negm'])
                p.act(ee, lg, AF.Exp, ['lg', 'negm'], ['ee'], bias=rt[:, 32:33])
                p.ts('vector', mk, lg, m8[:, 1:2], ALU.is_ge, ['lg', 'm8'], ['mk'])
                p.tt('vector', ee, ee, mk, ALU.mult, ['ee', 'mk'], ['ee'])
                p.op('vector', lambda e, ee=ee: e.tensor_reduce(out=rt[:, 33:34], in_=ee, axis=AX.X, op=ALU.add), ['ee'], ['den'])
                p.recip(rt[:, 34:35], rt[:, 33:34], ['den'], ['rden'])
                p.ts('vector', GW[:, tt, :], ee, rt[:, 34:35], ALU.mult, ['ee', 'rden'], ['GW'])
        aT = yT
        for blk in range(nblk):
            wb = wit % 2
            wit += 1
            p.dma('sync', WGU_t[wb], d['WGU'][blk], [], [('WGU_t', wb)], 'WGU_t%d' % wb)
            p.dma('sync', WD_t[wb], d['WD'][blk], [], [('WD_t', wb)], 'WD_t%d' % wb)
            for fc in range(FC):
                b = fc % 2
                gps, gk = PS[2 + b], PK(2 + b)
                ups, uk = PS[4 + b], PK(4 + b)
                for kc in range(8):
                    p.mm(gps, WGU_t[wb][:, kc, fc * 128:(fc + 1) * 128], hT[:, kc, :], kc == 0, kc == 7, ['hT', ('WGU_t', wb)], [gk])
                for kc in range(8):
                    p.mm(ups, WGU_t[wb][:, kc, F + fc * 128:F + (fc + 1) * 128], hT[:, kc, :], kc == 0, kc == 7, ['hT', ('WGU_t', wb)], [uk])
                p.act(sg[b], gps, AF.Silu, [gk], [('sg', b)])
                p.tt('vector', aT[:, fc, :], sg[b], ups, ALU.mult, [('sg', b), uk], ['yT'])
            k = 0
            for tt in range(4):
                for half in range(2):
                    b = k % 2
                    k += 1
                    ps, pk = PS[6 + b], PK(6 + b)
                    for fc in range(FC):
                        p.mm(ps, aT[:, fc, tt * 128:(tt + 1) * 128], WD_t[wb][:, fc, half * 512:(half + 1) * 512], fc == 0, fc == FC - 1, ['yT', ('WD_t', wb)], [pk])
                    xs = X4[:, tt, half * 512:(half + 1) * 512]
                    if moe:
                        p.stt(xs, ps, GW[:, tt, blk:blk + 1], xs, ALU.mult, ALU.add, [pk, 'GW', 'X4'], ['X4'])
                    else:
                        p.tt('vector', xs, ps, xs, ALU.add, [pk, 'X4'], ['X4'])
        p.dma('sync', d['xout'][T0:T0 + 512, :].rearrange("(t p) d -> p t d", p=128), X4, ['X4'], ['xout'], 'X4o')
        if d.get('XG') is not None:
            for kk in (2 * c, 2 * c + 1):
                p.cc("AllGather", ALU.bypass, GROUPS, d['xout'][kk * 256:(kk + 1) * 256, :], d['XG'][kk], ['xout'], [('XG', kk)])


def build_fused(S, n_layers=2):
    nc = bass.Bass("TRN2", target_bir_lowering=False)
    NT, NTOK, SL = S // 128, S // 4, S // 4
    g = {}

    def din(name, shape, dt=F32):
        g[name] = nc.dram_tensor(name, shape, dt, kind="ExternalInput").ap()

    def dint(name, shape, dt):
        g[name] = nc.dram_tensor(name, shape, dt, kind="Internal").ap()

    din('x', [S, D])
    din('xtok', [NTOK, D])
    for nm, shp, dt in (('cos', [128, SEQ // 128, 16], F32), ('sin', [128, SEQ // 128, 16], F32), ('ident', [128, 128], BF16),
                        ('triN', [128, 128], BF16), ('onesb', [128, 128], BF16), ('maskS', [128, 4, 512], BF16),
                        ('maskI', [128, 4, 512], BF16), ('onesf', [128, 64], F32), ('identf', [128, 128], F32), ('bvec', [10, 128, 128], F32)):
        din(nm, shp, dt)
    for L in range(n_layers):
        sfx = str(L)
        for nm, shp in (('w_in', [D, 1280]), ('g1', [128, 8]), ('wqb', [256, 96]), ('gqa', [128, 2]), ('wkvb', [256, 128]),
                        ('gkva', [128, 2]), ('g32', [288]), ('g96', [192]), ('sinks', [1, 2]), ('wgb', [128, 4096]),
                        ('bgate', [128, 32]), ('wbrc', [224, 1024]), ('g2', [1024]), ('wout', [1024, 1024])):
            din(nm + sfx, shp)
    din('wgu0', [1024, 4096])
    din('wdn0', [2048, 1024])
    if n_layers > 1:
        din('wr1', [1024, 8])
        din('br1', [8])
        din('wgu1', [8, 1024, 1536])
        din('wdn1', [8, 768, 1024])
    dint('QKT', [128, 8, S], BF16)
    dint('VSB', [128, NT, 64], BF16)
    dint('VML', [128, NT, 65], BF16)
    dint('VSW', [128, NT, 33], BF16)
    for i in range(3):
        dint('VD%d' % i, [S, 33], BF16)
    dint('GL', [128, S], BF16)
    dint('OUT', [224, S], BF16)
    SLp = min(SL, 1024)
    NP = SL // SLp
    NAG = NTOK // 256
    dint('YP', [NP, 4, 1024, SLp], F32)
    dint('YS', [NP, 1024, SLp], F32)
    dint('XS', [NTOK, D], F32)
    dint('XG', [NAG, 4 * 256, D], F32)
    dint('WGU0', [4, 128, 8, 1024], BF16)
    dint('WD0', [4, 128, 4, 1024], BF16)
    if n_layers > 1:
        dint('WGU1', [8, 128, 8, 1536], BF16)
        dint('WD1', [8, 128, 6, 1024], BF16)
    g['final'] = nc.dram_tensor('final', [NTOK, D], F32, kind="ExternalOutput").ap()
    shared = ('cos', 'sin', 'ident', 'triN', 'onesb', 'maskS', 'maskI', 'onesf', 'identf', 'bvec',
              'QKT', 'VSB', 'VML', 'VSW', 'VD0', 'VD1', 'VD2', 'GL', 'OUT', 'YP', 'YS')
    with ExitStack() as es:
        sb = Arena(nc, es, 200 * 1024)
        PSB = [es.enter_context(nc.psum_tensor("psb%d" % i, [128, 1024], F32))[:, :] for i in range(4)]
        PS = [PSB[i // 2][:, (i % 2) * 512:(i % 2 + 1) * 512] for i in range(8)]
        d_psb = PSB
        p = Prog(nc, es)
        block = es.enter_context(nc.Block())
        for L in range(n_layers):
            sfx = str(L)
            moe = (L % 2 == 1)
            d = {k: g[k] for k in shared}
            for nm in ('w_in', 'g1', 'wqb', 'gqa', 'wkvb', 'gkva', 'g32', 'g96', 'sinks', 'wgb', 'bgate', 'wbrc', 'g2', 'wout'):
                d[nm] = g[nm + sfx]
            if L == 0:
                d['xrows'] = lambda t: g['x'][t * 128:(t + 1) * 128, :]
            else:
                def xrows(t):
                    T = t * 128
                    r, w = T // NTOK, T % NTOK
                    return g['XG'][w // 256, r * 256 + (w % 256):r * 256 + (w % 256) + 128, :]
                d['xrows'] = xrows
                d['xkeys'] = lambda t: [('XG', ((t * 128) % NTOK) // 256)]
            d['xtok'] = g['xtok'] if L == 0 else g['XS']
            d['xout'] = g['final'] if L == n_layers - 1 else g['XS']
            d['WGU'], d['WD'] = g['WGU' + sfx], g['WD' + sfx]
            if moe:
                nblk, F = 8, 768
                d['wr'], d['br'] = g['wr1'], g['br1']
                d['wg'] = lambda blk: g['wgu1'][blk, :, 0:768]
                d['wu'] = lambda blk: g['wgu1'][blk, :, 768:1536]
                d['wd'] = lambda blk: g['wdn1'][blk, :, :]
            else:
                nblk, F = 4, 512
                d['wg'] = lambda blk: g['wgu0'][:, blk * 512:(blk + 1) * 512]
                d['wu'] = lambda blk: g['wgu0'][:, 2048 + blk * 512:2048 + (blk + 1) * 512]
                d['wd'] = lambda blk: g['wdn0'][blk * 512:(blk + 1) * 512, :]
            d['XG'] = g['XG'] if L < n_layers - 1 else None
            d['n_side'] = nblk * (8 + F // 128)
            d['PSB'] = d_psb
            for ph in (emit_inproj, emit_sb, emit_mla, emit_sw, emit_dil):
                sb.reset()
                if ph is emit_sb:
                    ph(p, sb, PS, d, S, side=lambda sb_, d=d, nblk=nblk, F=F: castw_iter(p, sb_, d, nblk, F, engs=('gpsimd',)))
                else:
                    ph(p, sb, PS, d, S)
                p.barrier(exclude_cc=True, keep=('XG',))
            sb.reset()
            emit_merge(p, sb, PS, d, S)
            p.barrier(exclude_cc=True, keep=('YS',))
            sb.reset()
            emit_B2(p, sb, PS, d, NTOK, nblk, F, moe)
            p.barrier(exclude_cc=True, keep=('XG',))
        p.barrier()
        p.finish(block)
    return nc, p


def inputs_fused(cA, inp, core, S, n_layers=2):
    b, h = core // 4, core % 4
    NTOK = S // 4
    m = {k: cA[k] for k in ('cos', 'sin', 'ident', 'triN', 'onesb', 'maskS', 'maskI', 'onesf')}
    m['identf'] = np.eye(128, dtype=np.float32)
    x = np.asarray(inp['x'], np.float32)
    m['x'] = np.ascontiguousarray(x[b, :S])
    m['xtok'] = np.ascontiguousarray(x[b, h * NTOK:(h + 1) * NTOK])
    bv = band_bias_vecs(inp['rel_bias'], h)
    kk = np.arange(128)
    m['bvec'] = np.ascontiguousarray(bv[:, kk[None, :] - kk[:, None] + 127])
    cols = core_cols(h)
    for l in range(n_layers):
        s = str(l)
        m['w_in' + s] = np.ascontiguousarray(np.concatenate([inp['w_in'][l][:, cols], inp['w_gate_a'][l]], axis=1))
        m['g1' + s] = np.ascontiguousarray(inp['norm1_g'][l].reshape(8, 128).T)
        m['wqb' + s] = np.ascontiguousarray(inp['w_qb'][l][:, h * 96:(h + 1) * 96])
        m['gqa' + s] = np.ascontiguousarray(inp['g_qa'][l].reshape(2, 128).T)
        m['wkvb' + s] = np.ascontiguousarray(inp['w_kvb'][l][:, h * 128:(h + 1) * 128])
        m['gkva' + s] = np.ascontiguousarray(inp['g_kva'][l].reshape(2, 128).T)
        gs, gd = inp['qk_g_sw'][l], inp['qk_g_dil'][l]
        m['g32' + s] = np.concatenate([gs[0], gd[0], gd[0], gs[1], gd[1], gd[1], gs[0], gd[0], gd[1]]).astype(np.float32)
        m['g96' + s] = np.concatenate([inp['qk_g_mla'][l][0], inp['qk_g_mla'][l][1]]).astype(np.float32)
        m['sinks' + s] = np.ascontiguousarray(inp['sinks'][l][2 * h:2 * h + 2].reshape(1, 2))
        m['wgb' + s] = np.ascontiguousarray(inp['w_gate_b'][l])
        m['bgate' + s] = np.ascontiguousarray(inp['b_gate'][l].reshape(32, 128).T)
        wb = inp['w_branch'][l]
        m['wbrc' + s] = np.ascontiguousarray(np.concatenate([wb[h * 64:(h + 1) * 64], wb[256 + h * 64:256 + (h + 1) * 64],
                                                             wb[512 + h * 64:512 + (h + 1) * 64], wb[768 + h * 32:768 + (h + 1) * 32]], axis=0))
        m['g2' + s] = np.ascontiguousarray(inp['norm2_g'][l])
        m['wout' + s] = np.ascontiguousarray(inp['w_out'][l])
    m['wgu0'] = np.ascontiguousarray(inp['w_gu_dense'][0])
    m['wdn0'] = np.ascontiguousarray(inp['w_down_dense'][0])
    if n_layers > 1:
        m['wr1'] = np.ascontiguousarray(inp['w_router'][0])
        m['br1'] = np.ascontiguousarray(inp['b_router'][0])
        m['wgu1'] = np.ascontiguousarray(inp['w_gu_exp'][0])
        m['wdn1'] = np.ascontiguousarray(inp['w_down_exp'][0])
    return m


def kernel_fused(inp, S=SEQ, n_layers=2):
    cA = consts_A()
    ncF, _ = _prog(('F', S, n_layers), lambda: build_fused(S, n_layers))
    in_maps = [inputs_fused(cA, inp, c, S, n_layers) for c in range(8)]
    res = run_bass_kernel_spmd(ncF, in_maps, core_ids=list(range(8)))
    NTOK = S // 4
    out = np.empty((BATCH, S, D), np.float32)
    for c in range(8):
        out[c // 4, (c % 4) * NTOK:(c % 4 + 1) * NTOK] = np.asarray(res.results[c]['final'])
    return out
```

```python
import math
from contextlib import ExitStack

import numpy as np
import ml_dtypes

import concourse.bass as bass
import concourse.mybir as mybir
from concourse.bass_utils import run_bass_kernel_spmd

F32 = mybir.dt.float32
BF16 = mybir.dt.bfloat16
U8 = mybir.dt.uint8
AF = mybir.ActivationFunctionType
ALU = mybir.AluOpType
AX = mybir.AxisListType
ENGS = ['sync', 'scalar', 'vector', 'gpsimd', 'tensor']

D = 1024
SEQ = 16384
BATCH = 2
EPS = 1e-6
NEG = -30000.0
O_AQ, O_AK, O_AV, O_CQ, O_CKV, O_KPE, O_SWQ, O_SWK, O_SWV, O_DQ, O_DK, O_DV = (
    0, 256, 512, 768, 1024, 1280, 1312, 1568, 1632, 1696, 2080, 2464)
DILS = (1, 4, 16)
STQ = 'sync'
LIMIT = 10 ** 12
SKIP = ()


class Prog:
    def __init__(self, nc, es):
        self.nc, self.es = nc, es
        self.ops = {e: [] for e in ENGS}
        self.sems, self.cnt = {}, {}
        self.known = {e: {} for e in ENGS}
        self.lastw, self.readers = {}, {}
        self.n_instr = 0
        for e in ENGS:
            self._mksem('E_' + e)

    def _mksem(self, name):
        if name not in self.sems:
            self.sems[name] = self.es.enter_context(self.nc.semaphore(name))
            self.cnt[name] = 0
        return self.sems[name]

    def _deps(self, reads, writes):
        deps = {}

        def add(d):
            if d is not None and deps.get(d[0], 0) < d[1]:
                deps[d[0]] = d[1]
        for k in reads:
            add(self.lastw.get(k))
        for k in writes:
            add(self.lastw.get(k))
            for s, v in self.readers.get(k, {}).items():
                add((s, v))
        return deps

    def _waits(self, eng, deps):
        for s, v in deps.items():
            if eng == 'tensor' and s == 'E_tensor':
                continue
            if self.known[eng].get(s, 0) >= v:
                continue
            self.known[eng][s] = v
            h = self.sems[s]
            self.ops[eng].append(lambda e, h=h, v=v: e.wait_ge(h, v))
            self.n_instr += 1

    def _record(self, s, v, reads, writes):
        for k in writes:
            self.lastw[k] = (s, v)
            self.readers[k] = {}
        for k in reads:
            self.readers.setdefault(k, {})[s] = v

    def op(self, eng, fn, reads=(), writes=()):
        self.n_ops = getattr(self, 'n_ops', 0) + 1
        if self.n_ops > LIMIT or self.n_ops in SKIP:
            return
        self._waits(eng, self._deps(reads, writes))
        s = 'E_' + eng
        self.cnt[s] += 1
        h = self.sems[s]
        self.ops[eng].append(lambda e, fn=fn, h=h: fn(e).then_inc(h, 1))
        self._record(s, self.cnt[s], reads, writes)
        self.n_instr += 1

    def dma(self, q, out, in_, reads, writes, slot):
        self.n_ops = getattr(self, 'n_ops', 0) + 1
        if self.n_ops > LIMIT:
            return
        self._waits(q, self._deps(reads, writes))
        s = 'D_' + slot
        h = self._mksem(s)
        self.cnt[s] += 16
        self.ops[q].append(lambda e, h=h, out=out, in_=in_: e.dma_start(out=out, in_=in_).then_inc(h, 16))
        self._record(s, self.cnt[s], reads, writes)
        self.n_instr += 1

    def cc(self, kind, op, groups, in_, out, reads, writes):
        self._waits('gpsimd', self._deps(reads, writes))
        s = 'C_all'
        h = self._mksem(s)
        self.cnt[s] += 1
        self.ops['gpsimd'].append(lambda e, h=h: e.collective_compute(kind, op, replica_groups=groups, ins=[in_], outs=[out]).then_inc(h, 1))
        self._record(s, self.cnt[s], reads, writes)
        self.n_instr += 1

    def barrier(self, exclude_cc=False, keep=()):
        allv = {s: v for s, v in self.cnt.items() if v > 0 and not (exclude_cc and s == 'C_all')}
        for e in ENGS:
            self._waits(e, allv)
        kept = {k: v for k, v in self.lastw.items() if isinstance(k, tuple) and k[0] in keep}
        self.lastw, self.readers = kept, {}

    def finish(self, block):
        for name in ENGS:
            def body(e, name=name):
                for f in self.ops[name]:
                    f(e)
            getattr(block, name)(body)

    def mm(self, out, lhsT, rhs, start, stop, reads, writes, skip=False):
        self.op('tensor', lambda e: e.matmul(out, lhsT, rhs, start=start, stop=stop, skip_group_check=skip), reads, writes)

    def tr(self, out, in_, ident, reads, writes):
        self.op('tensor', lambda e: e.transpose(out, in_, ident), reads, writes)

    def act(self, out, in_, func, reads, writes, bias=None, scale=None, accum_out=None):
        kw = {}
        if bias is not None:
            kw['bias'] = bias
        if scale is not None:
            kw['scale'] = scale
        if accum_out is not None:
            kw['accum_out'] = accum_out
        self.op('scalar', lambda e: e.activation(out=out, in_=in_, func=func, **kw), reads, writes)

    def tt(self, eng, out, in0, in1, op, reads, writes):
        self.op(eng, lambda e: e.tensor_tensor(out=out, in0=in0, in1=in1, op=op), reads, writes)

    def ts(self, eng, out, in0, s1, op0, reads, writes, s2=None, op1=None):
        if op1 is None:
            self.op(eng, lambda e: e.tensor_scalar(out=out, in0=in0, scalar1=s1, scalar2=None, op0=op0), reads, writes)
        else:
            self.op(eng, lambda e: e.tensor_scalar(out=out, in0=in0, scalar1=s1, scalar2=s2, op0=op0, op1=op1), reads, writes)

    def stt(self, out, in0, scalar, in1, op0, op1, reads, writes):
        self.op('vector', lambda e: e.scalar_tensor_tensor(out=out, in0=in0, scalar=scalar, in1=in1, op0=op0, op1=op1), reads, writes)

    def copy(self, eng, out, in_, reads, writes):
        if eng == 'scalar':
            self.op(eng, lambda e: e.copy(out=out, in_=in_), reads, writes)
        else:
            self.op(eng, lambda e: e.tensor_copy(out=out, in_=in_), reads, writes)

    def recip(self, out, in_, reads, writes):
        self.op('vector', lambda e: e.reciprocal(out=out, in_=in_), reads, writes)

    def memset(self, eng, ap, val, writes):
        self.op(eng, lambda e: e.memset(ap, val), (), writes)


class Arena:
    def __init__(self, nc, es, nbytes):
        self.big = es.enter_context(nc.sbuf_tensor("arena", [128, nbytes], U8))
        self.nbytes = nbytes
        self.off = 0

    def reset(self):
        self.off = 0

    def __call__(self, name, shape, dt):
        esz = 2 if dt == BF16 else 4
        n = int(np.prod(shape[1:]))
        nb = (n * esz + 63) // 64 * 64
        assert self.off + nb <= self.nbytes, (name, self.off, nb)
        ap = self.big[:, self.off:self.off + n * esz].bitcast(dt)
        self.off += nb
        if len(shape) > 2:
            names = " ".join("d%d" % i for i in range(len(shape) - 1))
            ap = ap.rearrange("p (%s) -> p %s" % (names, names), **{"d%d" % i: shape[i + 1] for i in range(len(shape) - 2)})
        if shape[0] < 128:
            ap = ap[0:shape[0]]
        return ap


def PK(i):
    return ('ps', i)


def core_cols(h):
    r = lambda o, n: list(range(o, o + n))
    swq0 = r(O_SWQ + (2 * h) * 32, 32)
    swq1 = r(O_SWQ + (2 * h + 1) * 32, 32)
    swk = r(O_SWK + (h // 2) * 32, 32)
    swv = r(O_SWV + (h // 2) * 32, 32)
    dq = [r(O_DQ + (g * 4 + h) * 32, 32) for g in range(3)]
    dk = [r(O_DK + (g * 4 + h) * 32, 32) for g in range(3)]
    dv = [r(O_DV + (g * 4 + h) * 32, 32) for g in range(3)]
    g1 = r(O_CQ, 256) + r(O_CKV, 256)
    n32 = swq0 + dq[0] + dq[1] + swk + dk[0] + dk[1] + swq1 + dq[2] + dk[2]
    g2 = n32 + r(O_AQ + h * 64, 64) + r(O_AK + h * 64, 64) + r(O_KPE, 32) + r(O_AV + h * 64, 64)
    g3 = swv + dv[0] + dv[1] + dv[2]
    return np.array(g1 + g2 + g3)


def emit_inproj(p, sb, PS, d, S):
    NT = S // 128
    identb = sb("identb", [128, 128], BF16)
    p.dma('sync', identb, d['ident'][:, :], [], ['identb'], 'identb')
    g1t = sb("g1t", [128, 8], F32)
    p.dma('sync', g1t, d['g1'][:, :], [], ['g1t'], 'g1t')
    gqat = sb("gqat", [128, 2], F32)
    p.dma('sync', gqat, d['gqa'][:, :], [], ['gqat'], 'gqat')
    gkvat = sb("gkvat", [128, 2], F32)
    p.dma('sync', gkvat, d['gkva'][:, :], [], ['gkvat'], 'gkvat')
    G32 = sb("G32", [128, 288], F32)
    p.dma('sync', G32, d['g32'].partition_broadcast(128), [], ['G32'], 'G32')
    G96 = sb("G96", [128, 2, 96], F32)
    p.dma('sync', G96.rearrange("p a d -> p (a d)"), d['g96'].partition_broadcast(128), [], ['G96'], 'G96')
    COS = sb("COS", [128, NT, 16], F32)
    SIN = sb("SIN", [128, NT, 16], F32)
    p.dma('sync', COS, d['cos'][:, 0:NT, :], [], ['COS'], 'COS')
    p.dma('sync', SIN, d['sin'][:, 0:NT, :], [], ['SIN'], 'SIN')
    Wb = sb("Wb", [128, 8, 1280], BF16)
    wst = [sb("wst%d" % i, [128, 1280], F32) for i in range(2)]
    for kc in range(8):
        b = kc % 2
        p.dma('sync', wst[b], d['w_in'][kc * 128:(kc + 1) * 128, :], [], [('wst', b)], 'wst%d' % b)
        p.act(Wb[:, kc, :], wst[b], AF.Copy, [('wst', b), 'g1t'], ['Wb'], scale=g1t[:, kc:kc + 1])
    Wqb = sb("Wqb", [128, 2, 96], BF16)
    Wkvb = sb("Wkvb", [128, 2, 128], BF16)
    for i in range(2):
        p.dma('sync', wst[0][:, 0:96], d['wqb'][i * 128:(i + 1) * 128, :], [], [('wst', 0)], 'wst0')
        p.act(Wqb[:, i, :], wst[0][:, 0:96], AF.Copy, [('wst', 0), 'gqat'], ['Wqb'], scale=gqat[:, i:i + 1])
        p.dma('sync', wst[1][:, 0:128], d['wkvb'][i * 128:(i + 1) * 128, :], [], [('wst', 1)], 'wst1')
        p.act(Wkvb[:, i, :], wst[1][:, 0:128], AF.Copy, [('wst', 1), 'gkvat'], ['Wkvb'], scale=gkvat[:, i:i + 1])

    X = [sb("x%d" % i, [128, 1024], F32) for i in range(2)]
    junk = sb("junk", [128, 1024], BF16)
    hb = [sb("hb%d" % i, [128, 1024], BF16) for i in range(2)]
    hT = [sb("hT%d" % i, [128, 8, 128], BF16) for i in range(2)]
    st = sb("stats", [128, 32], F32)
    cb = sb("cb", [128, 512], BF16)
    cT = sb("cT", [128, 4, 128], BF16)
    qk96 = sb("qk96", [128, 2, 96], F32)
    tmp96 = sb("tmp96", [128, 2, 96], F32)
    qkb = sb("qkb", [128, 2, 96], BF16)
    rA = sb("ropeA", [128, 2, 2, 16], F32)
    rB = sb("ropeB", [128, 2, 2, 16], F32)
    sq32 = sb("sq32", [128, 288], F32)
    tmp32 = sb("tmp32", [128, 288], F32)
    n32b = sb("n32b", [128, 288], BF16)
    sbqk = sb("sbqk", [128, 128], BF16)
    stageT = [sb("stageT%d" % i, [128, 8, 512], BF16) for i in range(2)]
    stVS = [sb("stVS%d" % i, [128, 4, 64], BF16) for i in range(2)]
    stVM = [sb("stVM%d" % i, [128, 4, 65], BF16) for i in range(2)]
    stVG = [sb("stVG%d" % i, [128, 4, 4, 33], BF16) for i in range(2)]
    stGL = [sb("stGL%d" % i, [128, 512], BF16) for i in range(2)]
    glb = sb("glb", [128, 128], BF16)
    for i in range(2):
        p.memset('gpsimd', stageT[i], 0.0, [('stageT', i)])
        p.memset('gpsimd', stVM[i], 1.0, [('stVM', i)])
        p.memset('gpsimd', stVG[i], 1.0, [('stVG', i)])
    psTb = PS[0].bitcast(BF16)
    ps1, ps2, ps3 = PS[1], PS[2], PS[3]
    ps4b = PS[4].bitcast(BF16)
    ps5 = PS[5]
    ps6b = PS[6].bitcast(BF16)
    xrows = d['xrows']

    xkeys = d.get('xkeys', lambda t: [])
    p.dma('sync', X[0], xrows(0), xkeys(0), [('x', 0)], 'x0')
    for t in range(NT):
        b = t % 2
        tt = t % 4
        sg = (t // 4) % 2
        xk, hbk, hTk = ('x', b), ('hb', b), ('hT', b)
        if t + 1 < NT:
            p.dma('sync', X[1 - b], xrows(t + 1), xkeys(t + 1), [('x', 1 - b)], 'x%d' % (1 - b))
        p.act(junk, X[b], AF.Square, [xk], ['junk', 'ssq'], accum_out=st[:, 0:1])
        p.act(st[:, 1:2], st[:, 0:1], AF.Sqrt, ['ssq'], ['rs'], scale=1.0 / D, bias=EPS)
        p.recip(st[:, 2:3], st[:, 1:2], ['rs'], ['rstd'])
        p.act(hb[b], X[b], AF.Copy, [xk, 'rstd'], [hbk], scale=st[:, 2:3])
        for kc in range(8):
            p.tr(psTb[:, kc * 128:(kc + 1) * 128], hb[b][:, kc * 128:(kc + 1) * 128], identb, [hbk, 'identb'], [PK(0)])
        p.copy('vector', hT[b].rearrange("p a d -> p (a d)"), psTb, [PK(0)], [hTk])
        for (ps, key, c0, c1) in ((ps1, PK(1), 0, 512), (ps2, PK(2), 512, 1024), (ps3, PK(3), 1024, 1280)):
            for kc in range(8):
                p.mm(ps[:, 0:c1 - c0], hT[b][:, kc, :], Wb[:, kc, c0:c1], kc == 0, kc == 7, [hTk, 'Wb'], [key])
        p.act(junk[:, 0:256], ps1[:, 0:256], AF.Square, [PK(1)], ['junk', 'ssq2a'], accum_out=st[:, 3:4])
        p.act(junk[:, 0:256], ps1[:, 256:512], AF.Square, [PK(1)], ['junk', 'ssq2b'], accum_out=st[:, 4:5])
        p.act(st[:, 5:7], st[:, 3:5], AF.Sqrt, ['ssq2a', 'ssq2b'], ['rs2'], scale=1.0 / 256, bias=EPS)
        p.recip(st[:, 7:9], st[:, 5:7], ['rs2'], ['rstd2'])
        p.copy('vector', cb, ps1, [PK(1)], ['cb'])
        for i in range(4):
            p.tr(ps4b[:, i * 128:(i + 1) * 128], cb[:, i * 128:(i + 1) * 128], identb, ['cb', 'identb'], [PK(4)])
        p.copy('vector', cT.rearrange("p a d -> p (a d)"), ps4b[:, 0:512], [PK(4)], ['cT'])
        for i in range(2):
            p.mm(ps5[:, 0:96], cT[:, i, :], Wqb[:, i, :], i == 0, i == 1, ['cT', 'Wqb'], [PK(5)])
        for i in range(2):
            p.mm(ps5[:, 128:256], cT[:, 2 + i, :], Wkvb[:, i, :], i == 0, i == 1, ['cT', 'Wkvb'], [PK(5)])
        p.act(qk96[:, 0, :], ps5[:, 0:96], AF.Copy, [PK(5), 'rstd2'], ['qk96'], scale=st[:, 7:8])
        p.act(qk96[:, 1, 0:64], ps5[:, 128:192], AF.Copy, [PK(5), 'rstd2'], ['qk96'], scale=st[:, 8:9])
        p.act(stVM[sg][:, tt, 0:64], ps5[:, 192:256], AF.Copy, [PK(5), 'rstd2'], [('stVM', sg)], scale=st[:, 8:9])
        p.copy('vector', qk96[:, 1, 64:96], ps2[:, 416:448], [PK(2)], ['qk96'])
        R = qk96[:, :, 64:96].rearrange("p a (h d) -> p a h d", h=2)
        cosb = COS[:, t, :].unsqueeze(1).unsqueeze(1).broadcast_to([128, 2, 2, 16])
        sinb = SIN[:, t, :].unsqueeze(1).broadcast_to([128, 2, 16])
        p.tt('vector', rA, R, cosb, ALU.mult, ['qk96', 'COS'], ['rA'])
        p.tt('vector', rB[:, :, 0, :], R[:, :, 1, :], sinb, ALU.mult, ['qk96', 'SIN'], ['rB'])
        p.tt('vector', rB[:, :, 1, :], R[:, :, 0, :], sinb, ALU.mult, ['qk96', 'SIN'], ['rB'])
        p.tt('vector', R[:, :, 0, :], rA[:, :, 0, :], rB[:, :, 0, :], ALU.subtract, ['rA', 'rB'], ['qk96'])
        p.tt('vector', R[:, :, 1, :], rA[:, :, 1, :], rB[:, :, 1, :], ALU.add, ['rA', 'rB'], ['qk96'])
        p.tt('vector', tmp96, qk96, qk96, ALU.mult, ['qk96'], ['tmp96'])
        p.op('vector', lambda e: e.tensor_reduce(out=st[:, 9:11], in_=tmp96, axis=AX.X, op=ALU.add), ['tmp96'], ['ssq96'])
        p.act(st[:, 11:13], st[:, 9:11], AF.Sqrt, ['ssq96'], ['rs96'], scale=1.0 / 96, bias=EPS)
        p.recip(st[:, 13:15], st[:, 11:13], ['rs96'], ['rstd96'])
        p.tt('vector', tmp96, qk96, G96, ALU.mult, ['qk96', 'G96'], ['tmp96'])
        p.tt('vector', qkb, tmp96, st[:, 13:15].unsqueeze(2).broadcast_to([128, 2, 96]), ALU.mult, ['tmp96', 'rstd96'], ['qkb'])
        p.act(sq32, ps2[:, 0:288], AF.Square, [PK(2)], ['sq32'])
        p.op('vector', lambda e: e.tensor_reduce(out=st[:, 15:24], in_=sq32.rearrange("p (a d) -> p a d", d=32), axis=AX.X, op=ALU.add), ['sq32'], ['ssq32'])
        p.act(st[:, 15:24], st[:, 15:24], AF.Sqrt, ['ssq32'], ['ssq32'], scale=1.0 / 32, bias=EPS)
        p.recip(st[:, 15:24], st[:, 15:24], ['ssq32'], ['ssq32'])
        p.tt('vector', tmp32, ps2[:, 0:288], G32, ALU.mult, [PK(2), 'G32'], ['tmp32'])
        p.tt('vector', n32b.rearrange("p (a d) -> p a d", d=32), tmp32.rearrange("p (a d) -> p a d", d=32),
             st[:, 15:24].unsqueeze(2).broadcast_to([128, 9, 32]), ALU.mult, ['tmp32', 'ssq32'], ['n32b'])
        p.ts('vector', sbqk[:, 0:64], ps2[:, 288:352], 0.125, ALU.mult, [PK(2)], ['sbqk'])
        p.copy('vector', sbqk[:, 64:128], ps2[:, 352:416], [PK(2)], ['sbqk'])
        p.copy('vector', stVS[sg][:, tt, :], ps2[:, 448:512], [PK(2)], [('stVS', sg)])
        p.copy('vector', stVG[sg][:, tt, :, 0:32], ps3[:, 0:128].rearrange("p (a d) -> p a d", d=32), [PK(3)], [('stVG', sg)])
        p.copy('vector', glb, ps3[:, 128:256], [PK(3)], ['glb'])
        p.tr(ps4b[:, 512:640], glb, identb, ['glb', 'identb'], [PK(4)])
        p.copy('vector', stGL[sg][:, tt * 128:(tt + 1) * 128], ps4b[:, 512:640], [PK(4)], [('stGL', sg)])
        p.tr(ps6b[0:64, 0:128], sbqk[:, 0:64], identb, ['sbqk', 'identb'], [PK(6)])
        p.tr(ps6b[0:64, 128:256], sbqk[:, 64:128], identb, ['sbqk', 'identb'], [PK(6)])
        p.tr(ps6b[0:96, 256:384], qkb[:, 0, :], identb, ['qkb', 'identb'], [PK(6)])
        p.tr(ps6b[0:96, 384:512], qkb[:, 1, :], identb, ['qkb', 'identb'], [PK(6)])
        p.tr(ps6b[0:96, 512:640], n32b[:, 0:96], identb, ['n32b', 'identb'], [PK(6)])
        p.tr(ps6b[0:96, 640:768], n32b[:, 96:192], identb, ['n32b', 'identb'], [PK(6)])
        p.tr(ps6b[0:64, 768:896], n32b[:, 192:256], identb, ['n32b', 'identb'], [PK(6)])
        p.tr(ps6b[0:64, 896:1024], n32b[:, 224:288], identb, ['n32b', 'identb'], [PK(6)])
        p.copy('vector', stageT[sg][0:96, 2:6, tt * 128:(tt + 1) * 128], ps6b[0:96, 256:768].rearrange("p (a d) -> p a d", d=128), [PK(6)], [('stageT', sg)])
        p.copy('vector', stageT[sg][0:64, 0:2, tt * 128:(tt + 1) * 128], ps6b[0:64, 0:256].rearrange("p (a d) -> p a d", d=128), [PK(6)], [('stageT', sg)])
        p.copy('vector', stageT[sg][0:64, 6:8, tt * 128:(tt + 1) * 128], ps6b[0:64, 768:1024].rearrange("p (a d) -> p a d", d=128), [PK(6)], [('stageT', sg)])
        if tt == 3:
            T0 = (t - 3) * 128
            j0 = t - 3
            p.dma(STQ, d['QKT'][0:96, :, T0:T0 + 512], stageT[sg][0:96], [('stageT', sg)], [('QKT', sg)], 'stageT%d' % sg)
            p.dma(STQ, d['GL'][:, T0:T0 + 512], stGL[sg], [('stGL', sg)], [('GL', sg)], 'stGL%d' % sg)
            p.dma(STQ, d['VSB'][:, j0:j0 + 4, :], stVS[sg], [('stVS', sg)], [('VSB', sg)], 'stVS%d' % sg)
            p.dma(STQ, d['VML'][:, j0:j0 + 4, :], stVM[sg], [('stVM', sg)], [('VML', sg)], 'stVM%d' % sg)
            p.dma(STQ, d['VSW'][:, j0:j0 + 4, :], stVG[sg][:, :, 0, :], [('stVG', sg)], [('VG', sg)], 'stVG%d' % sg)
            for g in range(3):
                p.dma(STQ, d['VD%d' % g][T0:T0 + 512, :].rearrange("(a p) d -> p a d", p=128), stVG[sg][:, :, 1 + g, :],
                      [('stVG', sg)], [('VG', sg)], 'stVG%d' % sg)


def load_cols(p, dst, dst_key, src, src_keys, slot, nsplit=4):
    n = src.shape[-1]
    w = n // nsplit
    for i in range(nsplit):
        p.dma('sync', dst[:, i * w:(i + 1) * w], src[:, i * w:(i + 1) * w], src_keys, [dst_key], slot)


def emit_sb(p, sb, PS, d, S, side=None):
    NT = S // 128
    QT = sb("sbQT", [64, S], BF16)
    KT = sb("sbKT", [64, S], BF16)
    V = sb("sbV", [128, NT, 64], BF16)
    load_cols(p, QT, 'sbQT', d['QKT'][0:64, 0, :], [], 'sbQT')
    load_cols(p, KT, 'sbKT', d['QKT'][0:64, 1, :], [], 'sbKT')
    p.dma('sync', V, d['VSB'][:, :, :], [], ['sbV'], 'sbV')
    triN = sb("triN", [128, 128], BF16)
    onesb = sb("onesb", [128, 128], BF16)
    maskS = sb("maskS", [128, 4, 512], BF16)
    p.dma('sync', triN, d['triN'][:, :], [], ['triN'], 'triN')
    p.dma('sync', onesb, d['onesb'][:, :], [], ['onesb'], 'onesb')
    p.dma('sync', maskS, d['maskS'][:, :, :], [], ['maskS'], 'maskS')
    e_t = [sb("sb_e%d" % i, [128, 1024], F32) for i in range(2)]
    sp_t = [sb("sb_sp%d" % i, [128, 1024], BF16) for i in range(2)]
    ex_t = [sb("sb_ex%d" % i, [128, 1024], F32) for i in range(2)]
    A_t = [sb("sb_A%d" % i, [128, 1024], BF16) for i in range(2)]
    Cb = sb("sb_Cb", [128, 512], F32)
    ost = [sb("sb_ost%d" % i, [64, 512], BF16) for i in range(2)]
    PSB = d['PSB']
    pairs = []
    for c in range(S // 512):
        js = list(range(4 * c + 3, -1, -1))
        for m in range(0, len(js), 2):
            pairs.append((c, m, js[m], js[m + 1]))
    NS = len(pairs)

    def stage1(pi):
        c, m, j0, j1 = pairs[pi]
        pb = pi % 2
        ek, spk = ('e', pb), ('sp', pb)
        for h, j in enumerate((j0, j1)):
            p.mm(PS[2 * pb + h], KT[:, j * 128:(j + 1) * 128], QT[:, c * 512:(c + 1) * 512], True, True, ['sbKT', 'sbQT'], [PK(2 * pb + h)])
        p.act(e_t[pb], PSB[pb], AF.Exp, [PK(2 * pb), PK(2 * pb + 1)], [ek])
        p.act(sp_t[pb], e_t[pb], AF.Ln, [ek], [spk], bias=1.0)
        for h, j in enumerate((j0, j1)):
            r = j - 4 * c
            if r >= 0:
                hs = slice(h * 512, (h + 1) * 512)
                p.tt('gpsimd', sp_t[pb][:, hs], sp_t[pb][:, hs], maskS[:, r, :], ALU.mult, [spk, 'maskS'], [spk])

    def stage2(pi):
        c, m, j0, j1 = pairs[pi]
        pb = pi % 2
        spk, exk, Ak = ('sp', pb), ('ex', pb), ('A', pb)
        for h, j in enumerate((j0, j1)):
            hs = slice(h * 512, (h + 1) * 512)
            zk, ck = PK(2 * pb + h), PK(4 + h)
            p.mm(PS[2 * pb + h], triN, sp_t[pb][:, hs], False, True, [spk, 'triN'], [zk], skip=True)
            p.mm(PS[4 + h], onesb, sp_t[pb][:, hs], True, True, [spk, 'onesb'], [ck])
            if m == 0 and h == 0:
                p.copy('vector', ex_t[pb][:, hs], PS[2 * pb + h], [zk], [exk])
                p.copy('vector', Cb, PS[4 + h], [ck], ['Cb'])
            else:
                p.tt('vector', ex_t[pb][:, hs], PS[2 * pb + h], Cb, ALU.subtract, [zk, 'Cb'], [exk])
                if j > 0:
                    p.tt('vector', Cb, PS[4 + h], Cb, ALU.add, [ck, 'Cb'], ['Cb'])
        p.act(A_t[pb], ex_t[pb], AF.Exp, [exk], [Ak])
        for h, j in enumerate((j0, j1)):
            r = j - 4 * c
            if r >= 0:
                hs = slice(h * 512, (h + 1) * 512)
                p.tt('gpsimd', A_t[pb][:, hs], A_t[pb][:, hs], maskS[:, r, :], ALU.mult, [Ak, 'maskS'], [Ak])

    def stage3(pi):
        c, m, j0, j1 = pairs[pi]
        pb = pi % 2
        ob = c % 2
        for h, j in enumerate((j0, j1)):
            hs = slice(h * 512, (h + 1) * 512)
            p.mm(PS[6 + ob][0:64, :], V[:, j, :], A_t[pb][:, hs], m == 0 and h == 0, j == 0, [('A', pb), 'sbV'], [PK(6 + ob)])
        if j1 == 0:
            p.copy('vector', ost[ob], PS[6 + ob][0:64, :], [PK(6 + ob)], [('ost', ob)])
            p.dma(STQ, d['OUT'][0:64, c * 512:(c + 1) * 512], ost[ob], [('ost', ob)], [('OUT', ob)], 'sb_ost%d' % ob)

    side_it = side(sb) if side is not None else None
    n_side = d.get('n_side', 0)
    every = max(1, NS // max(n_side, 1))
    for i in range(NS + 2):
        if i < NS:
            stage1(i)
        if 0 <= i - 1 < NS:
            stage2(i - 1)
        if 0 <= i - 2 < NS:
            stage3(i - 2)
        if side_it is not None and i % every == 0:
            next(side_it, None)
    if side_it is not None:
        for _ in side_it:
            pass


def emit_mla(p, sb, PS, d, S):
    NT = S // 128
    QT = sb("mlQT", [96, S], BF16)
    KT = sb("mlKT", [96, S], BF16)
    V = sb("mlV", [128, NT, 65], BF16)
    load_cols(p, QT, 'mlQT', d['QKT'][0:96, 2, :], [], 'mlQT')
    load_cols(p, KT, 'mlKT', d['QKT'][0:96, 3, :], [], 'mlKT')
    p.dma('sync', V, d['VML'][:, :, :], [], ['mlV'], 'mlV')
    maskI = sb("maskI", [128, 4, 512], BF16)
    p.dma('sync', maskI, d['maskI'][:, :, :], [], ['maskI'], 'maskI')
    onesf = sb("onesf", [128, 64], F32)
    p.dma('sync', onesf, d['onesf'][:, :], [], ['onesf'], 'onesf')
    P_t = [sb("ml_P%d" % i, [128, 1024], BF16) for i in range(3)]
    osb = sb("ml_osb", [128, 512], F32)
    rrow = sb("ml_rrow", [128, 512], F32)
    ost = [sb("ml_ost%d" % i, [64, 512], BF16) for i in range(2)]
    PSB = d['PSB']
    scale = 96 ** -0.5
    pairs = []
    for c in range(S // 512):
        js = list(range(4 * c + 3, -1, -1))
        for m in range(0, len(js), 2):
            pairs.append((c, m, js[m], js[m + 1]))
    NS = len(pairs)

    def stage1(pi):
        c, m, j0, j1 = pairs[pi]
        pb = pi % 3
        Pk = ('P', pb)
        for h, j in enumerate((j0, j1)):
            p.mm(PS[2 * pb + h], KT[:, j * 128:(j + 1) * 128], QT[:, c * 512:(c + 1) * 512], True, True, ['mlKT', 'mlQT'], [PK(2 * pb + h)])
        p.act(P_t[pb], PSB[pb], AF.Exp, [PK(2 * pb), PK(2 * pb + 1)], [Pk], scale=scale)
        for h, j in enumerate((j0, j1)):
            r = j - 4 * c
            if r >= 0:
                hs = slice(h * 512, (h + 1) * 512)
                p.tt('gpsimd', P_t[pb][:, hs], P_t[pb][:, hs], maskI[:, r, :], ALU.mult, [Pk, 'maskI'], [Pk])

    def stage2(pi):
        c, m, j0, j1 = pairs[pi]
        pb = pi % 3
        ob = c % 2
        oD, ok = PS[6], PK(6)
        for h, j in enumerate((j0, j1)):
            hs = slice(h * 512, (h + 1) * 512)
            p.mm(oD[0:65, :], V[:, j, :], P_t[pb][:, hs], m == 0 and h == 0, j == 0, [('P', pb), 'mlV'], [ok])
        if j1 == 0:
            p.copy('vector', osb[0:65, :], oD[0:65, :], [ok], ['ml_osb'])
            p.act(rrow[64:65, :], osb[64:65, :], AF.Ln, ['ml_osb'], ['ml_rrow'])
            p.act(rrow[64:65, :], rrow[64:65, :], AF.Exp, ['ml_rrow'], ['ml_rrow'], scale=-1.0)
            p.mm(PS[7][0:64, :], onesf[64:65, :], rrow[64:65, :], True, True, ['ml_rrow', 'onesf'], [PK(7)])
            p.tt('vector', ost[ob], osb[0:64, :], PS[7][0:64, :], ALU.mult, ['ml_osb', PK(7)], [('ml_ost', ob)])
            p.dma(STQ, d['OUT'][64:128, c * 512:(c + 1) * 512], ost[ob], [('ml_ost', ob)], [('OUT', 2 + ob)], 'ml_ost%d' % ob)

    for i in range(NS + 2):
        if i < NS:
            stage1(i)
        if 0 <= i - 2 < NS:
            stage2(i - 2)


def toeplitz(bmat, idx, rev=False):
    return bmat[idx, :, :]


def run_band_units(p, PS, units, t_sb, P_sb, scale):
    def front(ui):
        slots, Btile, Bkey, _ = units[ui]
        b = ui % 2
        s_ps, sk = PS[b], PK(b)
        for u, sl in enumerate(slots):
            p.mm(s_ps[:, u * 256:u * 256 + 128], sl['kc'], sl['q'], True, True, sl['rk'], [sk])
            p.mm(s_ps[:, u * 256 + 128:(u + 1) * 256], sl['kp'], sl['q'], True, True, sl['rk'], [sk])
        p.stt(t_sb[b], s_ps, scale, Btile, ALU.mult, ALU.add, [sk, Bkey], [('bt', b)])
        p.act(P_sb[b], t_sb[b], AF.Exp, [('bt', b)], [('bP', b)])

    def back(ui):
        slots, _, _, epi = units[ui]
        b = ui % 2
        o_ps, ok = PS[2 + b], PK(2 + b)
        for u, sl in enumerate(slots):
            p.mm(o_ps[0:33, u * 128:(u + 1) * 128], sl['vc'], P_sb[b][:, u * 256:u * 256 + 128], True, False, [('bP', b)] + sl['vk'], [ok])
            p.mm(o_ps[0:33, u * 128:(u + 1) * 128], sl['vp'], P_sb[b][:, u * 256 + 128:(u + 1) * 256], False, True, [('bP', b)] + sl['vk'], [ok])
        epi(o_ps, ok)

    n = len(units)
    for i in range(n + 1):
        if i < n:
            front(i)
        if i >= 1:
            back(i - 1)


def emit_sw(p, sb, PS, d, S):
    NT = S // 128
    Q = [sb("swQ%d" % i, [32, S], BF16) for i in range(2)]
    K = sb("swK", [32, S], BF16)
    V = sb("swV", [128, NT, 33], BF16)
    load_cols(p, Q[0], 'swQ0', d['QKT'][0:32, 4, :], [], 'swQ0')
    load_cols(p, Q[1], 'swQ1', d['QKT'][0:32, 6, :], [], 'swQ1')
    load_cols(p, K, 'swK', d['QKT'][0:32, 5, :], [], 'swK')
    p.dma('sync', V, d['VSW'][:, :, :], [], ['swV'], 'swV')
    B = sb("swB", [128, 2, 2, 128], F32)
    B0 = sb("swB0", [128, 2, 2, 128], F32)
    for h in range(2):
        for sel in range(2):
            p.dma('sync', B[:, h, sel, :], toeplitz(d['bvec'], h * 2 + sel), [], ['swB'], 'swB')
        p.dma('sync', B0[:, h, 0, :], toeplitz(d['bvec'], h * 2), [], ['swB0'], 'swB0')
        p.memset('gpsimd', B0[:, h, 1, :], NEG, ['swB0'])
    onesf = sb("onesf", [128, 64], F32)
    p.dma('sync', onesf, d['onesf'][:, :], [], ['onesf'], 'onesf')
    es = sb("sw_es", [128, 2], F32)
    p.dma('sync', es[32:33, :], d['sinks'][:, :], [], ['sw_es'], 'sw_es')
    p.act(es[32:33, :], es[32:33, :], AF.Exp, ['sw_es'], ['sw_es'])
    t_sb = [sb("bt%d" % i, [128, 512], F32) for i in range(2)]
    P_sb = [sb("bP%d" % i, [128, 512], BF16) for i in range(2)]
    osb = sb("sw_osb", [128, 2, 512], F32)
    rrow = sb("sw_rrow", [128, 2, 512], F32)
    ost = [sb("sw_ost%d" % i, [32, 2, 512], BF16) for i in range(2)]
    scale = 32 ** -0.5
    units = []
    for n in range(NT):
        cs = slice(n * 128, (n + 1) * 128)
        ps_ = slice(max(n - 1, 0) * 128, (max(n - 1, 0) + 1) * 128)
        slots = [dict(kc=K[:, cs], kp=K[:, ps_], q=Q[h][:, cs], vc=V[:, n, :], vp=V[:, max(n - 1, 0), :],
                      rk=['swK', 'swQ%d' % h], vk=['swV']) for h in range(2)]

        def epi(o_ps, ok, n=n):
            tt = n % 4
            p.copy('scalar', osb[0:33, :, tt * 128:(tt + 1) * 128], o_ps[0:33, 0:256].rearrange("p (a d) -> p a d", d=128), [ok], ['sw_osb'])
            if tt == 3:
                gi = (n // 4) % 2
                T0 = (n - 3) * 128
                for h in range(2):
                    p.act(rrow[32:33, h, :], osb[32:33, h, :], AF.Ln, ['sw_osb', 'sw_es'], ['sw_rrow'], bias=es[32:33, h:h + 1])
                    p.act(rrow[32:33, h, :], rrow[32:33, h, :], AF.Exp, ['sw_rrow'], ['sw_rrow'], scale=-1.0)
                    p.mm(PS[4 + h][0:32, :], onesf[32:33, 0:32], rrow[32:33, h, :], True, True, ['sw_rrow', 'onesf'], [PK(4 + h)])
                    p.tt('vector', ost[gi][:, h, :], osb[0:32, h, :], PS[4 + h][0:32, :], ALU.mult, ['sw_osb', PK(4 + h)], [('sw_ost', gi)])
                p.dma(STQ, d['OUT'][128:192, T0:T0 + 512].rearrange("(h p) t -> p h t", p=32), ost[gi], [('sw_ost', gi)], [('OUT', 4 + gi)], 'sw_ost%d' % gi)
        units.append((slots, (B0 if n == 0 else B).rearrange("p a b c -> p (a b c)"), 'swB0' if n == 0 else 'swB', epi))
    run_band_units(p, PS, units, t_sb, P_sb, scale)


def emit_dil(p, sb, PS, d, S):
    Qd = sb("dlQ", [96, S], BF16)
    Kd = sb("dlK", [96, S], BF16)
    Oacc = sb("dlO", [128, S], F32)
    onesf = sb("onesf", [128, 64], F32)
    p.dma('sync', onesf, d['onesf'][:, :], [], ['onesf'], 'onesf')
    Vg = sb("dlV", [128, S // 128, 33], BF16)
    Bt = [sb("dlB%d" % i, [128, 2, 2, 128], F32) for i in range(2)]
    t_sb = [sb("bt%d" % i, [128, 512], F32) for i in range(2)]
    P_sb = [sb("bP%d" % i, [128, 512], BF16) for i in range(2)]
    rrow = sb("dl_rrow", [128, 512], F32)
    ost = [sb("dl_ost%d" % i, [32, 512], BF16) for i in range(2)]
    scale = 32 ** -0.5
    src = [(4, 5, 32), (4, 5, 64), (6, 7, 32)]
    for g, dil in enumerate(DILS):
        qs_, ks_, pb = src[g]
        rows = slice(pb, pb + 32)
        M = S // dil
        nb = M // 128
        load_cols(p, Qd[rows], 'dlQ', d['QKT'][rows, qs_, :], [], 'dlQ')
        load_cols(p, Kd[rows], 'dlK', d['QKT'][rows, ks_, :], [], 'dlK')
        Vv = Vg.rearrange("p (r n) d -> p r n d", r=dil)
        vd = d['VD%d' % g]
        for r0 in range(dil):
            for n0 in range(0, nb, 16):
                nn = min(16, nb - n0)
                srcap = bass.AP(vd.tensor, r0 * 33 + n0 * 128 * dil * 33, [[dil * 33, 128], [128 * dil * 33, nn], [1, 33]])
                p.dma('sync', Vv[:, r0, n0:n0 + nn, :], srcap, [], ['dlV'], 'dlV')
        for v in range(2):
            for u in range(2):
                for sel in range(2):
                    if v == 1 and u == 0 and sel == 1:
                        p.memset('gpsimd', Bt[v][:, u, sel, :], NEG, [('dlB', v)])
                    else:
                        p.dma('sync', Bt[v][:, u, sel, :], toeplitz(d['bvec'], (2 + g) * 2 + sel), [], [('dlB', v)], 'dlB%d' % v)
        units = []
        for r in range(dil):
            for n in range(0, nb, 2):
                slots = []
                for u in range(2):
                    n1 = n + u
                    np_ = max(n1 - 1, 0)
                    qc = slice(r + dil * 128 * n1, r + dil * 128 * n1 + dil * 127 + 1, dil)
                    kc = slice(r + dil * 128 * np_, r + dil * 128 * np_ + dil * 127 + 1, dil)
                    slots.append(dict(kc=Kd[rows, qc], kp=Kd[rows, kc], q=Qd[rows, qc], vc=Vv[:, r, n1, :], vp=Vv[:, r, np_, :],
                                      rk=['dlK', 'dlQ'], vk=['dlV']))
                v = 1 if n == 0 else 0
                oc = slice(r + dil * 128 * n, r + dil * 128 * n + dil * 255 + 1, dil)

                def epi(o_ps, ok, oc=oc, g=g):
                    if g == 0:
                        p.copy('vector', Oacc[0:33, oc], o_ps[0:33, 0:256], [ok], ['dlO'])
                    else:
                        p.tt('vector', Oacc[0:33, oc], o_ps[0:33, 0:256], Oacc[0:33, oc], ALU.add, [ok, 'dlO'], ['dlO'])
                units.append((slots, Bt[v].rearrange("p a b c -> p (a b c)"), ('dlB', v), epi))
        run_band_units(p, PS, units, t_sb, P_sb, scale)
    for c in range(S // 512):
        gi = c % 2
        cs = slice(c * 512, (c + 1) * 512)
        p.act(rrow[32:33, :], Oacc[32:33, cs], AF.Ln, ['dlO'], ['dl_rrow'])
        p.act(rrow[32:33, :], rrow[32:33, :], AF.Exp, ['dl_rrow'], ['dl_rrow'], scale=-1.0)
        p.mm(PS[4 + gi][0:32, :], onesf[32:33, 0:32], rrow[32:33, :], True, True, ['dl_rrow', 'onesf'], [PK(4 + gi)])
        p.tt('vector', ost[gi], Oacc[0:32, cs], PS[4 + gi][0:32, :], ALU.mult, ['dlO', PK(4 + gi)], [('dl_ost', gi)])
        p.dma(STQ, d['OUT'][192:224, cs], ost[gi], [('dl_ost', gi)], [('OUT', 6 + gi)], 'dl_ost%d' % gi)


def build_A(S, phases=('inproj', 'sb', 'mla', 'sw', 'dil'), dbg=False):
    nc = bass.Bass("TRN2", target_bir_lowering=False)
    NT = S // 128
    d = {}

    def din(name, shape, dt=F32):
        d[name] = nc.dram_tensor(name, shape, dt, kind="ExternalInput").ap()

    din('x', [S, D])
    din('w_in', [D, 1280])
    din('g1', [128, 8])
    din('wqb', [256, 96])
    din('gqa', [128, 2])
    din('wkvb', [256, 128])
    din('gkva', [128, 2])
    din('g32', [288])
    din('g96', [192])
    din('cos', [128, SEQ // 128, 16])
    din('sin', [128, SEQ // 128, 16])
    din('ident', [128, 128], BF16)
    din('triN', [128, 128], BF16)
    din('onesb', [128, 128], BF16)
    din('maskS', [128, 4, 512], BF16)
    din('maskI', [128, 4, 512], BF16)
    din('onesf', [128, 64])
    din('bvec', [10, 128, 128])
    din('sinks', [1, 2])
    kind = "ExternalOutput" if dbg else "Internal"
    d['QKT'] = nc.dram_tensor('QKT', [128, 8, S], BF16, kind=kind).ap()
    d['VSB'] = nc.dram_tensor('VSB', [128, NT, 64], BF16, kind=kind).ap()
    d['VML'] = nc.dram_tensor('VML', [128, NT, 65], BF16, kind=kind).ap()
    d['VSW'] = nc.dram_tensor('VSW', [128, NT, 33], BF16, kind=kind).ap()
    for g in range(3):
        d['VD%d' % g] = nc.dram_tensor('VD%d' % g, [S, 33], BF16, kind=kind).ap()
    d['OUT'] = nc.dram_tensor('OUT', [224, S], BF16, kind="ExternalOutput").ap()
    with ExitStack() as es:
        sb = Arena(nc, es, 196 * 1024)
        PSB = [es.enter_context(nc.psum_tensor("psb%d" % i, [128, 1024], F32))[:, :] for i in range(4)]
        PS = [PSB[i // 2][:, (i % 2) * 512:(i % 2 + 1) * 512] for i in range(8)]
        d_psb = PSB
        p = Prog(nc, es)
        block = es.enter_context(nc.Block())
        d['xrows'] = lambda t: d['x'][t * 128:(t + 1) * 128, :]
        d['GL'] = nc.dram_tensor('GL', [128, S], BF16, kind="Internal").ap()
        d['PSB'] = d_psb
        emitters = dict(inproj=emit_inproj, sb=emit_sb, mla=emit_mla, sw=emit_sw, dil=emit_dil)
        for ph in phases:
            sb.reset()
            emitters[ph](p, sb, PS, d, S)
            p.barrier()
        p.finish(block)
    return nc, p


def bf(a):
    return np.ascontiguousarray(a).astype(ml_dtypes.bfloat16)


def t5_bucket_np(dist):
    dist = np.asarray(dist, np.int64)
    d_ = np.maximum(dist, 1).astype(np.float32)
    large = 16 + (np.log(d_ / np.float32(16)) / np.float32(math.log(2048 / 16)) * np.float32(16)).astype(np.int32)
    large = np.minimum(large, 31)
    return np.where(dist < 16, dist, large)


def consts_A():
    k = np.arange(128)
    c = {}
    c['ident'] = bf(np.eye(128, dtype=np.float32))
    c['triN'] = bf(-(k[:, None] >= k[None, :]).astype(np.float32))
    c['onesb'] = bf(np.ones((128, 128), np.float32))
    qi = np.arange(512)
    mS = np.zeros((128, 4, 512), np.float32)
    mI = np.zeros((128, 4, 512), np.float32)
    for r in range(4):
        mS[:, r, :] = (128 * r + k[:, None]) < qi[None, :]
        mI[:, r, :] = (128 * r + k[:, None]) <= qi[None, :]
    c['maskS'] = bf(mS)
    c['maskI'] = bf(mI)
    c['onesf'] = np.ones((128, 64), np.float32)
    half = 16
    inv = (10000.0 ** (-np.arange(half, dtype=np.float32) / half)).astype(np.float32)
    ang = np.arange(SEQ, dtype=np.float32)[:, None] * inv[None, :]
    c['cos'] = np.ascontiguousarray(np.cos(ang).astype(np.float32).reshape(SEQ // 128, 128, 16).transpose(1, 0, 2))
    c['sin'] = np.ascontiguousarray(np.sin(ang).astype(np.float32).reshape(SEQ // 128, 128, 16).transpose(1, 0, 2))
    return c


def band_bias_vecs(rel_bias, h):
    dd = np.arange(-127, 128)
    out = np.full((10, 255), NEG, np.float32)
    for i, hq in enumerate((2 * h, 2 * h + 1)):
        cur = dd >= 0
        out[2 * i, cur] = rel_bias[t5_bucket_np(dd[cur]), hq]
        prv = dd < 0
        out[2 * i + 1, prv] = rel_bias[t5_bucket_np(128 + dd[prv]), hq]
    for g, dil in enumerate(DILS):
        col = 8 + g * 4 + h
        cur = dd >= 0
        out[4 + 2 * g, cur] = rel_bias[t5_bucket_np(dd[cur] * dil), col]
        prv = dd <= 0
        out[5 + 2 * g, prv] = rel_bias[t5_bucket_np((128 + dd[prv]) * dil), col]
    return out


def inputs_A(c, l, b, h, inp, x_b, S):
    cols = core_cols(h)
    m = dict(c)
    m['x'] = np.ascontiguousarray(x_b[:S])
    m['w_in'] = np.ascontiguousarray(np.concatenate([inp['w_in'][l][:, cols], inp['w_gate_a'][l]], axis=1))
    m['g1'] = np.ascontiguousarray(inp['norm1_g'][l].reshape(8, 128).T)
    m['wqb'] = np.ascontiguousarray(inp['w_qb'][l][:, h * 96:(h + 1) * 96])
    m['gqa'] = np.ascontiguousarray(inp['g_qa'][l].reshape(2, 128).T)
    m['wkvb'] = np.ascontiguousarray(inp['w_kvb'][l][:, h * 128:(h + 1) * 128])
    m['gkva'] = np.ascontiguousarray(inp['g_kva'][l].reshape(2, 128).T)
    gs, gd = inp['qk_g_sw'][l], inp['qk_g_dil'][l]
    m['g32'] = np.concatenate([gs[0], gd[0], gd[0], gs[1], gd[1], gd[1], gs[0], gd[0], gd[1]]).astype(np.float32)
    m['g96'] = np.concatenate([inp['qk_g_mla'][l][0], inp['qk_g_mla'][l][1]]).astype(np.float32)
    bv = band_bias_vecs(inp['rel_bias'], h)
    kk = np.arange(128)
    m['bvec'] = np.ascontiguousarray(bv[:, kk[None, :] - kk[:, None] + 127])
    m['sinks'] = np.ascontiguousarray(inp['sinks'][l][2 * h:2 * h + 2].reshape(1, 2))
    return m


BR_CHUNKS = ((0, 1), (2, 3), (4, 5), (6,))


def castw_iter(p, sb, d, nblk, F, engs=('vector', 'gpsimd', 'scalar')):
    stg = [sb("cw_stg%d" % i, [128, 2 * F], F32) for i in range(2)]
    stb = [sb("cw_stb%d" % i, [128, 2 * F], BF16) for i in range(2)]
    it = 0
    for blk in range(nblk):
        for kc in range(8):
            b = it % 2
            rows = slice(kc * 128, (kc + 1) * 128)
            p.dma('sync', stg[b][:, 0:F], d['wg'](blk)[rows, :], [], [('cw_stg', b)], 'cw_stg%d' % b)
            p.dma('sync', stg[b][:, F:2 * F], d['wu'](blk)[rows, :], [], [('cw_stg', b)], 'cw_stg%d' % b)
            p.copy(engs[it % len(engs)], stb[b], stg[b], [('cw_stg', b)], [('cw_stb', b)])
            p.dma(STQ, d['WGU'][blk, :, kc, :], stb[b], [('cw_stb', b)], [('WGU', b)], 'cw_stb%d' % b)
            it += 1
            yield
        for fc in range(F // 128):
            b = it % 2
            p.dma('sync', stg[b][:, 0:1024], d['wd'](blk)[fc * 128:(fc + 1) * 128, :], [], [('cw_stg', b)], 'cw_stg%d' % b)
            p.copy(engs[it % len(engs)], stb[b][:, 0:1024], stg[b][:, 0:1024], [('cw_stg', b)], [('cw_stb', b)])
            p.dma(STQ, d['WD'][blk, :, fc, :], stb[b][:, 0:1024], [('cw_stb', b)], [('WD', b)], 'cw_stb%d' % b)
            it += 1
            yield


def emit_castw(p, sb, PS, d, nblk, F):
    for _ in castw_iter(p, sb, d, nblk, F):
        pass


def emit_B(p, sb, PS, d, NTOK, nblk, F, moe):
    NCH = NTOK // 512
    FC = F // 128
    identb = sb("identb", [128, 128], BF16)
    p.dma('sync', identb, d['ident'][:, :], [], ['identb'], 'identb')
    G1 = sb("G1", [128, 1024], F32)
    G2 = sb("G2", [128, 1024], F32)
    p.dma('sync', G1, d['g1'].partition_broadcast(128), [], ['G1'], 'G1')
    p.dma('sync', G2, d['g2'].partition_broadcast(128), [], ['G2'], 'G2')
    bgt = sb("bgt", [128, 32], F32)
    p.dma('sync', bgt, d['bgate'][:, :], [], ['bgt'], 'bgt')
    Wga = sb("Wga", [128, 8, 128], BF16)
    Wgb = sb("Wgb", [128, 4096], BF16)
    Wbr = sb("Wbr", [128, 7, 1024], BF16)
    Wout = sb("Wout", [128, 8, 1024], BF16)
    stg = [sb("w_stg%d" % i, [128, 1024], F32) for i in range(2)]
    engs = ['vector', 'gpsimd', 'scalar']
    it = 0

    def cast_in(dst, src, n):
        nonlocal it
        b = it % 2
        p.dma('sync', stg[b][:, 0:n], src, [], [('w_stg', b)], 'w_stg%d' % b)
        p.copy(engs[it % 3], dst, stg[b][:, 0:n], [('w_stg', b)], ['Wres'])
        it += 1
    for kc in range(8):
        cast_in(Wga[:, kc, :], d['wga'][kc * 128:(kc + 1) * 128, :], 128)
        cast_in(Wout[:, kc, :], d['wout'][kc * 128:(kc + 1) * 128, :], 1024)
    for i in range(4):
        cast_in(Wgb[:, i * 1024:(i + 1) * 1024], d['wgb'][:, i * 1024:(i + 1) * 1024], 1024)
    for rc in range(7):
        cast_in(Wbr[:, rc, :], d['wbr'][rc * 128:(rc + 1) * 128, :], 1024)
    if moe:
        identf = sb("identf", [128, 128], F32)
        p.dma('sync', identf, d['identf'][:, :], [], ['identf'], 'identf')
        Wr = sb("Wr", [128, 8, 8], F32)
        p.dma('sync', Wr, d['wr'].rearrange("(kc p) e -> p kc e", p=128), [], ['Wr'], 'Wr')
        brt = sb("brt", [128, 8], F32)
        p.dma('sync', brt, d['br'].partition_broadcast(128), [], ['brt'], 'brt')
        h2f = sb("h2f", [128, 1024], F32)
        h2fT = sb("h2fT", [128, 8, 128], F32)
        rt = sb("rt", [128, 64], F32)
        GW = sb("GW", [128, 4, 8], F32)
    X4 = sb("X4", [128, 4, 1024], F32)
    oTc = sb("oTc", [128, 7, 512], BF16)
    hT = sb("hT", [128, 8, 512], BF16)
    hb = sb("hb", [128, 1024], BF16)
    junk = sb("junk", [128, 1024], BF16)
    st = sb("st", [128, 8], F32)
    glT = sb("glT", [128, 512], BF16)
    gate = [sb("gate%d" % i, [128, 512], F32) for i in range(2)]
    tmpy = sb("tmpy", [128, 512], F32)
    yacc = sb("yacc", [128, 512], F32)
    yT = sb("yT", [128, 8, 512], BF16)
    sg = [sb("sg%d" % i, [128, 512], F32) for i in range(2)]
    WGU_t = [sb("WGU_t%d" % i, [128, 8, 2 * F], BF16) for i in range(2)]
    WD_t = [sb("WD_t%d" % i, [128, FC, 1024], BF16) for i in range(2)]
    psTb = PS[0].bitcast(BF16)
    x, out = d['x'], d['out']
    wit = 0

    def norm_T(tt, G, Gk):
        xt = X4[:, tt, :]
        p.act(junk, xt, AF.Square, ['X4'], ['junk', 'ssq'], accum_out=st[:, 0:1])
        p.act(st[:, 1:2], st[:, 0:1], AF.Sqrt, ['ssq'], ['rs'], scale=1.0 / D, bias=EPS)
        p.recip(st[:, 2:3], st[:, 1:2], ['rs'], ['rstd'])
        p.stt(hb, xt, st[:, 2:3], G, ALU.mult, ALU.mult, ['X4', 'rstd', Gk], ['hb'])
        for kc in range(8):
            p.tr(psTb[:, kc * 128:(kc + 1) * 128], hb[:, kc * 128:(kc + 1) * 128], identb, ['hb', 'identb'], [PK(0)])
        p.copy('vector', hT[:, :, tt * 128:(tt + 1) * 128], psTb.rearrange("p (a d) -> p a d", d=128), [PK(0)], ['hT'])

    for c in range(NCH):
        T0 = c * 512
        p.dma('sync', X4, x[T0:T0 + 512, :].rearrange("(t p) d -> p t d", p=128), [], ['X4'], 'X4')
        p.dma('sync', oTc, d['oT'][:, T0:T0 + 512].rearrange("(r p) t -> p r t", p=128), [], ['oTc'], 'oTc')
        for tt in range(4):
            norm_T(tt, G1, 'G1')
        for kc in range(8):
            p.mm(PS[1], Wga[:, kc, :], hT[:, kc, :], kc == 0, kc == 7, ['hT', 'Wres'], [PK(1)])
        p.copy('vector', glT, PS[1], [PK(1)], ['glT'])
        k = 0
        for oc in range(8):
            for i in range(4):
                b = k % 2
                k += 1
                gp, gk = PS[2 + b], PK(2 + b)
                bp, bk = PS[4 + b], PK(4 + b)
                col = i * 1024 + oc * 128
                p.mm(gp, Wgb[:, col:col + 128], glT, True, True, ['glT', 'Wres'], [gk])
                p.act(gate[b], gp, AF.Sigmoid, [gk, 'bgt'], [('gate', b)], bias=bgt[:, i * 8 + oc:i * 8 + oc + 1])
                rcs = BR_CHUNKS[i]
                for n_, rc in enumerate(rcs):
                    p.mm(bp, Wbr[:, rc, oc * 128:(oc + 1) * 128], oTc[:, rc, :], n_ == 0, n_ == len(rcs) - 1, ['oTc', 'Wres'], [bk])
                if i == 0:
                    p.tt('vector', yacc, gate[b], bp, ALU.mult, [('gate', b), bk], ['yacc'])
                else:
                    p.tt('vector', tmpy, gate[b], bp, ALU.mult, [('gate', b), bk], ['tmpy'])
                    if i < 3:
                        p.tt('gpsimd', yacc, yacc, tmpy, ALU.add, ['yacc', 'tmpy'], ['yacc'])
                    else:
                        p.tt('gpsimd', yT[:, oc, :], yacc, tmpy, ALU.add, ['yacc', 'tmpy'], ['yT'])
        k = 0
        for tt in range(4):
            for half in range(2):
                b = k % 2
                k += 1
                ps, pk = PS[6 + b], PK(6 + b)
                for oc in range(8):
                    p.mm(ps, yT[:, oc, tt * 128:(tt + 1) * 128], Wout[:, oc, half * 512:(half + 1) * 512], oc == 0, oc == 7, ['yT', 'Wres'], [pk])
                xs = X4[:, tt, half * 512:(half + 1) * 512]
                p.tt('vector', xs, ps, xs, ALU.add, [pk, 'X4'], ['X4'])
        for tt in range(4):
            norm_T(tt, G2, 'G2')
            if moe:
                p.stt(h2f, X4[:, tt, :], st[:, 2:3], G2, ALU.mult, ALU.mult, ['X4', 'rstd', 'G2'], ['h2f'])
                for kc in range(8):
                    bnk = 2 + kc // 4
                    p.tr(PS[bnk][:, (kc % 4) * 128:(kc % 4 + 1) * 128], h2f[:, kc * 128:(kc + 1) * 128], identf, ['h2f', 'identf'], [PK(bnk)])
                p.copy('vector', h2fT[:, 0:4, :].rearrange("p a d -> p (a d)"), PS[2], [PK(2)], ['h2fT'])
                p.copy('vector', h2fT[:, 4:8, :].rearrange("p a d -> p (a d)"), PS[3], [PK(3)], ['h2fT'])
                for kc in range(8):
                    p.mm(PS[1][:, 0:8], h2fT[:, kc, :], Wr[:, kc, :], kc == 0, kc == 7, ['h2fT', 'Wr'], [PK(1)])
                lg, m8, ee, mk = rt[:, 0:8], rt[:, 8:16], rt[:, 16:24], rt[:, 24:32]
                p.tt('vector', lg, PS[1][:, 0:8], brt, ALU.add, [PK(1), 'brt'], ['lg'])
                p.op('vector', lambda e, lg=lg, m8=m8: e.max(out=m8, in_=lg), ['lg'], ['m8'])
                p.ts('vector', rt[:, 32:33], m8[:, 0:1], -1.0, ALU.mult, ['m8'], ['negm'])
                p.act(ee, lg, AF.Exp, ['lg', 'negm'], ['ee'], bias=rt[:, 32:33])
                p.ts('vector', mk, lg, m8[:, 1:2], ALU.is_ge, ['lg', 'm8'], ['mk'])
                p.tt('vector', ee, ee, mk, ALU.mult, ['ee', 'mk'], ['ee'])
                p.op('vector', lambda e, ee=ee: e.tensor_reduce(out=rt[:, 33:34], in_=ee, axis=AX.X, op=ALU.add), ['ee'], ['den'])
                p.recip(rt[:, 34:35], rt[:, 33:34], ['den'], ['rden'])
                p.ts('vector', GW[:, tt, :], ee, rt[:, 34:35], ALU.mult, ['ee', 'rden'], ['GW'])
        aT = yT
        for blk in range(nblk):
            wb = wit % 2
            wit += 1
            p.dma('sync', WGU_t[wb], d['WGU'][blk], [], [('WGU_t', wb)], 'WGU_t%d' % wb)
            p.dma('sync', WD_t[wb], d['WD'][blk], [], [('WD_t', wb)], 'WD_t%d' % wb)
            for fc in range(FC):
                b = fc % 2
                gps, gk = PS[2 + b], PK(2 + b)
                ups, uk = PS[4 + b], PK(4 + b)
                for kc in range(8):
                    p.mm(gps, WGU_t[wb][:, kc, fc * 128:(fc + 1) * 128], hT[:, kc, :], kc == 0, kc == 7, ['hT', ('WGU_t', wb)], [gk])
                for kc in range(8):
                    p.mm(ups, WGU_t[wb][:, kc, F + fc * 128:F + (fc + 1) * 128], hT[:, kc, :], kc == 0, kc == 7, ['hT', ('WGU_t', wb)], [uk])
                p.act(sg[b], gps, AF.Silu, [gk], [('sg', b)])
                p.tt('vector', aT[:, fc, :], sg[b], ups, ALU.mult, [('sg', b), uk], ['yT'])
            k = 0
            for tt in range(4):
                for half in range(2):
                    b = k % 2
                    k += 1
                    ps, pk = PS[6 + b], PK(6 + b)
                    for fc in range(FC):
                        p.mm(ps, aT[:, fc, tt * 128:(tt + 1) * 128], WD_t[wb][:, fc, half * 512:(half + 1) * 512], fc == 0, fc == FC - 1, ['yT', ('WD_t', wb)], [pk])
                    xs = X4[:, tt, half * 512:(half + 1) * 512]
                    if moe:
                        p.stt(xs, ps, GW[:, tt, blk:blk + 1], xs, ALU.mult, ALU.add, [pk, 'GW', 'X4'], ['X4'])
                    else:
                        p.tt('vector', xs, ps, xs, ALU.add, [pk, 'X4'], ['X4'])
        p.dma(STQ, out[T0:T0 + 512, :].rearrange("(t p) d -> p t d", p=128), X4, ['X4'], ['out'], 'X4o')


def build_B(NTOK, moe):
    nc = bass.Bass("TRN2", target_bir_lowering=False)
    d = {}

    def din(name, shape, dt=F32):
        d[name] = nc.dram_tensor(name, shape, dt, kind="ExternalInput").ap()

    din('x', [NTOK, D])
    din('oT', [896, NTOK], BF16)
    din('ident', [128, 128], BF16)
    din('g1', [1024])
    din('g2', [1024])
    din('bgate', [128, 32])
    din('wga', [1024, 128])
    din('wgb', [128, 4096])
    din('wbr', [896, 1024])
    din('wout', [1024, 1024])
    if moe:
        nblk, F = 8, 768
        din('identf', [128, 128])
        din('wr', [1024, 8])
        din('br', [8])
        din('wgu', [8, 1024, 1536])
        din('wdn', [8, 768, 1024])
        d['wg'] = lambda blk: d['wgu'][blk, :, 0:768]
        d['wu'] = lambda blk: d['wgu'][blk, :, 768:1536]
        d['wd'] = lambda blk: d['wdn'][blk, :, :]
    else:
        nblk, F = 4, 512
        din('wgu', [1024, 4096])
        din('wdn', [2048, 1024])
        d['wg'] = lambda blk: d['wgu'][:, blk * 512:(blk + 1) * 512]
        d['wu'] = lambda blk: d['wgu'][:, 2048 + blk * 512:2048 + (blk + 1) * 512]
        d['wd'] = lambda blk: d['wdn'][blk * 512:(blk + 1) * 512, :]
    d['WGU'] = nc.dram_tensor('WGU', [nblk, 128, 8, 2 * F], BF16, kind="Internal").ap()
    d['WD'] = nc.dram_tensor('WD', [nblk, 128, F // 128, 1024], BF16, kind="Internal").ap()
    d['out'] = nc.dram_tensor('out', [NTOK, D], F32, kind="ExternalOutput").ap()
    with ExitStack() as es:
        sb = Arena(nc, es, 200 * 1024)
        PSB = [es.enter_context(nc.psum_tensor("psb%d" % i, [128, 1024], F32))[:, :] for i in range(4)]
        PS = [PSB[i // 2][:, (i % 2) * 512:(i % 2 + 1) * 512] for i in range(8)]
        d_psb = PSB
        p = Prog(nc, es)
        block = es.enter_context(nc.Block())
        emit_castw(p, sb, PS, d, nblk, F)
        p.barrier()
        sb.reset()
        emit_B(p, sb, PS, d, NTOK, nblk, F, moe)
        p.barrier()
        p.finish(block)
    return nc, p


def inputs_B(l, inp, x_tok, oT_tok, moe):
    m = {}
    m['x'] = np.ascontiguousarray(x_tok)
    m['oT'] = np.ascontiguousarray(oT_tok)
    m['ident'] = bf(np.eye(128, dtype=np.float32))
    m['g1'] = np.ascontiguousarray(inp['norm1_g'][l])
    m['g2'] = np.ascontiguousarray(inp['norm2_g'][l])
    m['bgate'] = np.ascontiguousarray(inp['b_gate'][l].reshape(32, 128).T)
    m['wga'] = np.ascontiguousarray(inp['w_gate_a'][l])
    m['wgb'] = np.ascontiguousarray(inp['w_gate_b'][l])
    m['wbr'] = np.ascontiguousarray(inp['w_branch'][l])
    m['wout'] = np.ascontiguousarray(inp['w_out'][l])
    if moe:
        m['identf'] = np.eye(128, dtype=np.float32)
        m['wr'] = np.ascontiguousarray(inp['w_router'][l // 2])
        m['br'] = np.ascontiguousarray(inp['b_router'][l // 2])
        m['wgu'] = np.ascontiguousarray(inp['w_gu_exp'][l // 2])
        m['wdn'] = np.ascontiguousarray(inp['w_down_exp'][l // 2])
    else:
        m['wgu'] = np.ascontiguousarray(inp['w_gu_dense'][l // 2])
        m['wdn'] = np.ascontiguousarray(inp['w_down_dense'][l // 2])
    return m


_CACHE = {}


def _prog(key, fn):
    if key not in _CACHE:
        _CACHE[key] = fn()
    return _CACHE[key]


def assemble_oT(outs):
    res = []
    for b in range(BATCH):
        rows = [None] * 4
        o = [np.asarray(outs[b * 4 + h]) for h in range(4)]
        sbp = np.concatenate([o[h][0:64] for h in range(4)], axis=0)
        mlp = np.concatenate([o[h][64:128] for h in range(4)], axis=0)
        swp = np.concatenate([o[h][128:192] for h in range(4)], axis=0)
        dlp = np.concatenate([o[h][192:224] for h in range(4)], axis=0)
        res.append(np.concatenate([sbp, mlp, swp, dlp], axis=0))
    return res


def kernel(**inp):
    inp = {k: np.asarray(v) for k, v in inp.items()}
    return kernel_fused(inp)


GROUPS = [[0, 1, 2, 3], [4, 5, 6, 7]]


def emit_merge(p, sb, PS, d, S):
    SL = S // 4
    bgt = sb("bgt", [128, 32], F32)
    p.dma('sync', bgt, d['bgate'][:, :], [], ['bgt'], 'bgt')
    Wgb = sb("Wgb", [128, 4096], BF16)
    Wbr = sb("Wbrc", [64, 4, 1024], BF16)
    stg = [sb("w_stg%d" % i, [128, 1024], F32) for i in range(2)]
    engs = ['vector', 'gpsimd', 'scalar']
    it = 0
    for i in range(4):
        b = it % 2
        p.dma('sync', stg[b], d['wgb'][:, i * 1024:(i + 1) * 1024], [], [('w_stg', b)], 'w_stg%d' % b)
        p.copy(engs[it % 3], Wgb[:, i * 1024:(i + 1) * 1024], stg[b], [('w_stg', b)], ['Wres'])
        it += 1
    rows = ((0, 64), (64, 64), (128, 64), (192, 32))
    for i, (r0, nr) in enumerate(rows):
        b = it % 2
        p.dma('sync', stg[b][0:nr, :], d['wbrc'][r0:r0 + nr, :], [], [('w_stg', b)], 'w_stg%d' % b)
        p.copy(engs[it % 3], Wbr[0:nr, i, :], stg[b][0:nr, :], [('w_stg', b)], ['Wres'])
        it += 1
    glT = [sb("glT%d" % i, [128, 512], BF16) for i in range(2)]
    oc_t = [sb("oc_t%d" % i, [64, 4, 512], BF16) for i in range(2)]
    gate = [sb("gate%d" % i, [128, 512], F32) for i in range(2)]
    tmpy2 = [sb("tmpy%d" % i, [128, 512], F32) for i in range(2)]
    yacc = sb("yacc", [128, 512], F32)
    yst = [sb("yst%d" % i, [128, 8, 512], F32) for i in range(2)]
    k = 0
    SLp = min(SL, 1024)
    NPc = SL // SLp
    order = [(sl * SL + q * SLp) // 512 + cc for q in range(NPc) for sl in range(4) for cc in range(SLp // 512)]
    for ci, c in enumerate(order):
        cb_ = ci % 2
        cs = slice(c * 512, (c + 1) * 512)
        p.dma('sync', glT[cb_], d['GL'][:, cs], [], [('glT', cb_)], 'glT%d' % cb_)
        for i, (r0, nr) in enumerate(rows):
            p.dma('sync', oc_t[cb_][0:nr, i, :], d['OUT'][r0:r0 + nr, cs], [], [('oc_t', cb_)], 'oc_t%d' % cb_)
        for oc in range(8):
            for i, (r0, nr) in enumerate(rows):
                b = k % 2
                k += 1
                gp, gk = PS[2 + b], PK(2 + b)
                bp, bk = PS[4 + b], PK(4 + b)
                col = i * 1024 + oc * 128
                p.mm(gp, Wgb[:, col:col + 128], glT[cb_], True, True, [('glT', cb_), 'Wres'], [gk])
                p.act(gate[b], gp, AF.Sigmoid, [gk, 'bgt'], [('gate', b)], bias=bgt[:, i * 8 + oc:i * 8 + oc + 1])
                p.mm(bp, Wbr[0:nr, i, oc * 128:(oc + 1) * 128], oc_t[cb_][0:nr, i, :], True, True, [('oc_t', cb_), 'Wres'], [bk])
                if i == 0:
                    p.tt('vector', yacc, gate[b], bp, ALU.mult, [('gate', b), bk], ['yacc'])
                else:
                    tmpy, tk = tmpy2[b], ('tmpy', b)
                    p.tt('vector', tmpy, gate[b], bp, ALU.mult, [('gate', b), bk], [tk])
                    if i < 3:
                        p.tt('gpsimd', yacc, yacc, tmpy, ALU.add, ['yacc', tk], ['yacc'])
                    else:
                        p.tt('gpsimd', yst[cb_][:, oc, :], yacc, tmpy, ALU.add, ['yacc', tk], [('yst', cb_)])
        sl, w = (c * 512) // SL, (c * 512) % SL
        q, t0 = w // SLp, w % SLp
        p.dma('sync', d['YP'][q, sl, :, t0:t0 + 512].rearrange("(oc p) t -> p oc t", p=128), yst[cb_], [('yst', cb_)], [('YP', cb_)], 'yst%d' % cb_)
        if (ci + 1) % (4 * (SLp // 512)) == 0:
            p.cc("ReduceScatter", ALU.add, GROUPS, d['YP'][q].rearrange("a c t -> (a c) t"), d['YS'][q], [('YP', 0), ('YP', 1)], [('YS', q)])


def emit_B2(p, sb, PS, d, NTOK, nblk, F, moe):
    NCH = NTOK // 512
    FC = F // 128
    identb = sb("identb", [128, 128], BF16)
    p.dma('sync', identb, d['ident'][:, :], [], ['identb'], 'identb')
    G2 = sb("G2", [128, 1024], F32)
    p.dma('sync', G2, d['g2'].partition_broadcast(128), [], ['G2'], 'G2')
    Wout = sb("Wout", [128, 8, 1024], BF16)
    stg = [sb("w_stg%d" % i, [128, 1024], F32) for i in range(2)]
    engs = ['vector', 'gpsimd', 'scalar']
    for kc in range(8):
        b = kc % 2
        p.dma('sync', stg[b], d['wout'][kc * 128:(kc + 1) * 128, :], [], [('w_stg', b)], 'w_stg%d' % b)
        p.copy(engs[kc % 3], Wout[:, kc, :], stg[b], [('w_stg', b)], ['Wres'])
    if moe:
        identf = sb("identf", [128, 128], F32)
        p.dma('sync', identf, d['identf'][:, :], [], ['identf'], 'identf')
        Wr = sb("Wr", [128, 8, 8], F32)
        p.dma('sync', Wr, d['wr'].rearrange("(kc p) e -> p kc e", p=128), [], ['Wr'], 'Wr')
        brt = sb("brt", [128, 8], F32)
        p.dma('sync', brt, d['br'].partition_broadcast(128), [], ['brt'], 'brt')
        h2f = sb("h2f", [128, 1024], F32)
        h2fT = sb("h2fT", [128, 8, 128], F32)
        rt = sb("rt", [128, 64], F32)
        GW = sb("GW", [128, 4, 8], F32)
    X4 = sb("X4", [128, 4, 1024], F32)
    yf = sb("yf", [128, 8, 512], F32)
    hT = sb("hT", [128, 8, 512], BF16)
    hb = sb("hb", [128, 1024], BF16)
    junk = sb("junk", [128, 1024], BF16)
    st = sb("st", [128, 8], F32)
    yT = sb("yT", [128, 8, 512], BF16)
    sg = [sb("sg%d" % i, [128, 512], F32) for i in range(2)]
    WGU_t = [sb("WGU_t%d" % i, [128, 8, 2 * F], BF16) for i in range(2)]
    WD_t = [sb("WD_t%d" % i, [128, FC, 1024], BF16) for i in range(2)]
    psTb = PS[0].bitcast(BF16)
    wit = 0

    def norm_T(tt, G, Gk):
        xt = X4[:, tt, :]
        p.act(junk, xt, AF.Square, ['X4'], ['junk', 'ssq'], accum_out=st[:, 0:1])
        p.act(st[:, 1:2], st[:, 0:1], AF.Sqrt, ['ssq'], ['rs'], scale=1.0 / D, bias=EPS)
        p.recip(st[:, 2:3], st[:, 1:2], ['rs'], ['rstd'])
        p.stt(hb, xt, st[:, 2:3], G, ALU.mult, ALU.mult, ['X4', 'rstd', Gk], ['hb'])
        for kc in range(8):
            p.tr(psTb[:, kc * 128:(kc + 1) * 128], hb[:, kc * 128:(kc + 1) * 128], identb, ['hb', 'identb'], [PK(0)])
        p.copy('vector', hT[:, :, tt * 128:(tt + 1) * 128], psTb.rearrange("p (a d) -> p a d", d=128), [PK(0)], ['hT'])

    for c in range(NCH):
        T0 = c * 512
        p.dma('sync', X4, d['xtok'][T0:T0 + 512, :].rearrange("(t p) d -> p t d", p=128), [], ['X4'], 'X4')
        SLp = min(NTOK, 1024)
        p.dma('sync', yf, d['YS'][T0 // SLp, :, T0 % SLp:T0 % SLp + 512].rearrange("(oc p) t -> p oc t", p=128), [('YS', T0 // SLp)], ['yf'], 'yf')
        p.copy('gpsimd', yT[:, 0:4, :], yf[:, 0:4, :], ['yf'], ['yT'])
        p.copy('vector', yT[:, 4:8, :], yf[:, 4:8, :], ['yf'], ['yT'])
        k = 0
        for tt in range(4):
            for half in range(2):
                b = k % 2
                k += 1
                ps, pk = PS[6 + b], PK(6 + b)
                for oc in range(8):
                    p.mm(ps, yT[:, oc, tt * 128:(tt + 1) * 128], Wout[:, oc, half * 512:(half + 1) * 512], oc == 0, oc == 7, ['yT', 'Wres'], [pk])
                xs = X4[:, tt, half * 512:(half + 1) * 512]
                p.tt('vector', xs, ps, xs, ALU.add, [pk, 'X4'], ['X4'])
        for tt in range(4):
            norm_T(tt, G2, 'G2')
            if moe:
                p.stt(h2f, X4[:, tt, :], st[:, 2:3], G2, ALU.mult, ALU.mult, ['X4', 'rstd', 'G2'], ['h2f'])
                for kc in range(8):
                    bnk = 2 + kc // 4
                    p.tr(PS[bnk][:, (kc % 4) * 128:(kc % 4 + 1) * 128], h2f[:, kc * 128:(kc + 1) * 128], identf, ['h2f', 'identf'], [PK(bnk)])
                p.copy('vector', h2fT[:, 0:4, :].rearrange("p a d -> p (a d)"), PS[2], [PK(2)], ['h2fT'])
                p.copy('vector', h2fT[:, 4:8, :].rearrange("p a d -> p (a d)"), PS[3], [PK(3)], ['h2fT'])
                for kc in range(8):
                    p.mm(PS[1][:, 0:8], h2fT[:, kc, :], Wr[:, kc, :], kc == 0, kc == 7, ['h2fT', 'Wr'], [PK(1)])
                lg, m8, ee, mk = rt[:, 0:8], rt[:, 8:16], rt[:, 16:24], rt[:, 24:32]
                p.tt('vector', lg, PS[1][:, 0:8], brt, ALU.add, [PK(1), 'brt'], ['lg'])
                p.op('vector', lambda e, lg=lg, m8=m8: e.max(out=m8, in_=lg), ['lg'], ['m8'])
                p.ts('vector', rt[:, 32:33], m8[:, 0:1], -1.0, ALU.mult, ['m8'], ['negm'])
                p.act(ee, lg, AF.Exp, ['lg', 'negm'], ['ee'], bias=rt[:, 32:33])
                p.ts('vector', mk, lg, m8[:, 1:2], ALU.is_ge, ['lg', 'm8'], ['mk'])
                p.tt('vector', ee, ee, mk, ALU.mult, ['ee', 'mk'], ['ee'])
                p.op('vector', lambda e, ee=ee: e.tensor_reduce(out=rt[:, 33:34], in_=ee, axis=AX.X, op=ALU.add), ['ee'], ['den'])
                p.recip(rt[:, 34:35], rt[:, 33:34], ['den'], ['rden'])
                p.ts('vector', GW[:, tt, :], ee, rt[:, 34:35], ALU.mult, ['ee', 'rden'], ['GW'])
        aT = yT
        for blk in range(nblk):
            wb = wit % 2
            wit += 1
            p.dma('sync', WGU_t[wb], d['WGU'][blk], [], [('WGU_t', wb)], 'WGU_t%d' % wb)
            p.dma('sync', WD_t[wb], d['WD'][blk], [], [('WD_t', wb)], 'WD_t%d' % wb)
            for fc in range(FC):
                b = fc % 2
                gps, gk = PS[2 + b], PK(2 + b)
                ups, uk = PS[4 + b], PK(4 + b)
                for kc in range(8):
                    p.mm(gps, WGU_t[wb][:, kc, fc * 128:(fc + 1) * 128], hT[:, kc, :], kc == 0, kc == 7, ['hT', ('WGU_t', wb)], [gk])
                for kc in range(8):
                    p.mm(ups, WGU_t[wb][:, kc, F + fc * 128:F + (fc + 1) * 128], hT[:, kc, :], kc == 0, kc == 7, ['hT', ('WGU_t', wb)], [uk])
                p.act(sg[b], gps, AF.Silu, [gk], [('sg', b)])
                p.tt('vector', aT[:, fc, :], sg[b], ups, ALU.mult, [('sg', b), uk], ['yT'])
            k = 0
            for tt in range(4):
                for half in range(2):
                    b = k % 2
                    k += 1
                    ps, pk = PS[6 + b], PK(6 + b)
                    for fc in range(FC):
                        p.mm(ps, aT[:, fc, tt * 128:(tt + 1) * 128], WD_t[wb][:, fc, half * 512:(half + 1) * 512], fc == 0, fc == FC - 1, ['yT', ('WD_t', wb)], [pk])
                    xs = X4[:, tt, half * 512:(half + 1) * 512]
                    if moe:
                        p.stt(xs, ps, GW[:, tt, blk:blk + 1], xs, ALU.mult, ALU.add, [pk, 'GW', 'X4'], ['X4'])
                    else:
                        p.tt('vector', xs, ps, xs, ALU.add, [pk, 'X4'], ['X4'])
        p.dma('sync', d['xout'][T0:T0 + 512, :].rearrange("(t p) d -> p t d", p=128), X4, ['X4'], ['xout'], 'X4o')
        if d.get('XG') is not None:
            for kk in (2 * c, 2 * c + 1):
                p.cc("AllGather", ALU.bypass, GROUPS, d['xout'][kk * 256:(kk + 1) * 256, :], d['XG'][kk], ['xout'], [('XG', kk)])


def build_fused(S, n_layers=2):
    nc = bass.Bass("TRN2", target_bir_lowering=False)
    NT, NTOK, SL = S // 128, S // 4, S // 4
    g = {}

    def din(name, shape, dt=F32):
        g[name] = nc.dram_tensor(name, shape, dt, kind="ExternalInput").ap()

    def dint(name, shape, dt):
        g[name] = nc.dram_tensor(name, shape, dt, kind="Internal").ap()

    din('x', [S, D])
    din('xtok', [NTOK, D])
    for nm, shp, dt in (('cos', [128, SEQ // 128, 16], F32), ('sin', [128, SEQ // 128, 16], F32), ('ident', [128, 128], BF16),
                        ('triN', [128, 128], BF16), ('onesb', [128, 128], BF16), ('maskS', [128, 4, 512], BF16),
                        ('maskI', [128, 4, 512], BF16), ('onesf', [128, 64], F32), ('identf', [128, 128], F32), ('bvec', [10, 128, 128], F32)):
        din(nm, shp, dt)
    for L in range(n_layers):
        sfx = str(L)
        for nm, shp in (('w_in', [D, 1280]), ('g1', [128, 8]), ('wqb', [256, 96]), ('gqa', [128, 2]), ('wkvb', [256, 128]),
                        ('gkva', [128, 2]), ('g32', [288]), ('g96', [192]), ('sinks', [1, 2]), ('wgb', [128, 4096]),
                        ('bgate', [128, 32]), ('wbrc', [224, 1024]), ('g2', [1024]), ('wout', [1024, 1024])):
            din(nm + sfx, shp)
    din('wgu0', [1024, 4096])
    din('wdn0', [2048, 1024])
    if n_layers > 1:
        din('wr1', [1024, 8])
        din('br1', [8])
        din('wgu1', [8, 1024, 1536])
        din('wdn1', [8, 768, 1024])
    dint('QKT', [128, 8, S], BF16)
    dint('VSB', [128, NT, 64], BF16)
    dint('VML', [128, NT, 65], BF16)
    dint('VSW', [128, NT, 33], BF16)
    for i in range(3):
        dint('VD%d' % i, [S, 33], BF16)
    dint('GL', [128, S], BF16)
    dint('OUT', [224, S], BF16)
    SLp = min(SL, 1024)
    NP = SL // SLp
    NAG = NTOK // 256
    dint('YP', [NP, 4, 1024, SLp], F32)
    dint('YS', [NP, 1024, SLp], F32)
    dint('XS', [NTOK, D], F32)
    dint('XG', [NAG, 4 * 256, D], F32)
    dint('WGU0', [4, 128, 8, 1024], BF16)
    dint('WD0', [4, 128, 4, 1024], BF16)
    if n_layers > 1:
        dint('WGU1', [8, 128, 8, 1536], BF16)
        dint('WD1', [8, 128, 6, 1024], BF16)
    g['final'] = nc.dram_tensor('final', [NTOK, D], F32, kind="ExternalOutput").ap()
    shared = ('cos', 'sin', 'ident', 'triN', 'onesb', 'maskS', 'maskI', 'onesf', 'identf', 'bvec',
              'QKT', 'VSB', 'VML', 'VSW', 'VD0', 'VD1', 'VD2', 'GL', 'OUT', 'YP', 'YS')
    with ExitStack() as es:
        sb = Arena(nc, es, 200 * 1024)
        PSB = [es.enter_context(nc.psum_tensor("psb%d" % i, [128, 1024], F32))[:, :] for i in range(4)]
        PS = [PSB[i // 2][:, (i % 2) * 512:(i % 2 + 1) * 512] for i in range(8)]
        d_psb = PSB
        p = Prog(nc, es)
        block = es.enter_context(nc.Block())
        for L in range(n_layers):
            sfx = str(L)
            moe = (L % 2 == 1)
            d = {k: g[k] for k in shared}
            for nm in ('w_in', 'g1', 'wqb', 'gqa', 'wkvb', 'gkva', 'g32', 'g96', 'sinks', 'wgb', 'bgate', 'wbrc', 'g2', 'wout'):
                d[nm] = g[nm + sfx]
            if L == 0:
                d['xrows'] = lambda t: g['x'][t * 128:(t + 1) * 128, :]
            else:
                def xrows(t):
                    T = t * 128
                    r, w = T // NTOK, T % NTOK
                    return g['XG'][w // 256, r * 256 + (w % 256):r * 256 + (w % 256) + 128, :]
                d['xrows'] = xrows
                d['xkeys'] = lambda t: [('XG', ((t * 128) % NTOK) // 256)]
            d['xtok'] = g['xtok'] if L == 0 else g['XS']
            d['xout'] = g['final'] if L == n_layers - 1 else g['XS']
            d['WGU'], d['WD'] = g['WGU' + sfx], g['WD' + sfx]
            if moe:
                nblk, F = 8, 768
                d['wr'], d['br'] = g['wr1'], g['br1']
                d['wg'] = lambda blk: g['wgu1'][blk, :, 0:768]
                d['wu'] = lambda blk: g['wgu1'][blk, :, 768:1536]
                d['wd'] = lambda blk: g['wdn1'][blk, :, :]
            else:
                nblk, F = 4, 512
                d['wg'] = lambda blk: g['wgu0'][:, blk * 512:(blk + 1) * 512]
                d['wu'] = lambda blk: g['wgu0'][:, 2048 + blk * 512:2048 + (blk + 1) * 512]
                d['wd'] = lambda blk: g['wdn0'][blk * 512:(blk + 1) * 512, :]
            d['XG'] = g['XG'] if L < n_layers - 1 else None
            d['n_side'] = nblk * (8 + F // 128)
            d['PSB'] = d_psb
            for ph in (emit_inproj, emit_sb, emit_mla, emit_sw, emit_dil):
                sb.reset()
                if ph is emit_sb:
                    ph(p, sb, PS, d, S, side=lambda sb_, d=d, nblk=nblk, F=F: castw_iter(p, sb_, d, nblk, F, engs=('gpsimd',)))
                else:
                    ph(p, sb, PS, d, S)
                p.barrier(exclude_cc=True, keep=('XG',))
            sb.reset()
            emit_merge(p, sb, PS, d, S)
            p.barrier(exclude_cc=True, keep=('YS',))
            sb.reset()
            emit_B2(p, sb, PS, d, NTOK, nblk, F, moe)
            p.barrier(exclude_cc=True, keep=('XG',))
        p.barrier()
        p.finish(block)
    return nc, p


def inputs_fused(cA, inp, core, S, n_layers=2):
    b, h = core // 4, core % 4
    NTOK = S // 4
    m = {k: cA[k] for k in ('cos', 'sin', 'ident', 'triN', 'onesb', 'maskS', 'maskI', 'onesf')}
    m['identf'] = np.eye(128, dtype=np.float32)
    x = np.asarray(inp['x'], np.float32)
    m['x'] = np.ascontiguousarray(x[b, :S])
    m['xtok'] = np.ascontiguousarray(x[b, h * NTOK:(h + 1) * NTOK])
    bv = band_bias_vecs(inp['rel_bias'], h)
    kk = np.arange(128)
    m['bvec'] = np.ascontiguousarray(bv[:, kk[None, :] - kk[:, None] + 127])
    cols = core_cols(h)
    for l in range(n_layers):
        s = str(l)
        m['w_in' + s] = np.ascontiguousarray(np.concatenate([inp['w_in'][l][:, cols], inp['w_gate_a'][l]], axis=1))
        m['g1' + s] = np.ascontiguousarray(inp['norm1_g'][l].reshape(8, 128).T)
        m['wqb' + s] = np.ascontiguousarray(inp['w_qb'][l][:, h * 96:(h + 1) * 96])
        m['gqa' + s] = np.ascontiguousarray(inp['g_qa'][l].reshape(2, 128).T)
        m['wkvb' + s] = np.ascontiguousarray(inp['w_kvb'][l][:, h * 128:(h + 1) * 128])
        m['gkva' + s] = np.ascontiguousarray(inp['g_kva'][l].reshape(2, 128).T)
        gs, gd = inp['qk_g_sw'][l], inp['qk_g_dil'][l]
        m['g32' + s] = np.concatenate([gs[0], gd[0], gd[0], gs[1], gd[1], gd[1], gs[0], gd[0], gd[1]]).astype(np.float32)
        m['g96' + s] = np.concatenate([inp['qk_g_mla'][l][0], inp['qk_g_mla'][l][1]]).astype(np.float32)
        m['sinks' + s] = np.ascontiguousarray(inp['sinks'][l][2 * h:2 * h + 2].reshape(1, 2))
        m['wgb' + s] = np.ascontiguousarray(inp['w_gate_b'][l])
        m['bgate' + s] = np.ascontiguousarray(inp['b_gate'][l].reshape(32, 128).T)
        wb = inp['w_branch'][l]
        m['wbrc' + s] = np.ascontiguousarray(np.concatenate([wb[h * 64:(h + 1) * 64], wb[256 + h * 64:256 + (h + 1) * 64],
                                                             wb[512 + h * 64:512 + (h + 1) * 64], wb[768 + h * 32:768 + (h + 1) * 32]], axis=0))
        m['g2' + s] = np.ascontiguousarray(inp['norm2_g'][l])
        m['wout' + s] = np.ascontiguousarray(inp['w_out'][l])
    m['wgu0'] = np.ascontiguousarray(inp['w_gu_dense'][0])
    m['wdn0'] = np.ascontiguousarray(inp['w_down_dense'][0])
    if n_layers > 1:
        m['wr1'] = np.ascontiguousarray(inp['w_router'][0])
        m['br1'] = np.ascontiguousarray(inp['b_router'][0])
        m['wgu1'] = np.ascontiguousarray(inp['w_gu_exp'][0])
        m['wdn1'] = np.ascontiguousarray(inp['w_down_exp'][0])
    return m


def kernel_fused(inp, S=SEQ, n_layers=2):
    cA = consts_A()
    ncF, _ = _prog(('F', S, n_layers), lambda: build_fused(S, n_layers))
    in_maps = [inputs_fused(cA, inp, c, S, n_layers) for c in range(8)]
    res = run_bass_kernel_spmd(ncF, in_maps, core_ids=list(range(8)))
    NTOK = S // 4
    out = np.empty((BATCH, S, D), np.float32)
    for c in range(8):
        out[c // 4, (c % 4) * NTOK:(c % 4 + 1) * NTOK] = np.asarray(res.results[c]['final'])
    return out
```

```python
import math
from contextlib import ExitStack

import numpy as np
import ml_dtypes

import concourse.bass as bass
import concourse.mybir as mybir
from concourse.bass_utils import run_bass_kernel_spmd

F32 = mybir.dt.float32
BF16 = mybir.dt.bfloat16
U8 = mybir.dt.uint8
AF = mybir.ActivationFunctionType
ALU = mybir.AluOpType
AX = mybir.AxisListType
ENGS = ['sync', 'scalar', 'vector', 'gpsimd', 'tensor']

D = 1024
SEQ = 16384
BATCH = 2
EPS = 1e-6
NEG = -30000.0
O_AQ, O_AK, O_AV, O_CQ, O_CKV, O_KPE, O_SWQ, O_SWK, O_SWV, O_DQ, O_DK, O_DV = (
    0, 256, 512, 768, 1024, 1280, 1312, 1568, 1632, 1696, 2080, 2464)
DILS = (1, 4, 16)
STQ = 'sync'
LIMIT = 10 ** 12
SKIP = ()


class Prog:
    def __init__(self, nc, es):
        self.nc, self.es = nc, es
        self.ops = {e: [] for e in ENGS}
        self.sems, self.cnt = {}, {}
        self.known = {e: {} for e in ENGS}
        self.lastw, self.readers = {}, {}
        self.n_instr = 0
        for e in ENGS:
            self._mksem('E_' + e)

    def _mksem(self, name):
        if name not in self.sems:
            self.sems[name] = self.es.enter_context(self.nc.semaphore(name))
            self.cnt[name] = 0
        return self.sems[name]

    def _deps(self, reads, writes):
        deps = {}

        def add(d):
            if d is not None and deps.get(d[0], 0) < d[1]:
                deps[d[0]] = d[1]
        for k in reads:
            add(self.lastw.get(k))
        for k in writes:
            add(self.lastw.get(k))
            for s, v in self.readers.get(k, {}).items():
                add((s, v))
        return deps

    def _waits(self, eng, deps):
        for s, v in deps.items():
            if eng == 'tensor' and s == 'E_tensor':
                continue
            if self.known[eng].get(s, 0) >= v:
                continue
            self.known[eng][s] = v
            h = self.sems[s]
            self.ops[eng].append(lambda e, h=h, v=v: e.wait_ge(h, v))
            self.n_instr += 1

    def _record(self, s, v, reads, writes):
        for k in writes:
            self.lastw[k] = (s, v)
            self.readers[k] = {}
        for k in reads:
            self.readers.setdefault(k, {})[s] = v

    def op(self, eng, fn, reads=(), writes=()):
        self.n_ops = getattr(self, 'n_ops', 0) + 1
        if self.n_ops > LIMIT or self.n_ops in SKIP:
            return
        self._waits(eng, self._deps(reads, writes))
        s = 'E_' + eng
        self.cnt[s] += 1
        h = self.sems[s]
        self.ops[eng].append(lambda e, fn=fn, h=h: fn(e).then_inc(h, 1))
        self._record(s, self.cnt[s], reads, writes)
        self.n_instr += 1

    def dma(self, q, out, in_, reads, writes, slot):
        self.n_ops = getattr(self, 'n_ops', 0) + 1
        if self.n_ops > LIMIT:
            return
        self._waits(q, self._deps(reads, writes))
        s = 'D_' + slot
        h = self._mksem(s)
        self.cnt[s] += 16
        self.ops[q].append(lambda e, h=h, out=out, in_=in_: e.dma_start(out=out, in_=in_).then_inc(h, 16))
        self._record(s, self.cnt[s], reads, writes)
        self.n_instr += 1

    def cc(self, kind, op, groups, in_, out, reads, writes):
        self._waits('gpsimd', self._deps(reads, writes))
        s = 'C_all'
        h = self._mksem(s)
        self.cnt[s] += 1
        self.ops['gpsimd'].append(lambda e, h=h: e.collective_compute(kind, op, replica_groups=groups, ins=[in_], outs=[out]).then_inc(h, 1))
        self._record(s, self.cnt[s], reads, writes)
        self.n_instr += 1

    def barrier(self, exclude_cc=False, keep=()):
        allv = {s: v for s, v in self.cnt.items() if v > 0 and not (exclude_cc and s == 'C_all')}
        for e in ENGS:
            self._waits(e, allv)
        kept = {k: v for k, v in self.lastw.items() if isinstance(k, tuple) and k[0] in keep}
        self.lastw, self.readers = kept, {}

    def finish(self, block):
        for name in ENGS:
            def body(e, name=name):
                for f in self.ops[name]:
                    f(e)
            getattr(block, name)(body)

    def mm(self, out, lhsT, rhs, start, stop, reads, writes, skip=False):
        self.op('tensor', lambda e: e.matmul(out, lhsT, rhs, start=start, stop=stop, skip_group_check=skip), reads, writes)

    def tr(self, out, in_, ident, reads, writes):
        self.op('tensor', lambda e: e.transpose(out, in_, ident), reads, writes)

    def act(self, out, in_, func, reads, writes, bias=None, scale=None, accum_out=None):
        kw = {}
        if bias is not None:
            kw['bias'] = bias
        if scale is not None:
            kw['scale'] = scale
        if accum_out is not None:
            kw['accum_out'] = accum_out
        self.op('scalar', lambda e: e.activation(out=out, in_=in_, func=func, **kw), reads, writes)

    def tt(self, eng, out, in0, in1, op, reads, writes):
        self.op(eng, lambda e: e.tensor_tensor(out=out, in0=in0, in1=in1, op=op), reads, writes)

    def ts(self, eng, out, in0, s1, op0, reads, writes, s2=None, op1=None):
        if op1 is None:
            self.op(eng, lambda e: e.tensor_scalar(out=out, in0=in0, scalar1=s1, scalar2=None, op0=op0), reads, writes)
        else:
            self.op(eng, lambda e: e.tensor_scalar(out=out, in0=in0, scalar1=s1, scalar2=s2, op0=op0, op1=op1), reads, writes)

    def stt(self, out, in0, scalar, in1, op0, op1, reads, writes):
        self.op('vector', lambda e: e.scalar_tensor_tensor(out=out, in0=in0, scalar=scalar, in1=in1, op0=op0, op1=op1), reads, writes)

    def copy(self, eng, out, in_, reads, writes):
        if eng == 'scalar':
            self.op(eng, lambda e: e.copy(out=out, in_=in_), reads, writes)
        else:
            self.op(eng, lambda e: e.tensor_copy(out=out, in_=in_), reads, writes)

    def recip(self, out, in_, reads, writes):
        self.op('vector', lambda e: e.reciprocal(out=out, in_=in_), reads, writes)

    def memset(self, eng, ap, val, writes):
        self.op(eng, lambda e: e.memset(ap, val), (), writes)


class Arena:
    def __init__(self, nc, es, nbytes):
        self.big = es.enter_context(nc.sbuf_tensor("arena", [128, nbytes], U8))
        self.nbytes = nbytes
        self.off = 0

    def reset(self):
        self.off = 0

    def __call__(self, name, shape, dt):
        esz = 2 if dt == BF16 else 4
        n = int(np.prod(shape[1:]))
        nb = (n * esz + 63) // 64 * 64
        assert self.off + nb <= self.nbytes, (name, self.off, nb)
        ap = self.big[:, self.off:self.off + n * esz].bitcast(dt)
        self.off += nb
        if len(shape) > 2:
            names = " ".join("d%d" % i for i in range(len(shape) - 1))
            ap = ap.rearrange("p (%s) -> p %s" % (names, names), **{"d%d" % i: shape[i + 1] for i in range(len(shape) - 2)})
        if shape[0] < 128:
            ap = ap[0:shape[0]]
        return ap


def PK(i):
    return ('ps', i)


def core_cols(h):
    r = lambda o, n: list(range(o, o + n))
    swq0 = r(O_SWQ + (2 * h) * 32, 32)
    swq1 = r(O_SWQ + (2 * h + 1) * 32, 32)
    swk = r(O_SWK + (h // 2) * 32, 32)
    swv = r(O_SWV + (h // 2) * 32, 32)
    dq = [r(O_DQ + (g * 4 + h) * 32, 32) for g in range(3)]
    dk = [r(O_DK + (g * 4 + h) * 32, 32) for g in range(3)]
    dv = [r(O_DV + (g * 4 + h) * 32, 32) for g in range(3)]
    g1 = r(O_CQ, 256) + r(O_CKV, 256)
    n32 = swq0 + dq[0] + dq[1] + swk + dk[0] + dk[1] + swq1 + dq[2] + dk[2]
    g2 = n32 + r(O_AQ + h * 64, 64) + r(O_AK + h * 64, 64) + r(O_KPE, 32) + r(O_AV + h * 64, 64)
    g3 = swv + dv[0] + dv[1] + dv[2]
    return np.array(g1 + g2 + g3)


def emit_inproj(p, sb, PS, d, S):
    NT = S // 128
    identb = sb("identb", [128, 128], BF16)
    p.dma('sync', identb, d['ident'][:, :], [], ['identb'], 'identb')
    g1t = sb("g1t", [128, 8], F32)
    p.dma('sync', g1t, d['g1'][:, :], [], ['g1t'], 'g1t')
    gqat = sb("gqat", [128, 2], F32)
    p.dma('sync', gqat, d['gqa'][:, :], [], ['gqat'], 'gqat')
    gkvat = sb("gkvat", [128, 2], F32)
    p.dma('sync', gkvat, d['gkva'][:, :], [], ['gkvat'], 'gkvat')
    G32 = sb("G32", [128, 288], F32)
    p.dma('sync', G32, d['g32'].partition_broadcast(128), [], ['G32'], 'G32')
    G96 = sb("G96", [128, 2, 96], F32)
    p.dma('sync', G96.rearrange("p a d -> p (a d)"), d['g96'].partition_broadcast(128), [], ['G96'], 'G96')
    COS = sb("COS", [128, NT, 16], F32)
    SIN = sb("SIN", [128, NT, 16], F32)
    p.dma('sync', COS, d['cos'][:, 0:NT, :], [], ['COS'], 'COS')
    p.dma('sync', SIN, d['sin'][:, 0:NT, :], [], ['SIN'], 'SIN')
    Wb = sb("Wb", [128, 8, 1280], BF16)
    wst = [sb("wst%d" % i, [128, 1280], F32) for i in range(2)]
    for kc in range(8):
        b = kc % 2
        p.dma('sync', wst[b], d['w_in'][kc * 128:(kc + 1) * 128, :], [], [('wst', b)], 'wst%d' % b)
        p.act(Wb[:, kc, :], wst[b], AF.Copy, [('wst', b), 'g1t'], ['Wb'], scale=g1t[:, kc:kc + 1])
    Wqb = sb("Wqb", [128, 2, 96], BF16)
    Wkvb = sb("Wkvb", [128, 2, 128], BF16)
    for i in range(2):
        p.dma('sync', wst[0][:, 0:96], d['wqb'][i * 128:(i + 1) * 128, :], [], [('wst', 0)], 'wst0')
        p.act(Wqb[:, i, :], wst[0][:, 0:96], AF.Copy, [('wst', 0), 'gqat'], ['Wqb'], scale=gqat[:, i:i + 1])
        p.dma('sync', wst[1][:, 0:128], d['wkvb'][i * 128:(i + 1) * 128, :], [], [('wst', 1)], 'wst1')
        p.act(Wkvb[:, i, :], wst[1][:, 0:128], AF.Copy, [('wst', 1), 'gkvat'], ['Wkvb'], scale=gkvat[:, i:i + 1])

    X = [sb("x%d" % i, [128, 1024], F32) for i in range(2)]
    junk = sb("junk", [128, 1024], BF16)
    hb = [sb("hb%d" % i, [128, 1024], BF16) for i in range(2)]
    hT = [sb("hT%d" % i, [128, 8, 128], BF16) for i in range(2)]
    st = sb("stats", [128, 32], F32)
    cb = sb("cb", [128, 512], BF16)
    cT = sb("cT", [128, 4, 128], BF16)
    qk96 = sb("qk96", [128, 2, 96], F32)
    tmp96 = sb("tmp96", [128, 2, 96], F32)
    qkb = sb("qkb", [128, 2, 96], BF16)
    rA = sb("ropeA", [128, 2, 2, 16], F32)
    rB = sb("ropeB", [128, 2, 2, 16], F32)
    sq32 = sb("sq32", [128, 288], F32)
    tmp32 = sb("tmp32", [128, 288], F32)
    n32b = sb("n32b", [128, 288], BF16)
    sbqk = sb("sbqk", [128, 128], BF16)
    stageT = [sb("stageT%d" % i, [128, 8, 512], BF16) for i in range(2)]
    stVS = [sb("stVS%d" % i, [128, 4, 64], BF16) for i in range(2)]
    stVM = [sb("stVM%d" % i, [128, 4, 65], BF16) for i in range(2)]
    stVG = [sb("stVG%d" % i, [128, 4, 4, 33], BF16) for i in range(2)]
    stGL = [sb("stGL%d" % i, [128, 512], BF16) for i in range(2)]
    glb = sb("glb", [128, 128], BF16)
    for i in range(2):
        p.memset('gpsimd', stageT[i], 0.0, [('stageT', i)])
        p.memset('gpsimd', stVM[i], 1.0, [('stVM', i)])
        p.memset('gpsimd', stVG[i], 1.0, [('stVG', i)])
    psTb = PS[0].bitcast(BF16)
    ps1, ps2, ps3 = PS[1], PS[2], PS[3]
    ps4b = PS[4].bitcast(BF16)
    ps5 = PS[5]
    ps6b = PS[6].bitcast(BF16)
    xrows = d['xrows']

    xkeys = d.get('xkeys', lambda t: [])
    def norm(t):
        b = t % 2
        xk, hbk = ('x', b), ('hb', b)
        if t + 1 < NT:
            p.dma('sync', X[1 - b], xrows(t + 1), xkeys(t + 1), [('x', 1 - b)], 'x%d' % (1 - b))
        p.act(junk, X[b], AF.Square, [xk], ['junk', 'ssq'], accum_out=st[:, 0:1])
        p.act(st[:, 1:2], st[:, 0:1], AF.Sqrt, ['ssq'], ['rs'], scale=1.0 / D, bias=EPS)
        p.recip(st[:, 2:3], st[:, 1:2], ['rs'], ['rstd'])
        p.act(hb[b], X[b], AF.Copy, [xk, 'rstd'], [hbk], scale=st[:, 2:3])

    p.dma('sync', X[0], xrows(0), xkeys(0), [('x', 0)], 'x0')
    norm(0)
    for t in range(NT):
        b = t % 2
        tt = t % 4
        sg = (t // 4) % 2
        xk, hbk, hTk = ('x', b), ('hb', b), ('hT', b)
        for kc in range(8):
            p.tr(psTb[:, kc * 128:(kc + 1) * 128], hb[b][:, kc * 128:(kc + 1) * 128], identb, [hbk, 'identb'], [PK(0)])
        p.copy('vector', hT[b].rearrange("p a d -> p (a d)"), psTb, [PK(0)], [hTk])
        for (ps, key, c0, c1) in ((ps1, PK(1), 0, 512), (ps2, PK(2), 512, 1024), (ps3, PK(3), 1024, 1280)):
            for kc in range(8):
                p.mm(ps[:, 0:c1 - c0], hT[b][:, kc, :], Wb[:, kc, c0:c1], kc == 0, kc == 7, [hTk, 'Wb'], [key])
        p.act(junk[:, 0:256], ps1[:, 0:256], AF.Square, [PK(1)], ['junk', 'ssq2a'], accum_out=st[:, 3:4])
        p.act(junk[:, 0:256], ps1[:, 256:512], AF.Square, [PK(1)], ['junk', 'ssq2b'], accum_out=st[:, 4:5])
        p.act(st[:, 5:7], st[:, 3:5], AF.Sqrt, ['ssq2a', 'ssq2b'], ['rs2'], scale=1.0 / 256, bias=EPS)
        p.recip(st[:, 7:9], st[:, 5:7], ['rs2'], ['rstd2'])
        p.copy('vector', cb, ps1, [PK(1)], ['cb'])
        if t + 1 < NT:
            norm(t + 1)
        for i in range(4):
            p.tr(ps4b[:, i * 128:(i + 1) * 128], cb[:, i * 128:(i + 1) * 128], identb, ['cb', 'identb'], [PK(4)])
        p.copy('vector', cT.rearrange("p a d -> p (a d)"), ps4b[:, 0:512], [PK(4)], ['cT'])
        for i in range(2):
            p.mm(ps5[:, 0:96], cT[:, i, :], Wqb[:, i, :], i == 0, i == 1, ['cT', 'Wqb'], [PK(5)])
        for i in range(2):
            p.mm(ps5[:, 128:256], cT[:, 2 + i, :], Wkvb[:, i, :], i == 0, i == 1, ['cT', 'Wkvb'], [PK(5)])
        p.act(qk96[:, 0, :], ps5[:, 0:96], AF.Copy, [PK(5), 'rstd2'], ['qk96'], scale=st[:, 7:8])
        p.act(qk96[:, 1, 0:64], ps5[:, 128:192], AF.Copy, [PK(5), 'rstd2'], ['qk96'], scale=st[:, 8:9])
        p.act(stVM[sg][:, tt, 0:64], ps5[:, 192:256], AF.Copy, [PK(5), 'rstd2'], [('stVM', sg)], scale=st[:, 8:9])
        p.copy('vector', qk96[:, 1, 64:96], ps2[:, 416:448], [PK(2)], ['qk96'])
        R = qk96[:, :, 64:96].rearrange("p a (h d) -> p a h d", h=2)
        cosb = COS[:, t, :].unsqueeze(1).unsqueeze(1).broadcast_to([128, 2, 2, 16])
        sinb = SIN[:, t, :].unsqueeze(1).broadcast_to([128, 2, 16])
        p.tt('vector', rA, R, cosb, ALU.mult, ['qk96', 'COS'], ['rA'])
        p.tt('vector', rB[:, :, 0, :], R[:, :, 1, :], sinb, ALU.mult, ['qk96', 'SIN'], ['rB'])
        p.tt('vector', rB[:, :, 1, :], R[:, :, 0, :], sinb, ALU.mult, ['qk96', 'SIN'], ['rB'])
        p.tt('vector', R[:, :, 0, :], rA[:, :, 0, :], rB[:, :, 0, :], ALU.subtract, ['rA', 'rB'], ['qk96'])
        p.tt('vector', R[:, :, 1, :], rA[:, :, 1, :], rB[:, :, 1, :], ALU.add, ['rA', 'rB'], ['qk96'])
        p.tt('vector', tmp96, qk96, qk96, ALU.mult, ['qk96'], ['tmp96'])
        p.op('vector', lambda e: e.tensor_reduce(out=st[:, 9:11], in_=tmp96, axis=AX.X, op=ALU.add), ['tmp96'], ['ssq96'])
        p.act(st[:, 11:13], st[:, 9:11], AF.Sqrt, ['ssq96'], ['rs96'], scale=1.0 / 96, bias=EPS)
        p.recip(st[:, 13:15], st[:, 11:13], ['rs96'], ['rstd96'])
        p.tt('vector', tmp96, qk96, G96, ALU.mult, ['qk96', 'G96'], ['tmp96'])
        p.tt('vector', qkb, tmp96, st[:, 13:15].unsqueeze(2).broadcast_to([128, 2, 96]), ALU.mult, ['tmp96', 'rstd96'], ['qkb'])
        p.act(sq32, ps2[:, 0:288], AF.Square, [PK(2)], ['sq32'])
        p.op('vector', lambda e: e.tensor_reduce(out=st[:, 15:24], in_=sq32.rearrange("p (a d) -> p a d", d=32), axis=AX.X, op=ALU.add), ['sq32'], ['ssq32'])
        p.act(st[:, 15:24], st[:, 15:24], AF.Sqrt, ['ssq32'], ['ssq32'], scale=1.0 / 32, bias=EPS)
        p.recip(st[:, 15:24], st[:, 15:24], ['ssq32'], ['ssq32'])
        p.tt('vector', tmp32, ps2[:, 0:288], G32, ALU.mult, [PK(2), 'G32'], ['tmp32'])
        p.tt('vector', n32b.rearrange("p (a d) -> p a d", d=32), tmp32.rearrange("p (a d) -> p a d", d=32),
             st[:, 15:24].unsqueeze(2).broadcast_to([128, 9, 32]), ALU.mult, ['tmp32', 'ssq32'], ['n32b'])
        p.ts('vector', sbqk[:, 0:64], ps2[:, 288:352], 0.125, ALU.mult, [PK(2)], ['sbqk'])
        p.copy('vector', sbqk[:, 64:128], ps2[:, 352:416], [PK(2)], ['sbqk'])
        p.copy('vector', stVS[sg][:, tt, :], ps2[:, 448:512], [PK(2)], [('stVS', sg)])
        p.copy('vector', stVG[sg][:, tt, :, 0:32], ps3[:, 0:128].rearrange("p (a d) -> p a d", d=32), [PK(3)], [('stVG', sg)])
        p.copy('vector', glb, ps3[:, 128:256], [PK(3)], ['glb'])
        p.tr(ps4b[:, 512:640], glb, identb, ['glb', 'identb'], [PK(4)])
        p.copy('vector', stGL[sg][:, tt * 128:(tt + 1) * 128], ps4b[:, 512:640], [PK(4)], [('stGL', sg)])
        p.tr(ps6b[0:64, 0:128], sbqk[:, 0:64], identb, ['sbqk', 'identb'], [PK(6)])
        p.tr(ps6b[0:64, 128:256], sbqk[:, 64:128], identb, ['sbqk', 'identb'], [PK(6)])
        p.tr(ps6b[0:96, 256:384], qkb[:, 0, :], identb, ['qkb', 'identb'], [PK(6)])
        p.tr(ps6b[0:96, 384:512], qkb[:, 1, :], identb, ['qkb', 'identb'], [PK(6)])
        p.tr(ps6b[0:96, 512:640], n32b[:, 0:96], identb, ['n32b', 'identb'], [PK(6)])
        p.tr(ps6b[0:96, 640:768], n32b[:, 96:192], identb, ['n32b', 'identb'], [PK(6)])
        p.tr(ps6b[0:64, 768:896], n32b[:, 192:256], identb, ['n32b', 'identb'], [PK(6)])
        p.tr(ps6b[0:64, 896:1024], n32b[:, 224:288], identb, ['n32b', 'identb'], [PK(6)])
        p.copy('vector', stageT[sg][0:96, 2:6, tt * 128:(tt + 1) * 128], ps6b[0:96, 256:768].rearrange("p (a d) -> p a d", d=128), [PK(6)], [('stageT', sg)])
        p.copy('vector', stageT[sg][0:64, 0:2, tt * 128:(tt + 1) * 128], ps6b[0:64, 0:256].rearrange("p (a d) -> p a d", d=128), [PK(6)], [('stageT', sg)])
        p.copy('vector', stageT[sg][0:64, 6:8, tt * 128:(tt + 1) * 128], ps6b[0:64, 768:1024].rearrange("p (a d) -> p a d", d=128), [PK(6)], [('stageT', sg)])
        if tt == 3:
            T0 = (t - 3) * 128
            j0 = t - 3
            p.dma(STQ, d['QKT'][0:96, :, T0:T0 + 512], stageT[sg][0:96], [('stageT', sg)], [('QKT', sg)], 'stageT%d' % sg)
            p.dma(STQ, d['GL'][:, T0:T0 + 512], stGL[sg], [('stGL', sg)], [('GL', sg)], 'stGL%d' % sg)
            p.dma(STQ, d['VSB'][:, j0:j0 + 4, :], stVS[sg], [('stVS', sg)], [('VSB', sg)], 'stVS%d' % sg)
            p.dma(STQ, d['VML'][:, j0:j0 + 4, :], stVM[sg], [('stVM', sg)], [('VML', sg)], 'stVM%d' % sg)
            p.dma(STQ, d['VSW'][:, j0:j0 + 4, :], stVG[sg][:, :, 0, :], [('stVG', sg)], [('VG', sg)], 'stVG%d' % sg)
            for g in range(3):
                p.dma(STQ, d['VD%d' % g][T0:T0 + 512, :].rearrange("(a p) d -> p a d", p=128), stVG[sg][:, :, 1 + g, :],
                      [('stVG', sg)], [('VG', sg)], 'stVG%d' % sg)


def load_cols(p, dst, dst_key, src, src_keys, slot, nsplit=4):
    n = src.shape[-1]
    w = n // nsplit
    for i in range(nsplit):
        p.dma('sync', dst[:, i * w:(i + 1) * w], src[:, i * w:(i + 1) * w], src_keys, [dst_key], slot)


def emit_sb(p, sb, PS, d, S, side=None):
    NT = S // 128
    QT = sb("sbQT", [64, S], BF16)
    KT = sb("sbKT", [64, S], BF16)
    V = sb("sbV", [128, NT, 64], BF16)
    load_cols(p, QT, 'sbQT', d['QKT'][0:64, 0, :], [], 'sbQT')
    load_cols(p, KT, 'sbKT', d['QKT'][0:64, 1, :], [], 'sbKT')
    p.dma('sync', V, d['VSB'][:, :, :], [], ['sbV'], 'sbV')
    triN = sb("triN", [128, 128], BF16)
    onesb = sb("onesb", [128, 128], BF16)
    maskS = sb("maskS", [128, 4, 512], BF16)
    p.dma('sync', triN, d['triN'][:, :], [], ['triN'], 'triN')
    p.dma('sync', onesb, d['onesb'][:, :], [], ['onesb'], 'onesb')
    p.dma('sync', maskS, d['maskS'][:, :, :], [], ['maskS'], 'maskS')
    e_t = [sb("sb_e%d" % i, [128, 1024], F32) for i in range(2)]
    sp_t = [sb("sb_sp%d" % i, [128, 1024], BF16) for i in range(2)]
    ex_t = [sb("sb_ex%d" % i, [128, 1024], F32) for i in range(2)]
    A_t = [sb("sb_A%d" % i, [128, 1024], BF16) for i in range(2)]
    Cb = sb("sb_Cb", [128, 512], F32)
    ost = [sb("sb_ost%d" % i, [64, 512], BF16) for i in range(2)]
    PSB = d['PSB']
    pairs = []
    for c in range(S // 512):
        js = list(range(4 * c + 3, -1, -1))
        for m in range(0, len(js), 2):
            pairs.append((c, m, js[m], js[m + 1]))
    NS = len(pairs)

    def stage1(pi):
        c, m, j0, j1 = pairs[pi]
        pb = pi % 2
        ek, spk = ('e', pb), ('sp', pb)
        for h, j in enumerate((j0, j1)):
            p.mm(PS[2 * pb + h], KT[:, j * 128:(j + 1) * 128], QT[:, c * 512:(c + 1) * 512], True, True, ['sbKT', 'sbQT'], [PK(2 * pb + h)])
        p.act(e_t[pb], PSB[pb], AF.Exp, [PK(2 * pb), PK(2 * pb + 1)], [ek])
        p.act(sp_t[pb], e_t[pb], AF.Ln, [ek], [spk], bias=1.0)
        for h, j in enumerate((j0, j1)):
            r = j - 4 * c
            if r >= 0:
                hs = slice(h * 512, (h + 1) * 512)
                p.tt('gpsimd', sp_t[pb][:, hs], sp_t[pb][:, hs], maskS[:, r, :], ALU.mult, [spk, 'maskS'], [spk])

    def stage2(pi):
        c, m, j0, j1 = pairs[pi]
        pb = pi % 2
        spk, exk, Ak = ('sp', pb), ('ex', pb), ('A', pb)
        for h, j in enumerate((j0, j1)):
            hs = slice(h * 512, (h + 1) * 512)
            zk, ck = PK(2 * pb + h), PK(4 + h)
            p.mm(PS[2 * pb + h], triN, sp_t[pb][:, hs], False, True, [spk, 'triN'], [zk], skip=True)
            p.mm(PS[4 + h], onesb, sp_t[pb][:, hs], True, True, [spk, 'onesb'], [ck])
            if m == 0 and h == 0:
                p.copy('vector', ex_t[pb][:, hs], PS[2 * pb + h], [zk], [exk])
                p.copy('vector', Cb, PS[4 + h], [ck], ['Cb'])
            else:
                p.tt('vector', ex_t[pb][:, hs], PS[2 * pb + h], Cb, ALU.subtract, [zk, 'Cb'], [exk])
                if j > 0:
                    p.tt('vector', Cb, PS[4 + h], Cb, ALU.add, [ck, 'Cb'], ['Cb'])
        p.act(A_t[pb], ex_t[pb], AF.Exp, [exk], [Ak])
        for h, j in enumerate((j0, j1)):
            r = j - 4 * c
            if r >= 0:
                hs = slice(h * 512, (h + 1) * 512)
                p.tt('gpsimd', A_t[pb][:, hs], A_t[pb][:, hs], maskS[:, r, :], ALU.mult, [Ak, 'maskS'], [Ak])

    def stage3(pi):
        c, m, j0, j1 = pairs[pi]
        pb = pi % 2
        ob = c % 2
        for h, j in enumerate((j0, j1)):
            hs = slice(h * 512, (h + 1) * 512)
            p.mm(PS[6 + ob][0:64, :], V[:, j, :], A_t[pb][:, hs], m == 0 and h == 0, j == 0, [('A', pb), 'sbV'], [PK(6 + ob)])
        if j1 == 0:
            p.copy('vector', ost[ob], PS[6 + ob][0:64, :], [PK(6 + ob)], [('ost', ob)])
            p.dma(STQ, d['OUT'][0:64, c * 512:(c + 1) * 512], ost[ob], [('ost', ob)], [('OUT', ob)], 'sb_ost%d' % ob)

    side_it = side(sb) if side is not None else None
    n_side = d.get('n_side', 0)
    every = max(1, NS // max(n_side, 1))
    for i in range(NS + 2):
        if i < NS:
            stage1(i)
        if 0 <= i - 1 < NS:
            stage2(i - 1)
        if 0 <= i - 2 < NS:
            stage3(i - 2)
        if side_it is not None and i % every == 0:
            next(side_it, None)
    if side_it is not None:
        for _ in side_it:
            pass


def emit_mla(p, sb, PS, d, S):
    NT = S // 128
    QT = sb("mlQT", [96, S], BF16)
    KT = sb("mlKT", [96, S], BF16)
    V = sb("mlV", [128, NT, 65], BF16)
    load_cols(p, QT, 'mlQT', d['QKT'][0:96, 2, :], [], 'mlQT')
    load_cols(p, KT, 'mlKT', d['QKT'][0:96, 3, :], [], 'mlKT')
    p.dma('sync', V, d['VML'][:, :, :], [], ['mlV'], 'mlV')
    maskI = sb("maskI", [128, 4, 512], BF16)
    p.dma('sync', maskI, d['maskI'][:, :, :], [], ['maskI'], 'maskI')
    onesf = sb("onesf", [128, 64], F32)
    p.dma('sync', onesf, d['onesf'][:, :], [], ['onesf'], 'onesf')
    P_t = [sb("ml_P%d" % i, [128, 1024], BF16) for i in range(3)]
    osb = sb("ml_osb", [128, 512], F32)
    rrow = sb("ml_rrow", [128, 512], F32)
    ost = [sb("ml_ost%d" % i, [64, 512], BF16) for i in range(2)]
    PSB = d['PSB']
    scale = 96 ** -0.5
    pairs = []
    for c in range(S // 512):
        js = list(range(4 * c + 3, -1, -1))
        for m in range(0, len(js), 2):
            pairs.append((c, m, js[m], js[m + 1]))
    NS = len(pairs)

    def stage1(pi):
        c, m, j0, j1 = pairs[pi]
        pb = pi % 3
        Pk = ('P', pb)
        for h, j in enumerate((j0, j1)):
            p.mm(PS[2 * pb + h], KT[:, j * 128:(j + 1) * 128], QT[:, c * 512:(c + 1) * 512], True, True, ['mlKT', 'mlQT'], [PK(2 * pb + h)])
        p.act(P_t[pb], PSB[pb], AF.Exp, [PK(2 * pb), PK(2 * pb + 1)], [Pk], scale=scale)
        for h, j in enumerate((j0, j1)):
            r = j - 4 * c
            if r >= 0:
                hs = slice(h * 512, (h + 1) * 512)
                p.tt('gpsimd', P_t[pb][:, hs], P_t[pb][:, hs], maskI[:, r, :], ALU.mult, [Pk, 'maskI'], [Pk])

    def stage2(pi):
        c, m, j0, j1 = pairs[pi]
        pb = pi % 3
        ob = c % 2
        oD, ok = PS[6], PK(6)
        for h, j in enumerate((j0, j1)):
            hs = slice(h * 512, (h + 1) * 512)
            p.mm(oD[0:65, :], V[:, j, :], P_t[pb][:, hs], m == 0 and h == 0, j == 0, [('P', pb), 'mlV'], [ok])
        if j1 == 0:
            p.copy('vector', osb[0:65, :], oD[0:65, :], [ok], ['ml_osb'])
            p.act(rrow[64:65, :], osb[64:65, :], AF.Ln, ['ml_osb'], ['ml_rrow'])
            p.act(rrow[64:65, :], rrow[64:65, :], AF.Exp, ['ml_rrow'], ['ml_rrow'], scale=-1.0)
            p.mm(PS[7][0:64, :], onesf[64:65, :], rrow[64:65, :], True, True, ['ml_rrow', 'onesf'], [PK(7)])
            p.tt('vector', ost[ob], osb[0:64, :], PS[7][0:64, :], ALU.mult, ['ml_osb', PK(7)], [('ml_ost', ob)])
            p.dma(STQ, d['OUT'][64:128, c * 512:(c + 1) * 512], ost[ob], [('ml_ost', ob)], [('OUT', 2 + ob)], 'ml_ost%d' % ob)

    for i in range(NS + 2):
        if i < NS:
            stage1(i)
        if 0 <= i - 2 < NS:
            stage2(i - 2)


def toeplitz(bmat, idx, rev=False):
    return bmat[idx, :, :]


def run_band_units(p, PS, units, t_sb, P_sb, scale):
    def front(ui):
        slots, Btile, Bkey, _ = units[ui]
        b = ui % 2
        s_ps, sk = PS[b], PK(b)
        for u, sl in enumerate(slots):
            p.mm(s_ps[:, u * 256:u * 256 + 128], sl['kc'], sl['q'], True, True, sl['rk'], [sk])
            p.mm(s_ps[:, u * 256 + 128:(u + 1) * 256], sl['kp'], sl['q'], True, True, sl['rk'], [sk])
        p.stt(t_sb[b], s_ps, scale, Btile, ALU.mult, ALU.add, [sk, Bkey], [('bt', b)])
        p.act(P_sb[b], t_sb[b], AF.Exp, [('bt', b)], [('bP', b)])

    def back(ui):
        slots, _, _, epi = units[ui]
        b = ui % 2
        o_ps, ok = PS[2 + b], PK(2 + b)
        for u, sl in enumerate(slots):
            p.mm(o_ps[0:33, u * 128:(u + 1) * 128], sl['vc'], P_sb[b][:, u * 256:u * 256 + 128], True, False, [('bP', b)] + sl['vk'], [ok])
            p.mm(o_ps[0:33, u * 128:(u + 1) * 128], sl['vp'], P_sb[b][:, u * 256 + 128:(u + 1) * 256], False, True, [('bP', b)] + sl['vk'], [ok])
        epi(o_ps, ok)

    n = len(units)
    for i in range(n + 1):
        if i < n:
            front(i)
        if i >= 1:
            back(i - 1)


def emit_sw(p, sb, PS, d, S):
    NT = S // 128
    Q = [sb("swQ%d" % i, [32, S], BF16) for i in range(2)]
    K = sb("swK", [32, S], BF16)
    V = sb("swV", [128, NT, 33], BF16)
    load_cols(p, Q[0], 'swQ0', d['QKT'][0:32, 4, :], [], 'swQ0')
    load_cols(p, Q[1], 'swQ1', d['QKT'][0:32, 6, :], [], 'swQ1')
    load_cols(p, K, 'swK', d['QKT'][0:32, 5, :], [], 'swK')
    p.dma('sync', V, d['VSW'][:, :, :], [], ['swV'], 'swV')
    B = sb("swB", [128, 2, 2, 128], F32)
    B0 = sb("swB0", [128, 2, 2, 128], F32)
    for h in range(2):
        for sel in range(2):
            p.dma('sync', B[:, h, sel, :], toeplitz(d['bvec'], h * 2 + sel), [], ['swB'], 'swB')
        p.dma('sync', B0[:, h, 0, :], toeplitz(d['bvec'], h * 2), [], ['swB0'], 'swB0')
        p.memset('gpsimd', B0[:, h, 1, :], NEG, ['swB0'])
    onesf = sb("onesf", [128, 64], F32)
    p.dma('sync', onesf, d['onesf'][:, :], [], ['onesf'], 'onesf')
    es = sb("sw_es", [128, 2], F32)
    p.dma('sync', es[32:33, :], d['sinks'][:, :], [], ['sw_es'], 'sw_es')
    p.act(es[32:33, :], es[32:33, :], AF.Exp, ['sw_es'], ['sw_es'])
    t_sb = [sb("bt%d" % i, [128, 512], F32) for i in range(2)]
    P_sb = [sb("bP%d" % i, [128, 512], BF16) for i in range(2)]
    osb = sb("sw_osb", [128, 2, 512], F32)
    rrow = sb("sw_rrow", [128, 2, 512], F32)
    ost = [sb("sw_ost%d" % i, [32, 2, 512], BF16) for i in range(2)]
    scale = 32 ** -0.5
    units = []
    for n in range(NT):
        cs = slice(n * 128, (n + 1) * 128)
        ps_ = slice(max(n - 1, 0) * 128, (max(n - 1, 0) + 1) * 128)
        slots = [dict(kc=K[:, cs], kp=K[:, ps_], q=Q[h][:, cs], vc=V[:, n, :], vp=V[:, max(n - 1, 0), :],
                      rk=['swK', 'swQ%d' % h], vk=['swV']) for h in range(2)]

        def epi(o_ps, ok, n=n):
            tt = n % 4
            p.copy('scalar', osb[0:33, :, tt * 128:(tt + 1) * 128], o_ps[0:33, 0:256].rearrange("p (a d) -> p a d", d=128), [ok], ['sw_osb'])
            if tt == 3:
                gi = (n // 4) % 2
                T0 = (n - 3) * 128
                for h in range(2):
                    p.act(rrow[32:33, h, :], osb[32:33, h, :], AF.Ln, ['sw_osb', 'sw_es'], ['sw_rrow'], bias=es[32:33, h:h + 1])
                    p.act(rrow[32:33, h, :], rrow[32:33, h, :], AF.Exp, ['sw_rrow'], ['sw_rrow'], scale=-1.0)
                    p.mm(PS[4 + h][0:32, :], onesf[32:33, 0:32], rrow[32:33, h, :], True, True, ['sw_rrow', 'onesf'], [PK(4 + h)])
                    p.tt('vector', ost[gi][:, h, :], osb[0:32, h, :], PS[4 + h][0:32, :], ALU.mult, ['sw_osb', PK(4 + h)], [('sw_ost', gi)])
                p.dma(STQ, d['OUT'][128:192, T0:T0 + 512].rearrange("(h p) t -> p h t", p=32), ost[gi], [('sw_ost', gi)], [('OUT', 4 + gi)], 'sw_ost%d' % gi)
        units.append((slots, (B0 if n == 0 else B).rearrange("p a b c -> p (a b c)"), 'swB0' if n == 0 else 'swB', epi))
    run_band_units(p, PS, units, t_sb, P_sb, scale)


def emit_dil(p, sb, PS, d, S):
    Qd = sb("dlQ", [96, S], BF16)
    Kd = sb("dlK", [96, S], BF16)
    Oacc = sb("dlO", [128, S], F32)
    onesf = sb("onesf", [128, 64], F32)
    p.dma('sync', onesf, d['onesf'][:, :], [], ['onesf'], 'onesf')
    Vg = sb("dlV", [128, S // 128, 33], BF16)
    Bt = [sb("dlB%d" % i, [128, 2, 2, 128], F32) for i in range(2)]
    t_sb = [sb("bt%d" % i, [128, 512], F32) for i in range(2)]
    P_sb = [sb("bP%d" % i, [128, 512], BF16) for i in range(2)]
    rrow = sb("dl_rrow", [128, 512], F32)
    ost = [sb("dl_ost%d" % i, [32, 512], BF16) for i in range(2)]
    scale = 32 ** -0.5
    src = [(4, 5, 32), (4, 5, 64), (6, 7, 32)]
    for g, dil in enumerate(DILS):
        qs_, ks_, pb = src[g]
        rows = slice(pb, pb + 32)
        M = S // dil
        nb = M // 128
        load_cols(p, Qd[rows], 'dlQ', d['QKT'][rows, qs_, :], [], 'dlQ')
        load_cols(p, Kd[rows], 'dlK', d['QKT'][rows, ks_, :], [], 'dlK')
        Vv = Vg.rearrange("p (r n) d -> p r n d", r=dil)
        vd = d['VD%d' % g]
        for r0 in range(dil):
            for n0 in range(0, nb, 16):
                nn = min(16, nb - n0)
                srcap = bass.AP(vd.tensor, r0 * 33 + n0 * 128 * dil * 33, [[dil * 33, 128], [128 * dil * 33, nn], [1, 33]])
                p.dma('sync', Vv[:, r0, n0:n0 + nn, :], srcap, [], ['dlV'], 'dlV')
        for v in range(2):
            for u in range(2):
                for sel in range(2):
                    if v == 1 and u == 0 and sel == 1:
                        p.memset('gpsimd', Bt[v][:, u, sel, :], NEG, [('dlB', v)])
                    else:
                        p.dma('sync', Bt[v][:, u, sel, :], toeplitz(d['bvec'], (2 + g) * 2 + sel), [], [('dlB', v)], 'dlB%d' % v)
        units = []
        for r in range(dil):
            for n in range(0, nb, 2):
                slots = []
                for u in range(2):
                    n1 = n + u
                    np_ = max(n1 - 1, 0)
                    qc = slice(r + dil * 128 * n1, r + dil * 128 * n1 + dil * 127 + 1, dil)
                    kc = slice(r + dil * 128 * np_, r + dil * 128 * np_ + dil * 127 + 1, dil)
                    slots.append(dict(kc=Kd[rows, qc], kp=Kd[rows, kc], q=Qd[rows, qc], vc=Vv[:, r, n1, :], vp=Vv[:, r, np_, :],
                                      rk=['dlK', 'dlQ'], vk=['dlV']))
                v = 1 if n == 0 else 0
                oc = slice(r + dil * 128 * n, r + dil * 128 * n + dil * 255 + 1, dil)

                def epi(o_ps, ok, oc=oc, g=g):
                    if g == 0:
                        p.copy('vector', Oacc[0:33, oc], o_ps[0:33, 0:256], [ok], ['dlO'])
                    else:
                        p.tt('vector', Oacc[0:33, oc], o_ps[0:33, 0:256], Oacc[0:33, oc], ALU.add, [ok, 'dlO'], ['dlO'])
                units.append((slots, Bt[v].rearrange("p a b c -> p (a b c)"), ('dlB', v), epi))
        run_band_units(p, PS, units, t_sb, P_sb, scale)
    for c in range(S // 512):
        gi = c % 2
        cs = slice(c * 512, (c + 1) * 512)
        p.act(rrow[32:33, :], Oacc[32:33, cs], AF.Ln, ['dlO'], ['dl_rrow'])
        p.act(rrow[32:33, :], rrow[32:33, :], AF.Exp, ['dl_rrow'], ['dl_rrow'], scale=-1.0)
        p.mm(PS[4 + gi][0:32, :], onesf[32:33, 0:32], rrow[32:33, :], True, True, ['dl_rrow', 'onesf'], [PK(4 + gi)])
        p.tt('vector', ost[gi], Oacc[0:32, cs], PS[4 + gi][0:32, :], ALU.mult, ['dlO', PK(4 + gi)], [('dl_ost', gi)])
        p.dma(STQ, d['OUT'][192:224, cs], ost[gi], [('dl_ost', gi)], [('OUT', 6 + gi)], 'dl_ost%d' % gi)


def build_A(S, phases=('inproj', 'sb', 'mla', 'sw', 'dil'), dbg=False):
    nc = bass.Bass("TRN2", target_bir_lowering=False)
    NT = S // 128
    d = {}

    def din(name, shape, dt=F32):
        d[name] = nc.dram_tensor(name, shape, dt, kind="ExternalInput").ap()

    din('x', [S, D])
    din('w_in', [D, 1280])
    din('g1', [128, 8])
    din('wqb', [256, 96])
    din('gqa', [128, 2])
    din('wkvb', [256, 128])
    din('gkva', [128, 2])
    din('g32', [288])
    din('g96', [192])
    din('cos', [128, SEQ // 128, 16])
    din('sin', [128, SEQ // 128, 16])
    din('ident', [128, 128], BF16)
    din('triN', [128, 128], BF16)
    din('onesb', [128, 128], BF16)
    din('maskS', [128, 4, 512], BF16)
    din('maskI', [128, 4, 512], BF16)
    din('onesf', [128, 64])
    din('bvec', [10, 128, 128])
    din('sinks', [1, 2])
    kind = "ExternalOutput" if dbg else "Internal"
    d['QKT'] = nc.dram_tensor('QKT', [128, 8, S], BF16, kind=kind).ap()
    d['VSB'] = nc.dram_tensor('VSB', [128, NT, 64], BF16, kind=kind).ap()
    d['VML'] = nc.dram_tensor('VML', [128, NT, 65], BF16, kind=kind).ap()
    d['VSW'] = nc.dram_tensor('VSW', [128, NT, 33], BF16, kind=kind).ap()
    for g in range(3):
        d['VD%d' % g] = nc.dram_tensor('VD%d' % g, [S, 33], BF16, kind=kind).ap()
    d['OUT'] = nc.dram_tensor('OUT', [224, S], BF16, kind="ExternalOutput").ap()
    with ExitStack() as es:
        sb = Arena(nc, es, 196 * 1024)
        PSB = [es.enter_context(nc.psum_tensor("psb%d" % i, [128, 1024], F32))[:, :] for i in range(4)]
        PS = [PSB[i // 2][:, (i % 2) * 512:(i % 2 + 1) * 512] for i in range(8)]
        d_psb = PSB
        p = Prog(nc, es)
        block = es.enter_context(nc.Block())
        d['xrows'] = lambda t: d['x'][t * 128:(t + 1) * 128, :]
        d['GL'] = nc.dram_tensor('GL', [128, S], BF16, kind="Internal").ap()
        d['PSB'] = d_psb
        emitters = dict(inproj=emit_inproj, sb=emit_sb, mla=emit_mla, sw=emit_sw, dil=emit_dil)
        for ph in phases:
            sb.reset()
            emitters[ph](p, sb, PS, d, S)
            p.barrier()
        p.finish(block)
    return nc, p


def bf(a):
    return np.ascontiguousarray(a).astype(ml_dtypes.bfloat16)


def t5_bucket_np(dist):
    dist = np.asarray(dist, np.int64)
    d_ = np.maximum(dist, 1).astype(np.float32)
    large = 16 + (np.log(d_ / np.float32(16)) / np.float32(math.log(2048 / 16)) * np.float32(16)).astype(np.int32)
    large = np.minimum(large, 31)
    return np.where(dist < 16, dist, large)


def consts_A():
    k = np.arange(128)
    c = {}
    c['ident'] = bf(np.eye(128, dtype=np.float32))
    c['triN'] = bf(-(k[:, None] >= k[None, :]).astype(np.float32))
    c['onesb'] = bf(np.ones((128, 128), np.float32))
    qi = np.arange(512)
    mS = np.zeros((128, 4, 512), np.float32)
    mI = np.zeros((128, 4, 512), np.float32)
    for r in range(4):
        mS[:, r, :] = (128 * r + k[:, None]) < qi[None, :]
        mI[:, r, :] = (128 * r + k[:, None]) <= qi[None, :]
    c['maskS'] = bf(mS)
    c['maskI'] = bf(mI)
    c['onesf'] = np.ones((128, 64), np.float32)
    half = 16
    inv = (10000.0 ** (-np.arange(half, dtype=np.float32) / half)).astype(np.float32)
    ang = np.arange(SEQ, dtype=np.float32)[:, None] * inv[None, :]
    c['cos'] = np.ascontiguousarray(np.cos(ang).astype(np.float32).reshape(SEQ // 128, 128, 16).transpose(1, 0, 2))
    c['sin'] = np.ascontiguousarray(np.sin(ang).astype(np.float32).reshape(SEQ // 128, 128, 16).transpose(1, 0, 2))
    return c


def band_bias_vecs(rel_bias, h):
    dd = np.arange(-127, 128)
    out = np.full((10, 255), NEG, np.float32)
    for i, hq in enumerate((2 * h, 2 * h + 1)):
        cur = dd >= 0
        out[2 * i, cur] = rel_bias[t5_bucket_np(dd[cur]), hq]
        prv = dd < 0
        out[2 * i + 1, prv] = rel_bias[t5_bucket_np(128 + dd[prv]), hq]
    for g, dil in enumerate(DILS):
        col = 8 + g * 4 + h
        cur = dd >= 0
        out[4 + 2 * g, cur] = rel_bias[t5_bucket_np(dd[cur] * dil), col]
        prv = dd <= 0
        out[5 + 2 * g, prv] = rel_bias[t5_bucket_np((128 + dd[prv]) * dil), col]
    return out


def inputs_A(c, l, b, h, inp, x_b, S):
    cols = core_cols(h)
    m = dict(c)
    m['x'] = np.ascontiguousarray(x_b[:S])
    m['w_in'] = np.ascontiguousarray(np.concatenate([inp['w_in'][l][:, cols], inp['w_gate_a'][l]], axis=1))
    m['g1'] = np.ascontiguousarray(inp['norm1_g'][l].reshape(8, 128).T)
    m['wqb'] = np.ascontiguousarray(inp['w_qb'][l][:, h * 96:(h + 1) * 96])
    m['gqa'] = np.ascontiguousarray(inp['g_qa'][l].reshape(2, 128).T)
    m['wkvb'] = np.ascontiguousarray(inp['w_kvb'][l][:, h * 128:(h + 1) * 128])
    m['gkva'] = np.ascontiguousarray(inp['g_kva'][l].reshape(2, 128).T)
    gs, gd = inp['qk_g_sw'][l], inp['qk_g_dil'][l]
    m['g32'] = np.concatenate([gs[0], gd[0], gd[0], gs[1], gd[1], gd[1], gs[0], gd[0], gd[1]]).astype(np.float32)
    m['g96'] = np.concatenate([inp['qk_g_mla'][l][0], inp['qk_g_mla'][l][1]]).astype(np.float32)
    bv = band_bias_vecs(inp['rel_bias'], h)
    kk = np.arange(128)
    m['bvec'] = np.ascontiguousarray(bv[:, kk[None, :] - kk[:, None] + 127])
    m['sinks'] = np.ascontiguousarray(inp['sinks'][l][2 * h:2 * h + 2].reshape(1, 2))
    return m


BR_CHUNKS = ((0, 1), (2, 3), (4, 5), (6,))


def castw_iter(p, sb, d, nblk, F, engs=('vector', 'gpsimd', 'scalar')):
    stg = [sb("cw_stg%d" % i, [128, 2 * F], F32) for i in range(2)]
    stb = [sb("cw_stb%d" % i, [128, 2 * F], BF16) for i in range(2)]
    it = 0
    for blk in range(nblk):
        for kc in range(8):
            b = it % 2
            rows = slice(kc * 128, (kc + 1) * 128)
            p.dma('sync', stg[b][:, 0:F], d['wg'](blk)[rows, :], [], [('cw_stg', b)], 'cw_stg%d' % b)
            p.dma('sync', stg[b][:, F:2 * F], d['wu'](blk)[rows, :], [], [('cw_stg', b)], 'cw_stg%d' % b)
            p.copy(engs[it % len(engs)], stb[b], stg[b], [('cw_stg', b)], [('cw_stb', b)])
            p.dma(STQ, d['WGU'][blk, :, kc, :], stb[b], [('cw_stb', b)], [('WGU', b)], 'cw_stb%d' % b)
            it += 1
            yield
        for fc in range(F // 128):
            b = it % 2
            p.dma('sync', stg[b][:, 0:1024], d['wd'](blk)[fc * 128:(fc + 1) * 128, :], [], [('cw_stg', b)], 'cw_stg%d' % b)
            p.copy(engs[it % len(engs)], stb[b][:, 0:1024], stg[b][:, 0:1024], [('cw_stg', b)], [('cw_stb', b)])
            p.dma(STQ, d['WD'][blk, :, fc, :], stb[b][:, 0:1024], [('cw_stb', b)], [('WD', b)], 'cw_stb%d' % b)
            it += 1
            yield


def emit_castw(p, sb, PS, d, nblk, F):
    for _ in castw_iter(p, sb, d, nblk, F):
        pass


def emit_B(p, sb, PS, d, NTOK, nblk, F, moe):
    NCH = NTOK // 512
    FC = F // 128
    identb = sb("identb", [128, 128], BF16)
    p.dma('sync', identb, d['ident'][:, :], [], ['identb'], 'identb')
    G1 = sb("G1", [128, 1024], F32)
    G2 = sb("G2", [128, 1024], F32)
    p.dma('sync', G1, d['g1'].partition_broadcast(128), [], ['G1'], 'G1')
    p.dma('sync', G2, d['g2'].partition_broadcast(128), [], ['G2'], 'G2')
    bgt = sb("bgt", [128, 32], F32)
    p.dma('sync', bgt, d['bgate'][:, :], [], ['bgt'], 'bgt')
    Wga = sb("Wga", [128, 8, 128], BF16)
    Wgb = sb("Wgb", [128, 4096], BF16)
    Wbr = sb("Wbr", [128, 7, 1024], BF16)
    Wout = sb("Wout", [128, 8, 1024], BF16)
    stg = [sb("w_stg%d" % i, [128, 1024], F32) for i in range(2)]
    engs = ['vector', 'gpsimd', 'scalar']
    it = 0

    def cast_in(dst, src, n):
        nonlocal it
        b = it % 2
        p.dma('sync', stg[b][:, 0:n], src, [], [('w_stg', b)], 'w_stg%d' % b)
        p.copy(engs[it % 3], dst, stg[b][:, 0:n], [('w_stg', b)], ['Wres'])
        it += 1
    for kc in range(8):
        cast_in(Wga[:, kc, :], d['wga'][kc * 128:(kc + 1) * 128, :], 128)
        cast_in(Wout[:, kc, :], d['wout'][kc * 128:(kc + 1) * 128, :], 1024)
    for i in range(4):
        cast_in(Wgb[:, i * 1024:(i + 1) * 1024], d['wgb'][:, i * 1024:(i + 1) * 1024], 1024)
    for rc in range(7):
        cast_in(Wbr[:, rc, :], d['wbr'][rc * 128:(rc + 1) * 128, :], 1024)
    if moe:
        identf = sb("identf", [128, 128], F32)
        p.dma('sync', identf, d['identf'][:, :], [], ['identf'], 'identf')
        Wr = sb("Wr", [128, 8, 8], F32)
        p.dma('sync', Wr, d['wr'].rearrange("(kc p) e -> p kc e", p=128), [], ['Wr'], 'Wr')
        brt = sb("brt", [128, 8], F32)
        p.dma('sync', brt, d['br'].partition_broadcast(128), [], ['brt'], 'brt')
        h2f = sb("h2f", [128, 1024], F32)
        h2fT = sb("h2fT", [128, 8, 128], F32)
        rt = sb("rt", [128, 64], F32)
        GW = sb("GW", [128, 4, 8], F32)
    X4 = sb("X4", [128, 4, 1024], F32)
    oTc = sb("oTc", [128, 7, 512], BF16)
    hT = sb("hT", [128, 8, 512], BF16)
    hb = sb("hb", [128, 1024], BF16)
    junk = sb("junk", [128, 1024], BF16)
    st = sb("st", [128, 8], F32)
    glT = sb("glT", [128, 512], BF16)
    gate = [sb("gate%d" % i, [128, 512], F32) for i in range(2)]
    tmpy = sb("tmpy", [128, 512], F32)
    yacc = sb("yacc", [128, 512], F32)
    yT = sb("yT", [128, 8, 512], BF16)
    sg = [sb("sg%d" % i, [128, 512], F32) for i in range(2)]
    WGU_t = [sb("WGU_t%d" % i, [128, 8, 2 * F], BF16) for i in range(2)]
    WD_t = [sb("WD_t%d" % i, [128, FC, 1024], BF16) for i in range(2)]
    psTb = PS[0].bitcast(BF16)
    x, out = d['x'], d['out']
    wit = 0

    def norm_T(tt, G, Gk):
        xt = X4[:, tt, :]
        p.act(junk, xt, AF.Square, ['X4'], ['junk', 'ssq'], accum_out=st[:, 0:1])
        p.act(st[:, 1:2], st[:, 0:1], AF.Sqrt, ['ssq'], ['rs'], scale=1.0 / D, bias=EPS)
        p.recip(st[:, 2:3], st[:, 1:2], ['rs'], ['rstd'])
        p.stt(hb, xt, st[:, 2:3], G, ALU.mult, ALU.mult, ['X4', 'rstd', Gk], ['hb'])
        for kc in range(8):
            p.tr(psTb[:, kc * 128:(kc + 1) * 128], hb[:, kc * 128:(kc + 1) * 128], identb, ['hb', 'identb'], [PK(0)])
        p.copy('vector', hT[:, :, tt * 128:(tt + 1) * 128], psTb.rearrange("p (a d) -> p a d", d=128), [PK(0)], ['hT'])

    for c in range(NCH):
        T0 = c * 512
        p.dma('sync', X4, x[T0:T0 + 512, :].rearrange("(t p) d -> p t d", p=128), [], ['X4'], 'X4')
        p.dma('sync', oTc, d['oT'][:, T0:T0 + 512].rearrange("(r p) t -> p r t", p=128), [], ['oTc'], 'oTc')
        for tt in range(4):
            norm_T(tt, G1, 'G1')
        for kc in range(8):
            p.mm(PS[1], Wga[:, kc, :], hT[:, kc, :], kc == 0, kc == 7, ['hT', 'Wres'], [PK(1)])
        p.copy('vector', glT, PS[1], [PK(1)], ['glT'])
        k = 0
        for oc in range(8):
            for i in range(4):
                b = k % 2
                k += 1
                gp, gk = PS[2 + b], PK(2 + b)
                bp, bk = PS[4 + b], PK(4 + b)
                col = i * 1024 + oc * 128
                p.mm(gp, Wgb[:, col:col + 128], glT, True, True, ['glT', 'Wres'], [gk])
                p.act(gate[b], gp, AF.Sigmoid, [gk, 'bgt'], [('gate', b)], bias=bgt[:, i * 8 + oc:i * 8 + oc + 1])
                rcs = BR_CHUNKS[i]
                for n_, rc in enumerate(rcs):
                    p.mm(bp, Wbr[:, rc, oc * 128:(oc + 1) * 128], oTc[:, rc, :], n_ == 0, n_ == len(rcs) - 1, ['oTc', 'Wres'], [bk])
                if i == 0:
                    p.tt('vector', yacc, gate[b], bp, ALU.mult, [('gate', b), bk], ['yacc'])
                else:
                    p.tt('vector', tmpy, gate[b], bp, ALU.mult, [('gate', b), bk], ['tmpy'])
                    if i < 3:
                        p.tt('gpsimd', yacc, yacc, tmpy, ALU.add, ['yacc', 'tmpy'], ['yacc'])
                    else:
                        p.tt('gpsimd', yT[:, oc, :], yacc, tmpy, ALU.add, ['yacc', 'tmpy'], ['yT'])
        k = 0
        for tt in range(4):
            for half in range(2):
                b = k % 2
                k += 1
                ps, pk = PS[6 + b], PK(6 + b)
                for oc in range(8):
                    p.mm(ps, yT[:, oc, tt * 128:(tt + 1) * 128], Wout[:, oc, half * 512:(half + 1) * 512], oc == 0, oc == 7, ['yT', 'Wres'], [pk])
                xs = X4[:, tt, half * 512:(half + 1) * 512]
                p.tt('vector', xs, ps, xs, ALU.add, [pk, 'X4'], ['X4'])
        for tt in range(4):
            norm_T(tt, G2, 'G2')
            if moe:
                p.stt(h2f, X4[:, tt, :], st[:, 2:3], G2, ALU.mult, ALU.mult, ['X4', 'rstd', 'G2'], ['h2f'])
                for kc in range(8):
                    bnk = 2 + kc // 4
                    p.tr(PS[bnk][:, (kc % 4) * 128:(kc % 4 + 1) * 128], h2f[:, kc * 128:(kc + 1) * 128], identf, ['h2f', 'identf'], [PK(bnk)])
                p.copy('vector', h2fT[:, 0:4, :].rearrange("p a d -> p (a d)"), PS[2], [PK(2)], ['h2fT'])
                p.copy('vector', h2fT[:, 4:8, :].rearrange("p a d -> p (a d)"), PS[3], [PK(3)], ['h2fT'])
                for kc in range(8):
                    p.mm(PS[1][:, 0:8], h2fT[:, kc, :], Wr[:, kc, :], kc == 0, kc == 7, ['h2fT', 'Wr'], [PK(1)])
                lg, m8, ee, mk = rt[:, 0:8], rt[:, 8:16], rt[:, 16:24], rt[:, 24:32]
                p.tt('vector', lg, PS[1][:, 0:8], brt, ALU.add, [PK(1), 'brt'], ['lg'])
                p.op('vector', lambda e, lg=lg, m8=m8: e.max(out=m8, in_=lg), ['lg'], ['m8'])
                p.ts('vector', rt[:, 32:33], m8[:, 0:1], -1.0, ALU.mult, ['m8'], ['negm'])
                p.act(ee, lg, AF.Exp, ['lg', 'negm'], ['ee'], bias=rt[:, 32:33])
                p.ts('vector', mk, lg, m8[:, 1:2], ALU.is_ge, ['lg', 'm8'], ['mk'])
                p.tt('vector', ee, ee, mk, ALU.mult, ['ee', 'mk'], ['ee'])
                p.op('vector', lambda e, ee=ee: e.tensor_reduce(out=rt[:, 33:34], in_=ee, axis=AX.X, op=ALU.add), ['ee'], ['den'])
                p.recip(rt[:, 34:35], rt[:, 33:34], ['den'], ['rden'])
                p.ts('vector', GW[:, tt, :], ee, rt[:, 34:35], ALU.mult, ['ee', 'rden'], ['GW'])
        aT = yT
        for blk in range(nblk):
            wb = wit % 2
            wit += 1
            p.dma('sync', WGU_t[wb], d['WGU'][blk], [], [('WGU_t', wb)], 'WGU_t%d' % wb)
            p.dma('sync', WD_t[wb], d['WD'][blk], [], [('WD_t', wb)], 'WD_t%d' % wb)
            for fc in range(FC):
                b = fc % 2
                gps, gk = PS[2 + b], PK(2 + b)
                ups, uk = PS[4 + b], PK(4 + b)
                for kc in range(8):
                    p.mm(gps, WGU_t[wb][:, kc, fc * 128:(fc + 1) * 128], hT[:, kc, :], kc == 0, kc == 7, ['hT', ('WGU_t', wb)], [gk])
                for kc in range(8):
                    p.mm(ups, WGU_t[wb][:, kc, F + fc * 128:F + (fc + 1) * 128], hT[:, kc, :], kc == 0, kc == 7, ['hT', ('WGU_t', wb)], [uk])
                p.act(sg[b], gps, AF.Silu, [gk], [('sg', b)])
                p.tt('vector', aT[:, fc, :], sg[b], ups, ALU.mult, [('sg', b), uk], ['yT'])
            k = 0
            for tt in range(4):
                for half in range(2):
                    b = k % 2
                    k += 1
                    ps, pk = PS[6 + b], PK(6 + b)
                    for fc in range(FC):
                        p.mm(ps, aT[:, fc, tt * 128:(tt + 1) * 128], WD_t[wb][:, fc, half * 512:(half + 1) * 512], fc == 0, fc == FC - 1, ['yT', ('WD_t', wb)], [pk])
                    xs = X4[:, tt, half * 512:(half + 1) * 512]
                    if moe:
                        p.stt(xs, ps, GW[:, tt, blk:blk + 1], xs, ALU.mult, ALU.add, [pk, 'GW', 'X4'], ['X4'])
                    else:
                        p.tt('vector', xs, ps, xs, ALU.add, [pk, 'X4'], ['X4'])
        p.dma(STQ, out[T0:T0 + 512, :].rearrange("(t p) d -> p t d", p=128), X4, ['X4'], ['out'], 'X4o')


def build_B(NTOK, moe):
    nc = bass.Bass("TRN2", target_bir_lowering=False)
    d = {}

    def din(name, shape, dt=F32):
        d[name] = nc.dram_tensor(name, shape, dt, kind="ExternalInput").ap()

    din('x', [NTOK, D])
    din('oT', [896, NTOK], BF16)
    din('ident', [128, 128], BF16)
    din('g1', [1024])
    din('g2', [1024])
    din('bgate', [128, 32])
    din('wga', [1024, 128])
    din('wgb', [128, 4096])
    din('wbr', [896, 1024])
    din('wout', [1024, 1024])
    if moe:
        nblk, F = 8, 768
        din('identf', [128, 128])
        din('wr', [1024, 8])
        din('br', [8])
        din('wgu', [8, 1024, 1536])
        din('wdn', [8, 768, 1024])
        d['wg'] = lambda blk: d['wgu'][blk, :, 0:768]
        d['wu'] = lambda blk: d['wgu'][blk, :, 768:1536]
        d['wd'] = lambda blk: d['wdn'][blk, :, :]
    else:
        nblk, F = 4, 512
        din('wgu', [1024, 4096])
        din('wdn', [2048, 1024])
        d['wg'] = lambda blk: d['wgu'][:, blk * 512:(blk + 1) * 512]
        d['wu'] = lambda blk: d['wgu'][:, 2048 + blk * 512:2048 + (blk + 1) * 512]
        d['wd'] = lambda blk: d['wdn'][blk * 512:(blk + 1) * 512, :]
    d['WGU'] = nc.dram_tensor('WGU', [nblk, 128, 8, 2 * F], BF16, kind="Internal").ap()
    d['WD'] = nc.dram_tensor('WD', [nblk, 128, F // 128, 1024], BF16, kind="Internal").ap()
    d['out'] = nc.dram_tensor('out', [NTOK, D], F32, kind="ExternalOutput").ap()
    with ExitStack() as es:
        sb = Arena(nc, es, 200 * 1024)
        PSB = [es.enter_context(nc.psum_tensor("psb%d" % i, [128, 1024], F32))[:, :] for i in range(4)]
        PS = [PSB[i // 2][:, (i % 2) * 512:(i % 2 + 1) * 512] for i in range(8)]
        d_psb = PSB
        p = Prog(nc, es)
        block = es.enter_context(nc.Block())
        emit_castw(p, sb, PS, d, nblk, F)
        p.barrier()
        sb.reset()
        emit_B(p, sb, PS, d, NTOK, nblk, F, moe)
        p.barrier()
        p.finish(block)
    return nc, p


def inputs_B(l, inp, x_tok, oT_tok, moe):
    m = {}
    m['x'] = np.ascontiguousarray(x_tok)
    m['oT'] = np.ascontiguousarray(oT_tok)
    m['ident'] = bf(np.eye(128, dtype=np.float32))
    m['g1'] = np.ascontiguousarray(inp['norm1_g'][l])
    m['g2'] = np.ascontiguousarray(inp['norm2_g'][l])
    m['bgate'] = np.ascontiguousarray(inp['b_gate'][l].reshape(32, 128).T)
    m['wga'] = np.ascontiguousarray(inp['w_gate_a'][l])
    m['wgb'] = np.ascontiguousarray(inp['w_gate_b'][l])
    m['wbr'] = np.ascontiguousarray(inp['w_branch'][l])
    m['wout'] = np.ascontiguousarray(inp['w_out'][l])
    if moe:
        m['identf'] = np.eye(128, dtype=np.float32)
        m['wr'] = np.ascontiguousarray(inp['w_router'][l // 2])
        m['br'] = np.ascontiguousarray(inp['b_router'][l // 2])
        m['wgu'] = np.ascontiguousarray(inp['w_gu_exp'][l // 2])
        m['wdn'] = np.ascontiguousarray(inp['w_down_exp'][l // 2])
    else:
        m['wgu'] = np.ascontiguousarray(inp['w_gu_dense'][l // 2])
        m['wdn'] = np.ascontiguousarray(inp['w_down_dense'][l // 2])
    return m


_CACHE = {}


def _prog(key, fn):
    if key not in _CACHE:
        _CACHE[key] = fn()
    return _CACHE[key]


def assemble_oT(outs):
    res = []
    for b in range(BATCH):
        rows = [None] * 4
        o = [np.asarray(outs[b * 4 + h]) for h in range(4)]
        sbp = np.concatenate([o[h][0:64] for h in range(4)], axis=0)
        mlp = np.concatenate([o[h][64:128] for h in range(4)], axis=0)
        swp = np.concatenate([o[h][128:192] for h in range(4)], axis=0)
        dlp = np.concatenate([o[h][192:224] for h in range(4)], axis=0)
        res.append(np.concatenate([sbp, mlp, swp, dlp], axis=0))
    return res


def kernel(**inp):
    inp = {k: np.asarray(v) for k, v in inp.items()}
    return kernel_fused(inp)


GROUPS = [[0, 1, 2, 3], [4, 5, 6, 7]]


def emit_merge(p, sb, PS, d, S):
    SL = S // 4
    bgt = sb("bgt", [128, 32], F32)
    p.dma('sync', bgt, d['bgate'][:, :], [], ['bgt'], 'bgt')
    Wgb = sb("Wgb", [128, 4096], BF16)
    Wbr = sb("Wbrc", [64, 4, 1024], BF16)
    stg = [sb("w_stg%d" % i, [128, 1024], F32) for i in range(2)]
    engs = ['vector', 'gpsimd', 'scalar']
    it = 0
    for i in range(4):
        b = it % 2
        p.dma('sync', stg[b], d['wgb'][:, i * 1024:(i + 1) * 1024], [], [('w_stg', b)], 'w_stg%d' % b)
        p.copy(engs[it % 3], Wgb[:, i * 1024:(i + 1) * 1024], stg[b], [('w_stg', b)], ['Wres'])
        it += 1
    rows = ((0, 64), (64, 64), (128, 64), (192, 32))
    for i, (r0, nr) in enumerate(rows):
        b = it % 2
        p.dma('sync', stg[b][0:nr, :], d['wbrc'][r0:r0 + nr, :], [], [('w_stg', b)], 'w_stg%d' % b)
        p.copy(engs[it % 3], Wbr[0:nr, i, :], stg[b][0:nr, :], [('w_stg', b)], ['Wres'])
        it += 1
    glT = [sb("glT%d" % i, [128, 512], BF16) for i in range(2)]
    oc_t = [sb("oc_t%d" % i, [64, 4, 512], BF16) for i in range(2)]
    gate = [sb("gate%d" % i, [128, 512], F32) for i in range(2)]
    tmpy2 = [sb("tmpy%d" % i, [128, 512], F32) for i in range(2)]
    yacc = sb("yacc", [128, 512], F32)
    yst = [sb("yst%d" % i, [128, 8, 512], F32) for i in range(2)]
    k = 0
    SLp = min(SL, 1024)
    NPc = SL // SLp
    order = [(sl * SL + q * SLp) // 512 + cc for q in range(NPc) for sl in range(4) for cc in range(SLp // 512)]
    for ci, c in enumerate(order):
        cb_ = ci % 2
        cs = slice(c * 512, (c + 1) * 512)
        p.dma('sync', glT[cb_], d['GL'][:, cs], [], [('glT', cb_)], 'glT%d' % cb_)
        for i, (r0, nr) in enumerate(rows):
            p.dma('sync', oc_t[cb_][0:nr, i, :], d['OUT'][r0:r0 + nr, cs], [], [('oc_t', cb_)], 'oc_t%d' % cb_)
        for oc in range(8):
            for i, (r0, nr) in enumerate(rows):
                b = k % 2
                k += 1
                gp, gk = PS[2 + b], PK(2 + b)
                bp, bk = PS[4 + b], PK(4 + b)
                col = i * 1024 + oc * 128
                p.mm(gp, Wgb[:, col:col + 128], glT[cb_], True, True, [('glT', cb_), 'Wres'], [gk])
                p.act(gate[b], gp, AF.Sigmoid, [gk, 'bgt'], [('gate', b)], bias=bgt[:, i * 8 + oc:i * 8 + oc + 1])
                p.mm(bp, Wbr[0:nr, i, oc * 128:(oc + 1) * 128], oc_t[cb_][0:nr, i, :], True, True, [('oc_t', cb_), 'Wres'], [bk])
                if i == 0:
                    p.tt('vector', yacc, gate[b], bp, ALU.mult, [('gate', b), bk], ['yacc'])
                else:
                    tmpy, tk = tmpy2[b], ('tmpy', b)
                    p.tt('vector', tmpy, gate[b], bp, ALU.mult, [('gate', b), bk], [tk])
                    if i < 3:
                        p.tt('gpsimd', yacc, yacc, tmpy, ALU.add, ['yacc', tk], ['yacc'])
                    else:
                        p.tt('gpsimd', yst[cb_][:, oc, :], yacc, tmpy, ALU.add, ['yacc', tk], [('yst', cb_)])
        sl, w = (c * 512) // SL, (c * 512) % SL
        q, t0 = w // SLp, w % SLp
        p.dma('sync', d['YP'][q, sl, :, t0:t0 + 512].rearrange("(oc p) t -> p oc t", p=128), yst[cb_], [('yst', cb_)], [('YP', cb_)], 'yst%d' % cb_)
        if (ci + 1) % (4 * (SLp // 512)) == 0:
            p.cc("ReduceScatter", ALU.add, GROUPS, d['YP'][q].rearrange("a c t -> (a c) t"), d['YS'][q], [('YP', 0), ('YP', 1)], [('YS', q)])


def emit_B2(p, sb, PS, d, NTOK, nblk, F, moe):
    NCH = NTOK // 512
    FC = F // 128
    identb = sb("identb", [128, 128], BF16)
    p.dma('sync', identb, d['ident'][:, :], [], ['identb'], 'identb')
    G2 = sb("G2", [128, 1024], F32)
    p.dma('sync', G2, d['g2'].partition_broadcast(128), [], ['G2'], 'G2')
    Wout = sb("Wout", [128, 8, 1024], BF16)
    stg = [sb("w_stg%d" % i, [128, 1024], F32) for i in range(2)]
    engs = ['vector', 'gpsimd', 'scalar']
    for kc in range(8):
        b = kc % 2
        p.dma('sync', stg[b], d['wout'][kc * 128:(kc + 1) * 128, :], [], [('w_stg', b)], 'w_stg%d' % b)
        p.copy(engs[kc % 3], Wout[:, kc, :], stg[b], [('w_stg', b)], ['Wres'])
    if moe:
        identf = sb("identf", [128, 128], F32)
        p.dma('sync', identf, d['identf'][:, :], [], ['identf'], 'identf')
        Wr = sb("Wr", [128, 8, 8], F32)
        p.dma('sync', Wr, d['wr'].rearrange("(kc p) e -> p kc e", p=128), [], ['Wr'], 'Wr')
        brt = sb("brt", [128, 8], F32)
        p.dma('sync', brt, d['br'].partition_broadcast(128), [], ['brt'], 'brt')
        h2f = sb("h2f", [128, 1024], F32)
        h2fT = sb("h2fT", [128, 8, 128], F32)
        rt = sb("rt", [128, 64], F32)
        GW = sb("GW", [128, 4, 8], F32)
    X4 = sb("X4", [128, 4, 1024], F32)
    yf = sb("yf", [128, 8, 512], F32)
    hT = sb("hT", [128, 8, 512], BF16)
    hb = sb("hb", [128, 1024], BF16)
    junk = sb("junk", [128, 1024], BF16)
    st = sb("st", [128, 8], F32)
    yT = sb("yT", [128, 8, 512], BF16)
    sg = [sb("sg%d" % i, [128, 512], F32) for i in range(2)]
    WGU_t = [sb("WGU_t%d" % i, [128, 8, 2 * F], BF16) for i in range(2)]
    WD_t = [sb("WD_t%d" % i, [128, FC, 1024], BF16) for i in range(2)]
    psTb = PS[0].bitcast(BF16)
    wit = 0

    def norm_T(tt, G, Gk):
        xt = X4[:, tt, :]
        p.act(junk, xt, AF.Square, ['X4'], ['junk', 'ssq'], accum_out=st[:, 0:1])
        p.act(st[:, 1:2], st[:, 0:1], AF.Sqrt, ['ssq'], ['rs'], scale=1.0 / D, bias=EPS)
        p.recip(st[:, 2:3], st[:, 1:2], ['rs'], ['rstd'])
        p.stt(hb, xt, st[:, 2:3], G, ALU.mult, ALU.mult, ['X4', 'rstd', Gk], ['hb'])
        for kc in range(8):
            p.tr(psTb[:, kc * 128:(kc + 1) * 128], hb[:, kc * 128:(kc + 1) * 128], identb, ['hb', 'identb'], [PK(0)])
        p.copy('vector', hT[:, :, tt * 128:(tt + 1) * 128], psTb.rearrange("p (a d) -> p a d", d=128), [PK(0)], ['hT'])

    for c in range(NCH):
        T0 = c * 512
        p.dma('sync', X4, d['xtok'][T0:T0 + 512, :].rearrange("(t p) d -> p t d", p=128), [], ['X4'], 'X4')
        SLp = min(NTOK, 1024)
        p.dma('sync', yf, d['YS'][T0 // SLp, :, T0 % SLp:T0 % SLp + 512].rearrange("(oc p) t -> p oc t", p=128), [('YS', T0 // SLp)], ['yf'], 'yf')
        p.copy('gpsimd', yT[:, 0:4, :], yf[:, 0:4, :], ['yf'], ['yT'])
        p.copy('vector', yT[:, 4:8, :], yf[:, 4:8, :], ['yf'], ['yT'])
        k = 0
        for tt in range(4):
            for half in range(2):
                b = k % 2
                k += 1
                ps, pk = PS[6 + b], PK(6 + b)
                for oc in range(8):
                    p.mm(ps, yT[:, oc, tt * 128:(tt + 1) * 128], Wout[:, oc, half * 512:(half + 1) * 512], oc == 0, oc == 7, ['yT', 'Wres'], [pk])
                xs = X4[:, tt, half * 512:(half + 1) * 512]
                p.tt('vector', xs, ps, xs, ALU.add, [pk, 'X4'], ['X4'])
        for tt in range(4):
            norm_T(tt, G2, 'G2')
            if moe:
                p.stt(h2f, X4[:, tt, :], st[:, 2:3], G2, ALU.mult, ALU.mult, ['X4', 'rstd', 'G2'], ['h2f'])
                for kc in range(8):
                    bnk = 2 + kc // 4
                    p.tr(PS[bnk][:, (kc % 4) * 128:(kc % 4 + 1) * 128], h2f[:, kc * 128:(kc + 1) * 128], identf, ['h2f', 'identf'], [PK(bnk)])
                p.copy('vector', h2fT[:, 0:4, :].rearrange("p a d -> p (a d)"), PS[2], [PK(2)], ['h2fT'])
                p.copy('vector', h2fT[:, 4:8, :].rearrange("p a d -> p (a d)"), PS[3], [PK(3)], ['h2fT'])
                for kc in range(8):
                    p.mm(PS[1][:, 0:8], h2fT[:, kc, :], Wr[:, kc, :], kc == 0, kc == 7, ['h2fT', 'Wr'], [PK(1)])
                lg, m8, ee, mk = rt[:, 0:8], rt[:, 8:16], rt[:, 16:24], rt[:, 24:32]
                p.tt('vector', lg, PS[1][:, 0:8], brt, ALU.add, [PK(1), 'brt'], ['lg'])
                p.op('vector', lambda e, lg=lg, m8=m8: e.max(out=m8, in_=lg), ['lg'], ['m8'])
                p.ts('vector', rt[:, 32:33], m8[:, 0:1], -1.0, ALU.mult, ['m8'], ['negm'])
                p.act(ee, lg, AF.Exp, ['lg', 'negm'], ['ee'], bias=rt[:, 32:33])
                p.ts('vector', mk, lg, m8[:, 1:2], ALU.is_ge, ['lg', 'm8'], ['mk'])
                p.tt('vector', ee, ee, mk, ALU.mult, ['ee', 'mk'], ['ee'])
                p.op('vector', lambda e, ee=ee: e.tensor_reduce(out=rt[:, 33:34], in_=ee, axis=AX.X, op=ALU.add), ['ee'], ['den'])
                p.recip(rt[:, 34:35], rt[:, 33:34], ['den'], ['rden'])
                p.ts('vector', GW[:, tt, :], ee, rt[:, 34:35], ALU.mult, ['ee', 'rden'], ['GW'])
        aT = yT
        for blk in range(nblk):
            wb = wit % 2
            wit += 1
            p.dma('sync', WGU_t[wb], d['WGU'][blk], [], [('WGU_t', wb)], 'WGU_t%d' % wb)
            p.dma('sync', WD_t[wb], d['WD'][blk], [], [('WD_t', wb)], 'WD_t%d' % wb)
            for fc in range(FC):
                b = fc % 2
                gps, gk = PS[2 + b], PK(2 + b)
                ups, uk = PS[4 + b], PK(4 + b)
                for kc in range(8):
                    p.mm(gps, WGU_t[wb][:, kc, fc * 128:(fc + 1) * 128], hT[:, kc, :], kc == 0, kc == 7, ['hT', ('WGU_t', wb)], [gk])
                for kc in range(8):
                    p.mm(ups, WGU_t[wb][:, kc, F + fc * 128:F + (fc + 1) * 128], hT[:, kc, :], kc == 0, kc == 7, ['hT', ('WGU_t', wb)], [uk])
                p.act(sg[b], gps, AF.Silu, [gk], [('sg', b)])
                p.tt('vector', aT[:, fc, :], sg[b], ups, ALU.mult, [('sg', b), uk], ['yT'])
            k = 0
            for tt in range(4):
                for half in range(2):
                    b = k % 2
                    k += 1
                    ps, pk = PS[6 + b], PK(6 + b)
                    for fc in range(FC):
                        p.mm(ps, aT[:, fc, tt * 128:(tt + 1) * 128], WD_t[wb][:, fc, half * 512:(half + 1) * 512], fc == 0, fc == FC - 1, ['yT', ('WD_t', wb)], [pk])
                    xs = X4[:, tt, half * 512:(half + 1) * 512]
                    if moe:
                        p.stt(xs, ps, GW[:, tt, blk:blk + 1], xs, ALU.mult, ALU.add, [pk, 'GW', 'X4'], ['X4'])
                    else:
                        p.tt('vector', xs, ps, xs, ALU.add, [pk, 'X4'], ['X4'])
        p.dma('sync', d['xout'][T0:T0 + 512, :].rearrange("(t p) d -> p t d", p=128), X4, ['X4'], ['xout'], 'X4o')
        if d.get('XG') is not None:
            for kk in (2 * c, 2 * c + 1):
                p.cc("AllGather", ALU.bypass, GROUPS, d['xout'][kk * 256:(kk + 1) * 256, :], d['XG'][kk], ['xout'], [('XG', kk)])


def build_fused(S, n_layers=2):
    nc = bass.Bass("TRN2", target_bir_lowering=False)
    NT, NTOK, SL = S // 128, S // 4, S // 4
    g = {}

    def din(name, shape, dt=F32):
        g[name] = nc.dram_tensor(name, shape, dt, kind="ExternalInput").ap()

    def dint(name, shape, dt):
        g[name] = nc.dram_tensor(name, shape, dt, kind="Internal").ap()

    din('x', [S, D])
    din('xtok', [NTOK, D])
    for nm, shp, dt in (('cos', [128, SEQ // 128, 16], F32), ('sin', [128, SEQ // 128, 16], F32), ('ident', [128, 128], BF16),
                        ('triN', [128, 128], BF16), ('onesb', [128, 128], BF16), ('maskS', [128, 4, 512], BF16),
                        ('maskI', [128, 4, 512], BF16), ('onesf', [128, 64], F32), ('identf', [128, 128], F32), ('bvec', [10, 128, 128], F32)):
        din(nm, shp, dt)
    for L in range(n_layers):
        sfx = str(L)
        for nm, shp in (('w_in', [D, 1280]), ('g1', [128, 8]), ('wqb', [256, 96]), ('gqa', [128, 2]), ('wkvb', [256, 128]),
                        ('gkva', [128, 2]), ('g32', [288]), ('g96', [192]), ('sinks', [1, 2]), ('wgb', [128, 4096]),
                        ('bgate', [128, 32]), ('wbrc', [224, 1024]), ('g2', [1024]), ('wout', [1024, 1024])):
            din(nm + sfx, shp)
    din('wgu0', [1024, 4096])
    din('wdn0', [2048, 1024])
    if n_layers > 1:
        din('wr1', [1024, 8])
        din('br1', [8])
        din('wgu1', [8, 1024, 1536])
        din('wdn1', [8, 768, 1024])
    dint('QKT', [128, 8, S], BF16)
    dint('VSB', [128, NT, 64], BF16)
    dint('VML', [128, NT, 65], BF16)
    dint('VSW', [128, NT, 33], BF16)
    for i in range(3):
        dint('VD%d' % i, [S, 33], BF16)
    dint('GL', [128, S], BF16)
    dint('OUT', [224, S], BF16)
    SLp = min(SL, 1024)
    NP = SL // SLp
    NAG = NTOK // 256
    dint('YP', [NP, 4, 1024, SLp], F32)
    dint('YS', [NP, 1024, SLp], F32)
    dint('XS', [NTOK, D], F32)
    dint('XG', [NAG, 4 * 256, D], F32)
    dint('WGU0', [4, 128, 8, 1024], BF16)
    dint('WD0', [4, 128, 4, 1024], BF16)
    if n_layers > 1:
        dint('WGU1', [8, 128, 8, 1536], BF16)
        dint('WD1', [8, 128, 6, 1024], BF16)
    g['final'] = nc.dram_tensor('final', [NTOK, D], F32, kind="ExternalOutput").ap()
    shared = ('cos', 'sin', 'ident', 'triN', 'onesb', 'maskS', 'maskI', 'onesf', 'identf', 'bvec',
              'QKT', 'VSB', 'VML', 'VSW', 'VD0', 'VD1', 'VD2', 'GL', 'OUT', 'YP', 'YS')
    with ExitStack() as es:
        sb = Arena(nc, es, 200 * 1024)
        PSB = [es.enter_context(nc.psum_tensor("psb%d" % i, [128, 1024], F32))[:, :] for i in range(4)]
        PS = [PSB[i // 2][:, (i % 2) * 512:(i % 2 + 1) * 512] for i in range(8)]
        d_psb = PSB
        p = Prog(nc, es)
        block = es.enter_context(nc.Block())
        for L in range(n_layers):
            sfx = str(L)
            moe = (L % 2 == 1)
            d = {k: g[k] for k in shared}
            for nm in ('w_in', 'g1', 'wqb', 'gqa', 'wkvb', 'gkva', 'g32', 'g96', 'sinks', 'wgb', 'bgate', 'wbrc', 'g2', 'wout'):
                d[nm] = g[nm + sfx]
            if L == 0:
                d['xrows'] = lambda t: g['x'][t * 128:(t + 1) * 128, :]
            else:
                def xrows(t):
                    T = t * 128
                    r, w = T // NTOK, T % NTOK
                    return g['XG'][w // 256, r * 256 + (w % 256):r * 256 + (w % 256) + 128, :]
                d['xrows'] = xrows
                d['xkeys'] = lambda t: [('XG', ((t * 128) % NTOK) // 256)]
            d['xtok'] = g['xtok'] if L == 0 else g['XS']
            d['xout'] = g['final'] if L == n_layers - 1 else g['XS']
            d['WGU'], d['WD'] = g['WGU' + sfx], g['WD' + sfx]
            if moe:
                nblk, F = 8, 768
                d['wr'], d['br'] = g['wr1'], g['br1']
                d['wg'] = lambda blk: g['wgu1'][blk, :, 0:768]
                d['wu'] = lambda blk: g['wgu1'][blk, :, 768:1536]
                d['wd'] = lambda blk: g['wdn1'][blk, :, :]
            else:
                nblk, F = 4, 512
                d['wg'] = lambda blk: g['wgu0'][:, blk * 512:(blk + 1) * 512]
                d['wu'] = lambda blk: g['wgu0'][:, 2048 + blk * 512:2048 + (blk + 1) * 512]
                d['wd'] = lambda blk: g['wdn0'][blk * 512:(blk + 1) * 512, :]
            d['XG'] = g['XG'] if L < n_layers - 1 else None
            d['n_side'] = nblk * (8 + F // 128)
            d['PSB'] = d_psb
            for ph in (emit_inproj, emit_sb, emit_mla, emit_sw, emit_dil):
                sb.reset()
                if ph is emit_sb:
                    ph(p, sb, PS, d, S, side=lambda sb_, d=d, nblk=nblk, F=F: castw_iter(p, sb_, d, nblk, F, engs=('gpsimd',)))
                else:
                    ph(p, sb, PS, d, S)
                p.barrier(exclude_cc=True, keep=('XG',))
            sb.reset()
            emit_merge(p, sb, PS, d, S)
            p.barrier(exclude_cc=True, keep=('YS',))
            sb.reset()
            emit_B2(p, sb, PS, d, NTOK, nblk, F, moe)
            p.barrier(exclude_cc=True, keep=('XG',))
        p.barrier()
        p.finish(block)
    return nc, p


def inputs_fused(cA, inp, core, S, n_layers=2):
    b, h = core // 4, core % 4
    NTOK = S // 4
    m = {k: cA[k] for k in ('cos', 'sin', 'ident', 'triN', 'onesb', 'maskS', 'maskI', 'onesf')}
    m['identf'] = np.eye(128, dtype=np.float32)
    x = np.asarray(inp['x'], np.float32)
    m['x'] = np.ascontiguousarray(x[b, :S])
    m['xtok'] = np.ascontiguousarray(x[b, h * NTOK:(h + 1) * NTOK])
    bv = band_bias_vecs(inp['rel_bias'], h)
    kk = np.arange(128)
    m['bvec'] = np.ascontiguousarray(bv[:, kk[None, :] - kk[:, None] + 127])
    cols = core_cols(h)
    for l in range(n_layers):
        s = str(l)
        m['w_in' + s] = np.ascontiguousarray(np.concatenate([inp['w_in'][l][:, cols], inp['w_gate_a'][l]], axis=1))
        m['g1' + s] = np.ascontiguousarray(inp['norm1_g'][l].reshape(8, 128).T)
        m['wqb' + s] = np.ascontiguousarray(inp['w_qb'][l][:, h * 96:(h + 1) * 96])
        m['gqa' + s] = np.ascontiguousarray(inp['g_qa'][l].reshape(2, 128).T)
        m['wkvb' + s] = np.ascontiguousarray(inp['w_kvb'][l][:, h * 128:(h + 1) * 128])
        m['gkva' + s] = np.ascontiguousarray(inp['g_kva'][l].reshape(2, 128).T)
        gs, gd = inp['qk_g_sw'][l], inp['qk_g_dil'][l]
        m['g32' + s] = np.concatenate([gs[0], gd[0], gd[0], gs[1], gd[1], gd[1], gs[0], gd[0], gd[1]]).astype(np.float32)
        m['g96' + s] = np.concatenate([inp['qk_g_mla'][l][0], inp['qk_g_mla'][l][1]]).astype(np.float32)
        m['sinks' + s] = np.ascontiguousarray(inp['sinks'][l][2 * h:2 * h + 2].reshape(1, 2))
        m['wgb' + s] = np.ascontiguousarray(inp['w_gate_b'][l])
        m['bgate' + s] = np.ascontiguousarray(inp['b_gate'][l].reshape(32, 128).T)
        wb = inp['w_branch'][l]
        m['wbrc' + s] = np.ascontiguousarray(np.concatenate([wb[h * 64:(h + 1) * 64], wb[256 + h * 64:256 + (h + 1) * 64],
                                                             wb[512 + h * 64:512 + (h + 1) * 64], wb[768 + h * 32:768 + (h + 1) * 32]], axis=0))
        m['g2' + s] = np.ascontiguousarray(inp['norm2_g'][l])
        m['wout' + s] = np.ascontiguousarray(inp['w_out'][l])
    m['wgu0'] = np.ascontiguousarray(inp['w_gu_dense'][0])
    m['wdn0'] = np.ascontiguousarray(inp['w_down_dense'][0])
    if n_layers > 1:
        m['wr1'] = np.ascontiguousarray(inp['w_router'][0])
        m['br1'] = np.ascontiguousarray(inp['b_router'][0])
        m['wgu1'] = np.ascontiguousarray(inp['w_gu_exp'][0])
        m['wdn1'] = np.ascontiguousarray(inp['w_down_exp'][0])
    return m


def kernel_fused(inp, S=SEQ, n_layers=2):
    cA = consts_A()
    ncF, _ = _prog(('F', S, n_layers), lambda: build_fused(S, n_layers))
    in_maps = [inputs_fused(cA, inp, c, S, n_layers) for c in range(8)]
    res = run_bass_kernel_spmd(ncF, in_maps, core_ids=list(range(8)))
    NTOK = S // 4
    out = np.empty((BATCH, S, D), np.float32)
    for c in range(8):
        out[c // 4, (c % 4) * NTOK:(c % 4 + 1) * NTOK] = np.asarray(res.results[c]['final'])
    return out
```

```python
import math
from contextlib import ExitStack

import numpy as np
import ml_dtypes

import concourse.bass as bass
import concourse.mybir as mybir
from concourse.bass_utils import run_bass_kernel_spmd

F32 = mybir.dt.float32
BF16 = mybir.dt.bfloat16
U8 = mybir.dt.uint8
AF = mybir.ActivationFunctionType
ALU = mybir.AluOpType
AX = mybir.AxisListType
ENGS = ['sync', 'scalar', 'vector', 'gpsimd', 'tensor']

D = 1024
SEQ = 16384
BATCH = 2
EPS = 1e-6
NEG = -30000.0
O_AQ, O_AK, O_AV, O_CQ, O_CKV, O_KPE, O_SWQ, O_SWK, O_SWV, O_DQ, O_DK, O_DV = (
    0, 256, 512, 768, 1024, 1280, 1312, 1568, 1632, 1696, 2080, 2464)
DILS = (1, 4, 16)
STQ = 'sync'
LIMIT = 10 ** 12
SKIP = ()


class Prog:
    def __init__(self, nc, es):
        self.nc, self.es = nc, es
        self.ops = {e: [] for e in ENGS}
        self.sems, self.cnt = {}, {}
        self.known = {e: {} for e in ENGS}
        self.lastw, self.readers = {}, {}
        self.n_instr = 0
        for e in ENGS:
            self._mksem('E_' + e)

    def _mksem(self, name):
        if name not in self.sems:
            self.sems[name] = self.es.enter_context(self.nc.semaphore(name))
            self.cnt[name] = 0
        return self.sems[name]

    def _deps(self, reads, writes):
        deps = {}

        def add(d):
            if d is not None and deps.get(d[0], 0) < d[1]:
                deps[d[0]] = d[1]
        for k in reads:
            add(self.lastw.get(k))
        for k in writes:
            add(self.lastw.get(k))
            for s, v in self.readers.get(k, {}).items():
                add((s, v))
        return deps

    def _waits(self, eng, deps):
        for s, v in deps.items():
            if eng == 'tensor' and s == 'E_tensor':
                continue
            if self.known[eng].get(s, 0) >= v:
                continue
            self.known[eng][s] = v
            h = self.sems[s]
            self.ops[eng].append(lambda e, h=h, v=v: e.wait_ge(h, v))
            self.n_instr += 1

    def _record(self, s, v, reads, writes):
        for k in writes:
            self.lastw[k] = (s, v)
            self.readers[k] = {}
        for k in reads:
            self.readers.setdefault(k, {})[s] = v

    def op(self, eng, fn, reads=(), writes=()):
        self.n_ops = getattr(self, 'n_ops', 0) + 1
        if self.n_ops > LIMIT or self.n_ops in SKIP:
            return
        self._waits(eng, self._deps(reads, writes))
        s = 'E_' + eng
        self.cnt[s] += 1
        h = self.sems[s]
        self.ops[eng].append(lambda e, fn=fn, h=h: fn(e).then_inc(h, 1))
        self._record(s, self.cnt[s], reads, writes)
        self.n_instr += 1

    def dma(self, q, out, in_, reads, writes, slot):
        self.n_ops = getattr(self, 'n_ops', 0) + 1
        if self.n_ops > LIMIT:
            return
        self._waits(q, self._deps(reads, writes))
        s = 'D_' + slot
        h = self._mksem(s)
        self.cnt[s] += 16
        self.ops[q].append(lambda e, h=h, out=out, in_=in_: e.dma_start(out=out, in_=in_).then_inc(h, 16))
        self._record(s, self.cnt[s], reads, writes)
        self.n_instr += 1

    def cc(self, kind, op, groups, in_, out, reads, writes):
        self._waits('gpsimd', self._deps(reads, writes))
        s = 'C_all'
        h = self._mksem(s)
        self.cnt[s] += 1
        self.ops['gpsimd'].append(lambda e, h=h: e.collective_compute(kind, op, replica_groups=groups, ins=[in_], outs=[out]).then_inc(h, 1))
        self._record(s, self.cnt[s], reads, writes)
        self.n_instr += 1

    def barrier(self, exclude_cc=False, keep=()):
        allv = {s: v for s, v in self.cnt.items() if v > 0 and not (exclude_cc and s == 'C_all')}
        for e in ENGS:
            self._waits(e, allv)
        kept = {k: v for k, v in self.lastw.items() if isinstance(k, tuple) and k[0] in keep}
        self.lastw, self.readers = kept, {}

    def finish(self, block):
        for name in ENGS:
            def body(e, name=name):
                for f in self.ops[name]:
                    f(e)
            getattr(block, name)(body)

    def mm(self, out, lhsT, rhs, start, stop, reads, writes, skip=False):
        self.op('tensor', lambda e: e.matmul(out, lhsT, rhs, start=start, stop=stop, skip_group_check=skip), reads, writes)

    def tr(self, out, in_, ident, reads, writes):
        self.op('tensor', lambda e: e.transpose(out, in_, ident), reads, writes)

    def act(self, out, in_, func, reads, writes, bias=None, scale=None, accum_out=None):
        kw = {}
        if bias is not None:
            kw['bias'] = bias
        if scale is not None:
            kw['scale'] = scale
        if accum_out is not None:
            kw['accum_out'] = accum_out
        self.op('scalar', lambda e: e.activation(out=out, in_=in_, func=func, **kw), reads, writes)

    def tt(self, eng, out, in0, in1, op, reads, writes):
        self.op(eng, lambda e: e.tensor_tensor(out=out, in0=in0, in1=in1, op=op), reads, writes)

    def ts(self, eng, out, in0, s1, op0, reads, writes, s2=None, op1=None):
        if op1 is None:
            self.op(eng, lambda e: e.tensor_scalar(out=out, in0=in0, scalar1=s1, scalar2=None, op0=op0), reads, writes)
        else:
            self.op(eng, lambda e: e.tensor_scalar(out=out, in0=in0, scalar1=s1, scalar2=s2, op0=op0, op1=op1), reads, writes)

    def stt(self, out, in0, scalar, in1, op0, op1, reads, writes):
        self.op('vector', lambda e: e.scalar_tensor_tensor(out=out, in0=in0, scalar=scalar, in1=in1, op0=op0, op1=op1), reads, writes)

    def copy(self, eng, out, in_, reads, writes):
        if eng == 'scalar':
            self.op(eng, lambda e: e.copy(out=out, in_=in_), reads, writes)
        else:
            self.op(eng, lambda e: e.tensor_copy(out=out, in_=in_), reads, writes)

    def recip(self, out, in_, reads, writes):
        self.op('vector', lambda e: e.reciprocal(out=out, in_=in_), reads, writes)

    def memset(self, eng, ap, val, writes):
        self.op(eng, lambda e: e.memset(ap, val), (), writes)


class Arena:
    def __init__(self, nc, es, nbytes):
        self.big = es.enter_context(nc.sbuf_tensor("arena", [128, nbytes], U8))
        self.nbytes = nbytes
        self.off = 0

    def reset(self):
        self.off = 0

    def __call__(self, name, shape, dt):
        esz = 2 if dt == BF16 else 4
        n = int(np.prod(shape[1:]))
        nb = (n * esz + 63) // 64 * 64
        assert self.off + nb <= self.nbytes, (name, self.off, nb)
        ap = self.big[:, self.off:self.off + n * esz].bitcast(dt)
        self.off += nb
        if len(shape) > 2:
            names = " ".join("d%d" % i for i in range(len(shape) - 1))
            ap = ap.rearrange("p (%s) -> p %s" % (names, names), **{"d%d" % i: shape[i + 1] for i in range(len(shape) - 2)})
        if shape[0] < 128:
            ap = ap[0:shape[0]]
        return ap


def PK(i):
    return ('ps', i)


def core_cols(h):
    r = lambda o, n: list(range(o, o + n))
    swq0 = r(O_SWQ + (2 * h) * 32, 32)
    swq1 = r(O_SWQ + (2 * h + 1) * 32, 32)
    swk = r(O_SWK + (h // 2) * 32, 32)
    swv = r(O_SWV + (h // 2) * 32, 32)
    dq = [r(O_DQ + (g * 4 + h) * 32, 32) for g in range(3)]
    dk = [r(O_DK + (g * 4 + h) * 32, 32) for g in range(3)]
    dv = [r(O_DV + (g * 4 + h) * 32, 32) for g in range(3)]
    g1 = r(O_CQ, 256) + r(O_CKV, 256)
    n32 = swq0 + dq[0] + dq[1] + swk + dk[0] + dk[1] + swq1 + dq[2] + dk[2]
    g2 = n32 + r(O_AQ + h * 64, 64) + r(O_AK + h * 64, 64) + r(O_KPE, 32) + r(O_AV + h * 64, 64)
    g3 = swv + dv[0] + dv[1] + dv[2]
    return np.array(g1 + g2 + g3)


def emit_inproj(p, sb, PS, d, S):
    NT = S // 128
    identb = sb("identb", [128, 128], BF16)
    p.dma('sync', identb, d['ident'][:, :], [], ['identb'], 'identb')
    g1t = sb("g1t", [128, 8], F32)
    p.dma('sync', g1t, d['g1'][:, :], [], ['g1t'], 'g1t')
    gqat = sb("gqat", [128, 2], F32)
    p.dma('sync', gqat, d['gqa'][:, :], [], ['gqat'], 'gqat')
    gkvat = sb("gkvat", [128, 2], F32)
    p.dma('sync', gkvat, d['gkva'][:, :], [], ['gkvat'], 'gkvat')
    G32 = sb("G32", [128, 288], F32)
    p.dma('sync', G32, d['g32'].partition_broadcast(128), [], ['G32'], 'G32')
    G96 = sb("G96", [128, 2, 96], F32)
    p.dma('sync', G96.rearrange("p a d -> p (a d)"), d['g96'].partition_broadcast(128), [], ['G96'], 'G96')
    COS = sb("COS", [128, NT, 16], F32)
    SIN = sb("SIN", [128, NT, 16], F32)
    p.dma('sync', COS, d['cos'][:, 0:NT, :], [], ['COS'], 'COS')
    p.dma('sync', SIN, d['sin'][:, 0:NT, :], [], ['SIN'], 'SIN')
    Wb = sb("Wb", [128, 8, 1280], BF16)
    wst = [sb("wst%d" % i, [128, 1280], F32) for i in range(2)]
    for kc in range(8):
        b = kc % 2
        p.dma('sync', wst[b], d['w_in'][kc * 128:(kc + 1) * 128, :], [], [('wst', b)], 'wst%d' % b)
        p.act(Wb[:, kc, :], wst[b], AF.Copy, [('wst', b), 'g1t'], ['Wb'], scale=g1t[:, kc:kc + 1])
    Wqb = sb("Wqb", [128, 2, 96], BF16)
    Wkvb = sb("Wkvb", [128, 2, 128], BF16)
    for i in range(2):
        p.dma('sync', wst[0][:, 0:96], d['wqb'][i * 128:(i + 1) * 128, :], [], [('wst', 0)], 'wst0')
        p.act(Wqb[:, i, :], wst[0][:, 0:96], AF.Copy, [('wst', 0), 'gqat'], ['Wqb'], scale=gqat[:, i:i + 1])
        p.dma('sync', wst[1][:, 0:128], d['wkvb'][i * 128:(i + 1) * 128, :], [], [('wst', 1)], 'wst1')
        p.act(Wkvb[:, i, :], wst[1][:, 0:128], AF.Copy, [('wst', 1), 'gkvat'], ['Wkvb'], scale=gkvat[:, i:i + 1])

    X = [sb("x%d" % i, [128, 1024], F32) for i in range(2)]
    junk = sb("junk", [128, 1024], BF16)
    hb = [sb("hb%d" % i, [128, 1024], BF16) for i in range(2)]
    hT = [sb("hT%d" % i, [128, 8, 128], BF16) for i in range(2)]
    st = sb("stats", [128, 32], F32)
    cb = sb("cb", [128, 512], BF16)
    cT = sb("cT", [128, 4, 128], BF16)
    qk96 = sb("qk96", [128, 2, 96], F32)
    tmp96 = sb("tmp96", [128, 2, 96], F32)
    qkb = sb("qkb", [128, 2, 96], BF16)
    rA = sb("ropeA", [128, 2, 2, 16], F32)
    rB = sb("ropeB", [128, 2, 2, 16], F32)
    sq32 = sb("sq32", [128, 288], F32)
    tmp32 = sb("tmp32", [128, 288], F32)
    n32b = sb("n32b", [128, 288], BF16)
    sbqk = sb("sbqk", [128, 128], BF16)
    stageT = [sb("stageT%d" % i, [128, 8, 512], BF16) for i in range(2)]
    stVS = [sb("stVS%d" % i, [128, 4, 64], BF16) for i in range(2)]
    stVM = [sb("stVM%d" % i, [128, 4, 65], BF16) for i in range(2)]
    stVG = [sb("stVG%d" % i, [128, 4, 4, 33], BF16) for i in range(2)]
    stGL = [sb("stGL%d" % i, [128, 512], BF16) for i in range(2)]
    glb = sb("glb", [128, 128], BF16)
    for i in range(2):
        p.memset('gpsimd', stageT[i], 0.0, [('stageT', i)])
        p.memset('gpsimd', stVM[i], 1.0, [('stVM', i)])
        p.memset('gpsimd', stVG[i], 1.0, [('stVG', i)])
    psTb = PS[0].bitcast(BF16)
    ps1, ps2, ps3 = PS[1], PS[2], PS[3]
    ps4b = PS[4].bitcast(BF16)
    ps5 = PS[5]
    ps6b = PS[6].bitcast(BF16)
    xrows = d['xrows']

    xkeys = d.get('xkeys', lambda t: [])
    def norm(t):
        b = t % 2
        xk, hbk = ('x', b), ('hb', b)
        if t + 1 < NT:
            p.dma('sync', X[1 - b], xrows(t + 1), xkeys(t + 1), [('x', 1 - b)], 'x%d' % (1 - b))
        p.act(junk, X[b], AF.Square, [xk], ['junk', 'ssq'], accum_out=st[:, 0:1])
        p.act(st[:, 1:2], st[:, 0:1], AF.Sqrt, ['ssq'], ['rs'], scale=1.0 / D, bias=EPS)
        p.recip(st[:, 2:3], st[:, 1:2], ['rs'], ['rstd'])
        p.act(hb[b], X[b], AF.Copy, [xk, 'rstd'], [hbk], scale=st[:, 2:3])

    p.dma('sync', X[0], xrows(0), xkeys(0), [('x', 0)], 'x0')
    norm(0)
    for t in range(NT):
        b = t % 2
        tt = t % 4
        sg = (t // 4) % 2
        xk, hbk, hTk = ('x', b), ('hb', b), ('hT', b)
        for kc in range(8):
            p.tr(psTb[:, kc * 128:(kc + 1) * 128], hb[b][:, kc * 128:(kc + 1) * 128], identb, [hbk, 'identb'], [PK(0)])
        p.copy('vector', hT[b].rearrange("p a d -> p (a d)"), psTb, [PK(0)], [hTk])
        for (ps, key, c0, c1) in ((ps1, PK(1), 0, 512), (ps2, PK(2), 512, 1024), (ps3, PK(3), 1024, 1280)):
            for kc in range(8):
                p.mm(ps[:, 0:c1 - c0], hT[b][:, kc, :], Wb[:, kc, c0:c1], kc == 0, kc == 7, [hTk, 'Wb'], [key])
        p.act(junk[:, 0:256], ps1[:, 0:256], AF.Square, [PK(1)], ['junk', 'ssq2a'], accum_out=st[:, 3:4])
        p.act(junk[:, 0:256], ps1[:, 256:512], AF.Square, [PK(1)], ['junk', 'ssq2b'], accum_out=st[:, 4:5])
        p.act(st[:, 5:7], st[:, 3:5], AF.Sqrt, ['ssq2a', 'ssq2b'], ['rs2'], scale=1.0 / 256, bias=EPS)
        p.recip(st[:, 7:9], st[:, 5:7], ['rs2'], ['rstd2'])
        p.copy('vector', cb, ps1, [PK(1)], ['cb'])
        if t + 1 < NT:
            norm(t + 1)
        for i in range(4):
            p.tr(ps4b[:, i * 128:(i + 1) * 128], cb[:, i * 128:(i + 1) * 128], identb, ['cb', 'identb'], [PK(4)])
        p.copy('vector', cT.rearrange("p a d -> p (a d)"), ps4b[:, 0:512], [PK(4)], ['cT'])
        for i in range(2):
            p.mm(ps5[:, 0:96], cT[:, i, :], Wqb[:, i, :], i == 0, i == 1, ['cT', 'Wqb'], [PK(5)])
        for i in range(2):
            p.mm(ps5[:, 128:256], cT[:, 2 + i, :], Wkvb[:, i, :], i == 0, i == 1, ['cT', 'Wkvb'], [PK(5)])
        p.act(qk96[:, 0, :], ps5[:, 0:96], AF.Copy, [PK(5), 'rstd2'], ['qk96'], scale=st[:, 7:8])
        p.act(qk96[:, 1, 0:64], ps5[:, 128:192], AF.Copy, [PK(5), 'rstd2'], ['qk96'], scale=st[:, 8:9])
        p.act(stVM[sg][:, tt, 0:64], ps5[:, 192:256], AF.Copy, [PK(5), 'rstd2'], [('stVM', sg)], scale=st[:, 8:9])
        p.copy('vector', qk96[:, 1, 64:96], ps2[:, 416:448], [PK(2)], ['qk96'])
        R = qk96[:, :, 64:96].rearrange("p a (h d) -> p a h d", h=2)
        cosb = COS[:, t, :].unsqueeze(1).unsqueeze(1).broadcast_to([128, 2, 2, 16])
        sinb = SIN[:, t, :].unsqueeze(1).broadcast_to([128, 2, 16])
        p.tt('vector', rA, R, cosb, ALU.mult, ['qk96', 'COS'], ['rA'])
        p.tt('vector', rB[:, :, 0, :], R[:, :, 1, :], sinb, ALU.mult, ['qk96', 'SIN'], ['rB'])
        p.tt('vector', rB[:, :, 1, :], R[:, :, 0, :], sinb, ALU.mult, ['qk96', 'SIN'], ['rB'])
        p.tt('vector', R[:, :, 0, :], rA[:, :, 0, :], rB[:, :, 0, :], ALU.subtract, ['rA', 'rB'], ['qk96'])
        p.tt('vector', R[:, :, 1, :], rA[:, :, 1, :], rB[:, :, 1, :], ALU.add, ['rA', 'rB'], ['qk96'])
        p.tt('vector', tmp96, qk96, qk96, ALU.mult, ['qk96'], ['tmp96'])
        p.op('vector', lambda e: e.tensor_reduce(out=st[:, 9:11], in_=tmp96, axis=AX.X, op=ALU.add), ['tmp96'], ['ssq96'])
        p.act(st[:, 11:13], st[:, 9:11], AF.Sqrt, ['ssq96'], ['rs96'], scale=1.0 / 96, bias=EPS)
        p.recip(st[:, 13:15], st[:, 11:13], ['rs96'], ['rstd96'])
        p.tt('vector', tmp96, qk96, G96, ALU.mult, ['qk96', 'G96'], ['tmp96'])
        p.tt('vector', qkb, tmp96, st[:, 13:15].unsqueeze(2).broadcast_to([128, 2, 96]), ALU.mult, ['tmp96', 'rstd96'], ['qkb'])
        p.act(sq32, ps2[:, 0:288], AF.Square, [PK(2)], ['sq32'])
        p.op('vector', lambda e: e.tensor_reduce(out=st[:, 15:24], in_=sq32.rearrange("p (a d) -> p a d", d=32), axis=AX.X, op=ALU.add), ['sq32'], ['ssq32'])
        p.act(st[:, 15:24], st[:, 15:24], AF.Sqrt, ['ssq32'], ['ssq32'], scale=1.0 / 32, bias=EPS)
        p.recip(st[:, 15:24], st[:, 15:24], ['ssq32'], ['ssq32'])
        p.tt('vector', tmp32, ps2[:, 0:288], G32, ALU.mult, [PK(2), 'G32'], ['tmp32'])
        p.tt('vector', n32b.rearrange("p (a d) -> p a d", d=32), tmp32.rearrange("p (a d) -> p a d", d=32),
             st[:, 15:24].unsqueeze(2).broadcast_to([128, 9, 32]), ALU.mult, ['tmp32', 'ssq32'], ['n32b'])
        p.ts('vector', sbqk[:, 0:64], ps2[:, 288:352], 0.125, ALU.mult, [PK(2)], ['sbqk'])
        p.copy('vector', sbqk[:, 64:128], ps2[:, 352:416], [PK(2)], ['sbqk'])
        p.copy('vector', stVS[sg][:, tt, :], ps2[:, 448:512], [PK(2)], [('stVS', sg)])
        p.copy('vector', stVG[sg][:, tt, :, 0:32], ps3[:, 0:128].rearrange("p (a d) -> p a d", d=32), [PK(3)], [('stVG', sg)])
        p.copy('vector', glb, ps3[:, 128:256], [PK(3)], ['glb'])
        p.tr(ps4b[:, 512:640], glb, identb, ['glb', 'identb'], [PK(4)])
        p.copy('vector', stGL[sg][:, tt * 128:(tt + 1) * 128], ps4b[:, 512:640], [PK(4)], [('stGL', sg)])
        p.tr(ps6b[0:64, 0:128], sbqk[:, 0:64], identb, ['sbqk', 'identb'], [PK(6)])
        p.tr(ps6b[0:64, 128:256], sbqk[:, 64:128], identb, ['sbqk', 'identb'], [PK(6)])
        p.tr(ps6b[0:96, 256:384], qkb[:, 0, :], identb, ['qkb', 'identb'], [PK(6)])
        p.tr(ps6b[0:96, 384:512], qkb[:, 1, :], identb, ['qkb', 'identb'], [PK(6)])
        p.tr(ps6b[0:96, 512:640], n32b[:, 0:96], identb, ['n32b', 'identb'], [PK(6)])
        p.tr(ps6b[0:96, 640:768], n32b[:, 96:192], identb, ['n32b', 'identb'], [PK(6)])
        p.tr(ps6b[0:64, 768:896], n32b[:, 192:256], identb, ['n32b', 'identb'], [PK(6)])
        p.tr(ps6b[0:64, 896:1024], n32b[:, 224:288], identb, ['n32b', 'identb'], [PK(6)])
        p.copy('vector', stageT[sg][0:96, 2:6, tt * 128:(tt + 1) * 128], ps6b[0:96, 256:768].rearrange("p (a d) -> p a d", d=128), [PK(6)], [('stageT', sg)])
        p.copy('vector', stageT[sg][0:64, 0:2, tt * 128:(tt + 1) * 128], ps6b[0:64, 0:256].rearrange("p (a d) -> p a d", d=128), [PK(6)], [('stageT', sg)])
        p.copy('vector', stageT[sg][0:64, 6:8, tt * 128:(tt + 1) * 128], ps6b[0:64, 768:1024].rearrange("p (a d) -> p a d", d=128), [PK(6)], [('stageT', sg)])
        if tt == 3:
            T0 = (t - 3) * 128
            j0 = t - 3
            p.dma(STQ, d['QKT'][0:96, :, T0:T0 + 512], stageT[sg][0:96], [('stageT', sg)], [('QKT', sg)], 'stageT%d' % sg)
            p.dma(STQ, d['GL'][:, T0:T0 + 512], stGL[sg], [('stGL', sg)], [('GL', sg)], 'stGL%d' % sg)
            p.dma(STQ, d['VSB'][:, j0:j0 + 4, :], stVS[sg], [('stVS', sg)], [('VSB', sg)], 'stVS%d' % sg)
            p.dma(STQ, d['VML'][:, j0:j0 + 4, :], stVM[sg], [('stVM', sg)], [('VML', sg)], 'stVM%d' % sg)
            p.dma(STQ, d['VSW'][:, j0:j0 + 4, :], stVG[sg][:, :, 0, :], [('stVG', sg)], [('VG', sg)], 'stVG%d' % sg)
            for g in range(3):
                p.dma(STQ, d['VD%d' % g][T0:T0 + 512, :].rearrange("(a p) d -> p a d", p=128), stVG[sg][:, :, 1 + g, :],
                      [('stVG', sg)], [('VG', sg)], 'stVG%d' % sg)


def load_cols(p, dst, dst_key, src, src_keys, slot, nsplit=4):
    n = src.shape[-1]
    w = n // nsplit
    for i in range(nsplit):
        p.dma('sync', dst[:, i * w:(i + 1) * w], src[:, i * w:(i + 1) * w], src_keys, [dst_key], slot)


def emit_sb(p, sb, PS, d, S, side=None):
    NT = S // 128
    QT = sb("sbQT", [64, S], BF16)
    KT = sb("sbKT", [64, S], BF16)
    V = sb("sbV", [128, NT, 64], BF16)
    load_cols(p, QT, 'sbQT', d['QKT'][0:64, 0, :], [], 'sbQT')
    load_cols(p, KT, 'sbKT', d['QKT'][0:64, 1, :], [], 'sbKT')
    p.dma('sync', V, d['VSB'][:, :, :], [], ['sbV'], 'sbV')
    triN = sb("triN", [128, 128], BF16)
    onesb = sb("onesb", [128, 128], BF16)
    maskS = sb("maskS", [128, 4, 512], BF16)
    p.dma('sync', triN, d['triN'][:, :], [], ['triN'], 'triN')
    p.dma('sync', onesb, d['onesb'][:, :], [], ['onesb'], 'onesb')
    p.dma('sync', maskS, d['maskS'][:, :, :], [], ['maskS'], 'maskS')
    e_t = [sb("sb_e%d" % i, [128, 1024], F32) for i in range(2)]
    sp_t = [sb("sb_sp%d" % i, [128, 1024], BF16) for i in range(2)]
    ex_t = [sb("sb_ex%d" % i, [128, 1024], F32) for i in range(2)]
    A_t = [sb("sb_A%d" % i, [128, 1024], BF16) for i in range(2)]
    Cb = sb("sb_Cb", [128, 512], F32)
    ost = [sb("sb_ost%d" % i, [64, 512], BF16) for i in range(2)]
    PSB = d['PSB']
    pairs = []
    for c in range(S // 512):
        js = list(range(4 * c + 3, -1, -1))
        for m in range(0, len(js), 2):
            pairs.append((c, m, js[m], js[m + 1]))
    NS = len(pairs)

    def stage1(pi):
        c, m, j0, j1 = pairs[pi]
        pb = pi % 2
        ek, spk = ('e', pb), ('sp', pb)
        for h, j in enumerate((j0, j1)):
            p.mm(PS[2 * pb + h], KT[:, j * 128:(j + 1) * 128], QT[:, c * 512:(c + 1) * 512], True, True, ['sbKT', 'sbQT'], [PK(2 * pb + h)])
        p.act(e_t[pb], PSB[pb], AF.Exp, [PK(2 * pb), PK(2 * pb + 1)], [ek])
        p.act(sp_t[pb], e_t[pb], AF.Ln, [ek], [spk], bias=1.0)
        for h, j in enumerate((j0, j1)):
            r = j - 4 * c
            if r >= 0:
                hs = slice(h * 512, (h + 1) * 512)
                p.tt('gpsimd', sp_t[pb][:, hs], sp_t[pb][:, hs], maskS[:, r, :], ALU.mult, [spk, 'maskS'], [spk])

    def stage2(pi):
        c, m, j0, j1 = pairs[pi]
        pb = pi % 2
        spk, exk, Ak = ('sp', pb), ('ex', pb), ('A', pb)
        for h, j in enumerate((j0, j1)):
            hs = slice(h * 512, (h + 1) * 512)
            zk, ck = PK(2 * pb + h), PK(4 + h)
            p.mm(PS[2 * pb + h], triN, sp_t[pb][:, hs], False, True, [spk, 'triN'], [zk], skip=True)
            p.mm(PS[4 + h], onesb, sp_t[pb][:, hs], True, True, [spk, 'onesb'], [ck])
            if m == 0 and h == 0:
                p.copy('vector', ex_t[pb][:, hs], PS[2 * pb + h], [zk], [exk])
                p.copy('vector', Cb, PS[4 + h], [ck], ['Cb'])
            else:
                p.tt('vector', ex_t[pb][:, hs], PS[2 * pb + h], Cb, ALU.subtract, [zk, 'Cb'], [exk])
                if j > 0:
                    p.tt('vector', Cb, PS[4 + h], Cb, ALU.add, [ck, 'Cb'], ['Cb'])
        p.act(A_t[pb], ex_t[pb], AF.Exp, [exk], [Ak])
        for h, j in enumerate((j0, j1)):
            r = j - 4 * c
            if r >= 0:
                hs = slice(h * 512, (h + 1) * 512)
                p.tt('gpsimd', A_t[pb][:, hs], A_t[pb][:, hs], maskS[:, r, :], ALU.mult, [Ak, 'maskS'], [Ak])

    def stage3(pi):
        c, m, j0, j1 = pairs[pi]
        pb = pi % 2
        ob = c % 2
        for h, j in enumerate((j0, j1)):
            hs = slice(h * 512, (h + 1) * 512)
            p.mm(PS[6 + ob][0:64, :], V[:, j, :], A_t[pb][:, hs], m == 0 and h == 0, j == 0, [('A', pb), 'sbV'], [PK(6 + ob)])
        if j1 == 0:
            p.copy('vector', ost[ob], PS[6 + ob][0:64, :], [PK(6 + ob)], [('ost', ob)])
            p.dma(STQ, d['OUT'][0:64, c * 512:(c + 1) * 512], ost[ob], [('ost', ob)], [('OUT', ob)], 'sb_ost%d' % ob)

    side_it = side(sb) if side is not None else None
    n_side = d.get('n_side', 0)
    every = max(1, NS // max(n_side, 1))
    for i in range(NS + 2):
        if i < NS:
            stage1(i)
        if 0 <= i - 1 < NS:
            stage2(i - 1)
        if 0 <= i - 2 < NS:
            stage3(i - 2)
        if side_it is not None and i % every == 0:
            next(side_it, None)
    if side_it is not None:
        for _ in side_it:
            pass


def emit_mla(p, sb, PS, d, S):
    NT = S // 128
    QT = sb("mlQT", [96, S], BF16)
    KT = sb("mlKT", [96, S], BF16)
    V = sb("mlV", [128, NT, 65], BF16)
    load_cols(p, QT, 'mlQT', d['QKT'][0:96, 2, :], [], 'mlQT')
    load_cols(p, KT, 'mlKT', d['QKT'][0:96, 3, :], [], 'mlKT')
    p.dma('sync', V, d['VML'][:, :, :], [], ['mlV'], 'mlV')
    maskI = sb("maskI", [128, 4, 512], BF16)
    p.dma('sync', maskI, d['maskI'][:, :, :], [], ['maskI'], 'maskI')
    onesf = sb("onesf", [128, 64], F32)
    p.dma('sync', onesf, d['onesf'][:, :], [], ['onesf'], 'onesf')
    P_t = [sb("ml_P%d" % i, [128, 1024], BF16) for i in range(3)]
    osb = sb("ml_osb", [128, 512], F32)
    rrow = sb("ml_rrow", [128, 512], F32)
    ost = [sb("ml_ost%d" % i, [64, 512], BF16) for i in range(2)]
    PSB = d['PSB']
    scale = 96 ** -0.5
    pairs = []
    for c in range(S // 512):
        js = list(range(4 * c + 3, -1, -1))
        for m in range(0, len(js), 2):
            pairs.append((c, m, js[m], js[m + 1]))
    NS = len(pairs)

    def stage1(pi):
        c, m, j0, j1 = pairs[pi]
        pb = pi % 3
        Pk = ('P', pb)
        for h, j in enumerate((j0, j1)):
            p.mm(PS[2 * pb + h], KT[:, j * 128:(j + 1) * 128], QT[:, c * 512:(c + 1) * 512], True, True, ['mlKT', 'mlQT'], [PK(2 * pb + h)])
        p.act(P_t[pb], PSB[pb], AF.Exp, [PK(2 * pb), PK(2 * pb + 1)], [Pk], scale=scale)
        for h, j in enumerate((j0, j1)):
            r = j - 4 * c
            if r >= 0:
                hs = slice(h * 512, (h + 1) * 512)
                p.tt('gpsimd', P_t[pb][:, hs], P_t[pb][:, hs], maskI[:, r, :], ALU.mult, [Pk, 'maskI'], [Pk])

    def stage2(pi):
        c, m, j0, j1 = pairs[pi]
        pb = pi % 3
        ob = c % 2
        oD, ok = PS[6], PK(6)
        for h, j in enumerate((j0, j1)):
            hs = slice(h * 512, (h + 1) * 512)
            p.mm(oD[0:65, :], V[:, j, :], P_t[pb][:, hs], m == 0 and h == 0, j == 0, [('P', pb), 'mlV'], [ok])
        if j1 == 0:
            p.copy('vector', osb[0:65, :], oD[0:65, :], [ok], ['ml_osb'])
            p.act(rrow[64:65, :], osb[64:65, :], AF.Ln, ['ml_osb'], ['ml_rrow'])
            p.act(rrow[64:65, :], rrow[64:65, :], AF.Exp, ['ml_rrow'], ['ml_rrow'], scale=-1.0)
            p.mm(PS[7][0:64, :], onesf[64:65, :], rrow[64:65, :], True, True, ['ml_rrow', 'onesf'], [PK(7)])
            p.tt('vector', ost[ob], osb[0:64, :], PS[7][0:64, :], ALU.mult, ['ml_osb', PK(7)], [('ml_ost', ob)])
            p.dma(STQ, d['OUT'][64:128, c * 512:(c + 1) * 512], ost[ob], [('ml_ost', ob)], [('OUT', 2 + ob)], 'ml_ost%d' % ob)

    for i in range(NS + 2):
        if i < NS:
            stage1(i)
        if 0 <= i - 2 < NS:
            stage2(i - 2)


def toeplitz(bmat, idx, rev=False):
    return bmat[idx, :, :]


def run_band_units(p, PS, units, t_sb, P_sb, scale):
    def front(ui):
        slots, Btile, Bkey, _ = units[ui]
        b = ui % 2
        s_ps, sk = PS[b], PK(b)
        for u, sl in enumerate(slots):
            p.mm(s_ps[:, u * 256:u * 256 + 128], sl['kc'], sl['q'], True, True, sl['rk'], [sk])
            p.mm(s_ps[:, u * 256 + 128:(u + 1) * 256], sl['kp'], sl['q'], True, True, sl['rk'], [sk])
        p.stt(t_sb[b], s_ps, scale, Btile, ALU.mult, ALU.add, [sk, Bkey], [('bt', b)])
        p.act(P_sb[b], t_sb[b], AF.Exp, [('bt', b)], [('bP', b)])

    def back(ui):
        slots, _, _, epi = units[ui]
        b = ui % 2
        o_ps, ok = PS[2 + b], PK(2 + b)
        for u, sl in enumerate(slots):
            p.mm(o_ps[0:33, u * 128:(u + 1) * 128], sl['vc'], P_sb[b][:, u * 256:u * 256 + 128], True, False, [('bP', b)] + sl['vk'], [ok])
            p.mm(o_ps[0:33, u * 128:(u + 1) * 128], sl['vp'], P_sb[b][:, u * 256 + 128:(u + 1) * 256], False, True, [('bP', b)] + sl['vk'], [ok])
        epi(o_ps, ok)

    n = len(units)
    for i in range(n + 1):
        if i < n:
            front(i)
        if i >= 1:
            back(i - 1)


def emit_sw(p, sb, PS, d, S):
    NT = S // 128
    Q = [sb("swQ%d" % i, [32, S], BF16) for i in range(2)]
    K = sb("swK", [32, S], BF16)
    V = sb("swV", [128, NT, 33], BF16)
    load_cols(p, Q[0], 'swQ0', d['QKT'][0:32, 4, :], [], 'swQ0')
    load_cols(p, Q[1], 'swQ1', d['QKT'][0:32, 6, :], [], 'swQ1')
    load_cols(p, K, 'swK', d['QKT'][0:32, 5, :], [], 'swK')
    p.dma('sync', V, d['VSW'][:, :, :], [], ['swV'], 'swV')
    B = sb("swB", [128, 2, 2, 128], F32)
    B0 = sb("swB0", [128, 2, 2, 128], F32)
    for h in range(2):
        for sel in range(2):
            p.dma('sync', B[:, h, sel, :], toeplitz(d['bvec'], h * 2 + sel), [], ['swB'], 'swB')
        p.dma('sync', B0[:, h, 0, :], toeplitz(d['bvec'], h * 2), [], ['swB0'], 'swB0')
        p.memset('gpsimd', B0[:, h, 1, :], NEG, ['swB0'])
    onesf = sb("onesf", [128, 64], F32)
    p.dma('sync', onesf, d['onesf'][:, :], [], ['onesf'], 'onesf')
    es = sb("sw_es", [128, 2], F32)
    p.dma('sync', es[32:33, :], d['sinks'][:, :], [], ['sw_es'], 'sw_es')
    p.act(es[32:33, :], es[32:33, :], AF.Exp, ['sw_es'], ['sw_es'])
    t_sb = [sb("bt%d" % i, [128, 512], F32) for i in range(2)]
    P_sb = [sb("bP%d" % i, [128, 512], BF16) for i in range(2)]
    osb = sb("sw_osb", [128, 2, 512], F32)
    rrow = sb("sw_rrow", [128, 2, 512], F32)
    ost = [sb("sw_ost%d" % i, [32, 2, 512], BF16) for i in range(2)]
    scale = 32 ** -0.5
    units = []
    for n in range(NT):
        cs = slice(n * 128, (n + 1) * 128)
        ps_ = slice(max(n - 1, 0) * 128, (max(n - 1, 0) + 1) * 128)
        slots = [dict(kc=K[:, cs], kp=K[:, ps_], q=Q[h][:, cs], vc=V[:, n, :], vp=V[:, max(n - 1, 0), :],
                      rk=['swK', 'swQ%d' % h], vk=['swV']) for h in range(2)]

        def epi(o_ps, ok, n=n):
            tt = n % 4
            p.copy('scalar', osb[0:33, :, tt * 128:(tt + 1) * 128], o_ps[0:33, 0:256].rearrange("p (a d) -> p a d", d=128), [ok], ['sw_osb'])
            if tt == 3:
                gi = (n // 4) % 2
                T0 = (n - 3) * 128
                for h in range(2):
                    p.act(rrow[32:33, h, :], osb[32:33, h, :], AF.Ln, ['sw_osb', 'sw_es'], ['sw_rrow'], bias=es[32:33, h:h + 1])
                    p.act(rrow[32:33, h, :], rrow[32:33, h, :], AF.Exp, ['sw_rrow'], ['sw_rrow'], scale=-1.0)
                    p.mm(PS[4 + h][0:32, :], onesf[32:33, 0:32], rrow[32:33, h, :], True, True, ['sw_rrow', 'onesf'], [PK(4 + h)])
                    p.tt('vector', ost[gi][:, h, :], osb[0:32, h, :], PS[4 + h][0:32, :], ALU.mult, ['sw_osb', PK(4 + h)], [('sw_ost', gi)])
                p.dma(STQ, d['OUT'][128:192, T0:T0 + 512].rearrange("(h p) t -> p h t", p=32), ost[gi], [('sw_ost', gi)], [('OUT', 4 + gi)], 'sw_ost%d' % gi)
        units.append((slots, (B0 if n == 0 else B).rearrange("p a b c -> p (a b c)"), 'swB0' if n == 0 else 'swB', epi))
    run_band_units(p, PS, units, t_sb, P_sb, scale)


def emit_dil(p, sb, PS, d, S):
    Qd = sb("dlQ", [96, S], BF16)
    Kd = sb("dlK", [96, S], BF16)
    Oacc = sb("dlO", [128, S], F32)
    onesf = sb("onesf", [128, 64], F32)
    p.dma('sync', onesf, d['onesf'][:, :], [], ['onesf'], 'onesf')
    Vg = sb("dlV", [128, S // 128, 33], BF16)
    Bt = [sb("dlB%d" % i, [128, 2, 2, 128], F32) for i in range(2)]
    t_sb = [sb("bt%d" % i, [128, 512], F32) for i in range(2)]
    P_sb = [sb("bP%d" % i, [128, 512], BF16) for i in range(2)]
    rrow = sb("dl_rrow", [128, 512], F32)
    ost = [sb("dl_ost%d" % i, [32, 512], BF16) for i in range(2)]
    scale = 32 ** -0.5
    src = [(4, 5, 32), (4, 5, 64), (6, 7, 32)]
    for g, dil in enumerate(DILS):
        qs_, ks_, pb = src[g]
        rows = slice(pb, pb + 32)
        M = S // dil
        nb = M // 128
        load_cols(p, Qd[rows], 'dlQ', d['QKT'][rows, qs_, :], [], 'dlQ')
        load_cols(p, Kd[rows], 'dlK', d['QKT'][rows, ks_, :], [], 'dlK')
        Vv = Vg.rearrange("p (r n) d -> p r n d", r=dil)
        vd = d['VD%d' % g]
        for r0 in range(dil):
            for n0 in range(0, nb, 16):
                nn = min(16, nb - n0)
                srcap = bass.AP(vd.tensor, r0 * 33 + n0 * 128 * dil * 33, [[dil * 33, 128], [128 * dil * 33, nn], [1, 33]])
                p.dma('sync', Vv[:, r0, n0:n0 + nn, :], srcap, [], ['dlV'], 'dlV')
        for v in range(2):
            for u in range(2):
                for sel in range(2):
                    if v == 1 and u == 0 and sel == 1:
                        p.memset('gpsimd', Bt[v][:, u, sel, :], NEG, [('dlB', v)])
                    else:
                        p.dma('sync', Bt[v][:, u, sel, :], toeplitz(d['bvec'], (2 + g) * 2 + sel), [], [('dlB', v)], 'dlB%d' % v)
        units = []
        for r in range(dil):
            for n in range(0, nb, 2):
                slots = []
                for u in range(2):
                    n1 = n + u
                    np_ = max(n1 - 1, 0)
                    qc = slice(r + dil * 128 * n1, r + dil * 128 * n1 + dil * 127 + 1, dil)
                    kc = slice(r + dil * 128 * np_, r + dil * 128 * np_ + dil * 127 + 1, dil)
                    slots.append(dict(kc=Kd[rows, qc], kp=Kd[rows, kc], q=Qd[rows, qc], vc=Vv[:, r, n1, :], vp=Vv[:, r, np_, :],
                                      rk=['dlK', 'dlQ'], vk=['dlV']))
                v = 1 if n == 0 else 0
                oc = slice(r + dil * 128 * n, r + dil * 128 * n + dil * 255 + 1, dil)

                def epi(o_ps, ok, oc=oc, g=g):
                    if g == 0:
                        p.copy('vector', Oacc[0:33, oc], o_ps[0:33, 0:256], [ok], ['dlO'])
                    else:
                        p.tt('vector', Oacc[0:33, oc], o_ps[0:33, 0:256], Oacc[0:33, oc], ALU.add, [ok, 'dlO'], ['dlO'])
                units.append((slots, Bt[v].rearrange("p a b c -> p (a b c)"), ('dlB', v), epi))
        run_band_units(p, PS, units, t_sb, P_sb, scale)
    for c in range(S // 512):
        gi = c % 2
        cs = slice(c * 512, (c + 1) * 512)
        p.act(rrow[32:33, :], Oacc[32:33, cs], AF.Ln, ['dlO'], ['dl_rrow'])
        p.act(rrow[32:33, :], rrow[32:33, :], AF.Exp, ['dl_rrow'], ['dl_rrow'], scale=-1.0)
        p.mm(PS[4 + gi][0:32, :], onesf[32:33, 0:32], rrow[32:33, :], True, True, ['dl_rrow', 'onesf'], [PK(4 + gi)])
        p.tt('vector', ost[gi], Oacc[0:32, cs], PS[4 + gi][0:32, :], ALU.mult, ['dlO', PK(4 + gi)], [('dl_ost', gi)])
        p.dma(STQ, d['OUT'][192:224, cs], ost[gi], [('dl_ost', gi)], [('OUT', 6 + gi)], 'dl_ost%d' % gi)


def build_A(S, phases=('inproj', 'sb', 'mla', 'sw', 'dil'), dbg=False):
    nc = bass.Bass("TRN2", target_bir_lowering=False)
    NT = S // 128
    d = {}

    def din(name, shape, dt=F32):
        d[name] = nc.dram_tensor(name, shape, dt, kind="ExternalInput").ap()

    din('x', [S, D])
    din('w_in', [D, 1280])
    din('g1', [128, 8])
    din('wqb', [256, 96])
    din('gqa', [128, 2])
    din('wkvb', [256, 128])
    din('gkva', [128, 2])
    din('g32', [288])
    din('g96', [192])
    din('cos', [128, SEQ // 128, 16])
    din('sin', [128, SEQ // 128, 16])
    din('ident', [128, 128], BF16)
    din('triN', [128, 128], BF16)
    din('onesb', [128, 128], BF16)
    din('maskS', [128, 4, 512], BF16)
    din('maskI', [128, 4, 512], BF16)
    din('onesf', [128, 64])
    din('bvec', [10, 128, 128])
    din('sinks', [1, 2])
    kind = "ExternalOutput" if dbg else "Internal"
    d['QKT'] = nc.dram_tensor('QKT', [128, 8, S], BF16, kind=kind).ap()
    d['VSB'] = nc.dram_tensor('VSB', [128, NT, 64], BF16, kind=kind).ap()
    d['VML'] = nc.dram_tensor('VML', [128, NT, 65], BF16, kind=kind).ap()
    d['VSW'] = nc.dram_tensor('VSW', [128, NT, 33], BF16, kind=kind).ap()
    for g in range(3):
        d['VD%d' % g] = nc.dram_tensor('VD%d' % g, [S, 33], BF16, kind=kind).ap()
    d['OUT'] = nc.dram_tensor('OUT', [224, S], BF16, kind="ExternalOutput").ap()
    with ExitStack() as es:
        sb = Arena(nc, es, 196 * 1024)
        PSB = [es.enter_context(nc.psum_tensor("psb%d" % i, [128, 1024], F32))[:, :] for i in range(4)]
        PS = [PSB[i // 2][:, (i % 2) * 512:(i % 2 + 1) * 512] for i in range(8)]
        d_psb = PSB
        p = Prog(nc, es)
        block = es.enter_context(nc.Block())
        d['xrows'] = lambda t: d['x'][t * 128:(t + 1) * 128, :]
        d['GL'] = nc.dram_tensor('GL', [128, S], BF16, kind="Internal").ap()
        d['PSB'] = d_psb
        emitters = dict(inproj=emit_inproj, sb=emit_sb, mla=emit_mla, sw=emit_sw, dil=emit_dil)
        for ph in phases:
            sb.reset()
            emitters[ph](p, sb, PS, d, S)
            p.barrier()
        p.finish(block)
    return nc, p


def bf(a):
    return np.ascontiguousarray(a).astype(ml_dtypes.bfloat16)


def t5_bucket_np(dist):
    dist = np.asarray(dist, np.int64)
    d_ = np.maximum(dist, 1).astype(np.float32)
    large = 16 + (np.log(d_ / np.float32(16)) / np.float32(math.log(2048 / 16)) * np.float32(16)).astype(np.int32)
    large = np.minimum(large, 31)
    return np.where(dist < 16, dist, large)


def consts_A():
    k = np.arange(128)
    c = {}
    c['ident'] = bf(np.eye(128, dtype=np.float32))
    c['triN'] = bf(-(k[:, None] >= k[None, :]).astype(np.float32))
    c['onesb'] = bf(np.ones((128, 128), np.float32))
    qi = np.arange(512)
    mS = np.zeros((128, 4, 512), np.float32)
    mI = np.zeros((128, 4, 512), np.float32)
    for r in range(4):
        mS[:, r, :] = (128 * r + k[:, None]) < qi[None, :]
        mI[:, r, :] = (128 * r + k[:, None]) <= qi[None, :]
    c['maskS'] = bf(mS)
    c['maskI'] = bf(mI)
    c['onesf'] = np.ones((128, 64), np.float32)
    half = 16
    inv = (10000.0 ** (-np.arange(half, dtype=np.float32) / half)).astype(np.float32)
    ang = np.arange(SEQ, dtype=np.float32)[:, None] * inv[None, :]
    c['cos'] = np.ascontiguousarray(np.cos(ang).astype(np.float32).reshape(SEQ // 128, 128, 16).transpose(1, 0, 2))
    c['sin'] = np.ascontiguousarray(np.sin(ang).astype(np.float32).reshape(SEQ // 128, 128, 16).transpose(1, 0, 2))
    return c


def band_bias_vecs(rel_bias, h):
    dd = np.arange(-127, 128)
    out = np.full((10, 255), NEG, np.float32)
    for i, hq in enumerate((2 * h, 2 * h + 1)):
        cur = dd >= 0
        out[2 * i, cur] = rel_bias[t5_bucket_np(dd[cur]), hq]
        prv = dd < 0
        out[2 * i + 1, prv] = rel_bias[t5_bucket_np(128 + dd[prv]), hq]
    for g, dil in enumerate(DILS):
        col = 8 + g * 4 + h
        cur = dd >= 0
        out[4 + 2 * g, cur] = rel_bias[t5_bucket_np(dd[cur] * dil), col]
        prv = dd <= 0
        out[5 + 2 * g, prv] = rel_bias[t5_bucket_np((128 + dd[prv]) * dil), col]
    return out


def inputs_A(c, l, b, h, inp, x_b, S):
    cols = core_cols(h)
    m = dict(c)
    m['x'] = np.ascontiguousarray(x_b[:S])
    m['w_in'] = np.ascontiguousarray(np.concatenate([inp['w_in'][l][:, cols], inp['w_gate_a'][l]], axis=1))
    m['g1'] = np.ascontiguousarray(inp['norm1_g'][l].reshape(8, 128).T)
    m['wqb'] = np.ascontiguousarray(inp['w_qb'][l][:, h * 96:(h + 1) * 96])
    m['gqa'] = np.ascontiguousarray(inp['g_qa'][l].reshape(2, 128).T)
    m['wkvb'] = np.ascontiguousarray(inp['w_kvb'][l][:, h * 128:(h + 1) * 128])
    m['gkva'] = np.ascontiguousarray(inp['g_kva'][l].reshape(2, 128).T)
    gs, gd = inp['qk_g_sw'][l], inp['qk_g_dil'][l]
    m['g32'] = np.concatenate([gs[0], gd[0], gd[0], gs[1], gd[1], gd[1], gs[0], gd[0], gd[1]]).astype(np.float32)
    m['g96'] = np.concatenate([inp['qk_g_mla'][l][0], inp['qk_g_mla'][l][1]]).astype(np.float32)
    bv = band_bias_vecs(inp['rel_bias'], h)
    kk = np.arange(128)
    m['bvec'] = np.ascontiguousarray(bv[:, kk[None, :] - kk[:, None] + 127])
    m['sinks'] = np.ascontiguousarray(inp['sinks'][l][2 * h:2 * h + 2].reshape(1, 2))
    return m


BR_CHUNKS = ((0, 1), (2, 3), (4, 5), (6,))


def castw_iter(p, sb, d, nblk, F, engs=('vector', 'gpsimd', 'scalar')):
    stg = [sb("cw_stg%d" % i, [128, 2 * F], F32) for i in range(2)]
    stb = [sb("cw_stb%d" % i, [128, 2 * F], BF16) for i in range(2)]
    it = 0
    for blk in range(nblk):
        for kc in range(8):
            b = it % 2
            rows = slice(kc * 128, (kc + 1) * 128)
            p.dma('sync', stg[b][:, 0:F], d['wg'](blk)[rows, :], [], [('cw_stg', b)], 'cw_stg%d' % b)
            p.dma('sync', stg[b][:, F:2 * F], d['wu'](blk)[rows, :], [], [('cw_stg', b)], 'cw_stg%d' % b)
            p.copy(engs[it % len(engs)], stb[b], stg[b], [('cw_stg', b)], [('cw_stb', b)])
            p.dma(STQ, d['WGU'][blk, :, kc, :], stb[b], [('cw_stb', b)], [('WGU', b)], 'cw_stb%d' % b)
            it += 1
            yield
        for fc in range(F // 128):
            b = it % 2
            p.dma('sync', stg[b][:, 0:1024], d['wd'](blk)[fc * 128:(fc + 1) * 128, :], [], [('cw_stg', b)], 'cw_stg%d' % b)
            p.copy(engs[it % len(engs)], stb[b][:, 0:1024], stg[b][:, 0:1024], [('cw_stg', b)], [('cw_stb', b)])
            p.dma(STQ, d['WD'][blk, :, fc, :], stb[b][:, 0:1024], [('cw_stb', b)], [('WD', b)], 'cw_stb%d' % b)
            it += 1
            yield


def emit_castw(p, sb, PS, d, nblk, F):
    for _ in castw_iter(p, sb, d, nblk, F):
        pass


def emit_B(p, sb, PS, d, NTOK, nblk, F, moe):
    NCH = NTOK // 512
    FC = F // 128
    identb = sb("identb", [128, 128], BF16)
    p.dma('sync', identb, d['ident'][:, :], [], ['identb'], 'identb')
    G1 = sb("G1", [128, 1024], F32)
    G2 = sb("G2", [128, 1024], F32)
    p.dma('sync', G1, d['g1'].partition_broadcast(128), [], ['G1'], 'G1')
    p.dma('sync', G2, d['g2'].partition_broadcast(128), [], ['G2'], 'G2')
    bgt = sb("bgt", [128, 32], F32)
    p.dma('sync', bgt, d['bgate'][:, :], [], ['bgt'], 'bgt')
    Wga = sb("Wga", [128, 8, 128], BF16)
    Wgb = sb("Wgb", [128, 4096], BF16)
    Wbr = sb("Wbr", [128, 7, 1024], BF16)
    Wout = sb("Wout", [128, 8, 1024], BF16)
    stg = [sb("w_stg%d" % i, [128, 1024], F32) for i in range(2)]
    engs = ['vector', 'gpsimd', 'scalar']
    it = 0

    def cast_in(dst, src, n):
        nonlocal it
        b = it % 2
        p.dma('sync', stg[b][:, 0:n], src, [], [('w_stg', b)], 'w_stg%d' % b)
        p.copy(engs[it % 3], dst, stg[b][:, 0:n], [('w_stg', b)], ['Wres'])
        it += 1
    for kc in range(8):
        cast_in(Wga[:, kc, :], d['wga'][kc * 128:(kc + 1) * 128, :], 128)
        cast_in(Wout[:, kc, :], d['wout'][kc * 128:(kc + 1) * 128, :], 1024)
    for i in range(4):
        cast_in(Wgb[:, i * 1024:(i + 1) * 1024], d['wgb'][:, i * 1024:(i + 1) * 1024], 1024)
    for rc in range(7):
        cast_in(Wbr[:, rc, :], d['wbr'][rc * 128:(rc + 1) * 128, :], 1024)
    if moe:
        identf = sb("identf", [128, 128], F32)
        p.dma('sync', identf, d['identf'][:, :], [], ['identf'], 'identf')
        Wr = sb("Wr", [128, 8, 8], F32)
        p.dma('sync', Wr, d['wr'].rearrange("(kc p) e -> p kc e", p=128), [], ['Wr'], 'Wr')
        brt = sb("brt", [128, 8], F32)
        p.dma('sync', brt, d['br'].partition_broadcast(128), [], ['brt'], 'brt')
        h2f = sb("h2f", [128, 1024], F32)
        h2fT = sb("h2fT", [128, 8, 128], F32)
        rt = sb("rt", [128, 64], F32)
        GW = sb("GW", [128, 4, 8], F32)
    X4 = sb("X4", [128, 4, 1024], F32)
    oTc = sb("oTc", [128, 7, 512], BF16)
    hT = sb("hT", [128, 8, 512], BF16)
    hb = sb("hb", [128, 1024], BF16)
    junk = sb("junk", [128, 1024], BF16)
    st = sb("st", [128, 8], F32)
    glT = sb("glT", [128, 512], BF16)
    gate = [sb("gate%d" % i, [128, 512], F32) for i in range(2)]
    tmpy = sb("tmpy", [128, 512], F32)
    yacc = sb("yacc", [128, 512], F32)
    yT = sb("yT", [128, 8, 512], BF16)
    sg = [sb("sg%d" % i, [128, 512], F32) for i in range(2)]
    WGU_t = [sb("WGU_t%d" % i, [128, 8, 2 * F], BF16) for i in range(2)]
    WD_t = [sb("WD_t%d" % i, [128, FC, 1024], BF16) for i in range(2)]
    psTb = PS[0].bitcast(BF16)
    x, out = d['x'], d['out']
    wit = 0

    def norm_T(tt, G, Gk):
        xt = X4[:, tt, :]
        p.act(junk, xt, AF.Square, ['X4'], ['junk', 'ssq'], accum_out=st[:, 0:1])
        p.act(st[:, 1:2], st[:, 0:1], AF.Sqrt, ['ssq'], ['rs'], scale=1.0 / D, bias=EPS)
        p.recip(st[:, 2:3], st[:, 1:2], ['rs'], ['rstd'])
        p.stt(hb, xt, st[:, 2:3], G, ALU.mult, ALU.mult, ['X4', 'rstd', Gk], ['hb'])
        for kc in range(8):
            p.tr(psTb[:, kc * 128:(kc + 1) * 128], hb[:, kc * 128:(kc + 1) * 128], identb, ['hb', 'identb'], [PK(0)])
        p.copy('vector', hT[:, :, tt * 128:(tt + 1) * 128], psTb.rearrange("p (a d) -> p a d", d=128), [PK(0)], ['hT'])

    for c in range(NCH):
        T0 = c * 512
        p.dma('sync', X4, x[T0:T0 + 512, :].rearrange("(t p) d -> p t d", p=128), [], ['X4'], 'X4')
        p.dma('sync', oTc, d['oT'][:, T0:T0 + 512].rearrange("(r p) t -> p r t", p=128), [], ['oTc'], 'oTc')
        for tt in range(4):
            norm_T(tt, G1, 'G1')
        for kc in range(8):
            p.mm(PS[1], Wga[:, kc, :], hT[:, kc, :], kc == 0, kc == 7, ['hT', 'Wres'], [PK(1)])
        p.copy('vector', glT, PS[1], [PK(1)], ['glT'])
        k = 0
        for oc in range(8):
            for i in range(4):
                b = k % 2
                k += 1
                gp, gk = PS[2 + b], PK(2 + b)
                bp, bk = PS[4 + b], PK(4 + b)
                col = i * 1024 + oc * 128
                p.mm(gp, Wgb[:, col:col + 128], glT, True, True, ['glT', 'Wres'], [gk])
                p.act(gate[b], gp, AF.Sigmoid, [gk, 'bgt'], [('gate', b)], bias=bgt[:, i * 8 + oc:i * 8 + oc + 1])
                rcs = BR_CHUNKS[i]
                for n_, rc in enumerate(rcs):
                    p.mm(bp, Wbr[:, rc, oc * 128:(oc + 1) * 128], oTc[:, rc, :], n_ == 0, n_ == len(rcs) - 1, ['oTc', 'Wres'], [bk])
                if i == 0:
                    p.tt('vector', yacc, gate[b], bp, ALU.mult, [('gate', b), bk], ['yacc'])
                else:
                    p.tt('vector', tmpy, gate[b], bp, ALU.mult, [('gate', b), bk], ['tmpy'])
                    if i < 3:
                        p.tt('gpsimd', yacc, yacc, tmpy, ALU.add, ['yacc', 'tmpy'], ['yacc'])
                    else:
                        p.tt('gpsimd', yT[:, oc, :], yacc, tmpy, ALU.add, ['yacc', 'tmpy'], ['yT'])
        k = 0
        for tt in range(4):
            for half in range(2):
                b = k % 2
                k += 1
                ps, pk = PS[6 + b], PK(6 + b)
                for oc in range(8):
                    p.mm(ps, yT[:, oc, tt * 128:(tt + 1) * 128], Wout[:, oc, half * 512:(half + 1) * 512], oc == 0, oc == 7, ['yT', 'Wres'], [pk])
                xs = X4[:, tt, half * 512:(half + 1) * 512]
                p.tt('vector', xs, ps, xs, ALU.add, [pk, 'X4'], ['X4'])
        for tt in range(4):
            norm_T(tt, G2, 'G2')
            if moe:
                p.stt(h2f, X4[:, tt, :], st[:, 2:3], G2, ALU.mult, ALU.mult, ['X4', 'rstd', 'G2'], ['h2f'])
                for kc in range(8):
                    bnk = 2 + kc // 4
                    p.tr(PS[bnk][:, (kc % 4) * 128:(kc % 4 + 1) * 128], h2f[:, kc * 128:(kc + 1) * 128], identf, ['h2f', 'identf'], [PK(bnk)])
                p.copy('vector', h2fT[:, 0:4, :].rearrange("p a d -> p (a d)"), PS[2], [PK(2)], ['h2fT'])
                p.copy('vector', h2fT[:, 4:8, :].rearrange("p a d -> p (a d)"), PS[3], [PK(3)], ['h2fT'])
                for kc in range(8):
                    p.mm(PS[1][:, 0:8], h2fT[:, kc, :], Wr[:, kc, :], kc == 0, kc == 7, ['h2fT', 'Wr'], [PK(1)])
                lg, m8, ee, mk = rt[:, 0:8], rt[:, 8:16], rt[:, 16:24], rt[:, 24:32]
                p.tt('vector', lg, PS[1][:, 0:8], brt, ALU.add, [PK(1), 'brt'], ['lg'])
                p.op('vector', lambda e, lg=lg, m8=m8: e.max(out=m8, in_=lg), ['lg'], ['m8'])
                p.ts('vector', rt[:, 32:33], m8[:, 0:1], -1.0, ALU.mult, ['m8'], ['negm'])
                p.act(ee, lg, AF.Exp, ['lg', 'negm'], ['ee'], bias=rt[:, 32:33])
                p.ts('vector', mk, lg, m8[:, 1:2], ALU.is_ge, ['lg', 'm8'], ['mk'])
                p.tt('vector', ee, ee, mk, ALU.mult, ['ee', 'mk'], ['ee'])
                p.op('vector', lambda e, ee=ee: e.tensor_reduce(out=rt[:, 33:34], in_=ee, axis=AX.X, op=ALU.add), ['ee'], ['den'])
                p.recip(rt[:, 34:35], rt[:, 33:34], ['den'], ['rden'])
                p.ts('vector', GW[:, tt, :], ee, rt[:, 34:35], ALU.mult, ['ee', 'rden'], ['GW'])
        aT = yT
        for blk in range(nblk):
            wb = wit % 2
            wit += 1
            p.dma('sync', WGU_t[wb], d['WGU'][blk], [], [('WGU_t', wb)], 'WGU_t%d' % wb)
            p.dma('sync', WD_t[wb], d['WD'][blk], [], [('WD_t', wb)], 'WD_t%d' % wb)
            for fc in range(FC):
                b = fc % 2
                gps, gk = PS[2 + b], PK(2 + b)
                ups, uk = PS[4 + b], PK(4 + b)
                for kc in range(8):
                    p.mm(gps, WGU_t[wb][:, kc, fc * 128:(fc + 1) * 128], hT[:, kc, :], kc == 0, kc == 7, ['hT', ('WGU_t', wb)], [gk])
                for kc in range(8):
                    p.mm(ups, WGU_t[wb][:, kc, F + fc * 128:F + (fc + 1) * 128], hT[:, kc, :], kc == 0, kc == 7, ['hT', ('WGU_t', wb)], [uk])
                p.act(sg[b], gps, AF.Silu, [gk], [('sg', b)])
                p.tt('vector', aT[:, fc, :], sg[b], ups, ALU.mult, [('sg', b), uk], ['yT'])
            k = 0
            for tt in range(4):
                for half in range(2):
                    b = k % 2
                    k += 1
                    ps, pk = PS[6 + b], PK(6 + b)
                    for fc in range(FC):
                        p.mm(ps, aT[:, fc, tt * 128:(tt + 1) * 128], WD_t[wb][:, fc, half * 512:(half + 1) * 512], fc == 0, fc == FC - 1, ['yT', ('WD_t', wb)], [pk])
                    xs = X4[:, tt, half * 512:(half + 1) * 512]
                    if moe:
                        p.stt(xs, ps, GW[:, tt, blk:blk + 1], xs, ALU.mult, ALU.add, [pk, 'GW', 'X4'], ['X4'])
                    else:
                        p.tt('vector', xs, ps, xs, ALU.add, [pk, 'X4'], ['X4'])
        p.dma(STQ, out[T0:T0 + 512, :].rearrange("(t p) d -> p t d", p=128), X4, ['X4'], ['out'], 'X4o')


def build_B(NTOK, moe):
    nc = bass.Bass("TRN2", target_bir_lowering=False)
    d = {}

    def din(name, shape, dt=F32):
        d[name] = nc.dram_tensor(name, shape, dt, kind="ExternalInput").ap()

    din('x', [NTOK, D])
    din('oT', [896, NTOK], BF16)
    din('ident', [128, 128], BF16)
    din('g1', [1024])
    din('g2', [1024])
    din('bgate', [128, 32])
    din('wga', [1024, 128])
    din('wgb', [128, 4096])
    din('wbr', [896, 1024])
    din('wout', [1024, 1024])
    if moe:
        nblk, F = 8, 768
        din('identf', [128, 128])
        din('wr', [1024, 8])
        din('br', [8])
        din('wgu', [8, 1024, 1536])
        din('wdn', [8, 768, 1024])
        d['wg'] = lambda blk: d['wgu'][blk, :, 0:768]
        d['wu'] = lambda blk: d['wgu'][blk, :, 768:1536]
        d['wd'] = lambda blk: d['wdn'][blk, :, :]
    else:
        nblk, F = 4, 512
        din('wgu', [1024, 4096])
        din('wdn', [2048, 1024])
        d['wg'] = lambda blk: d['wgu'][:, blk * 512:(blk + 1) * 512]
        d['wu'] = lambda blk: d['wgu'][:, 2048 + blk * 512:2048 + (blk + 1) * 512]
        d['wd'] = lambda blk: d['wdn'][blk * 512:(blk + 1) * 512, :]
    d['WGU'] = nc.dram_tensor('WGU', [nblk, 128, 8, 2 * F], BF16, kind="Internal").ap()
    d['WD'] = nc.dram_tensor('WD', [nblk, 128, F // 128, 1024], BF16, kind="Internal").ap()
    d['out'] = nc.dram_tensor('out', [NTOK, D], F32, kind="ExternalOutput").ap()
    with ExitStack() as es:
        sb = Arena(nc, es, 200 * 1024)
        PSB = [es.enter_context(nc.psum_tensor("psb%d" % i, [128, 1024], F32))[:, :] for i in range(4)]
        PS = [PSB[i // 2][:, (i % 2) * 512:(i % 2 + 1) * 512] for i in range(8)]
        d_psb = PSB
        p = Prog(nc, es)
        block = es.enter_context(nc.Block())
        emit_castw(p, sb, PS, d, nblk, F)
        p.barrier()
        sb.reset()
        emit_B(p, sb, PS, d, NTOK, nblk, F, moe)
        p.barrier()
        p.finish(block)
    return nc, p


def inputs_B(l, inp, x_tok, oT_tok, moe):
    m = {}
    m['x'] = np.ascontiguousarray(x_tok)
    m['oT'] = np.ascontiguousarray(oT_tok)
    m['ident'] = bf(np.eye(128, dtype=np.float32))
    m['g1'] = np.ascontiguousarray(inp['norm1_g'][l])
    m['g2'] = np.ascontiguousarray(inp['norm2_g'][l])
    m['bgate'] = np.ascontiguousarray(inp['b_gate'][l].reshape(32, 128).T)
    m['wga'] = np.ascontiguousarray(inp['w_gate_a'][l])
    m['wgb'] = np.ascontiguousarray(inp['w_gate_b'][l])
    m['wbr'] = np.ascontiguousarray(inp['w_branch'][l])
    m['wout'] = np.ascontiguousarray(inp['w_out'][l])
    if moe:
        m['identf'] = np.eye(128, dtype=np.float32)
        m['wr'] = np.ascontiguousarray(inp['w_router'][l // 2])
        m['br'] = np.ascontiguousarray(inp['b_router'][l // 2])
        m['wgu'] = np.ascontiguousarray(inp['w_gu_exp'][l // 2])
        m['wdn'] = np.ascontiguousarray(inp['w_down_exp'][l // 2])
    else:
        m['wgu'] = np.ascontiguousarray(inp['w_gu_dense'][l // 2])
        m['wdn'] = np.ascontiguousarray(inp['w_down_dense'][l // 2])
    return m


_CACHE = {}


def _prog(key, fn):
    if key not in _CACHE:
        _CACHE[key] = fn()
    return _CACHE[key]


def assemble_oT(outs):
    res = []
    for b in range(BATCH):
        rows = [None] * 4
        o = [np.asarray(outs[b * 4 + h]) for h in range(4)]
        sbp = np.concatenate([o[h][0:64] for h in range(4)], axis=0)
        mlp = np.concatenate([o[h][64:128] for h in range(4)], axis=0)
        swp = np.concatenate([o[h][128:192] for h in range(4)], axis=0)
        dlp = np.concatenate([o[h][192:224] for h in range(4)], axis=0)
        res.append(np.concatenate([sbp, mlp, swp, dlp], axis=0))
    return res


def kernel(**inp):
    inp = {k: np.asarray(v) for k, v in inp.items()}
    return kernel_fused(inp)


GROUPS = [[0, 1, 2, 3], [4, 5, 6, 7]]


def emit_merge(p, sb, PS, d, S):
    SL = S // 4
    bgt = sb("bgt", [128, 32], F32)
    p.dma('sync', bgt, d['bgate'][:, :], [], ['bgt'], 'bgt')
    Wgb = sb("Wgb", [128, 4096], BF16)
    Wbr = sb("Wbrc", [64, 4, 1024], BF16)
    stg = [sb("w_stg%d" % i, [128, 1024], F32) for i in range(2)]
    engs = ['vector', 'gpsimd', 'scalar']
    it = 0
    for i in range(4):
        b = it % 2
        p.dma('sync', stg[b], d['wgb'][:, i * 1024:(i + 1) * 1024], [], [('w_stg', b)], 'w_stg%d' % b)
        p.copy(engs[it % 3], Wgb[:, i * 1024:(i + 1) * 1024], stg[b], [('w_stg', b)], ['Wres'])
        it += 1
    rows = ((0, 64), (64, 64), (128, 64), (192, 32))
    for i, (r0, nr) in enumerate(rows):
        b = it % 2
        p.dma('sync', stg[b][0:nr, :], d['wbrc'][r0:r0 + nr, :], [], [('w_stg', b)], 'w_stg%d' % b)
        p.copy(engs[it % 3], Wbr[0:nr, i, :], stg[b][0:nr, :], [('w_stg', b)], ['Wres'])
        it += 1
    glT = [sb("glT%d" % i, [128, 512], BF16) for i in range(2)]
    oc_t = [sb("oc_t%d" % i, [64, 4, 512], BF16) for i in range(2)]
    gate = [sb("gate%d" % i, [128, 512], F32) for i in range(2)]
    tmpy2 = [sb("tmpy%d" % i, [128, 512], F32) for i in range(2)]
    yacc2 = [sb("yacc%d" % i, [128, 512], F32) for i in range(2)]
    yst = [sb("yst%d" % i, [128, 8, 512], F32) for i in range(2)]
    k = 0
    SLp = min(SL, 1024)
    NPc = SL // SLp
    order = [(sl * SL + q * SLp) // 512 + cc for q in range(NPc) for sl in range(4) for cc in range(SLp // 512)]
    for ci, c in enumerate(order):
        cb_ = ci % 2
        cs = slice(c * 512, (c + 1) * 512)
        p.dma('sync', glT[cb_], d['GL'][:, cs], [], [('glT', cb_)], 'glT%d' % cb_)
        for i, (r0, nr) in enumerate(rows):
            p.dma('sync', oc_t[cb_][0:nr, i, :], d['OUT'][r0:r0 + nr, cs], [], [('oc_t', cb_)], 'oc_t%d' % cb_)
        for oc in range(8):
            yacc, yk = yacc2[oc % 2], ('yacc', oc % 2)
            for i, (r0, nr) in enumerate(rows):
                b = k % 2
                k += 1
                gp, gk = PS[2 + b], PK(2 + b)
                bp, bk = PS[4 + b], PK(4 + b)
                col = i * 1024 + oc * 128
                p.mm(gp, Wgb[:, col:col + 128], glT[cb_], True, True, [('glT', cb_), 'Wres'], [gk])
                p.act(gate[b], gp, AF.Sigmoid, [gk, 'bgt'], [('gate', b)], bias=bgt[:, i * 8 + oc:i * 8 + oc + 1])
                p.mm(bp, Wbr[0:nr, i, oc * 128:(oc + 1) * 128], oc_t[cb_][0:nr, i, :], True, True, [('oc_t', cb_), 'Wres'], [bk])
                if i == 0:
                    p.tt('vector', yacc, gate[b], bp, ALU.mult, [('gate', b), bk], [yk])
                else:
                    tmpy, tk = tmpy2[b], ('tmpy', b)
                    p.tt('vector', tmpy, gate[b], bp, ALU.mult, [('gate', b), bk], [tk])
                    if i < 3:
                        p.tt('gpsimd', yacc, yacc, tmpy, ALU.add, [yk, tk], [yk])
                    else:
                        p.tt('gpsimd', yst[cb_][:, oc, :], yacc, tmpy, ALU.add, [yk, tk], [('yst', cb_)])
        sl, w = (c * 512) // SL, (c * 512) % SL
        q, t0 = w // SLp, w % SLp
        p.dma('sync', d['YP'][q, sl, :, t0:t0 + 512].rearrange("(oc p) t -> p oc t", p=128), yst[cb_], [('yst', cb_)], [('YP', cb_)], 'yst%d' % cb_)
        if (ci + 1) % (4 * (SLp // 512)) == 0:
            p.cc("ReduceScatter", ALU.add, GROUPS, d['YP'][q].rearrange("a c t -> (a c) t"), d['YS'][q], [('YP', 0), ('YP', 1)], [('YS', q)])


def emit_B2(p, sb, PS, d, NTOK, nblk, F, moe):
    NCH = NTOK // 512
    FC = F // 128
    identb = sb("identb", [128, 128], BF16)
    p.dma('sync', identb, d['ident'][:, :], [], ['identb'], 'identb')
    G2 = sb("G2", [128, 1024], F32)
    p.dma('sync', G2, d['g2'].partition_broadcast(128), [], ['G2'], 'G2')
    Wout = sb("Wout", [128, 8, 1024], BF16)
    stg = [sb("w_stg%d" % i, [128, 1024], F32) for i in range(2)]
    engs = ['vector', 'gpsimd', 'scalar']
    for kc in range(8):
        b = kc % 2
        p.dma('sync', stg[b], d['wout'][kc * 128:(kc + 1) * 128, :], [], [('w_stg', b)], 'w_stg%d' % b)
        p.copy(engs[kc % 3], Wout[:, kc, :], stg[b], [('w_stg', b)], ['Wres'])
    if moe:
        identf = sb("identf", [128, 128], F32)
        p.dma('sync', identf, d['identf'][:, :], [], ['identf'], 'identf')
        Wr = sb("Wr", [128, 8, 8], F32)
        p.dma('sync', Wr, d['wr'].rearrange("(kc p) e -> p kc e", p=128), [], ['Wr'], 'Wr')
        brt = sb("brt", [128, 8], F32)
        p.dma('sync', brt, d['br'].partition_broadcast(128), [], ['brt'], 'brt')
        h2f = sb("h2f", [128, 1024], F32)
        h2fT = sb("h2fT", [128, 8, 128], F32)
        rt = sb("rt", [128, 64], F32)
        GW = sb("GW", [128, 4, 8], F32)
    X4 = sb("X4", [128, 4, 1024], F32)
    yf = sb("yf", [128, 8, 512], F32)
    hT = sb("hT", [128, 8, 512], BF16)
    hb = sb("hb", [128, 1024], BF16)
    junk = sb("junk", [128, 1024], BF16)
    st = sb("st", [128, 8], F32)
    yT = sb("yT", [128, 8, 512], BF16)
    sg = [sb("sg%d" % i, [128, 512], F32) for i in range(2)]
    WGU_t = [sb("WGU_t%d" % i, [128, 8, 2 * F], BF16) for i in range(2)]
    WD_t = [sb("WD_t%d" % i, [128, FC, 1024], BF16) for i in range(2)]
    psTb = PS[0].bitcast(BF16)
    wit = 0

    def norm_T(tt, G, Gk):
        xt = X4[:, tt, :]
        p.act(junk, xt, AF.Square, ['X4'], ['junk', 'ssq'], accum_out=st[:, 0:1])
        p.act(st[:, 1:2], st[:, 0:1], AF.Sqrt, ['ssq'], ['rs'], scale=1.0 / D, bias=EPS)
        p.recip(st[:, 2:3], st[:, 1:2], ['rs'], ['rstd'])
        p.stt(hb, xt, st[:, 2:3], G, ALU.mult, ALU.mult, ['X4', 'rstd', Gk], ['hb'])
        for kc in range(8):
            p.tr(psTb[:, kc * 128:(kc + 1) * 128], hb[:, kc * 128:(kc + 1) * 128], identb, ['hb', 'identb'], [PK(0)])
        p.copy('vector', hT[:, :, tt * 128:(tt + 1) * 128], psTb.rearrange("p (a d) -> p a d", d=128), [PK(0)], ['hT'])

    for c in range(NCH):
        T0 = c * 512
        p.dma('sync', X4, d['xtok'][T0:T0 + 512, :].rearrange("(t p) d -> p t d", p=128), [], ['X4'], 'X4')
        SLp = min(NTOK, 1024)
        p.dma('sync', yf, d['YS'][T0 // SLp, :, T0 % SLp:T0 % SLp + 512].rearrange("(oc p) t -> p oc t", p=128), [('YS', T0 // SLp)], ['yf'], 'yf')
        p.copy('gpsimd', yT[:, 0:4, :], yf[:, 0:4, :], ['yf'], ['yT'])
        p.copy('vector', yT[:, 4:8, :], yf[:, 4:8, :], ['yf'], ['yT'])
        k = 0
        for tt in range(4):
            for half in range(2):
                b = k % 2
                k += 1
                ps, pk = PS[6 + b], PK(6 + b)
                for oc in range(8):
                    p.mm(ps, yT[:, oc, tt * 128:(tt + 1) * 128], Wout[:, oc, half * 512:(half + 1) * 512], oc == 0, oc == 7, ['yT', 'Wres'], [pk])
                xs = X4[:, tt, half * 512:(half + 1) * 512]
                p.tt('vector', xs, ps, xs, ALU.add, [pk, 'X4'], ['X4'])
        for tt in range(4):
            norm_T(tt, G2, 'G2')
            if moe:
                p.stt(h2f, X4[:, tt, :], st[:, 2:3], G2, ALU.mult, ALU.mult, ['X4', 'rstd', 'G2'], ['h2f'])
                for kc in range(8):
                    bnk = 2 + kc // 4
                    p.tr(PS[bnk][:, (kc % 4) * 128:(kc % 4 + 1) * 128], h2f[:, kc * 128:(kc + 1) * 128], identf, ['h2f', 'identf'], [PK(bnk)])
                p.copy('vector', h2fT[:, 0:4, :].rearrange("p a d -> p (a d)"), PS[2], [PK(2)], ['h2fT'])
                p.copy('vector', h2fT[:, 4:8, :].rearrange("p a d -> p (a d)"), PS[3], [PK(3)], ['h2fT'])
                for kc in range(8):
                    p.mm(PS[1][:, 0:8], h2fT[:, kc, :], Wr[:, kc, :], kc == 0, kc == 7, ['h2fT', 'Wr'], [PK(1)])
                lg, m8, ee, mk = rt[:, 0:8], rt[:, 8:16], rt[:, 16:24], rt[:, 24:32]
                p.tt('vector', lg, PS[1][:, 0:8], brt, ALU.add, [PK(1), 'brt'], ['lg'])
                p.op('vector', lambda e, lg=lg, m8=m8: e.max(out=m8, in_=lg), ['lg'], ['m8'])
                p.ts('vector', rt[:, 32:33], m8[:, 0:1], -1.0, ALU.mult, ['m8'], ['negm'])
                p.act(ee, lg, AF.Exp, ['lg', 'negm'], ['ee'], bias=rt[:, 32:33])
                p.ts('vector', mk, lg, m8[:, 1:2], ALU.is_ge, ['lg', 'm8'], ['mk'])
                p.tt('vector', ee, ee, mk, ALU.mult, ['ee', 'mk'], ['ee'])
                p.op('vector', lambda e, ee=ee: e.tensor_reduce(out=rt[:, 33:34], in_=ee, axis=AX.X, op=ALU.add), ['ee'], ['den'])
                p.recip(rt[:, 34:35], rt[:, 33:34], ['den'], ['rden'])
                p.ts('vector', GW[:, tt, :], ee, rt[:, 34:35], ALU.mult, ['ee', 'rden'], ['GW'])
        aT = yT
        for blk in range(nblk):
            wb = wit % 2
            wit += 1
            p.dma('sync', WGU_t[wb], d['WGU'][blk], [], [('WGU_t', wb)], 'WGU_t%d' % wb)
            p.dma('sync', WD_t[wb], d['WD'][blk], [], [('WD_t', wb)], 'WD_t%d' % wb)
            for fc in range(FC):
                b = fc % 2
                gps, gk = PS[2 + b], PK(2 + b)
                ups, uk = PS[4 + b], PK(4 + b)
                for kc in range(8):
                    p.mm(gps, WGU_t[wb][:, kc, fc * 128:(fc + 1) * 128], hT[:, kc, :], kc == 0, kc == 7, ['hT', ('WGU_t', wb)], [gk])
                for kc in range(8):
                    p.mm(ups, WGU_t[wb][:, kc, F + fc * 128:F + (fc + 1) * 128], hT[:, kc, :], kc == 0, kc == 7, ['hT', ('WGU_t', wb)], [uk])
                p.act(sg[b], gps, AF.Silu, [gk], [('sg', b)])
                p.tt('vector', aT[:, fc, :], sg[b], ups, ALU.mult, [('sg', b), uk], ['yT'])
            k = 0
            for tt in range(4):
                for half in range(2):
                    b = k % 2
                    k += 1
                    ps, pk = PS[6 + b], PK(6 + b)
                    for fc in range(FC):
                        p.mm(ps, aT[:, fc, tt * 128:(tt + 1) * 128], WD_t[wb][:, fc, half * 512:(half + 1) * 512], fc == 0, fc == FC - 1, ['yT', ('WD_t', wb)], [pk])
                    xs = X4[:, tt, half * 512:(half + 1) * 512]
                    if moe:
                        p.stt(xs, ps, GW[:, tt, blk:blk + 1], xs, ALU.mult, ALU.add, [pk, 'GW', 'X4'], ['X4'])
                    else:
                        p.tt('vector', xs, ps, xs, ALU.add, [pk, 'X4'], ['X4'])
        p.dma('sync', d['xout'][T0:T0 + 512, :].rearrange("(t p) d -> p t d", p=128), X4, ['X4'], ['xout'], 'X4o')
        if d.get('XG') is not None:
            for kk in (2 * c, 2 * c + 1):
                p.cc("AllGather", ALU.bypass, GROUPS, d['xout'][kk * 256:(kk + 1) * 256, :], d['XG'][kk], ['xout'], [('XG', kk)])


def build_fused(S, n_layers=2):
    nc = bass.Bass("TRN2", target_bir_lowering=False)
    NT, NTOK, SL = S // 128, S // 4, S // 4
    g = {}

    def din(name, shape, dt=F32):
        g[name] = nc.dram_tensor(name, shape, dt, kind="ExternalInput").ap()

    def dint(name, shape, dt):
        g[name] = nc.dram_tensor(name, shape, dt, kind="Internal").ap()

    din('x', [S, D])
    din('xtok', [NTOK, D])
    for nm, shp, dt in (('cos', [128, SEQ // 128, 16], F32), ('sin', [128, SEQ // 128, 16], F32), ('ident', [128, 128], BF16),
                        ('triN', [128, 128], BF16), ('onesb', [128, 128], BF16), ('maskS', [128, 4, 512], BF16),
                        ('maskI', [128, 4, 512], BF16), ('onesf', [128, 64], F32), ('identf', [128, 128], F32), ('bvec', [10, 128, 128], F32)):
        din(nm, shp, dt)
    for L in range(n_layers):
        sfx = str(L)
        for nm, shp in (('w_in', [D, 1280]), ('g1', [128, 8]), ('wqb', [256, 96]), ('gqa', [128, 2]), ('wkvb', [256, 128]),
                        ('gkva', [128, 2]), ('g32', [288]), ('g96', [192]), ('sinks', [1, 2]), ('wgb', [128, 4096]),
                        ('bgate', [128, 32]), ('wbrc', [224, 1024]), ('g2', [1024]), ('wout', [1024, 1024])):
            din(nm + sfx, shp)
    din('wgu0', [1024, 4096])
    din('wdn0', [2048, 1024])
    if n_layers > 1:
        din('wr1', [1024, 8])
        din('br1', [8])
        din('wgu1', [8, 1024, 1536])
        din('wdn1', [8, 768, 1024])
    dint('QKT', [128, 8, S], BF16)
    dint('VSB', [128, NT, 64], BF16)
    dint('VML', [128, NT, 65], BF16)
    dint('VSW', [128, NT, 33], BF16)
    for i in range(3):
        dint('VD%d' % i, [S, 33], BF16)
    dint('GL', [128, S], BF16)
    dint('OUT', [224, S], BF16)
    SLp = min(SL, 1024)
    NP = SL // SLp
    NAG = NTOK // 256
    dint('YP', [NP, 4, 1024, SLp], F32)
    dint('YS', [NP, 1024, SLp], F32)
    dint('XS', [NTOK, D], F32)
    dint('XG', [NAG, 4 * 256, D], F32)
    dint('WGU0', [4, 128, 8, 1024], BF16)
    dint('WD0', [4, 128, 4, 1024], BF16)
    if n_layers > 1:
        dint('WGU1', [8, 128, 8, 1536], BF16)
        dint('WD1', [8, 128, 6, 1024], BF16)
    g['final'] = nc.dram_tensor('final', [NTOK, D], F32, kind="ExternalOutput").ap()
    shared = ('cos', 'sin', 'ident', 'triN', 'onesb', 'maskS', 'maskI', 'onesf', 'identf', 'bvec',
              'QKT', 'VSB', 'VML', 'VSW', 'VD0', 'VD1', 'VD2', 'GL', 'OUT', 'YP', 'YS')
    with ExitStack() as es:
        sb = Arena(nc, es, 200 * 1024)
        PSB = [es.enter_context(nc.psum_tensor("psb%d" % i, [128, 1024], F32))[:, :] for i in range(4)]
        PS = [PSB[i // 2][:, (i % 2) * 512:(i % 2 + 1) * 512] for i in range(8)]
        d_psb = PSB
        p = Prog(nc, es)
        block = es.enter_context(nc.Block())
        for L in range(n_layers):
            sfx = str(L)
            moe = (L % 2 == 1)
            d = {k: g[k] for k in shared}
            for nm in ('w_in', 'g1', 'wqb', 'gqa', 'wkvb', 'gkva', 'g32', 'g96', 'sinks', 'wgb', 'bgate', 'wbrc', 'g2', 'wout'):
                d[nm] = g[nm + sfx]
            if L == 0:
                d['xrows'] = lambda t: g['x'][t * 128:(t + 1) * 128, :]
            else:
                def xrows(t):
                    T = t * 128
                    r, w = T // NTOK, T % NTOK
                    return g['XG'][w // 256, r * 256 + (w % 256):r * 256 + (w % 256) + 128, :]
                d['xrows'] = xrows
                d['xkeys'] = lambda t: [('XG', ((t * 128) % NTOK) // 256)]
            d['xtok'] = g['xtok'] if L == 0 else g['XS']
            d['xout'] = g['final'] if L == n_layers - 1 else g['XS']
            d['WGU'], d['WD'] = g['WGU' + sfx], g['WD' + sfx]
            if moe:
                nblk, F = 8, 768
                d['wr'], d['br'] = g['wr1'], g['br1']
                d['wg'] = lambda blk: g['wgu1'][blk, :, 0:768]
                d['wu'] = lambda blk: g['wgu1'][blk, :, 768:1536]
                d['wd'] = lambda blk: g['wdn1'][blk, :, :]
            else:
                nblk, F = 4, 512
                d['wg'] = lambda blk: g['wgu0'][:, blk * 512:(blk + 1) * 512]
                d['wu'] = lambda blk: g['wgu0'][:, 2048 + blk * 512:2048 + (blk + 1) * 512]
                d['wd'] = lambda blk: g['wdn0'][blk * 512:(blk + 1) * 512, :]
            d['XG'] = g['XG'] if L < n_layers - 1 else None
            d['n_side'] = nblk * (8 + F // 128)
            d['PSB'] = d_psb
            for ph in (emit_inproj, emit_sb, emit_mla, emit_sw, emit_dil):
                sb.reset()
                if ph is emit_sb:
                    ph(p, sb, PS, d, S, side=lambda sb_, d=d, nblk=nblk, F=F: castw_iter(p, sb_, d, nblk, F, engs=('gpsimd',)))
                else:
                    ph(p, sb, PS, d, S)
                p.barrier(exclude_cc=True, keep=('XG',))
            sb.reset()
            emit_merge(p, sb, PS, d, S)
            p.barrier(exclude_cc=True, keep=('YS',))
            sb.reset()
            emit_B2(p, sb, PS, d, NTOK, nblk, F, moe)
            p.barrier(exclude_cc=True, keep=('XG',))
        p.barrier()
        p.finish(block)
    return nc, p


def inputs_fused(cA, inp, core, S, n_layers=2):
    b, h = core // 4, core % 4
    NTOK = S // 4
    m = {k: cA[k] for k in ('cos', 'sin', 'ident', 'triN', 'onesb', 'maskS', 'maskI', 'onesf')}
    m['identf'] = np.eye(128, dtype=np.float32)
    x = np.asarray(inp['x'], np.float32)
    m['x'] = np.ascontiguousarray(x[b, :S])
    m['xtok'] = np.ascontiguousarray(x[b, h * NTOK:(h + 1) * NTOK])
    bv = band_bias_vecs(inp['rel_bias'], h)
    kk = np.arange(128)
    m['bvec'] = np.ascontiguousarray(bv[:, kk[None, :] - kk[:, None] + 127])
    cols = core_cols(h)
    for l in range(n_layers):
        s = str(l)
        m['w_in' + s] = np.ascontiguousarray(np.concatenate([inp['w_in'][l][:, cols], inp['w_gate_a'][l]], axis=1))
        m['g1' + s] = np.ascontiguousarray(inp['norm1_g'][l].reshape(8, 128).T)
        m['wqb' + s] = np.ascontiguousarray(inp['w_qb'][l][:, h * 96:(h + 1) * 96])
        m['gqa' + s] = np.ascontiguousarray(inp['g_qa'][l].reshape(2, 128).T)
        m['wkvb' + s] = np.ascontiguousarray(inp['w_kvb'][l][:, h * 128:(h + 1) * 128])
        m['gkva' + s] = np.ascontiguousarray(inp['g_kva'][l].reshape(2, 128).T)
        gs, gd = inp['qk_g_sw'][l], inp['qk_g_dil'][l]
        m['g32' + s] = np.concatenate([gs[0], gd[0], gd[0], gs[1], gd[1], gd[1], gs[0], gd[0], gd[1]]).astype(np.float32)
        m['g96' + s] = np.concatenate([inp['qk_g_mla'][l][0], inp['qk_g_mla'][l][1]]).astype(np.float32)
        m['sinks' + s] = np.ascontiguousarray(inp['sinks'][l][2 * h:2 * h + 2].reshape(1, 2))
        m['wgb' + s] = np.ascontiguousarray(inp['w_gate_b'][l])
        m['bgate' + s] = np.ascontiguousarray(inp['b_gate'][l].reshape(32, 128).T)
        wb = inp['w_branch'][l]
        m['wbrc' + s] = np.ascontiguousarray(np.concatenate([wb[h * 64:(h + 1) * 64], wb[256 + h * 64:256 + (h + 1) * 64],
                                                             wb[512 + h * 64:512 + (h + 1) * 64], wb[768 + h * 32:768 + (h + 1) * 32]], axis=0))
        m['g2' + s] = np.ascontiguousarray(inp['norm2_g'][l])
        m['wout' + s] = np.ascontiguousarray(inp['w_out'][l])
    m['wgu0'] = np.ascontiguousarray(inp['w_gu_dense'][0])
    m['wdn0'] = np.ascontiguousarray(inp['w_down_dense'][0])
    if n_layers > 1:
        m['wr1'] = np.ascontiguousarray(inp['w_router'][0])
        m['br1'] = np.ascontiguousarray(inp['b_router'][0])
        m['wgu1'] = np.ascontiguousarray(inp['w_gu_exp'][0])
        m['wdn1'] = np.ascontiguousarray(inp['w_down_exp'][0])
    return m


def kernel_fused(inp, S=SEQ, n_layers=2):
    cA = consts_A()
    ncF, _ = _prog(('F', S, n_layers), lambda: build_fused(S, n_layers))
    in_maps = [inputs_fused(cA, inp, c, S, n_layers) for c in range(8)]
    res = run_bass_kernel_spmd(ncF, in_maps, core_ids=list(range(8)))
    NTOK = S // 4
    out = np.empty((BATCH, S, D), np.float32)
    for c in range(8):
        out[c // 4, (c % 4) * NTOK:(c % 4 + 1) * NTOK] = np.asarray(res.results[c]['final'])
    return out
```

```python
import math
from contextlib import ExitStack

import numpy as np
import ml_dtypes

import concourse.bass as bass
import concourse.mybir as mybir
from concourse.bass_utils import run_bass_kernel_spmd

F32 = mybir.dt.float32
BF16 = mybir.dt.bfloat16
U8 = mybir.dt.uint8
AF = mybir.ActivationFunctionType
ALU = mybir.AluOpType
AX = mybir.AxisListType
ENGS = ['sync', 'scalar', 'vector', 'gpsimd', 'tensor']

D = 1024
SEQ = 16384
BATCH = 2
EPS = 1e-6
NEG = -30000.0
O_AQ, O_AK, O_AV, O_CQ, O_CKV, O_KPE, O_SWQ, O_SWK, O_SWV, O_DQ, O_DK, O_DV = (
    0, 256, 512, 768, 1024, 1280, 1312, 1568, 1632, 1696, 2080, 2464)
DILS = (1, 4, 16)
STQ = 'sync'
LIMIT = 10 ** 12
SKIP = ()


class Prog:
    def __init__(self, nc, es):
        self.nc, self.es = nc, es
        self.ops = {e: [] for e in ENGS}
        self.sems, self.cnt = {}, {}
        self.known = {e: {} for e in ENGS}
        self.lastw, self.readers = {}, {}
        self.n_instr = 0
        for e in ENGS:
            self._mksem('E_' + e)

    def _mksem(self, name):
        if name not in self.sems:
            self.sems[name] = self.es.enter_context(self.nc.semaphore(name))
            self.cnt[name] = 0
        return self.sems[name]

    def _deps(self, reads, writes):
        deps = {}

        def add(d):
            if d is not None and deps.get(d[0], 0) < d[1]:
                deps[d[0]] = d[1]
        for k in reads:
            add(self.lastw.get(k))
        for k in writes:
            add(self.lastw.get(k))
            for s, v in self.readers.get(k, {}).items():
                add((s, v))
        return deps

    def _waits(self, eng, deps):
        for s, v in deps.items():
            if eng == 'tensor' and s == 'E_tensor':
                continue
            if self.known[eng].get(s, 0) >= v:
                continue
            self.known[eng][s] = v
            h = self.sems[s]
            self.ops[eng].append(lambda e, h=h, v=v: e.wait_ge(h, v))
            self.n_instr += 1

    def _record(self, s, v, reads, writes):
        for k in writes:
            self.lastw[k] = (s, v)
            self.readers[k] = {}
        for k in reads:
            self.readers.setdefault(k, {})[s] = v

    def op(self, eng, fn, reads=(), writes=()):
        self.n_ops = getattr(self, 'n_ops', 0) + 1
        if self.n_ops > LIMIT or self.n_ops in SKIP:
            return
        self._waits(eng, self._deps(reads, writes))
        s = 'E_' + eng
        self.cnt[s] += 1
        h = self.sems[s]
        self.ops[eng].append(lambda e, fn=fn, h=h: fn(e).then_inc(h, 1))
        self._record(s, self.cnt[s], reads, writes)
        self.n_instr += 1

    def dma(self, q, out, in_, reads, writes, slot):
        self.n_ops = getattr(self, 'n_ops', 0) + 1
        if self.n_ops > LIMIT:
            return
        self._waits(q, self._deps(reads, writes))
        s = 'D_' + slot
        h = self._mksem(s)
        self.cnt[s] += 16
        self.ops[q].append(lambda e, h=h, out=out, in_=in_: e.dma_start(out=out, in_=in_).then_inc(h, 16))
        self._record(s, self.cnt[s], reads, writes)
        self.n_instr += 1

    def cc(self, kind, op, groups, in_, out, reads, writes):
        self._waits('gpsimd', self._deps(reads, writes))
        s = 'C_all'
        h = self._mksem(s)
        self.cnt[s] += 1
        self.ops['gpsimd'].append(lambda e, h=h: e.collective_compute(kind, op, replica_groups=groups, ins=[in_], outs=[out]).then_inc(h, 1))
        self._record(s, self.cnt[s], reads, writes)
        self.n_instr += 1

    def barrier(self, exclude_cc=False, keep=()):
        allv = {s: v for s, v in self.cnt.items() if v > 0 and not (exclude_cc and s == 'C_all')}
        for e in ENGS:
            self._waits(e, allv)
        kept = {k: v for k, v in self.lastw.items() if isinstance(k, tuple) and k[0] in keep}
        self.lastw, self.readers = kept, {}

    def finish(self, block):
        for name in ENGS:
            def body(e, name=name):
                for f in self.ops[name]:
                    f(e)
            getattr(block, name)(body)

    def mm(self, out, lhsT, rhs, start, stop, reads, writes, skip=False):
        self.op('tensor', lambda e: e.matmul(out, lhsT, rhs, start=start, stop=stop, skip_group_check=skip), reads, writes)

    def tr(self, out, in_, ident, reads, writes):
        self.op('tensor', lambda e: e.transpose(out, in_, ident), reads, writes)

    def act(self, out, in_, func, reads, writes, bias=None, scale=None, accum_out=None):
        kw = {}
        if bias is not None:
            kw['bias'] = bias
        if scale is not None:
            kw['scale'] = scale
        if accum_out is not None:
            kw['accum_out'] = accum_out
        self.op('scalar', lambda e: e.activation(out=out, in_=in_, func=func, **kw), reads, writes)

    def tt(self, eng, out, in0, in1, op, reads, writes):
        self.op(eng, lambda e: e.tensor_tensor(out=out, in0=in0, in1=in1, op=op), reads, writes)

    def ts(self, eng, out, in0, s1, op0, reads, writes, s2=None, op1=None):
        if op1 is None:
            self.op(eng, lambda e: e.tensor_scalar(out=out, in0=in0, scalar1=s1, scalar2=None, op0=op0), reads, writes)
        else:
            self.op(eng, lambda e: e.tensor_scalar(out=out, in0=in0, scalar1=s1, scalar2=s2, op0=op0, op1=op1), reads, writes)

    def stt(self, out, in0, scalar, in1, op0, op1, reads, writes):
        self.op('vector', lambda e: e.scalar_tensor_tensor(out=out, in0=in0, scalar=scalar, in1=in1, op0=op0, op1=op1), reads, writes)

    def copy(self, eng, out, in_, reads, writes):
        if eng == 'scalar':
            self.op(eng, lambda e: e.copy(out=out, in_=in_), reads, writes)
        else:
            self.op(eng, lambda e: e.tensor_copy(out=out, in_=in_), reads, writes)

    def recip(self, out, in_, reads, writes):
        self.op('vector', lambda e: e.reciprocal(out=out, in_=in_), reads, writes)

    def memset(self, eng, ap, val, writes):
        self.op(eng, lambda e: e.memset(ap, val), (), writes)


class Arena:
    def __init__(self, nc, es, nbytes):
        self.big = es.enter_context(nc.sbuf_tensor("arena", [128, nbytes], U8))
        self.nbytes = nbytes
        self.off = 0

    def reset(self):
        self.off = 0

    def __call__(self, name, shape, dt):
        esz = 2 if dt == BF16 else 4
        n = int(np.prod(shape[1:]))
        nb = (n * esz + 63) // 64 * 64
        assert self.off + nb <= self.nbytes, (name, self.off, nb)
        ap = self.big[:, self.off:self.off + n * esz].bitcast(dt)
        self.off += nb
        if len(shape) > 2:
            names = " ".join("d%d" % i for i in range(len(shape) - 1))
            ap = ap.rearrange("p (%s) -> p %s" % (names, names), **{"d%d" % i: shape[i + 1] for i in range(len(shape) - 2)})
        if shape[0] < 128:
            ap = ap[0:shape[0]]
        return ap


def PK(i):
    return ('ps', i)


def core_cols(h):
    r = lambda o, n: list(range(o, o + n))
    swq0 = r(O_SWQ + (2 * h) * 32, 32)
    swq1 = r(O_SWQ + (2 * h + 1) * 32, 32)
    swk = r(O_SWK + (h // 2) * 32, 32)
    swv = r(O_SWV + (h // 2) * 32, 32)
    dq = [r(O_DQ + (g * 4 + h) * 32, 32) for g in range(3)]
    dk = [r(O_DK + (g * 4 + h) * 32, 32) for g in range(3)]
    dv = [r(O_DV + (g * 4 + h) * 32, 32) for g in range(3)]
    g1 = r(O_CQ, 256) + r(O_CKV, 256)
    n32 = swq0 + dq[0] + dq[1] + swk + dk[0] + dk[1] + swq1 + dq[2] + dk[2]
    g2 = n32 + r(O_AQ + h * 64, 64) + r(O_AK + h * 64, 64) + r(O_KPE, 32) + r(O_AV + h * 64, 64)
    g3 = swv + dv[0] + dv[1] + dv[2]
    return np.array(g1 + g2 + g3)


def emit_inproj(p, sb, PS, d, S):
    NT = S // 128
    identb = sb("identb", [128, 128], BF16)
    p.dma('sync', identb, d['ident'][:, :], [], ['identb'], 'identb')
    g1t = sb("g1t", [128, 8], F32)
    p.dma('sync', g1t, d['g1'][:, :], [], ['g1t'], 'g1t')
    gqat = sb("gqat", [128, 2], F32)
    p.dma('sync', gqat, d['gqa'][:, :], [], ['gqat'], 'gqat')
    gkvat = sb("gkvat", [128, 2], F32)
    p.dma('sync', gkvat, d['gkva'][:, :], [], ['gkvat'], 'gkvat')
    G32 = sb("G32", [128, 288], F32)
    p.dma('sync', G32, d['g32'].partition_broadcast(128), [], ['G32'], 'G32')
    G96 = sb("G96", [128, 2, 96], F32)
    p.dma('sync', G96.rearrange("p a d -> p (a d)"), d['g96'].partition_broadcast(128), [], ['G96'], 'G96')
    COS = sb("COS", [128, NT, 16], F32)
    SIN = sb("SIN", [128, NT, 16], F32)
    p.dma('sync', COS, d['cos'][:, 0:NT, :], [], ['COS'], 'COS')
    p.dma('sync', SIN, d['sin'][:, 0:NT, :], [], ['SIN'], 'SIN')
    Wb = sb("Wb", [128, 8, 1280], BF16)
    wst = [sb("wst%d" % i, [128, 1280], F32) for i in range(2)]
    for kc in range(8):
        b = kc % 2
        p.dma('sync', wst[b], d['w_in'][kc * 128:(kc + 1) * 128, :], [], [('wst', b)], 'wst%d' % b)
        p.act(Wb[:, kc, :], wst[b], AF.Copy, [('wst', b), 'g1t'], ['Wb'], scale=g1t[:, kc:kc + 1])
    Wqb = sb("Wqb", [128, 2, 96], BF16)
    Wkvb = sb("Wkvb", [128, 2, 128], BF16)
    for i in range(2):
        p.dma('sync', wst[0][:, 0:96], d['wqb'][i * 128:(i + 1) * 128, :], [], [('wst', 0)], 'wst0')
        p.act(Wqb[:, i, :], wst[0][:, 0:96], AF.Copy, [('wst', 0), 'gqat'], ['Wqb'], scale=gqat[:, i:i + 1])
        p.dma('sync', wst[1][:, 0:128], d['wkvb'][i * 128:(i + 1) * 128, :], [], [('wst', 1)], 'wst1')
        p.act(Wkvb[:, i, :], wst[1][:, 0:128], AF.Copy, [('wst', 1), 'gkvat'], ['Wkvb'], scale=gkvat[:, i:i + 1])

    X = [sb("x%d" % i, [128, 1024], F32) for i in range(2)]
    junk = sb("junk", [128, 1024], BF16)
    hb = [sb("hb%d" % i, [128, 1024], BF16) for i in range(2)]
    hT = [sb("hT%d" % i, [128, 8, 128], BF16) for i in range(2)]
    st = sb("stats", [128, 32], F32)
    cb = sb("cb", [128, 512], BF16)
    cT = sb("cT", [128, 4, 128], BF16)
    qk96 = sb("qk96", [128, 2, 96], F32)
    tmp96 = sb("tmp96", [128, 2, 96], F32)
    qkb = sb("qkb", [128, 2, 96], BF16)
    rA = sb("ropeA", [128, 2, 2, 16], F32)
    rB = sb("ropeB", [128, 2, 2, 16], F32)
    sq32 = sb("sq32", [128, 288], F32)
    tmp32 = sb("tmp32", [128, 288], F32)
    n32b = sb("n32b", [128, 288], BF16)
    sbqk = sb("sbqk", [128, 128], BF16)
    stageT = [sb("stageT%d" % i, [128, 8, 512], BF16) for i in range(2)]
    stVS = [sb("stVS%d" % i, [128, 4, 64], BF16) for i in range(2)]
    stVM = [sb("stVM%d" % i, [128, 4, 65], BF16) for i in range(2)]
    stVG = [sb("stVG%d" % i, [128, 4, 4, 33], BF16) for i in range(2)]
    stGL = [sb("stGL%d" % i, [128, 512], BF16) for i in range(2)]
    glb = sb("glb", [128, 128], BF16)
    for i in range(2):
        p.memset('gpsimd', stageT[i], 0.0, [('stageT', i)])
        p.memset('gpsimd', stVM[i], 1.0, [('stVM', i)])
        p.memset('gpsimd', stVG[i], 1.0, [('stVG', i)])
    psTb = PS[0].bitcast(BF16)
    ps1, ps2, ps3 = PS[1], PS[2], PS[3]
    ps4b = PS[4].bitcast(BF16)
    ps5 = PS[5]
    ps6b = PS[6].bitcast(BF16)
    xrows = d['xrows']

    xkeys = d.get('xkeys', lambda t: [])
    def norm(t):
        b = t % 2
        xk, hbk = ('x', b), ('hb', b)
        if t + 1 < NT:
            p.dma('sync', X[1 - b], xrows(t + 1), xkeys(t + 1), [('x', 1 - b)], 'x%d' % (1 - b))
        p.act(junk, X[b], AF.Square, [xk], ['junk', 'ssq'], accum_out=st[:, 0:1])
        p.act(st[:, 1:2], st[:, 0:1], AF.Sqrt, ['ssq'], ['rs'], scale=1.0 / D, bias=EPS)
        p.recip(st[:, 2:3], st[:, 1:2], ['rs'], ['rstd'])
        p.act(hb[b], X[b], AF.Copy, [xk, 'rstd'], [hbk], scale=st[:, 2:3])

    p.dma('sync', X[0], xrows(0), xkeys(0), [('x', 0)], 'x0')
    norm(0)
    for t in range(NT):
        b = t % 2
        tt = t % 4
        sg = (t // 4) % 2
        xk, hbk, hTk = ('x', b), ('hb', b), ('hT', b)
        for kc in range(8):
            p.tr(psTb[:, kc * 128:(kc + 1) * 128], hb[b][:, kc * 128:(kc + 1) * 128], identb, [hbk, 'identb'], [PK(0)])
        p.copy('vector', hT[b].rearrange("p a d -> p (a d)"), psTb, [PK(0)], [hTk])
        for (ps, key, c0, c1) in ((ps1, PK(1), 0, 512), (ps2, PK(2), 512, 1024), (ps3, PK(3), 1024, 1280)):
            for kc in range(8):
                p.mm(ps[:, 0:c1 - c0], hT[b][:, kc, :], Wb[:, kc, c0:c1], kc == 0, kc == 7, [hTk, 'Wb'], [key])
        p.act(junk[:, 0:256], ps1[:, 0:256], AF.Square, [PK(1)], ['junk', 'ssq2a'], accum_out=st[:, 3:4])
        p.act(junk[:, 0:256], ps1[:, 256:512], AF.Square, [PK(1)], ['junk', 'ssq2b'], accum_out=st[:, 4:5])
        p.act(st[:, 5:7], st[:, 3:5], AF.Sqrt, ['ssq2a', 'ssq2b'], ['rs2'], scale=1.0 / 256, bias=EPS)
        p.recip(st[:, 7:9], st[:, 5:7], ['rs2'], ['rstd2'])
        p.copy('vector', cb, ps1, [PK(1)], ['cb'])
        if t + 1 < NT:
            norm(t + 1)
        for i in range(4):
            p.tr(ps4b[:, i * 128:(i + 1) * 128], cb[:, i * 128:(i + 1) * 128], identb, ['cb', 'identb'], [PK(4)])
        p.copy('vector', cT.rearrange("p a d -> p (a d)"), ps4b[:, 0:512], [PK(4)], ['cT'])
        for i in range(2):
            p.mm(ps5[:, 0:96], cT[:, i, :], Wqb[:, i, :], i == 0, i == 1, ['cT', 'Wqb'], [PK(5)])
        for i in range(2):
            p.mm(ps5[:, 128:256], cT[:, 2 + i, :], Wkvb[:, i, :], i == 0, i == 1, ['cT', 'Wkvb'], [PK(5)])
        p.act(qk96[:, 0, :], ps5[:, 0:96], AF.Copy, [PK(5), 'rstd2'], ['qk96'], scale=st[:, 7:8])
        p.act(qk96[:, 1, 0:64], ps5[:, 128:192], AF.Copy, [PK(5), 'rstd2'], ['qk96'], scale=st[:, 8:9])
        p.act(stVM[sg][:, tt, 0:64], ps5[:, 192:256], AF.Copy, [PK(5), 'rstd2'], [('stVM', sg)], scale=st[:, 8:9])
        p.copy('vector', qk96[:, 1, 64:96], ps2[:, 416:448], [PK(2)], ['qk96'])
        R = qk96[:, :, 64:96].rearrange("p a (h d) -> p a h d", h=2)
        cosb = COS[:, t, :].unsqueeze(1).unsqueeze(1).broadcast_to([128, 2, 2, 16])
        sinb = SIN[:, t, :].unsqueeze(1).broadcast_to([128, 2, 16])
        p.tt('vector', rA, R, cosb, ALU.mult, ['qk96', 'COS'], ['rA'])
        p.tt('vector', rB[:, :, 0, :], R[:, :, 1, :], sinb, ALU.mult, ['qk96', 'SIN'], ['rB'])
        p.tt('vector', rB[:, :, 1, :], R[:, :, 0, :], sinb, ALU.mult, ['qk96', 'SIN'], ['rB'])
        p.tt('vector', R[:, :, 0, :], rA[:, :, 0, :], rB[:, :, 0, :], ALU.subtract, ['rA', 'rB'], ['qk96'])
        p.tt('vector', R[:, :, 1, :], rA[:, :, 1, :], rB[:, :, 1, :], ALU.add, ['rA', 'rB'], ['qk96'])
        p.tt('vector', tmp96, qk96, qk96, ALU.mult, ['qk96'], ['tmp96'])
        p.op('vector', lambda e: e.tensor_reduce(out=st[:, 9:11], in_=tmp96, axis=AX.X, op=ALU.add), ['tmp96'], ['ssq96'])
        p.act(st[:, 11:13], st[:, 9:11], AF.Sqrt, ['ssq96'], ['rs96'], scale=1.0 / 96, bias=EPS)
        p.recip(st[:, 13:15], st[:, 11:13], ['rs96'], ['rstd96'])
        p.tt('vector', tmp96, qk96, G96, ALU.mult, ['qk96', 'G96'], ['tmp96'])
        p.tt('vector', qkb, tmp96, st[:, 13:15].unsqueeze(2).broadcast_to([128, 2, 96]), ALU.mult, ['tmp96', 'rstd96'], ['qkb'])
        p.act(sq32, ps2[:, 0:288], AF.Square, [PK(2)], ['sq32'])
        p.op('vector', lambda e: e.tensor_reduce(out=st[:, 15:24], in_=sq32.rearrange("p (a d) -> p a d", d=32), axis=AX.X, op=ALU.add), ['sq32'], ['ssq32'])
        p.act(st[:, 15:24], st[:, 15:24], AF.Sqrt, ['ssq32'], ['ssq32'], scale=1.0 / 32, bias=EPS)
        p.recip(st[:, 15:24], st[:, 15:24], ['ssq32'], ['ssq32'])
        p.tt('vector', tmp32, ps2[:, 0:288], G32, ALU.mult, [PK(2), 'G32'], ['tmp32'])
        p.tt('vector', n32b.rearrange("p (a d) -> p a d", d=32), tmp32.rearrange("p (a d) -> p a d", d=32),
             st[:, 15:24].unsqueeze(2).broadcast_to([128, 9, 32]), ALU.mult, ['tmp32', 'ssq32'], ['n32b'])
        p.ts('vector', sbqk[:, 0:64], ps2[:, 288:352], 0.125, ALU.mult, [PK(2)], ['sbqk'])
        p.copy('vector', sbqk[:, 64:128], ps2[:, 352:416], [PK(2)], ['sbqk'])
        p.copy('vector', stVS[sg][:, tt, :], ps2[:, 448:512], [PK(2)], [('stVS', sg)])
        p.copy('vector', stVG[sg][:, tt, :, 0:32], ps3[:, 0:128].rearrange("p (a d) -> p a d", d=32), [PK(3)], [('stVG', sg)])
        p.copy('vector', glb, ps3[:, 128:256], [PK(3)], ['glb'])
        p.tr(ps4b[:, 512:640], glb, identb, ['glb', 'identb'], [PK(4)])
        p.copy('vector', stGL[sg][:, tt * 128:(tt + 1) * 128], ps4b[:, 512:640], [PK(4)], [('stGL', sg)])
        p.tr(ps6b[0:64, 0:128], sbqk[:, 0:64], identb, ['sbqk', 'identb'], [PK(6)])
        p.tr(ps6b[0:64, 128:256], sbqk[:, 64:128], identb, ['sbqk', 'identb'], [PK(6)])
        p.tr(ps6b[0:96, 256:384], qkb[:, 0, :], identb, ['qkb', 'identb'], [PK(6)])
        p.tr(ps6b[0:96, 384:512], qkb[:, 1, :], identb, ['qkb', 'identb'], [PK(6)])
        p.tr(ps6b[0:96, 512:640], n32b[:, 0:96], identb, ['n32b', 'identb'], [PK(6)])
        p.tr(ps6b[0:96, 640:768], n32b[:, 96:192], identb, ['n32b', 'identb'], [PK(6)])
        p.tr(ps6b[0:64, 768:896], n32b[:, 192:256], identb, ['n32b', 'identb'], [PK(6)])
        p.tr(ps6b[0:64, 896:1024], n32b[:, 224:288], identb, ['n32b', 'identb'], [PK(6)])
        p.copy('vector', stageT[sg][0:96, 2:6, tt * 128:(tt + 1) * 128], ps6b[0:96, 256:768].rearrange("p (a d) -> p a d", d=128), [PK(6)], [('stageT', sg)])
        p.copy('vector', stageT[sg][0:64, 0:2, tt * 128:(tt + 1) * 128], ps6b[0:64, 0:256].rearrange("p (a d) -> p a d", d=128), [PK(6)], [('stageT', sg)])
        p.copy('vector', stageT[sg][0:64, 6:8, tt * 128:(tt + 1) * 128], ps6b[0:64, 768:1024].rearrange("p (a d) -> p a d", d=128), [PK(6)], [('stageT', sg)])
        if tt == 3:
            T0 = (t - 3) * 128
            j0 = t - 3
            p.dma(STQ, d['QKT'][0:96, :, T0:T0 + 512], stageT[sg][0:96], [('stageT', sg)], [('QKT', sg)], 'stageT%d' % sg)
            p.dma(STQ, d['GL'][:, T0:T0 + 512], stGL[sg], [('stGL', sg)], [('GL', sg)], 'stGL%d' % sg)
            p.dma(STQ, d['VSB'][:, j0:j0 + 4, :], stVS[sg], [('stVS', sg)], [('VSB', sg)], 'stVS%d' % sg)
            p.dma(STQ, d['VML'][:, j0:j0 + 4, :], stVM[sg], [('stVM', sg)], [('VML', sg)], 'stVM%d' % sg)
            p.dma(STQ, d['VSW'][:, j0:j0 + 4, :], stVG[sg][:, :, 0, :], [('stVG', sg)], [('VG', sg)], 'stVG%d' % sg)
            for g in range(3):
                p.dma(STQ, d['VD%d' % g][T0:T0 + 512, :].rearrange("(a p) d -> p a d", p=128), stVG[sg][:, :, 1 + g, :],
                      [('stVG', sg)], [('VG', sg)], 'stVG%d' % sg)


def load_cols(p, dst, dst_key, src, src_keys, slot, nsplit=4):
    n = src.shape[-1]
    w = n // nsplit
    for i in range(nsplit):
        p.dma('sync' if i % 2 == 0 else 'gpsimd', dst[:, i * w:(i + 1) * w], src[:, i * w:(i + 1) * w], src_keys, [dst_key], slot)


def emit_sb(p, sb, PS, d, S, side=None):
    NT = S // 128
    QT = sb("sbQT", [64, S], BF16)
    KT = sb("sbKT", [64, S], BF16)
    V = sb("sbV", [128, NT, 64], BF16)
    load_cols(p, QT, 'sbQT', d['QKT'][0:64, 0, :], [], 'sbQT')
    load_cols(p, KT, 'sbKT', d['QKT'][0:64, 1, :], [], 'sbKT')
    p.dma('sync', V, d['VSB'][:, :, :], [], ['sbV'], 'sbV')
    triN = sb("triN", [128, 128], BF16)
    onesb = sb("onesb", [128, 128], BF16)
    maskS = sb("maskS", [128, 4, 512], BF16)
    p.dma('sync', triN, d['triN'][:, :], [], ['triN'], 'triN')
    p.dma('sync', onesb, d['onesb'][:, :], [], ['onesb'], 'onesb')
    p.dma('sync', maskS, d['maskS'][:, :, :], [], ['maskS'], 'maskS')
    e_t = [sb("sb_e%d" % i, [128, 1024], F32) for i in range(2)]
    sp_t = [sb("sb_sp%d" % i, [128, 1024], BF16) for i in range(2)]
    ex_t = [sb("sb_ex%d" % i, [128, 1024], F32) for i in range(2)]
    A_t = [sb("sb_A%d" % i, [128, 1024], BF16) for i in range(2)]
    Cb = sb("sb_Cb", [128, 512], F32)
    ost = [sb("sb_ost%d" % i, [64, 512], BF16) for i in range(2)]
    PSB = d['PSB']
    pairs = []
    for c in range(S // 512):
        js = list(range(4 * c + 3, -1, -1))
        for m in range(0, len(js), 2):
            pairs.append((c, m, js[m], js[m + 1]))
    NS = len(pairs)

    def stage1(pi):
        c, m, j0, j1 = pairs[pi]
        pb = pi % 2
        ek, spk = ('e', pb), ('sp', pb)
        for h, j in enumerate((j0, j1)):
            p.mm(PS[2 * pb + h], KT[:, j * 128:(j + 1) * 128], QT[:, c * 512:(c + 1) * 512], True, True, ['sbKT', 'sbQT'], [PK(2 * pb + h)])
        p.act(e_t[pb], PSB[pb], AF.Exp, [PK(2 * pb), PK(2 * pb + 1)], [ek])
        p.act(sp_t[pb], e_t[pb], AF.Ln, [ek], [spk], bias=1.0)
        for h, j in enumerate((j0, j1)):
            r = j - 4 * c
            if r >= 0:
                hs = slice(h * 512, (h + 1) * 512)
                p.tt('gpsimd', sp_t[pb][:, hs], sp_t[pb][:, hs], maskS[:, r, :], ALU.mult, [spk, 'maskS'], [spk])

    def stage2(pi):
        c, m, j0, j1 = pairs[pi]
        pb = pi % 2
        spk, exk, Ak = ('sp', pb), ('ex', pb), ('A', pb)
        for h, j in enumerate((j0, j1)):
            hs = slice(h * 512, (h + 1) * 512)
            zk, ck = PK(2 * pb + h), PK(4 + h)
            p.mm(PS[2 * pb + h], triN, sp_t[pb][:, hs], False, True, [spk, 'triN'], [zk], skip=True)
            p.mm(PS[4 + h], onesb, sp_t[pb][:, hs], True, True, [spk, 'onesb'], [ck])
            if m == 0 and h == 0:
                p.copy('vector', ex_t[pb][:, hs], PS[2 * pb + h], [zk], [exk])
                p.copy('vector', Cb, PS[4 + h], [ck], ['Cb'])
            else:
                p.tt('vector', ex_t[pb][:, hs], PS[2 * pb + h], Cb, ALU.subtract, [zk, 'Cb'], [exk])
                if j > 0:
                    p.tt('vector', Cb, PS[4 + h], Cb, ALU.add, [ck, 'Cb'], ['Cb'])
        p.act(A_t[pb], ex_t[pb], AF.Exp, [exk], [Ak])
        for h, j in enumerate((j0, j1)):
            r = j - 4 * c
            if r >= 0:
                hs = slice(h * 512, (h + 1) * 512)
                p.tt('gpsimd', A_t[pb][:, hs], A_t[pb][:, hs], maskS[:, r, :], ALU.mult, [Ak, 'maskS'], [Ak])

    def stage3(pi):
        c, m, j0, j1 = pairs[pi]
        pb = pi % 2
        ob = c % 2
        for h, j in enumerate((j0, j1)):
            hs = slice(h * 512, (h + 1) * 512)
            p.mm(PS[6 + ob][0:64, :], V[:, j, :], A_t[pb][:, hs], m == 0 and h == 0, j == 0, [('A', pb), 'sbV'], [PK(6 + ob)])
        if j1 == 0:
            p.copy('vector', ost[ob], PS[6 + ob][0:64, :], [PK(6 + ob)], [('ost', ob)])
            p.dma(STQ, d['OUT'][0:64, c * 512:(c + 1) * 512], ost[ob], [('ost', ob)], [('OUT', ob)], 'sb_ost%d' % ob)

    side_it = side(sb) if side is not None else None
    n_side = d.get('n_side', 0)
    every = max(1, NS // max(n_side, 1))
    for i in range(NS + 2):
        if i < NS:
            stage1(i)
        if 0 <= i - 1 < NS:
            stage2(i - 1)
        if 0 <= i - 2 < NS:
            stage3(i - 2)
        if side_it is not None and i % every == 0:
            next(side_it, None)
    if side_it is not None:
        for _ in side_it:
            pass


def emit_mla(p, sb, PS, d, S):
    NT = S // 128
    QT = sb("mlQT", [96, S], BF16)
    KT = sb("mlKT", [96, S], BF16)
    V = sb("mlV", [128, NT, 65], BF16)
    load_cols(p, QT, 'mlQT', d['QKT'][0:96, 2, :], [], 'mlQT')
    load_cols(p, KT, 'mlKT', d['QKT'][0:96, 3, :], [], 'mlKT')
    p.dma('sync', V, d['VML'][:, :, :], [], ['mlV'], 'mlV')
    maskI = sb("maskI", [128, 4, 512], BF16)
    p.dma('sync', maskI, d['maskI'][:, :, :], [], ['maskI'], 'maskI')
    onesf = sb("onesf", [128, 64], F32)
    p.dma('sync', onesf, d['onesf'][:, :], [], ['onesf'], 'onesf')
    P_t = [sb("ml_P%d" % i, [128, 1024], BF16) for i in range(3)]
    osb = sb("ml_osb", [128, 512], F32)
    rrow = sb("ml_rrow", [128, 512], F32)
    ost = [sb("ml_ost%d" % i, [64, 512], BF16) for i in range(2)]
    PSB = d['PSB']
    scale = 96 ** -0.5
    pairs = []
    for c in range(S // 512):
        js = list(range(4 * c + 3, -1, -1))
        for m in range(0, len(js), 2):
            pairs.append((c, m, js[m], js[m + 1]))
    NS = len(pairs)

    def stage1(pi):
        c, m, j0, j1 = pairs[pi]
        pb = pi % 3
        Pk = ('P', pb)
        for h, j in enumerate((j0, j1)):
            p.mm(PS[2 * pb + h], KT[:, j * 128:(j + 1) * 128], QT[:, c * 512:(c + 1) * 512], True, True, ['mlKT', 'mlQT'], [PK(2 * pb + h)])
        p.act(P_t[pb], PSB[pb], AF.Exp, [PK(2 * pb), PK(2 * pb + 1)], [Pk], scale=scale)
        for h, j in enumerate((j0, j1)):
            r = j - 4 * c
            if r >= 0:
                hs = slice(h * 512, (h + 1) * 512)
                p.tt('gpsimd', P_t[pb][:, hs], P_t[pb][:, hs], maskI[:, r, :], ALU.mult, [Pk, 'maskI'], [Pk])

    def stage2(pi):
        c, m, j0, j1 = pairs[pi]
        pb = pi % 3
        ob = c % 2
        oD, ok = PS[6], PK(6)
        for h, j in enumerate((j0, j1)):
            hs = slice(h * 512, (h + 1) * 512)
            p.mm(oD[0:65, :], V[:, j, :], P_t[pb][:, hs], m == 0 and h == 0, j == 0, [('P', pb), 'mlV'], [ok])
        if j1 == 0:
            p.copy('vector', osb[0:65, :], oD[0:65, :], [ok], ['ml_osb'])
            p.act(rrow[64:65, :], osb[64:65, :], AF.Ln, ['ml_osb'], ['ml_rrow'])
            p.act(rrow[64:65, :], rrow[64:65, :], AF.Exp, ['ml_rrow'], ['ml_rrow'], scale=-1.0)
            p.mm(PS[7][0:64, :], onesf[64:65, :], rrow[64:65, :], True, True, ['ml_rrow', 'onesf'], [PK(7)])
            p.tt('vector', ost[ob], osb[0:64, :], PS[7][0:64, :], ALU.mult, ['ml_osb', PK(7)], [('ml_ost', ob)])
            p.dma(STQ, d['OUT'][64:128, c * 512:(c + 1) * 512], ost[ob], [('ml_ost', ob)], [('OUT', 2 + ob)], 'ml_ost%d' % ob)

    for i in range(NS + 2):
        if i < NS:
            stage1(i)
        if 0 <= i - 2 < NS:
            stage2(i - 2)


def toeplitz(bmat, idx, rev=False):
    return bmat[idx, :, :]


def run_band_units(p, PS, units, t_sb, P_sb, scale):
    def front(ui):
        slots, Btile, Bkey, _ = units[ui]
        b = ui % 2
        s_ps, sk = PS[b], PK(b)
        for u, sl in enumerate(slots):
            p.mm(s_ps[:, u * 256:u * 256 + 128], sl['kc'], sl['q'], True, True, sl['rk'], [sk])
            p.mm(s_ps[:, u * 256 + 128:(u + 1) * 256], sl['kp'], sl['q'], True, True, sl['rk'], [sk])
        p.stt(t_sb[b], s_ps, scale, Btile, ALU.mult, ALU.add, [sk, Bkey], [('bt', b)])
        p.act(P_sb[b], t_sb[b], AF.Exp, [('bt', b)], [('bP', b)])

    def back(ui):
        slots, _, _, epi = units[ui]
        b = ui % 2
        o_ps, ok = PS[2 + b], PK(2 + b)
        for u, sl in enumerate(slots):
            p.mm(o_ps[0:33, u * 128:(u + 1) * 128], sl['vc'], P_sb[b][:, u * 256:u * 256 + 128], True, False, [('bP', b)] + sl['vk'], [ok])
            p.mm(o_ps[0:33, u * 128:(u + 1) * 128], sl['vp'], P_sb[b][:, u * 256 + 128:(u + 1) * 256], False, True, [('bP', b)] + sl['vk'], [ok])
        epi(o_ps, ok)

    n = len(units)
    for i in range(n + 1):
        if i < n:
            front(i)
        if i >= 1:
            back(i - 1)


def emit_sw(p, sb, PS, d, S):
    NT = S // 128
    Q = [sb("swQ%d" % i, [32, S], BF16) for i in range(2)]
    K = sb("swK", [32, S], BF16)
    V = sb("swV", [128, NT, 33], BF16)
    load_cols(p, Q[0], 'swQ0', d['QKT'][0:32, 4, :], [], 'swQ0')
    load_cols(p, Q[1], 'swQ1', d['QKT'][0:32, 6, :], [], 'swQ1')
    load_cols(p, K, 'swK', d['QKT'][0:32, 5, :], [], 'swK')
    p.dma('sync', V, d['VSW'][:, :, :], [], ['swV'], 'swV')
    B = sb("swB", [128, 2, 2, 128], F32)
    B0 = sb("swB0", [128, 2, 2, 128], F32)
    for h in range(2):
        for sel in range(2):
            p.dma('sync', B[:, h, sel, :], toeplitz(d['bvec'], h * 2 + sel), [], ['swB'], 'swB')
        p.dma('sync', B0[:, h, 0, :], toeplitz(d['bvec'], h * 2), [], ['swB0'], 'swB0')
        p.memset('gpsimd', B0[:, h, 1, :], NEG, ['swB0'])
    onesf = sb("onesf", [128, 64], F32)
    p.dma('sync', onesf, d['onesf'][:, :], [], ['onesf'], 'onesf')
    es = sb("sw_es", [128, 2], F32)
    p.dma('sync', es[32:33, :], d['sinks'][:, :], [], ['sw_es'], 'sw_es')
    p.act(es[32:33, :], es[32:33, :], AF.Exp, ['sw_es'], ['sw_es'])
    t_sb = [sb("bt%d" % i, [128, 512], F32) for i in range(2)]
    P_sb = [sb("bP%d" % i, [128, 512], BF16) for i in range(2)]
    osb = sb("sw_osb", [128, 2, 512], F32)
    rrow = sb("sw_rrow", [128, 2, 512], F32)
    ost = [sb("sw_ost%d" % i, [32, 2, 512], BF16) for i in range(2)]
    scale = 32 ** -0.5
    units = []
    for n in range(NT):
        cs = slice(n * 128, (n + 1) * 128)
        ps_ = slice(max(n - 1, 0) * 128, (max(n - 1, 0) + 1) * 128)
        slots = [dict(kc=K[:, cs], kp=K[:, ps_], q=Q[h][:, cs], vc=V[:, n, :], vp=V[:, max(n - 1, 0), :],
                      rk=['swK', 'swQ%d' % h], vk=['swV']) for h in range(2)]

        def epi(o_ps, ok, n=n):
            tt = n % 4
            p.copy('scalar', osb[0:33, :, tt * 128:(tt + 1) * 128], o_ps[0:33, 0:256].rearrange("p (a d) -> p a d", d=128), [ok], ['sw_osb'])
            if tt == 3:
                gi = (n // 4) % 2
                T0 = (n - 3) * 128
                for h in range(2):
                    p.act(rrow[32:33, h, :], osb[32:33, h, :], AF.Ln, ['sw_osb', 'sw_es'], ['sw_rrow'], bias=es[32:33, h:h + 1])
                    p.act(rrow[32:33, h, :], rrow[32:33, h, :], AF.Exp, ['sw_rrow'], ['sw_rrow'], scale=-1.0)
                    p.mm(PS[4 + h][0:32, :], onesf[32:33, 0:32], rrow[32:33, h, :], True, True, ['sw_rrow', 'onesf'], [PK(4 + h)])
                    p.tt('vector', ost[gi][:, h, :], osb[0:32, h, :], PS[4 + h][0:32, :], ALU.mult, ['sw_osb', PK(4 + h)], [('sw_ost', gi)])
                p.dma(STQ, d['OUT'][128:192, T0:T0 + 512].rearrange("(h p) t -> p h t", p=32), ost[gi], [('sw_ost', gi)], [('OUT', 4 + gi)], 'sw_ost%d' % gi)
        units.append((slots, (B0 if n == 0 else B).rearrange("p a b c -> p (a b c)"), 'swB0' if n == 0 else 'swB', epi))
    run_band_units(p, PS, units, t_sb, P_sb, scale)


def emit_dil(p, sb, PS, d, S):
    Qd = sb("dlQ", [96, S], BF16)
    Kd = sb("dlK", [96, S], BF16)
    Oacc = sb("dlO", [128, S], F32)
    onesf = sb("onesf", [128, 64], F32)
    p.dma('sync', onesf, d['onesf'][:, :], [], ['onesf'], 'onesf')
    Vg = sb("dlV", [128, S // 128, 33], BF16)
    Bt = [sb("dlB%d" % i, [128, 2, 2, 128], F32) for i in range(2)]
    t_sb = [sb("bt%d" % i, [128, 512], F32) for i in range(2)]
    P_sb = [sb("bP%d" % i, [128, 512], BF16) for i in range(2)]
    rrow = sb("dl_rrow", [128, 512], F32)
    ost = [sb("dl_ost%d" % i, [32, 512], BF16) for i in range(2)]
    scale = 32 ** -0.5
    src = [(4, 5, 32), (4, 5, 64), (6, 7, 32)]
    for g, dil in enumerate(DILS):
        qs_, ks_, pb = src[g]
        rows = slice(pb, pb + 32)
        M = S // dil
        nb = M // 128
        load_cols(p, Qd[rows], 'dlQ', d['QKT'][rows, qs_, :], [], 'dlQ')
        load_cols(p, Kd[rows], 'dlK', d['QKT'][rows, ks_, :], [], 'dlK')
        Vv = Vg.rearrange("p (r n) d -> p r n d", r=dil)
        vd = d['VD%d' % g]
        for r0 in range(dil):
            for n0 in range(0, nb, 16):
                nn = min(16, nb - n0)
                srcap = bass.AP(vd.tensor, r0 * 33 + n0 * 128 * dil * 33, [[dil * 33, 128], [128 * dil * 33, nn], [1, 33]])
                p.dma('sync', Vv[:, r0, n0:n0 + nn, :], srcap, [], ['dlV'], 'dlV')
        for v in range(2):
            for u in range(2):
                for sel in range(2):
                    if v == 1 and u == 0 and sel == 1:
                        p.memset('gpsimd', Bt[v][:, u, sel, :], NEG, [('dlB', v)])
                    else:
                        p.dma('sync', Bt[v][:, u, sel, :], toeplitz(d['bvec'], (2 + g) * 2 + sel), [], [('dlB', v)], 'dlB%d' % v)
        units = []
        for r in range(dil):
            for n in range(0, nb, 2):
                slots = []
                for u in range(2):
                    n1 = n + u
                    np_ = max(n1 - 1, 0)
                    qc = slice(r + dil * 128 * n1, r + dil * 128 * n1 + dil * 127 + 1, dil)
                    kc = slice(r + dil * 128 * np_, r + dil * 128 * np_ + dil * 127 + 1, dil)
                    slots.append(dict(kc=Kd[rows, qc], kp=Kd[rows, kc], q=Qd[rows, qc], vc=Vv[:, r, n1, :], vp=Vv[:, r, np_, :],
                                      rk=['dlK', 'dlQ'], vk=['dlV']))
                v = 1 if n == 0 else 0
                oc = slice(r + dil * 128 * n, r + dil * 128 * n + dil * 255 + 1, dil)

                def epi(o_ps, ok, oc=oc, g=g):
                    if g == 0:
                        p.copy('vector', Oacc[0:33, oc], o_ps[0:33, 0:256], [ok], ['dlO'])
                    else:
                        p.tt('vector', Oacc[0:33, oc], o_ps[0:33, 0:256], Oacc[0:33, oc], ALU.add, [ok, 'dlO'], ['dlO'])
                units.append((slots, Bt[v].rearrange("p a b c -> p (a b c)"), ('dlB', v), epi))
        run_band_units(p, PS, units, t_sb, P_sb, scale)
    for c in range(S // 512):
        gi = c % 2
        cs = slice(c * 512, (c + 1) * 512)
        p.act(rrow[32:33, :], Oacc[32:33, cs], AF.Ln, ['dlO'], ['dl_rrow'])
        p.act(rrow[32:33, :], rrow[32:33, :], AF.Exp, ['dl_rrow'], ['dl_rrow'], scale=-1.0)
        p.mm(PS[4 + gi][0:32, :], onesf[32:33, 0:32], rrow[32:33, :], True, True, ['dl_rrow', 'onesf'], [PK(4 + gi)])
        p.tt('vector', ost[gi], Oacc[0:32, cs], PS[4 + gi][0:32, :], ALU.mult, ['dlO', PK(4 + gi)], [('dl_ost', gi)])
        p.dma(STQ, d['OUT'][192:224, cs], ost[gi], [('dl_ost', gi)], [('OUT', 6 + gi)], 'dl_ost%d' % gi)


def build_A(S, phases=('inproj', 'sb', 'mla', 'sw', 'dil'), dbg=False):
    nc = bass.Bass("TRN2", target_bir_lowering=False)
    NT = S // 128
    d = {}

    def din(name, shape, dt=F32):
        d[name] = nc.dram_tensor(name, shape, dt, kind="ExternalInput").ap()

    din('x', [S, D])
    din('w_in', [D, 1280])
    din('g1', [128, 8])
    din('wqb', [256, 96])
    din('gqa', [128, 2])
    din('wkvb', [256, 128])
    din('gkva', [128, 2])
    din('g32', [288])
    din('g96', [192])
    din('cos', [128, SEQ // 128, 16])
    din('sin', [128, SEQ // 128, 16])
    din('ident', [128, 128], BF16)
    din('triN', [128, 128], BF16)
    din('onesb', [128, 128], BF16)
    din('maskS', [128, 4, 512], BF16)
    din('maskI', [128, 4, 512], BF16)
    din('onesf', [128, 64])
    din('bvec', [10, 128, 128])
    din('sinks', [1, 2])
    kind = "ExternalOutput" if dbg else "Internal"
    d['QKT'] = nc.dram_tensor('QKT', [128, 8, S], BF16, kind=kind).ap()
    d['VSB'] = nc.dram_tensor('VSB', [128, NT, 64], BF16, kind=kind).ap()
    d['VML'] = nc.dram_tensor('VML', [128, NT, 65], BF16, kind=kind).ap()
    d['VSW'] = nc.dram_tensor('VSW', [128, NT, 33], BF16, kind=kind).ap()
    for g in range(3):
        d['VD%d' % g] = nc.dram_tensor('VD%d' % g, [S, 33], BF16, kind=kind).ap()
    d['OUT'] = nc.dram_tensor('OUT', [224, S], BF16, kind="ExternalOutput").ap()
    with ExitStack() as es:
        sb = Arena(nc, es, 196 * 1024)
        PSB = [es.enter_context(nc.psum_tensor("psb%d" % i, [128, 1024], F32))[:, :] for i in range(4)]
        PS = [PSB[i // 2][:, (i % 2) * 512:(i % 2 + 1) * 512] for i in range(8)]
        d_psb = PSB
        p = Prog(nc, es)
        block = es.enter_context(nc.Block())
        d['xrows'] = lambda t: d['x'][t * 128:(t + 1) * 128, :]
        d['GL'] = nc.dram_tensor('GL', [128, S], BF16, kind="Internal").ap()
        d['PSB'] = d_psb
        emitters = dict(inproj=emit_inproj, sb=emit_sb, mla=emit_mla, sw=emit_sw, dil=emit_dil)
        for ph in phases:
            sb.reset()
            emitters[ph](p, sb, PS, d, S)
            p.barrier()
        p.finish(block)
    return nc, p


def bf(a):
    return np.ascontiguousarray(a).astype(ml_dtypes.bfloat16)


def t5_bucket_np(dist):
    dist = np.asarray(dist, np.int64)
    d_ = np.maximum(dist, 1).astype(np.float32)
    large = 16 + (np.log(d_ / np.float32(16)) / np.float32(math.log(2048 / 16)) * np.float32(16)).astype(np.int32)
    large = np.minimum(large, 31)
    return np.where(dist < 16, dist, large)


def consts_A():
    k = np.arange(128)
    c = {}
    c['ident'] = bf(np.eye(128, dtype=np.float32))
    c['triN'] = bf(-(k[:, None] >= k[None, :]).astype(np.float32))
    c['onesb'] = bf(np.ones((128, 128), np.float32))
    qi = np.arange(512)
    mS = np.zeros((128, 4, 512), np.float32)
    mI = np.zeros((128, 4, 512), np.float32)
    for r in range(4):
        mS[:, r, :] = (128 * r + k[:, None]) < qi[None, :]
        mI[:, r, :] = (128 * r + k[:, None]) <= qi[None, :]
    c['maskS'] = bf(mS)
    c['maskI'] = bf(mI)
    c['onesf'] = np.ones((128, 64), np.float32)
    half = 16
    inv = (10000.0 ** (-np.arange(half, dtype=np.float32) / half)).astype(np.float32)
    ang = np.arange(SEQ, dtype=np.float32)[:, None] * inv[None, :]
    c['cos'] = np.ascontiguousarray(np.cos(ang).astype(np.float32).reshape(SEQ // 128, 128, 16).transpose(1, 0, 2))
    c['sin'] = np.ascontiguousarray(np.sin(ang).astype(np.float32).reshape(SEQ // 128, 128, 16).transpose(1, 0, 2))
    return c


def band_bias_vecs(rel_bias, h):
    dd = np.arange(-127, 128)
    out = np.full((10, 255), NEG, np.float32)
    for i, hq in enumerate((2 * h, 2 * h + 1)):
        cur = dd >= 0
        out[2 * i, cur] = rel_bias[t5_bucket_np(dd[cur]), hq]
        prv = dd < 0
        out[2 * i + 1, prv] = rel_bias[t5_bucket_np(128 + dd[prv]), hq]
    for g, dil in enumerate(DILS):
        col = 8 + g * 4 + h
        cur = dd >= 0
        out[4 + 2 * g, cur] = rel_bias[t5_bucket_np(dd[cur] * dil), col]
        prv = dd <= 0
        out[5 + 2 * g, prv] = rel_bias[t5_bucket_np((128 + dd[prv]) * dil), col]
    return out


def inputs_A(c, l, b, h, inp, x_b, S):
    cols = core_cols(h)
    m = dict(c)
    m['x'] = np.ascontiguousarray(x_b[:S])
    m['w_in'] = np.ascontiguousarray(np.concatenate([inp['w_in'][l][:, cols], inp['w_gate_a'][l]], axis=1))
    m['g1'] = np.ascontiguousarray(inp['norm1_g'][l].reshape(8, 128).T)
    m['wqb'] = np.ascontiguousarray(inp['w_qb'][l][:, h * 96:(h + 1) * 96])
    m['gqa'] = np.ascontiguousarray(inp['g_qa'][l].reshape(2, 128).T)
    m['wkvb'] = np.ascontiguousarray(inp['w_kvb'][l][:, h * 128:(h + 1) * 128])
    m['gkva'] = np.ascontiguousarray(inp['g_kva'][l].reshape(2, 128).T)
    gs, gd = inp['qk_g_sw'][l], inp['qk_g_dil'][l]
    m['g32'] = np.concatenate([gs[0], gd[0], gd[0], gs[1], gd[1], gd[1], gs[0], gd[0], gd[1]]).astype(np.float32)
    m['g96'] = np.concatenate([inp['qk_g_mla'][l][0], inp['qk_g_mla'][l][1]]).astype(np.float32)
    bv = band_bias_vecs(inp['rel_bias'], h)
    kk = np.arange(128)
    m['bvec'] = np.ascontiguousarray(bv[:, kk[None, :] - kk[:, None] + 127])
    m['sinks'] = np.ascontiguousarray(inp['sinks'][l][2 * h:2 * h + 2].reshape(1, 2))
    return m


BR_CHUNKS = ((0, 1), (2, 3), (4, 5), (6,))


def castw_iter(p, sb, d, nblk, F, engs=('vector', 'gpsimd', 'scalar')):
    stg = [sb("cw_stg%d" % i, [128, 2 * F], F32) for i in range(2)]
    stb = [sb("cw_stb%d" % i, [128, 2 * F], BF16) for i in range(2)]
    it = 0
    for blk in range(nblk):
        for kc in range(8):
            b = it % 2
            rows = slice(kc * 128, (kc + 1) * 128)
            p.dma('sync', stg[b][:, 0:F], d['wg'](blk)[rows, :], [], [('cw_stg', b)], 'cw_stg%d' % b)
            p.dma('sync', stg[b][:, F:2 * F], d['wu'](blk)[rows, :], [], [('cw_stg', b)], 'cw_stg%d' % b)
            p.copy(engs[it % len(engs)], stb[b], stg[b], [('cw_stg', b)], [('cw_stb', b)])
            p.dma(STQ, d['WGU'][blk, :, kc, :], stb[b], [('cw_stb', b)], [('WGU', b)], 'cw_stb%d' % b)
            it += 1
            yield
        for fc in range(F // 128):
            b = it % 2
            p.dma('sync', stg[b][:, 0:1024], d['wd'](blk)[fc * 128:(fc + 1) * 128, :], [], [('cw_stg', b)], 'cw_stg%d' % b)
            p.copy(engs[it % len(engs)], stb[b][:, 0:1024], stg[b][:, 0:1024], [('cw_stg', b)], [('cw_stb', b)])
            p.dma(STQ, d['WD'][blk, :, fc, :], stb[b][:, 0:1024], [('cw_stb', b)], [('WD', b)], 'cw_stb%d' % b)
            it += 1
            yield


def emit_castw(p, sb, PS, d, nblk, F):
    for _ in castw_iter(p, sb, d, nblk, F):
        pass


def emit_B(p, sb, PS, d, NTOK, nblk, F, moe):
    NCH = NTOK // 512
    FC = F // 128
    identb = sb("identb", [128, 128], BF16)
    p.dma('sync', identb, d['ident'][:, :], [], ['identb'], 'identb')
    G1 = sb("G1", [128, 1024], F32)
    G2 = sb("G2", [128, 1024], F32)
    p.dma('sync', G1, d['g1'].partition_broadcast(128), [], ['G1'], 'G1')
    p.dma('sync', G2, d['g2'].partition_broadcast(128), [], ['G2'], 'G2')
    bgt = sb("bgt", [128, 32], F32)
    p.dma('sync', bgt, d['bgate'][:, :], [], ['bgt'], 'bgt')
    Wga = sb("Wga", [128, 8, 128], BF16)
    Wgb = sb("Wgb", [128, 4096], BF16)
    Wbr = sb("Wbr", [128, 7, 1024], BF16)
    Wout = sb("Wout", [128, 8, 1024], BF16)
    stg = [sb("w_stg%d" % i, [128, 1024], F32) for i in range(2)]
    engs = ['vector', 'gpsimd', 'scalar']
    it = 0

    def cast_in(dst, src, n):
        nonlocal it
        b = it % 2
        p.dma('sync', stg[b][:, 0:n], src, [], [('w_stg', b)], 'w_stg%d' % b)
        p.copy(engs[it % 3], dst, stg[b][:, 0:n], [('w_stg', b)], ['Wres'])
        it += 1
    for kc in range(8):
        cast_in(Wga[:, kc, :], d['wga'][kc * 128:(kc + 1) * 128, :], 128)
        cast_in(Wout[:, kc, :], d['wout'][kc * 128:(kc + 1) * 128, :], 1024)
    for i in range(4):
        cast_in(Wgb[:, i * 1024:(i + 1) * 1024], d['wgb'][:, i * 1024:(i + 1) * 1024], 1024)
    for rc in range(7):
        cast_in(Wbr[:, rc, :], d['wbr'][rc * 128:(rc + 1) * 128, :], 1024)
    if moe:
        identf = sb("identf", [128, 128], F32)
        p.dma('sync', identf, d['identf'][:, :], [], ['identf'], 'identf')
        Wr = sb("Wr", [128, 8, 8], F32)
        p.dma('sync', Wr, d['wr'].rearrange("(kc p) e -> p kc e", p=128), [], ['Wr'], 'Wr')
        brt = sb("brt", [128, 8], F32)
        p.dma('sync', brt, d['br'].partition_broadcast(128), [], ['brt'], 'brt')
        h2f = sb("h2f", [128, 1024], F32)
        h2fT = sb("h2fT", [128, 8, 128], F32)
        rt = sb("rt", [128, 64], F32)
        GW = sb("GW", [128, 4, 8], F32)
    X4 = sb("X4", [128, 4, 1024], F32)
    oTc = sb("oTc", [128, 7, 512], BF16)
    hT = sb("hT", [128, 8, 512], BF16)
    hb = sb("hb", [128, 1024], BF16)
    junk = sb("junk", [128, 1024], BF16)
    st = sb("st", [128, 8], F32)
    glT = sb("glT", [128, 512], BF16)
    gate = [sb("gate%d" % i, [128, 512], F32) for i in range(2)]
    tmpy = sb("tmpy", [128, 512], F32)
    yacc = sb("yacc", [128, 512], F32)
    yT = sb("yT", [128, 8, 512], BF16)
    sg = [sb("sg%d" % i, [128, 512], F32) for i in range(2)]
    WGU_t = [sb("WGU_t%d" % i, [128, 8, 2 * F], BF16) for i in range(2)]
    WD_t = [sb("WD_t%d" % i, [128, FC, 1024], BF16) for i in range(2)]
    psTb = PS[0].bitcast(BF16)
    x, out = d['x'], d['out']
    wit = 0

    def norm_T(tt, G, Gk):
        xt = X4[:, tt, :]
        p.act(junk, xt, AF.Square, ['X4'], ['junk', 'ssq'], accum_out=st[:, 0:1])
        p.act(st[:, 1:2], st[:, 0:1], AF.Sqrt, ['ssq'], ['rs'], scale=1.0 / D, bias=EPS)
        p.recip(st[:, 2:3], st[:, 1:2], ['rs'], ['rstd'])
        p.stt(hb, xt, st[:, 2:3], G, ALU.mult, ALU.mult, ['X4', 'rstd', Gk], ['hb'])
        for kc in range(8):
            p.tr(psTb[:, kc * 128:(kc + 1) * 128], hb[:, kc * 128:(kc + 1) * 128], identb, ['hb', 'identb'], [PK(0)])
        p.copy('vector', hT[:, :, tt * 128:(tt + 1) * 128], psTb.rearrange("p (a d) -> p a d", d=128), [PK(0)], ['hT'])

    for c in range(NCH):
        T0 = c * 512
        p.dma('sync', X4, x[T0:T0 + 512, :].rearrange("(t p) d -> p t d", p=128), [], ['X4'], 'X4')
        p.dma('sync', oTc, d['oT'][:, T0:T0 + 512].rearrange("(r p) t -> p r t", p=128), [], ['oTc'], 'oTc')
        for tt in range(4):
            norm_T(tt, G1, 'G1')
        for kc in range(8):
            p.mm(PS[1], Wga[:, kc, :], hT[:, kc, :], kc == 0, kc == 7, ['hT', 'Wres'], [PK(1)])
        p.copy('vector', glT, PS[1], [PK(1)], ['glT'])
        k = 0
        for oc in range(8):
            for i in range(4):
                b = k % 2
                k += 1
                gp, gk = PS[2 + b], PK(2 + b)
                bp, bk = PS[4 + b], PK(4 + b)
                col = i * 1024 + oc * 128
                p.mm(gp, Wgb[:, col:col + 128], glT, True, True, ['glT', 'Wres'], [gk])
                p.act(gate[b], gp, AF.Sigmoid, [gk, 'bgt'], [('gate', b)], bias=bgt[:, i * 8 + oc:i * 8 + oc + 1])
                rcs = BR_CHUNKS[i]
                for n_, rc in enumerate(rcs):
                    p.mm(bp, Wbr[:, rc, oc * 128:(oc + 1) * 128], oTc[:, rc, :], n_ == 0, n_ == len(rcs) - 1, ['oTc', 'Wres'], [bk])
                if i == 0:
                    p.tt('vector', yacc, gate[b], bp, ALU.mult, [('gate', b), bk], ['yacc'])
                else:
                    p.tt('vector', tmpy, gate[b], bp, ALU.mult, [('gate', b), bk], ['tmpy'])
                    if i < 3:
                        p.tt('gpsimd', yacc, yacc, tmpy, ALU.add, ['yacc', 'tmpy'], ['yacc'])
                    else:
                        p.tt('gpsimd', yT[:, oc, :], yacc, tmpy, ALU.add, ['yacc', 'tmpy'], ['yT'])
        k = 0
        for tt in range(4):
            for half in range(2):
                b = k % 2
                k += 1
                ps, pk = PS[6 + b], PK(6 + b)
                for oc in range(8):
                    p.mm(ps, yT[:, oc, tt * 128:(tt + 1) * 128], Wout[:, oc, half * 512:(half + 1) * 512], oc == 0, oc == 7, ['yT', 'Wres'], [pk])
                xs = X4[:, tt, half * 512:(half + 1) * 512]
                p.tt('vector', xs, ps, xs, ALU.add, [pk, 'X4'], ['X4'])
        for tt in range(4):
            norm_T(tt, G2, 'G2')
            if moe:
                p.stt(h2f, X4[:, tt, :], st[:, 2:3], G2, ALU.mult, ALU.mult, ['X4', 'rstd', 'G2'], ['h2f'])
                for kc in range(8):
                    bnk = 2 + kc // 4
                    p.tr(PS[bnk][:, (kc % 4) * 128:(kc % 4 + 1) * 128], h2f[:, kc * 128:(kc + 1) * 128], identf, ['h2f', 'identf'], [PK(bnk)])
                p.copy('vector', h2fT[:, 0:4, :].rearrange("p a d -> p (a d)"), PS[2], [PK(2)], ['h2fT'])
                p.copy('vector', h2fT[:, 4:8, :].rearrange("p a d -> p (a d)"), PS[3], [PK(3)], ['h2fT'])
                for kc in range(8):
                    p.mm(PS[1][:, 0:8], h2fT[:, kc, :], Wr[:, kc, :], kc == 0, kc == 7, ['h2fT', 'Wr'], [PK(1)])
                lg, m8, ee, mk = rt[:, 0:8], rt[:, 8:16], rt[:, 16:24], rt[:, 24:32]
                p.tt('vector', lg, PS[1][:, 0:8], brt, ALU.add, [PK(1), 'brt'], ['lg'])
                p.op('vector', lambda e, lg=lg, m8=m8: e.max(out=m8, in_=lg), ['lg'], ['m8'])
                p.ts('vector', rt[:, 32:33], m8[:, 0:1], -1.0, ALU.mult, ['m8'], ['negm'])
                p.act(ee, lg, AF.Exp, ['lg', 'negm'], ['ee'], bias=rt[:, 32:33])
                p.ts('vector', mk, lg, m8[:, 1:2], ALU.is_ge, ['lg', 'm8'], ['mk'])
                p.tt('vector', ee, ee, mk, ALU.mult, ['ee', 'mk'], ['ee'])
                p.op('vector', lambda e, ee=ee: e.tensor_reduce(out=rt[:, 33:34], in_=ee, axis=AX.X, op=ALU.add), ['ee'], ['den'])
                p.recip(rt[:, 34:35], rt[:, 33:34], ['den'], ['rden'])
                p.ts('vector', GW[:, tt, :], ee, rt[:, 34:35], ALU.mult, ['ee', 'rden'], ['GW'])
        aT = yT
        for blk in range(nblk):
            wb = wit % 2
            wit += 1
            p.dma('sync', WGU_t[wb], d['WGU'][blk], [], [('WGU_t', wb)], 'WGU_t%d' % wb)
            p.dma('sync', WD_t[wb], d['WD'][blk], [], [('WD_t', wb)], 'WD_t%d' % wb)
            for fc in range(FC):
                b = fc % 2
                gps, gk = PS[2 + b], PK(2 + b)
                ups, uk = PS[4 + b], PK(4 + b)
                for kc in range(8):
                    p.mm(gps, WGU_t[wb][:, kc, fc * 128:(fc + 1) * 128], hT[:, kc, :], kc == 0, kc == 7, ['hT', ('WGU_t', wb)], [gk])
                for kc in range(8):
                    p.mm(ups, WGU_t[wb][:, kc, F + fc * 128:F + (fc + 1) * 128], hT[:, kc, :], kc == 0, kc == 7, ['hT', ('WGU_t', wb)], [uk])
                p.act(sg[b], gps, AF.Silu, [gk], [('sg', b)])
                p.tt('vector', aT[:, fc, :], sg[b], ups, ALU.mult, [('sg', b), uk], ['yT'])
            k = 0
            for tt in range(4):
                for half in range(2):
                    b = k % 2
                    k += 1
                    ps, pk = PS[6 + b], PK(6 + b)
                    for fc in range(FC):
                        p.mm(ps, aT[:, fc, tt * 128:(tt + 1) * 128], WD_t[wb][:, fc, half * 512:(half + 1) * 512], fc == 0, fc == FC - 1, ['yT', ('WD_t', wb)], [pk])
                    xs = X4[:, tt, half * 512:(half + 1) * 512]
                    if moe:
                        p.stt(xs, ps, GW[:, tt, blk:blk + 1], xs, ALU.mult, ALU.add, [pk, 'GW', 'X4'], ['X4'])
                    else:
                        p.tt('vector', xs, ps, xs, ALU.add, [pk, 'X4'], ['X4'])
        p.dma(STQ, out[T0:T0 + 512, :].rearrange("(t p) d -> p t d", p=128), X4, ['X4'], ['out'], 'X4o')


def build_B(NTOK, moe):
    nc = bass.Bass("TRN2", target_bir_lowering=False)
    d = {}

    def din(name, shape, dt=F32):
        d[name] = nc.dram_tensor(name, shape, dt, kind="ExternalInput").ap()

    din('x', [NTOK, D])
    din('oT', [896, NTOK], BF16)
    din('ident', [128, 128], BF16)
    din('g1', [1024])
    din('g2', [1024])
    din('bgate', [128, 32])
    din('wga', [1024, 128])
    din('wgb', [128, 4096])
    din('wbr', [896, 1024])
    din('wout', [1024, 1024])
    if moe:
        nblk, F = 8, 768
        din('identf', [128, 128])
        din('wr', [1024, 8])
        din('br', [8])
        din('wgu', [8, 1024, 1536])
        din('wdn', [8, 768, 1024])
        d['wg'] = lambda blk: d['wgu'][blk, :, 0:768]
        d['wu'] = lambda blk: d['wgu'][blk, :, 768:1536]
        d['wd'] = lambda blk: d['wdn'][blk, :, :]
    else:
        nblk, F = 4, 512
        din('wgu', [1024, 4096])
        din('wdn', [2048, 1024])
        d['wg'] = lambda blk: d['wgu'][:, blk * 512:(blk + 1) * 512]
        d['wu'] = lambda blk: d['wgu'][:, 2048 + blk * 512:2048 + (blk + 1) * 512]
        d['wd'] = lambda blk: d['wdn'][blk * 512:(blk + 1) * 512, :]
    d['WGU'] = nc.dram_tensor('WGU', [nblk, 128, 8, 2 * F], BF16, kind="Internal").ap()
    d['WD'] = nc.dram_tensor('WD', [nblk, 128, F // 128, 1024], BF16, kind="Internal").ap()
    d['out'] = nc.dram_tensor('out', [NTOK, D], F32, kind="ExternalOutput").ap()
    with ExitStack() as es:
        sb = Arena(nc, es, 200 * 1024)
        PSB = [es.enter_context(nc.psum_tensor("psb%d" % i, [128, 1024], F32))[:, :] for i in range(4)]
        PS = [PSB[i // 2][:, (i % 2) * 512:(i % 2 + 1) * 512] for i in range(8)]
        d_psb = PSB
        p = Prog(nc, es)
        block = es.enter_context(nc.Block())
        emit_castw(p, sb, PS, d, nblk, F)
        p.barrier()
        sb.reset()
        emit_B(p, sb, PS, d, NTOK, nblk, F, moe)
        p.barrier()
        p.finish(block)
    return nc, p


def inputs_B(l, inp, x_tok, oT_tok, moe):
    m = {}
    m['x'] = np.ascontiguousarray(x_tok)
    m['oT'] = np.ascontiguousarray(oT_tok)
    m['ident'] = bf(np.eye(128, dtype=np.float32))
    m['g1'] = np.ascontiguousarray(inp['norm1_g'][l])
    m['g2'] = np.ascontiguousarray(inp['norm2_g'][l])
    m['bgate'] = np.ascontiguousarray(inp['b_gate'][l].reshape(32, 128).T)
    m['wga'] = np.ascontiguousarray(inp['w_gate_a'][l])
    m['wgb'] = np.ascontiguousarray(inp['w_gate_b'][l])
    m['wbr'] = np.ascontiguousarray(inp['w_branch'][l])
    m['wout'] = np.ascontiguousarray(inp['w_out'][l])
    if moe:
        m['identf'] = np.eye(128, dtype=np.float32)
        m['wr'] = np.ascontiguousarray(inp['w_router'][l // 2])
        m['br'] = np.ascontiguousarray(inp['b_router'][l // 2])
        m['wgu'] = np.ascontiguousarray(inp['w_gu_exp'][l // 2])
        m['wdn'] = np.ascontiguousarray(inp['w_down_exp'][l // 2])
    else:
        m['wgu'] = np.ascontiguousarray(inp['w_gu_dense'][l // 2])
        m['wdn'] = np.ascontiguousarray(inp['w_down_dense'][l // 2])
    return m


_CACHE = {}


def _prog(key, fn):
    if key not in _CACHE:
        _CACHE[key] = fn()
    return _CACHE[key]


def assemble_oT(outs):
    res = []
    for b in range(BATCH):
        rows = [None] * 4
        o = [np.asarray(outs[b * 4 + h]) for h in range(4)]
        sbp = np.concatenate([o[h][0:64] for h in range(4)], axis=0)
        mlp = np.concatenate([o[h][64:128] for h in range(4)], axis=0)
        swp = np.concatenate([o[h][128:192] for h in range(4)], axis=0)
        dlp = np.concatenate([o[h][192:224] for h in range(4)], axis=0)
        res.append(np.concatenate([sbp, mlp, swp, dlp], axis=0))
    return res


def kernel(**inp):
    inp = {k: np.asarray(v) for k, v in inp.items()}
    return kernel_fused(inp)


GROUPS = [[0, 1, 2, 3], [4, 5, 6, 7]]


def emit_merge(p, sb, PS, d, S):
    SL = S // 4
    bgt = sb("bgt", [128, 32], F32)
    p.dma('sync', bgt, d['bgate'][:, :], [], ['bgt'], 'bgt')
    Wgb = sb("Wgb", [128, 4096], BF16)
    Wbr = sb("Wbrc", [64, 4, 1024], BF16)
    stg = [sb("w_stg%d" % i, [128, 1024], F32) for i in range(2)]
    engs = ['vector', 'gpsimd', 'scalar']
    it = 0
    for i in range(4):
        b = it % 2
        p.dma('sync', stg[b], d['wgb'][:, i * 1024:(i + 1) * 1024], [], [('w_stg', b)], 'w_stg%d' % b)
        p.copy(engs[it % 3], Wgb[:, i * 1024:(i + 1) * 1024], stg[b], [('w_stg', b)], ['Wres'])
        it += 1
    rows = ((0, 64), (64, 64), (128, 64), (192, 32))
    for i, (r0, nr) in enumerate(rows):
        b = it % 2
        p.dma('sync', stg[b][0:nr, :], d['wbrc'][r0:r0 + nr, :], [], [('w_stg', b)], 'w_stg%d' % b)
        p.copy(engs[it % 3], Wbr[0:nr, i, :], stg[b][0:nr, :], [('w_stg', b)], ['Wres'])
        it += 1
    glT = [sb("glT%d" % i, [128, 512], BF16) for i in range(2)]
    oc_t = [sb("oc_t%d" % i, [64, 4, 512], BF16) for i in range(2)]
    gate = [sb("gate%d" % i, [128, 512], F32) for i in range(2)]
    tmpy2 = [sb("tmpy%d" % i, [128, 512], F32) for i in range(2)]
    yacc2 = [sb("yacc%d" % i, [128, 512], F32) for i in range(2)]
    yst = [sb("yst%d" % i, [128, 8, 512], F32) for i in range(2)]
    k = 0
    SLp = min(SL, 1024)
    NPc = SL // SLp
    order = [(sl * SL + q * SLp) // 512 + cc for q in range(NPc) for sl in range(4) for cc in range(SLp // 512)]
    for ci, c in enumerate(order):
        cb_ = ci % 2
        cs = slice(c * 512, (c + 1) * 512)
        p.dma('sync', glT[cb_], d['GL'][:, cs], [], [('glT', cb_)], 'glT%d' % cb_)
        for i, (r0, nr) in enumerate(rows):
            p.dma('sync', oc_t[cb_][0:nr, i, :], d['OUT'][r0:r0 + nr, cs], [], [('oc_t', cb_)], 'oc_t%d' % cb_)
        for oc in range(8):
            yacc, yk = yacc2[oc % 2], ('yacc', oc % 2)
            for i, (r0, nr) in enumerate(rows):
                b = k % 2
                k += 1
                gp, gk = PS[2 + b], PK(2 + b)
                bp, bk = PS[4 + b], PK(4 + b)
                col = i * 1024 + oc * 128
                p.mm(gp, Wgb[:, col:col + 128], glT[cb_], True, True, [('glT', cb_), 'Wres'], [gk])
                p.act(gate[b], gp, AF.Sigmoid, [gk, 'bgt'], [('gate', b)], bias=bgt[:, i * 8 + oc:i * 8 + oc + 1])
                p.mm(bp, Wbr[0:nr, i, oc * 128:(oc + 1) * 128], oc_t[cb_][0:nr, i, :], True, True, [('oc_t', cb_), 'Wres'], [bk])
                if i == 0:
                    p.tt('vector', yacc, gate[b], bp, ALU.mult, [('gate', b), bk], [yk])
                else:
                    tmpy, tk = tmpy2[b], ('tmpy', b)
                    p.tt('vector', tmpy, gate[b], bp, ALU.mult, [('gate', b), bk], [tk])
                    if i < 3:
                        p.tt('gpsimd', yacc, yacc, tmpy, ALU.add, [yk, tk], [yk])
                    else:
                        p.tt('gpsimd', yst[cb_][:, oc, :], yacc, tmpy, ALU.add, [yk, tk], [('yst', cb_)])
        sl, w = (c * 512) // SL, (c * 512) % SL
        q, t0 = w // SLp, w % SLp
        p.dma('sync', d['YP'][q, sl, :, t0:t0 + 512].rearrange("(oc p) t -> p oc t", p=128), yst[cb_], [('yst', cb_)], [('YP', cb_)], 'yst%d' % cb_)
        if (ci + 1) % (4 * (SLp // 512)) == 0:
            p.cc("ReduceScatter", ALU.add, GROUPS, d['YP'][q].rearrange("a c t -> (a c) t"), d['YS'][q], [('YP', 0), ('YP', 1)], [('YS', q)])


def emit_B2(p, sb, PS, d, NTOK, nblk, F, moe):
    NCH = NTOK // 512
    FC = F // 128
    identb = sb("identb", [128, 128], BF16)
    p.dma('sync', identb, d['ident'][:, :], [], ['identb'], 'identb')
    G2 = sb("G2", [128, 1024], F32)
    p.dma('sync', G2, d['g2'].partition_broadcast(128), [], ['G2'], 'G2')
    Wout = sb("Wout", [128, 8, 1024], BF16)
    stg = [sb("w_stg%d" % i, [128, 1024], F32) for i in range(2)]
    engs = ['vector', 'gpsimd', 'scalar']
    for kc in range(8):
        b = kc % 2
        p.dma('sync', stg[b], d['wout'][kc * 128:(kc + 1) * 128, :], [], [('w_stg', b)], 'w_stg%d' % b)
        p.copy(engs[kc % 3], Wout[:, kc, :], stg[b], [('w_stg', b)], ['Wres'])
    if moe:
        identf = sb("identf", [128, 128], F32)
        p.dma('sync', identf, d['identf'][:, :], [], ['identf'], 'identf')
        Wr = sb("Wr", [128, 8, 8], F32)
        p.dma('sync', Wr, d['wr'].rearrange("(kc p) e -> p kc e", p=128), [], ['Wr'], 'Wr')
        brt = sb("brt", [128, 8], F32)
        p.dma('sync', brt, d['br'].partition_broadcast(128), [], ['brt'], 'brt')
        h2f = sb("h2f", [128, 1024], F32)
        h2fT = sb("h2fT", [128, 8, 128], F32)
        rt = sb("rt", [128, 64], F32)
        GW = sb("GW", [128, 4, 8], F32)
    X4 = sb("X4", [128, 4, 1024], F32)
    yf = sb("yf", [128, 8, 512], F32)
    hT = sb("hT", [128, 8, 512], BF16)
    hb = sb("hb", [128, 1024], BF16)
    junk = sb("junk", [128, 1024], BF16)
    st = sb("st", [128, 8], F32)
    yT = sb("yT", [128, 8, 512], BF16)
    sg = [sb("sg%d" % i, [128, 512], F32) for i in range(2)]
    WGU_t = [sb("WGU_t%d" % i, [128, 8, 2 * F], BF16) for i in range(2)]
    WD_t = [sb("WD_t%d" % i, [128, FC, 1024], BF16) for i in range(2)]
    psTb = PS[0].bitcast(BF16)
    wit = 0

    def norm_T(tt, G, Gk):
        xt = X4[:, tt, :]
        p.act(junk, xt, AF.Square, ['X4'], ['junk', 'ssq'], accum_out=st[:, 0:1])
        p.act(st[:, 1:2], st[:, 0:1], AF.Sqrt, ['ssq'], ['rs'], scale=1.0 / D, bias=EPS)
        p.recip(st[:, 2:3], st[:, 1:2], ['rs'], ['rstd'])
        p.stt(hb, xt, st[:, 2:3], G, ALU.mult, ALU.mult, ['X4', 'rstd', Gk], ['hb'])
        for kc in range(8):
            p.tr(psTb[:, kc * 128:(kc + 1) * 128], hb[:, kc * 128:(kc + 1) * 128], identb, ['hb', 'identb'], [PK(0)])
        p.copy('vector', hT[:, :, tt * 128:(tt + 1) * 128], psTb.rearrange("p (a d) -> p a d", d=128), [PK(0)], ['hT'])

    for c in range(NCH):
        T0 = c * 512
        p.dma('sync', X4, d['xtok'][T0:T0 + 512, :].rearrange("(t p) d -> p t d", p=128), [], ['X4'], 'X4')
        SLp = min(NTOK, 1024)
        p.dma('sync', yf, d['YS'][T0 // SLp, :, T0 % SLp:T0 % SLp + 512].rearrange("(oc p) t -> p oc t", p=128), [('YS', T0 // SLp)], ['yf'], 'yf')
        p.copy('gpsimd', yT[:, 0:4, :], yf[:, 0:4, :], ['yf'], ['yT'])
        p.copy('vector', yT[:, 4:8, :], yf[:, 4:8, :], ['yf'], ['yT'])
        k = 0
        for tt in range(4):
            for half in range(2):
                b = k % 2
                k += 1
                ps, pk = PS[6 + b], PK(6 + b)
                for oc in range(8):
                    p.mm(ps, yT[:, oc, tt * 128:(tt + 1) * 128], Wout[:, oc, half * 512:(half + 1) * 512], oc == 0, oc == 7, ['yT', 'Wres'], [pk])
                xs = X4[:, tt, half * 512:(half + 1) * 512]
                p.tt('vector', xs, ps, xs, ALU.add, [pk, 'X4'], ['X4'])
        for tt in range(4):
            norm_T(tt, G2, 'G2')
            if moe:
                p.stt(h2f, X4[:, tt, :], st[:, 2:3], G2, ALU.mult, ALU.mult, ['X4', 'rstd', 'G2'], ['h2f'])
                for kc in range(8):
                    bnk = 2 + kc // 4
                    p.tr(PS[bnk][:, (kc % 4) * 128:(kc % 4 + 1) * 128], h2f[:, kc * 128:(kc + 1) * 128], identf, ['h2f', 'identf'], [PK(bnk)])
                p.copy('vector', h2fT[:, 0:4, :].rearrange("p a d -> p (a d)"), PS[2], [PK(2)], ['h2fT'])
                p.copy('vector', h2fT[:, 4:8, :].rearrange("p a d -> p (a d)"), PS[3], [PK(3)], ['h2fT'])
                for kc in range(8):
                    p.mm(PS[1][:, 0:8], h2fT[:, kc, :], Wr[:, kc, :], kc == 0, kc == 7, ['h2fT', 'Wr'], [PK(1)])
                lg, m8, ee, mk = rt[:, 0:8], rt[:, 8:16], rt[:, 16:24], rt[:, 24:32]
                p.tt('vector', lg, PS[1][:, 0:8], brt, ALU.add, [PK(1), 'brt'], ['lg'])
                p.op('vector', lambda e, lg=lg, m8=m8: e.max(out=m8, in_=lg), ['lg'], ['m8'])
                p.ts('vector', rt[:, 32:33], m8[:, 0:1], -1.0, ALU.mult, ['m8'], ['negm'])
                p.act(ee, lg, AF.Exp, ['lg', 'negm'], ['ee'], bias=rt[:, 32:33])
                p.ts('vector', mk, lg, m8[:, 1:2], ALU.is_ge, ['lg', 'm8'], ['mk'])
                p.tt('vector', ee, ee, mk, ALU.mult, ['ee', 'mk'], ['ee'])
                p.op('vector', lambda e, ee=ee: e.tensor_reduce(out=rt[:, 33:34], in_=ee, axis=AX.X, op=ALU.add), ['ee'], ['den'])
                p.recip(rt[:, 34:35], rt[:, 33:34], ['den'], ['rden'])
                p.ts('vector', GW[:, tt, :], ee, rt[:, 34:35], ALU.mult, ['ee', 'rden'], ['GW'])
        aT = yT
        for blk in range(nblk):
            wb = wit % 2
            wit += 1
            p.dma('sync', WGU_t[wb], d['WGU'][blk], [], [('WGU_t', wb)], 'WGU_t%d' % wb)
            p.dma('sync', WD_t[wb], d['WD'][blk], [], [('WD_t', wb)], 'WD_t%d' % wb)
            for fc in range(FC):
                b = fc % 2
                gps, gk = PS[2 + b], PK(2 + b)
                ups, uk = PS[4 + b], PK(4 + b)
                for kc in range(8):
                    p.mm(gps, WGU_t[wb][:, kc, fc * 128:(fc + 1) * 128], hT[:, kc, :], kc == 0, kc == 7, ['hT', ('WGU_t', wb)], [gk])
                for kc in range(8):
                    p.mm(ups, WGU_t[wb][:, kc, F + fc * 128:F + (fc + 1) * 128], hT[:, kc, :], kc == 0, kc == 7, ['hT', ('WGU_t', wb)], [uk])
                p.act(sg[b], gps, AF.Silu, [gk], [('sg', b)])
                p.tt('vector', aT[:, fc, :], sg[b], ups, ALU.mult, [('sg', b), uk], ['yT'])
            k = 0
            for tt in range(4):
                for half in range(2):
                    b = k % 2
                    k += 1
                    ps, pk = PS[6 + b], PK(6 + b)
                    for fc in range(FC):
                        p.mm(ps, aT[:, fc, tt * 128:(tt + 1) * 128], WD_t[wb][:, fc, half * 512:(half + 1) * 512], fc == 0, fc == FC - 1, ['yT', ('WD_t', wb)], [pk])
                    xs = X4[:, tt, half * 512:(half + 1) * 512]
                    if moe:
                        p.stt(xs, ps, GW[:, tt, blk:blk + 1], xs, ALU.mult, ALU.add, [pk, 'GW', 'X4'], ['X4'])
                    else:
                        p.tt('vector', xs, ps, xs, ALU.add, [pk, 'X4'], ['X4'])
        p.dma('sync', d['xout'][T0:T0 + 512, :].rearrange("(t p) d -> p t d", p=128), X4, ['X4'], ['xout'], 'X4o')
        if d.get('XG') is not None:
            for kk in (2 * c, 2 * c + 1):
                p.cc("AllGather", ALU.bypass, GROUPS, d['xout'][kk * 256:(kk + 1) * 256, :], d['XG'][kk], ['xout'], [('XG', kk)])


def build_fused(S, n_layers=2):
    nc = bass.Bass("TRN2", target_bir_lowering=False)
    NT, NTOK, SL = S // 128, S // 4, S // 4
    g = {}

    def din(name, shape, dt=F32):
        g[name] = nc.dram_tensor(name, shape, dt, kind="ExternalInput").ap()

    def dint(name, shape, dt):
        g[name] = nc.dram_tensor(name, shape, dt, kind="Internal").ap()

    din('x', [S, D])
    din('xtok', [NTOK, D])
    for nm, shp, dt in (('cos', [128, SEQ // 128, 16], F32), ('sin', [128, SEQ // 128, 16], F32), ('ident', [128, 128], BF16),
                        ('triN', [128, 128], BF16), ('onesb', [128, 128], BF16), ('maskS', [128, 4, 512], BF16),
                        ('maskI', [128, 4, 512], BF16), ('onesf', [128, 64], F32), ('identf', [128, 128], F32), ('bvec', [10, 128, 128], F32)):
        din(nm, shp, dt)
    for L in range(n_layers):
        sfx = str(L)
        for nm, shp in (('w_in', [D, 1280]), ('g1', [128, 8]), ('wqb', [256, 96]), ('gqa', [128, 2]), ('wkvb', [256, 128]),
                        ('gkva', [128, 2]), ('g32', [288]), ('g96', [192]), ('sinks', [1, 2]), ('wgb', [128, 4096]),
                        ('bgate', [128, 32]), ('wbrc', [224, 1024]), ('g2', [1024]), ('wout', [1024, 1024])):
            din(nm + sfx, shp)
    din('wgu0', [1024, 4096])
    din('wdn0', [2048, 1024])
    if n_layers > 1:
        din('wr1', [1024, 8])
        din('br1', [8])
        din('wgu1', [8, 1024, 1536])
        din('wdn1', [8, 768, 1024])
    dint('QKT', [128, 8, S], BF16)
    dint('VSB', [128, NT, 64], BF16)
    dint('VML', [128, NT, 65], BF16)
    dint('VSW', [128, NT, 33], BF16)
    for i in range(3):
        dint('VD%d' % i, [S, 33], BF16)
    dint('GL', [128, S], BF16)
    dint('OUT', [224, S], BF16)
    SLp = min(SL, 1024)
    NP = SL // SLp
    NAG = NTOK // 256
    dint('YP', [NP, 4, 1024, SLp], F32)
    dint('YS', [NP, 1024, SLp], F32)
    dint('XS', [NTOK, D], F32)
    dint('XG', [NAG, 4 * 256, D], F32)
    dint('WGU0', [4, 128, 8, 1024], BF16)
    dint('WD0', [4, 128, 4, 1024], BF16)
    if n_layers > 1:
        dint('WGU1', [8, 128, 8, 1536], BF16)
        dint('WD1', [8, 128, 6, 1024], BF16)
    g['final'] = nc.dram_tensor('final', [NTOK, D], F32, kind="ExternalOutput").ap()
    shared = ('cos', 'sin', 'ident', 'triN', 'onesb', 'maskS', 'maskI', 'onesf', 'identf', 'bvec',
              'QKT', 'VSB', 'VML', 'VSW', 'VD0', 'VD1', 'VD2', 'GL', 'OUT', 'YP', 'YS')
    with ExitStack() as es:
        sb = Arena(nc, es, 200 * 1024)
        PSB = [es.enter_context(nc.psum_tensor("psb%d" % i, [128, 1024], F32))[:, :] for i in range(4)]
        PS = [PSB[i // 2][:, (i % 2) * 512:(i % 2 + 1) * 512] for i in range(8)]
        d_psb = PSB
        p = Prog(nc, es)
        block = es.enter_context(nc.Block())
        for L in range(n_layers):
            sfx = str(L)
            moe = (L % 2 == 1)
            d = {k: g[k] for k in shared}
            for nm in ('w_in', 'g1', 'wqb', 'gqa', 'wkvb', 'gkva', 'g32', 'g96', 'sinks', 'wgb', 'bgate', 'wbrc', 'g2', 'wout'):
                d[nm] = g[nm + sfx]
            if L == 0:
                d['xrows'] = lambda t: g['x'][t * 128:(t + 1) * 128, :]
            else:
                def xrows(t):
                    T = t * 128
                    r, w = T // NTOK, T % NTOK
                    return g['XG'][w // 256, r * 256 + (w % 256):r * 256 + (w % 256) + 128, :]
                d['xrows'] = xrows
                d['xkeys'] = lambda t: [('XG', ((t * 128) % NTOK) // 256)]
            d['xtok'] = g['xtok'] if L == 0 else g['XS']
            d['xout'] = g['final'] if L == n_layers - 1 else g['XS']
            d['WGU'], d['WD'] = g['WGU' + sfx], g['WD' + sfx]
            if moe:
                nblk, F = 8, 768
                d['wr'], d['br'] = g['wr1'], g['br1']
                d['wg'] = lambda blk: g['wgu1'][blk, :, 0:768]
                d['wu'] = lambda blk: g['wgu1'][blk, :, 768:1536]
                d['wd'] = lambda blk: g['wdn1'][blk, :, :]
            else:
                nblk, F = 4, 512
                d['wg'] = lambda blk: g['wgu0'][:, blk * 512:(blk + 1) * 512]
                d['wu'] = lambda blk: g['wgu0'][:, 2048 + blk * 512:2048 + (blk + 1) * 512]
                d['wd'] = lambda blk: g['wdn0'][blk * 512:(blk + 1) * 512, :]
            d['XG'] = g['XG'] if L < n_layers - 1 else None
            d['n_side'] = nblk * (8 + F // 128)
            d['PSB'] = d_psb
            for ph in (emit_inproj, emit_sb, emit_mla, emit_sw, emit_dil):
                sb.reset()
                if ph is emit_sb:
                    ph(p, sb, PS, d, S, side=lambda sb_, d=d, nblk=nblk, F=F: castw_iter(p, sb_, d, nblk, F, engs=('gpsimd',)))
                else:
                    ph(p, sb, PS, d, S)
                p.barrier(exclude_cc=True, keep=('XG',))
            sb.reset()
            emit_merge(p, sb, PS, d, S)
            p.barrier(exclude_cc=True, keep=('YS',))
            sb.reset()
            emit_B2(p, sb, PS, d, NTOK, nblk, F, moe)
            p.barrier(exclude_cc=True, keep=('XG',))
        p.barrier()
        p.finish(block)
    return nc, p


def inputs_fused(cA, inp, core, S, n_layers=2):
    b, h = core // 4, core % 4
    NTOK = S // 4
    m = {k: cA[k] for k in ('cos', 'sin', 'ident', 'triN', 'onesb', 'maskS', 'maskI', 'onesf')}
    m['identf'] = np.eye(128, dtype=np.float32)
    x = np.asarray(inp['x'], np.float32)
    m['x'] = np.ascontiguousarray(x[b, :S])
    m['xtok'] = np.ascontiguousarray(x[b, h * NTOK:(h + 1) * NTOK])
    bv = band_bias_vecs(inp['rel_bias'], h)
    kk = np.arange(128)
    m['bvec'] = np.ascontiguousarray(bv[:, kk[None, :] - kk[:, None] + 127])
    cols = core_cols(h)
    for l in range(n_layers):
        s = str(l)
        m['w_in' + s] = np.ascontiguousarray(np.concatenate([inp['w_in'][l][:, cols], inp['w_gate_a'][l]], axis=1))
        m['g1' + s] = np.ascontiguousarray(inp['norm1_g'][l].reshape(8, 128).T)
        m['wqb' + s] = np.ascontiguousarray(inp['w_qb'][l][:, h * 96:(h + 1) * 96])
        m['gqa' + s] = np.ascontiguousarray(inp['g_qa'][l].reshape(2, 128).T)
        m['wkvb' + s] = np.ascontiguousarray(inp['w_kvb'][l][:, h * 128:(h + 1) * 128])
        m['gkva' + s] = np.ascontiguousarray(inp['g_kva'][l].reshape(2, 128).T)
        gs, gd = inp['qk_g_sw'][l], inp['qk_g_dil'][l]
        m['g32' + s] = np.concatenate([gs[0], gd[0], gd[0], gs[1], gd[1], gd[1], gs[0], gd[0], gd[1]]).astype(np.float32)
        m['g96' + s] = np.concatenate([inp['qk_g_mla'][l][0], inp['qk_g_mla'][l][1]]).astype(np.float32)
        m['sinks' + s] = np.ascontiguousarray(inp['sinks'][l][2 * h:2 * h + 2].reshape(1, 2))
        m['wgb' + s] = np.ascontiguousarray(inp['w_gate_b'][l])
        m['bgate' + s] = np.ascontiguousarray(inp['b_gate'][l].reshape(32, 128).T)
        wb = inp['w_branch'][l]
        m['wbrc' + s] = np.ascontiguousarray(np.concatenate([wb[h * 64:(h + 1) * 64], wb[256 + h * 64:256 + (h + 1) * 64],
                                                             wb[512 + h * 64:512 + (h + 1) * 64], wb[768 + h * 32:768 + (h + 1) * 32]], axis=0))
        m['g2' + s] = np.ascontiguousarray(inp['norm2_g'][l])
        m['wout' + s] = np.ascontiguousarray(inp['w_out'][l])
    m['wgu0'] = np.ascontiguousarray(inp['w_gu_dense'][0])
    m['wdn0'] = np.ascontiguousarray(inp['w_down_dense'][0])
    if n_layers > 1:
        m['wr1'] = np.ascontiguousarray(inp['w_router'][0])
        m['br1'] = np.ascontiguousarray(inp['b_router'][0])
        m['wgu1'] = np.ascontiguousarray(inp['w_gu_exp'][0])
        m['wdn1'] = np.ascontiguousarray(inp['w_down_exp'][0])
    return m


def kernel_fused(inp, S=SEQ, n_layers=2):
    cA = consts_A()
    ncF, _ = _prog(('F', S, n_layers), lambda: build_fused(S, n_layers))
    in_maps = [inputs_fused(cA, inp, c, S, n_layers) for c in range(8)]
    res = run_bass_kernel_spmd(ncF, in_maps, core_ids=list(range(8)))
    NTOK = S // 4
    out = np.empty((BATCH, S, D), np.float32)
    for c in range(8):
        out[c // 4, (c % 4) * NTOK:(c % 4 + 1) * NTOK] = np.asarray(res.results[c]['final'])
    return out
```
